# Optimizing a Trainium2 kernel written in Bass

```python
import math
import jax, jax.numpy as jnp
from jax import lax
import numpy as np

D_MODEL = 1024
BATCH = 4
SEQ = 8192
DEPTH = 1

HEAD_DIM = D_MODEL // 16
N_HEADS_SGU = 8
N_HEADS_FOX = 8
D_SGU = N_HEADS_SGU * HEAD_DIM
D_FOX = N_HEADS_FOX * HEAD_DIM
D_MIX = D_SGU + D_FOX
D_IN_PROJ = 2 * D_SGU + 3 * D_FOX + N_HEADS_FOX
CHUNK = 128
Q_BLOCK = 128
N_GROUPS = 4
EXPERTS_PER_GROUP = 4
N_EXPERTS = N_GROUPS * EXPERTS_PER_GROUP
TOP_K_INNER = 2
D_EXPERT = D_MODEL // 2
N_ADA = 6
EPS = 1e-6

kernel_name = "hybrid_sgu_fox_hmoe_adaln"


def rms_norm(x, g):
    xf = x.astype(jnp.float32)
    y = xf * lax.rsqrt(jnp.mean(xf * xf, axis=-1, keepdims=True) + EPS)
    return (y * g.astype(jnp.float32)).astype(x.dtype)


def sgu_mixer(u, v, g_sgu, w_spatial, b_spatial):
    B, S, _ = u.shape
    u = jax.nn.gelu(u)
    v = jax.nn.gelu(v).reshape(B, S // CHUNK, CHUNK, N_HEADS_SGU, HEAD_DIM)
    v = rms_norm(v, g_sgu.reshape(N_HEADS_SGU, HEAD_DIM))
    mask = jnp.tril(jnp.ones((CHUNK, CHUNK), dtype=bool))
    ws = jnp.where(mask[None], w_spatial, jnp.zeros((), w_spatial.dtype)).astype(v.dtype)
    z = jnp.einsum('hts,bnshd->bnthd', ws, v) + b_spatial.T[None, None, :, :, None].astype(v.dtype)
    return u * z.reshape(B, S, D_SGU)


def fox_attention(q, k, v, f_logit, b_forget):
    B, S, H, hd = q.shape
    log_f = jax.nn.log_sigmoid(f_logit.astype(jnp.float32) + b_forget.astype(jnp.float32))
    F = jnp.cumsum(log_f, axis=1).transpose(0, 2, 1)
    q = q.transpose(0, 2, 1, 3)
    k = k.transpose(0, 2, 1, 3)
    v = v.transpose(0, 2, 1, 3)
    scale = 1.0 / math.sqrt(hd)
    kpos = jnp.arange(S)

    def block(i):
        start = i * Q_BLOCK
        qi = lax.dynamic_slice_in_dim(q, start, Q_BLOCK, axis=2)
        Fi = lax.dynamic_slice_in_dim(F, start, Q_BLOCK, axis=2)
        s = jnp.einsum('bhqd,bhkd->bhqk', qi, k, preferred_element_type=jnp.float32) * scale
        s = s + Fi[..., :, None] - F[..., None, :]
        qpos = start + jnp.arange(Q_BLOCK)
        s = jnp.where(kpos[None, :] <= qpos[:, None], s, -jnp.inf)
        p = jax.nn.softmax(s, axis=-1).astype(v.dtype)
        return jnp.einsum('bhqk,bhkd->bhqd', p, v)

    out = lax.map(block, jnp.arange(S // Q_BLOCK))
    return out.transpose(1, 0, 3, 2, 4).reshape(B, S, H, hd)


def hybrid_mixer(h, w_in, g_sgu, w_spatial, b_spatial, b_forget, g_out_sgu, g_out_fox, w_out):
    B, S, _ = h.shape
    proj = h @ w_in
    splits = np.cumsum([D_SGU, D_SGU, D_FOX, D_FOX, D_FOX]).tolist()
    u, vs, q, k, vf, fl = jnp.split(proj, splits, axis=-1)
    y_sgu = sgu_mixer(u, vs, g_sgu, w_spatial, b_spatial)
    shp = (B, S, N_HEADS_FOX, HEAD_DIM)
    y_fox = fox_attention(q.reshape(shp), k.reshape(shp), vf.reshape(shp), fl, b_forget).reshape(B, S, D_FOX)
    y = jnp.concatenate([rms_norm(y_sgu, g_out_sgu), rms_norm(y_fox, g_out_fox)], axis=-1)
    return y @ w_out


def hier_moe(h, w_rg, b_rg, w_re, b_re, w_gate, w_up, w_down):
    B, S, D = h.shape
    t = h.reshape(-1, D)
    lg = (t @ w_rg + b_rg).astype(jnp.float32)
    p_group = jax.nn.softmax(lg, axis=-1)
    g_sel = jnp.argmax(lg, axis=-1)
    p_sel = jnp.take_along_axis(p_group, g_sel[:, None], axis=1)
    le = (jnp.einsum('nd,gde->nge', t, w_re) + b_re).astype(jnp.float32)
    le_sel = jnp.take_along_axis(le, g_sel[:, None, None], axis=1)[:, 0]
    top_v, top_i = lax.top_k(le_sel, TOP_K_INNER)
    w_k = p_sel * jax.nn.softmax(top_v, axis=-1)
    expert_idx = g_sel[:, None] * EXPERTS_PER_GROUP + top_i
    combine = jnp.sum(jax.nn.one_hot(expert_idx, N_EXPERTS, dtype=jnp.float32) * w_k[..., None],
                      axis=1).astype(h.dtype)
    y = jnp.zeros_like(t)
    for e in range(N_EXPERTS):
        a = jax.nn.silu(t @ w_gate[e]) * (t @ w_up[e])
        y = y + combine[:, e:e + 1] * (a @ w_down[e])
    return y.reshape(B, S, D)


def setup_inputs(seed: int = 0) -> dict:
    key = jax.random.key(seed)
    ks = jax.random.split(key, 24)
    n = lambda k, shape, s: jax.random.normal(k, shape, jnp.float32) * s
    L = DEPTH
    return {
        "x": n(ks[0], (BATCH, SEQ, D_MODEL), 1.0),
        "c": n(ks[1], (BATCH, D_MODEL), 1.0),
        "w_ada": n(ks[2], (L, D_MODEL, N_ADA * D_MODEL), 0.02),
        "b_ada": n(ks[3], (L, N_ADA * D_MODEL), 0.02),
        "g_norm_mix": 1.0 + n(ks[4], (L, D_MODEL), 0.02),
        "w_in": n(ks[5], (L, D_MODEL, D_IN_PROJ), D_MODEL ** -0.5),
        "g_sgu": 1.0 + n(ks[6], (L, D_SGU), 0.02),
        "w_spatial": n(ks[7], (L, N_HEADS_SGU, CHUNK, CHUNK), 0.5 * CHUNK ** -0.5),
        "b_spatial": 1.0 + n(ks[8], (L, N_HEADS_SGU, CHUNK), 0.1),
        "b_forget": 3.0 + n(ks[9], (L, N_HEADS_FOX), 0.1),
        "g_out_sgu": 1.0 + n(ks[10], (L, D_SGU), 0.02),
        "g_out_fox": 1.0 + n(ks[11], (L, D_FOX), 0.02),
        "w_out": n(ks[12], (L, D_MIX, D_MODEL), D_MIX ** -0.5),
        "g_norm_ffn": 1.0 + n(ks[13], (L, D_MODEL), 0.02),
        "w_router_group": n(ks[14], (L, D_MODEL, N_GROUPS), D_MODEL ** -0.5),
        "b_router_group": n(ks[15], (L, N_GROUPS), 0.01),
        "w_router_expert": n(ks[16], (L, N_GROUPS, D_MODEL, EXPERTS_PER_GROUP), D_MODEL ** -0.5),
        "b_router_expert": n(ks[17], (L, N_GROUPS, EXPERTS_PER_GROUP), 0.01),
        "w_gate": n(ks[18], (L, N_EXPERTS, D_MODEL, D_EXPERT), D_MODEL ** -0.5),
        "w_up": n(ks[19], (L, N_EXPERTS, D_MODEL, D_EXPERT), D_MODEL ** -0.5),
        "w_down": n(ks[20], (L, N_EXPERTS, D_EXPERT, D_MODEL), D_EXPERT ** -0.5),
        "g_final": 1.0 + n(ks[21], (D_MODEL,), 0.02),
    }


def reference(x, c, w_ada, b_ada, g_norm_mix, w_in, g_sgu, w_spatial, b_spatial, b_forget,
              g_out_sgu, g_out_fox, w_out, g_norm_ffn, w_router_group, b_router_group,
              w_router_expert, b_router_expert, w_gate, w_up, w_down, g_final):
    c_act = jax.nn.silu(c)
    for l in range(DEPTH):
        ada = (c_act @ w_ada[l] + b_ada[l])[:, None, :]
        sh1, sc1, gt1, sh2, sc2, gt2 = jnp.split(ada, N_ADA, axis=-1)
        h = rms_norm(x, g_norm_mix[l]) * (1 + sc1) + sh1
        x = x + gt1 * hybrid_mixer(h, w_in[l], g_sgu[l], w_spatial[l], b_spatial[l], b_forget[l],
                                   g_out_sgu[l], g_out_fox[l], w_out[l])
        h = rms_norm(x, g_norm_ffn[l]) * (1 + sc2) + sh2
        x = x + gt2 * hier_moe(h, w_router_group[l], b_router_group[l], w_router_expert[l],
                               b_router_expert[l], w_gate[l], w_up[l], w_down[l])
    return rms_norm(x, g_final)
```

```python
import numpy as np
from contextlib import ExitStack
import concourse.bass as bass
import concourse.mybir as mybir
from concourse.bass_utils import run_bass_kernel_spmd

F32 = mybir.dt.float32
BF16 = mybir.dt.bfloat16
U32 = mybir.dt.uint32
I32 = mybir.dt.int32
AF = mybir.ActivationFunctionType
ALU = mybir.AluOpType
AX = mybir.AxisListType
ENGS = ['sync', 'scalar', 'vector', 'gpsimd', 'tensor']
EPS = 1e-6
NEG = -1.0e30
OWN_RUNS = {0: [0, 3, 4, 7, 8, 11, 12, 15], 1: [1, 2, 5, 6, 9, 10, 13, 14]}
WARM = 18
DEBUG = {"x1": False, "same": True, "phases": "ABC", "dump": []}


def NK(i):
    return 16 * (i // 2) + 8 + 8 * (i % 2)


class Tracker:
    def __init__(self, nc, same_engine_sync=False):
        self.nc = nc
        self.streams = {e: [] for e in ENGS}
        self.sems = {e: nc.alloc_semaphore("c_" + e) for e in ENGS}
        self.cnt = {e: 0 for e in ENGS}
        self.waited = {e: {} for e in ENGS}
        self.res = {}
        self.slots = {}
        self.same = same_engine_sync
        self.slot_q = {}

    def _deps(self, reads, writes):
        deps = []
        for r in reads:
            st = self.res.get(r)
            if st and st[0]:
                deps.append(st[0])
        for w in writes:
            st = self.res.get(w)
            if st:
                if st[0]:
                    deps.append(st[0])
                deps.extend(st[1])
        return deps

    def _update(self, reads, writes, tag):
        for r in reads:
            st = self.res.setdefault(r, [None, []])
            st[1].append(tag)
        for w in writes:
            self.res[w] = [tag, []]

    def _emit_waits(self, eng, deps, skip_same):
        mx = {}
        for key, val in deps:
            mx[key] = max(mx.get(key, 0), val)
        for key, val in mx.items():
            if key == eng and skip_same:
                continue
            if self.waited[eng].get(key, 0) >= val:
                continue
            self.waited[eng][key] = val
            sem = self.sems[key] if key in self.sems else self.slots[key][0]
            self.streams[eng].append(lambda e, sem=sem, val=val: e.wait_ge(sem, val))

    def op(self, eng, fn, reads=(), writes=()):
        deps = self._deps(reads, writes)
        self._emit_waits(eng, deps, (not self.same) or eng == 'tensor')
        self.cnt[eng] += 1
        sem = self.sems[eng]
        self.streams[eng].append(lambda e, fn=fn, sem=sem: fn(e).then_inc(sem, 1))
        self._update(reads, writes, (eng, self.cnt[eng]))

    def dma(self, q, slot, out, in_, reads=(), writes=()):
        self.slot_q[slot] = q
        if slot not in self.slots:
            self.slots[slot] = [self.nc.alloc_semaphore("d_" + slot), 0]
        deps = self._deps(reads, writes)
        self._emit_waits(q, deps, False)
        s = self.slots[slot]
        s[1] += 16
        sem = s[0]
        self.streams[q].append(lambda e, out=out, in_=in_, sem=sem: e.dma_start(out=out, in_=in_).then_inc(sem, 16))
        self._update(reads, writes, (slot, s[1]))

    def idma(self, slot, out, out_off, in_, in_off, reads=(), writes=()):
        q = 'gpsimd'
        self.slot_q[slot] = q
        if slot not in self.slots:
            self.slots[slot] = [self.nc.alloc_semaphore("d_" + slot), 0]
        deps = self._deps(reads, writes)
        self._emit_waits(q, deps, False)
        s = self.slots[slot]
        s[1] += 16
        sem = s[0]
        self.streams[q].append(lambda e, out=out, in_=in_, sem=sem, oo=out_off, io=in_off:
                               e.indirect_dma_start(out=out, out_offset=oo, in_=in_, in_offset=io).then_inc(sem, 16))
        self._update(reads, writes, (slot, s[1]))

    def cond_begin(self, cnt_ap, thr, drain_slots):
        for e in ENGS:
            deps = [(e, self.cnt[e])] if self.cnt[e] else []
            deps += [(k, self.slots[k][1]) for k in drain_slots if k in self.slots and self.slot_q.get(k) == e]
            self._emit_waits(e, deps, False)
        self.snap_cnt = dict(self.cnt)
        self.snap_slots = {k: v[1] for k, v in self.slots.items()}
        self.snap_waited = {e: dict(w) for e, w in self.waited.items()}
        for e in ENGS:
            self.streams[e].append(('regload', cnt_ap))
            self.streams[e].append(('if', thr))

    def cond_end(self):
        for e in ENGS:
            self.streams[e].append(('else',))
            n = self.cnt[e] - self.snap_cnt[e]
            if n:
                self.streams[e].append(lambda eng, sem=self.sems[e], n=n: eng.sem_inc(sem, n))
        for slot, (sem, c) in self.slots.items():
            d = c - self.snap_slots.get(slot, 0)
            if d:
                self.streams[self.slot_q[slot]].append(lambda eng, sem=sem, d=d: eng.sem_inc(sem, d))
        for e in ENGS:
            self.streams[e].append(('endif',))
        self.waited = self.snap_waited

    def barrier(self):
        deps = [(e, c) for e, c in self.cnt.items() if c > 0]
        deps += [(k, v[1]) for k, v in self.slots.items() if v[1] > 0]
        for e in ENGS:
            self._emit_waits(e, deps, True)

    def emit(self):
        self.barrier()
        streams = self.streams

        def run(e, items):
            creg = e.alloc_register("creg")
            i = 0
            n = len(items)
            while i < n:
                it = items[i]
                if not isinstance(it, tuple):
                    it(e)
                    i += 1
                    continue
                if it[0] == 'regload':
                    e.reg_load(creg, it[1])
                    i += 1
                    continue
                assert it[0] == 'if'
                thr = it[1]
                j = i + 1
                body = []
                while not (isinstance(items[j], tuple) and items[j][0] == 'else'):
                    body.append(items[j])
                    j += 1
                j += 1
                fix = []
                while not (isinstance(items[j], tuple) and items[j][0] == 'endif'):
                    fix.append(items[j])
                    j += 1
                with e.If_lt(creg, thr + 1):
                    for f in fix:
                        f(e)
                with e.Else():
                    for f in body:
                        f(e)
                i = j + 1

        with self.nc.Block() as block:
            @block.sync
            def _(e):
                run(e, streams['sync'])

            @block.scalar
            def _(e):
                run(e, streams['scalar'])

            @block.vector
            def _(e):
                run(e, streams['vector'])

            @block.gpsimd
            def _(e):
                run(e, streams['gpsimd'])

            @block.tensor
            def _(e):
                run(e, streams['tensor'])


def mmgrp(lst):
    def fn(e):
        ins = None
        for (out, lhsT, rhs, st, sp) in lst:
            ins = e.matmul(out, lhsT=lhsT, rhs=rhs, start=st, stop=sp)
        return ins
    return fn


def tpgrp(lst):
    def fn(e):
        ins = None
        for (out, in_, ident) in lst:
            ins = e.transpose(out, in_, ident)
        return ins
    return fn


def ACT(out, in_, func, **kw):
    return lambda e: e.activation(out=out, in_=in_, func=func, **kw)


def TT(out, in0, in1, op):
    return lambda e: e.tensor_tensor(out=out, in0=in0, in1=in1, op=op)


def TS(out, in0, s1, s2, op0, op1=None):
    if op1 is None:
        return lambda e: e.tensor_scalar(out=out, in0=in0, scalar1=s1, scalar2=None, op0=op0)
    return lambda e: e.tensor_scalar(out=out, in0=in0, scalar1=s1, scalar2=s2, op0=op0, op1=op1)


def STT(out, in0, scalar, in1, op0, op1):
    return lambda e: e.scalar_tensor_tensor(out=out, in0=in0, scalar=scalar, in1=in1, op0=op0, op1=op1)


def CP(out, in_):
    return lambda e: e.tensor_copy(out=out, in_=in_)


def RED(out, in_, op):
    return lambda e: e.tensor_reduce(out=out, in_=in_, axis=AX.X, op=op)


def RECIP(out, in_):
    return lambda e: e.reciprocal(out=out, in_=in_)


def MEMSET(ap, v):
    return lambda e: e.memset(ap, v)


def ASEL(out, in_, pattern, cmp, fill, base, cm):
    return lambda e: e.affine_select(out=out, in_=in_, pattern=pattern, compare_op=cmp, fill=fill, base=base,
                                     channel_multiplier=cm)


def build_program():
    nc = bass.Bass("TRN2", target_bir_lowering=False)
    T = Tracker(nc, same_engine_sync=DEBUG["same"])
    din = lambda name, shape: nc.dram_tensor(name, shape, F32, kind="ExternalInput").ap()
    xall = din("xall", [8192, 1024])
    xown = din("xown", [4096, 1024])
    cT_d = din("cT", [128, 8])
    wada_d = din("w_ada", [1024, 6144])
    bada_d = din("b_ada", [1, 6144])
    g1T_d = din("g1T", [128, 8])
    g2T_d = din("g2T", [128, 8])
    win_d = din("w_in", [1024, 2568])
    gsgu_d = din("gsgu", [1, 512])
    wsT_d = din("wsT", [128, 8, 128])
    bsp_d = din("bsp", [8, 128])
    bfg_d = din("bfg", [1, 8])
    gos_d = din("gos", [128, 4])
    gof_d = din("gof", [128, 4])
    wout_d = din("w_out", [1024, 1024])
    wr_d = din("wr", [1024, 20])
    br_d = din("br", [1, 20])
    wg_d = din("w_gate", [16, 1024, 512])
    wu_d = din("w_up", [16, 1024, 512])
    wd_d = din("w_down", [16, 512, 1024])
    gfin_d = din("gfin", [1, 1024])
    masks_d = din("masks", [16, 128, 512])
    rsel_d = din("rsel", [1, 512])
    eb_d = din("eb", [1, 16])
    out_d = nc.dram_tensor("out", [4096, 1024], F32, kind="ExternalOutput").ap()
    KT_d = nc.dram_tensor("KT_d", [4, 128, 8192], BF16).ap()
    V_d = nc.dram_tensor("V_d", [4, 128, 64, 192], BF16).ap()
    Xs_d = nc.dram_tensor("Xs_d", [65536, 1024], BF16).ap()
    Ys_d = nc.dram_tensor("Ys_d", [65536, 1024], F32).ap()
    CNT_d = nc.dram_tensor("CNT_d", [1, 16], I32).ap()
    if DEBUG["x1"]:
        X1_d = nc.dram_tensor("X1_d", [4096, 1024], F32, kind="ExternalOutput").ap()
    else:
        X1_d = nc.dram_tensor("X1_d", [4096, 1024], F32).ap()

    win_v = win_d.rearrange("(kt p) n -> p kt n", p=128)
    dumps = DEBUG.get("dump", [])

    def dump(name, ap, shape, dt, rname):
        if name not in dumps:
            return
        d = nc.dram_tensor("dbg_" + name, shape, dt, kind="ExternalOutput").ap()
        T.dma('sync', 'dbg_' + name, d, ap, reads=rname, writes=['dbg_' + name])

    top = ExitStack()
    uid = [0]

    def sb(es, shape, dt, name=None):
        uid[0] += 1
        return es.enter_context(nc.sbuf_tensor(f"{name or 't'}_{uid[0]}", shape, dt))

    def ps(es, shape, dt, name=None):
        uid[0] += 1
        return es.enter_context(nc.psum_tensor(f"{name or 'p'}_{uid[0]}", shape, dt))

    ident_f = sb(top, [128, 128], F32, "identf")
    ident_b = sb(top, [128, 128], BF16, "identb")
    Umat = sb(top, [128, 128], F32, "U")
    Sel127 = sb(top, [128, 128], F32, "sel127")
    eps_t = sb(top, [128, 1], F32, "eps")
    one_t = sb(top, [128, 1], F32, "one")
    ones_bf = sb(top, [128, 1], BF16, "onesbf")
    A1 = sb(top, [128, 8], F32, "A1")
    B1 = sb(top, [128, 8], F32, "B1")
    A2 = sb(top, [128, 8], F32, "A2")
    B2 = sb(top, [128, 8], F32, "B2")
    GT1 = sb(top, [128, 1024], F32, "GT1")
    GT2 = sb(top, [128, 1024], F32, "GT2")
    C_all = sb(top, [128, 64, 8], F32, "Call")
    RC = sb(top, [128, 8, 8], F32, "RC")

    T.op('gpsimd', MEMSET(ident_f[:], 0.0), writes=['identf'])
    T.op('gpsimd', ASEL(ident_f[:], ident_f[:], [[-1, 128]], ALU.not_equal, 1.0, 0, 1), reads=['identf'], writes=['identf'])
    T.op('gpsimd', MEMSET(Umat[:], 1.0), writes=['U'])
    T.op('gpsimd', ASEL(Umat[:], Umat[:], [[1, 128]], ALU.is_ge, 0.0, 0, -1), reads=['U'], writes=['U'])
    T.op('gpsimd', MEMSET(Sel127[:], 1.0), writes=['sel127'])
    T.op('gpsimd', ASEL(Sel127[:], Sel127[:], [[0, 128]], ALU.is_ge, 0.0, -127, 1), reads=['sel127'], writes=['sel127'])
    T.op('gpsimd', MEMSET(eps_t[:], EPS), writes=['eps'])
    T.op('gpsimd', MEMSET(one_t[:], 1.0), writes=['one'])
    T.op('gpsimd', MEMSET(ones_bf[:], 1.0), writes=['onesbf'])
    T.op('vector', CP(ident_b[:], ident_f[:]), reads=['identf'], writes=['identb'])

    with ExitStack() as es:
        cT = sb(es, [128, 8], F32)
        CB = sb(es, [128, 8, 128], F32)
        WA = [sb(es, [128, 8, 1024], F32, "wada") for _ in range(2)]
        badab = sb(es, [128, 1024], F32)
        gT1 = sb(es, [128, 8], F32)
        gT2 = sb(es, [128, 8], F32)
        rowv = sb(es, [128, 1024], F32)
        dtmp = sb(es, [128, 8, 128], F32)
        colv = sb(es, [128, 8], F32)
        pp = [ps(es, [128, 512], F32) for _ in range(2)]
        T.dma('sync', 'cT', cT[:], cT_d, writes=['cT'])
        T.dma('sync', 'gT1', gT1[:], g1T_d, writes=['gT1'])
        T.dma('sync', 'gT2', gT2[:], g2T_d, writes=['gT2'])
        T.op('scalar', ACT(cT[:], cT[:], AF.Silu), reads=['cT'], writes=['cT'])
        T.op('vector', CP(CB[:], cT[:].unsqueeze(2).broadcast_to([128, 8, 128])), reads=['cT'], writes=['CB'])
        wada_v = wada_d.rearrange("(kt p) n -> p kt n", p=128)
        order = [1, 0, 2, 4, 3, 5]
        for n, v in enumerate(order):
            wb = n % 2
            for kt in range(8):
                T.dma('sync', f'wada{wb}_{kt}', WA[wb][:, kt, :], wada_v[:, kt, v * 1024:(v + 1) * 1024],
                      writes=[f'wada{wb}_{kt}'])
            T.dma('gpsimd', 'badab', badab[:], bada_d[0:1, v * 1024:(v + 1) * 1024].broadcast_to([128, 1024]),
                  writes=['badab'])
            for hf in range(2):
                T.op('tensor', mmgrp([(pp[hf][:], CB[:, kt, :], WA[wb][:, kt, hf * 512:(hf + 1) * 512], kt == 0, kt == 7)
                                      for kt in range(8)]),
                     reads=['CB'] + [f'wada{wb}_{kt}' for kt in range(8)], writes=[f'pp{hf}'])
                dst = {2: GT1, 5: GT2}.get(v, rowv)
                dname = {2: 'GT1', 5: 'GT2'}.get(v, 'rowv') + str(hf)
                T.op('vector', TT(dst[:, hf * 512:(hf + 1) * 512], pp[hf][:], badab[:, hf * 512:(hf + 1) * 512], ALU.add),
                     reads=[f'pp{hf}', 'badab'], writes=[dname])
            if v in (2, 5):
                continue
            T.op('vector', TT(dtmp[:], rowv[:].rearrange("p (k t) -> p k t", k=8),
                              ident_f[:].unsqueeze(1).broadcast_to([128, 8, 128]), ALU.mult),
                 reads=['rowv0', 'rowv1', 'identf'], writes=['dtmp'])
            T.op('vector', RED(colv[:], dtmp[:], ALU.add), reads=['dtmp'], writes=['colv'])
            if v == 1:
                T.op('vector', STT(A1[:], colv[:], 1.0, gT1[:], ALU.add, ALU.mult), reads=['colv', 'gT1'], writes=['A1'])
            elif v == 0:
                T.op('vector', CP(B1[:], colv[:]), reads=['colv'], writes=['B1'])
            elif v == 4:
                T.op('vector', STT(A2[:], colv[:], 1.0, gT2[:], ALU.add, ALU.mult), reads=['colv', 'gT2'], writes=['A2'])
            elif v == 3:
                T.op('vector', CP(B2[:], colv[:]), reads=['colv'], writes=['B2'])
        T.barrier()

    def ln1_block(xsrc_rows, XA, XN, junk, ssq, rs, tp, tmpf, HT_dst, bi, htname):
        b = bi % 2
        T.dma('sync', f'xa{b}', XA[b][:], xsrc_rows, writes=[f'xa{b}'])
        T.op('scalar', ACT(XN[b][:], XA[b][:], AF.Square, accum_out=ssq[:, bi:bi + 1]), reads=[f'xa{b}', 'ssqall'],
             writes=[f'xn{b}', f'ssq_{bi}'])
        T.op('scalar', ACT(rs[b][:], ssq[:, bi:bi + 1], AF.Ln, scale=1.0 / 1024, bias=eps_t[:]), reads=[f'ssq_{bi}', 'eps'],
             writes=[f'rs{b}'])
        T.op('scalar', ACT(rs[b][:], rs[b][:], AF.Exp, scale=-0.5), reads=[f'rs{b}'], writes=[f'rs{b}'])
        T.op('vector', TS(XN[b][:], XA[b][:], rs[b][:, 0:1], None, ALU.mult), reads=[f'xa{b}', f'rs{b}'], writes=[f'xn{b}'])
        T.op('tensor', tpgrp([(tp[:, kt, :], XN[b][:, kt * 128:(kt + 1) * 128], ident_b[:]) for kt in range(8)]),
             reads=[f'xn{b}', 'identb'], writes=['tp'])
        T.op('vector', TT(tmpf[:], tp[:], A1[:].unsqueeze(2).broadcast_to([128, 8, 128]), ALU.mult), reads=['tp', 'A1'],
             writes=['tmpf'])
        T.op('vector', TT(HT_dst, tmpf[:], B1[:].unsqueeze(2).broadcast_to([128, 8, 128]), ALU.add), reads=['tmpf', 'B1'],
             writes=[htname])

    if "A" in DEBUG["phases"]:
      with ExitStack() as es:
        WKV = sb(es, [128, 8, 1032], BF16, "wkvf")
        XA = [sb(es, [128, 1024], F32) for _ in range(2)]
        XN = [sb(es, [128, 1024], BF16) for _ in range(2)]
        junk = None
        ssq = sb(es, [128, 64], F32)
        T.op('vector', MEMSET(ssq[:], 0.0), writes=['ssqall'])
        rs = [sb(es, [128, 1], F32) for _ in range(2)]
        tmpf = sb(es, [128, 8, 128], F32)
        HT = [sb(es, [128, 8, 512], BF16) for _ in range(2)]
        KTs = [sb(es, [128, 4, 512], BF16) for _ in range(2)]
        VBs = [sb(es, [128, 4, 4, 3, 64], BF16) for _ in range(2)]
        bfb = sb(es, [128, 8], F32)
        ft = sb(es, [128, 8], F32)
        tp = ps(es, [128, 8, 128], BF16)
        mm = [ps(es, [128, 512], F32) for _ in range(3)]
        sm = ps(es, [128, 512], F32)
        T.dma('gpsimd', 'wkvf', WKV[:], win_v[:, :, 1536:2568], writes=['wkvf'])
        T.dma('sync', 'bfb', bfb[:], bfg_d[0:1, :].broadcast_to([128, 8]), writes=['bfb'])
        for k in range(2):
            T.op('gpsimd', MEMSET(VBs[k][:, :, :, 1, :], 1.0), writes=[f'vbs{k}'])
        mi = 0
        for r in range(16):
            hb = r % 2
            for bl in range(4):
                g = r * 4 + bl
                ln1_block(xall[g * 128:(g + 1) * 128, :], XA, XN, junk, ssq, rs, tp, tmpf,
                          HT[hb][:, :, bl * 128:(bl + 1) * 128], g, f'ht{hb}')
            for p in range(4):
                m = mm[mi % 3]; mn = f'mm{mi % 3}'; mi += 1
                T.op('tensor', mmgrp([(m[:], WKV[:, kt, p * 128:(p + 1) * 128], HT[hb][:, kt, :], kt == 0, kt == 7)
                                      for kt in range(8)]), reads=['wkvf', f'ht{hb}'], writes=[mn])
                T.op('scalar', ACT(KTs[hb][:, p, :], m[:], AF.Copy), reads=[mn], writes=[f'kts{hb}'])
            T.dma('gpsimd', f'kts{hb}', KT_d[:, :, r * 512:(r + 1) * 512].rearrange("q p t -> p q t"), KTs[hb][:],
                  reads=[f'kts{hb}'], writes=[f'KT_d{r}'])
            for bl in range(4):
                g = r * 4 + bl
                m = mm[mi % 3]; mn = f'mm{mi % 3}'; mi += 1
                T.op('tensor', mmgrp([(m[:], HT[hb][:, kt, bl * 128:(bl + 1) * 128], WKV[:, kt, 512:1024], kt == 0, kt == 7)
                                      for kt in range(8)]), reads=['wkvf', f'ht{hb}'], writes=[mn])
                T.op('scalar', ACT(VBs[hb][:, bl, :, 0::2, :], m[:].rearrange("t (p two d) -> t p two d", p=4, two=2),
                                   AF.Copy), reads=[mn], writes=[f'vbs{hb}'])
                T.op('tensor', mmgrp([(sm[:, 0:8], HT[hb][:, kt, bl * 128:(bl + 1) * 128], WKV[:, kt, 1024:1032], kt == 0, kt == 7)
                                      for kt in range(8)]), reads=['wkvf', f'ht{hb}'], writes=['sm'])
                T.op('vector', TT(ft[:], sm[:, 0:8], bfb[:], ALU.add), reads=['sm', 'bfb'], writes=['ft'])
                T.op('scalar', ACT(ft[:], ft[:], AF.Exp, scale=-1.0), reads=['ft'], writes=['ft'])
                T.op('scalar', ACT(ft[:], ft[:], AF.Ln, bias=one_t[:]), reads=['ft', 'one'], writes=['ft'])
                lst = [(sm[:, 8:16], Umat[:], ft[:], True, g == 0)]
                if g > 0:
                    lst.append((sm[:, 8:16], Sel127[:], C_all[:, g - 1, :], False, True))
                T.op('tensor', mmgrp(lst), reads=['ft', 'U', 'sel127', 'Call'], writes=['sm'])
                T.op('vector', CP(C_all[:, g, :], sm[:, 8:16]), reads=['sm'], writes=['Call'])
            for q in range(4):
                T.dma('gpsimd', f'vbs{hb}_{q}', V_d[q, :, r * 4:(r + 1) * 4, :],
                      VBs[hb][:, :, q, :, :].rearrange("t b three d -> t b (three d)"), reads=[f'vbs{hb}'],
                      writes=[f'V_d{r}_{q}'])
        T.barrier()

    with ExitStack() as es:
        rselb = sb(es, [128, 8, 64], F32)
        csel = sb(es, [128, 8, 8], F32)
        tmp3 = sb(es, [128, 8, 64], F32)
        pr = ps(es, [128, 512], F32)
        T.dma('sync', 'rselb', rselb[:], rsel_d[0:1, :].broadcast_to([128, 512]).rearrange("p (i b) -> p i b", i=8),
              writes=['rselb'])
        for i in range(8):
            T.op('vector', TT(tmp3[:], C_all[:].rearrange("p b h -> p h b"),
                              rselb[:, i, :].unsqueeze(1).broadcast_to([128, 8, 64]), ALU.mult),
                 reads=['Call', 'rselb'], writes=['tmp3'])
            T.op('vector', RED(csel[:, i, :], tmp3[:], ALU.add), reads=['tmp3'], writes=['csel'])
        T.op('tensor', mmgrp([(pr[:, 0:64], Sel127[:], csel[:].rearrange("p i h -> p (i h)"), True, True)]),
             reads=['csel', 'sel127'], writes=['pr'])
        T.op('vector', CP(RC[:].rearrange("p i h -> p (i h)"), pr[:, 0:64]), reads=['pr'], writes=['RC'])
        T.barrier()

    if "B" in DEBUG["phases"]:
      with ExitStack() as es:
        WQ = sb(es, [128, 8, 1536], BF16, "wq")
        WO = sb(es, [128, 8, 1024], BF16, "wo")
        MK = sb(es, [128, 16, 512], BF16, "mk")
        WsT = sb(es, [128, 8, 128], BF16, "wst")
        Bb = sb(es, [128, 4, 128], F32)
        gsg = sb(es, [128, 512], F32)
        gos = sb(es, [128, 4], F32)
        gof = sb(es, [128, 4], F32)
        XA = [sb(es, [128, 1024], F32) for _ in range(2)]
        XR = [sb(es, [128, 1024], F32) for _ in range(2)]
        XN = [sb(es, [128, 1024], BF16) for _ in range(2)]
        wstage = XA
        wsf = XR[0][:].rearrange("p (h t) -> p h t", h=8)
        junk = None
        ssq = sb(es, [128, 64], F32)
        T.op('vector', MEMSET(ssq[:], 0.0), writes=['ssqall'])
        rs = [sb(es, [128, 1], F32) for _ in range(2)]
        tmpf = sb(es, [128, 8, 128], F32)
        HT = sb(es, [128, 8, 512], BF16)
        QT = sb(es, [128, 4, 512], BF16)
        UT = sb(es, [128, 4, 512], BF16)
        VG = sb(es, [128, 512], F32)
        VG2 = sb(es, [128, 512], F32)
        v8 = sb(es, [128, 8], F32)
        VN = sb(es, [128, 4, 4, 3, 64], BF16)
        YS = sb(es, [128, 4, 512], BF16)
        YF = sb(es, [128, 4, 512], BF16)
        SQ = sb(es, [128, 4, 512], BF16)
        zt = sb(es, [128, 4, 128], F32)
        KB = [sb(es, [128, 2048], BF16) for _ in range(2)]
        VB = [sb(es, [128, 16, 192], BF16) for _ in range(2)]
        PT = [sb(es, [128, 512], BF16) for _ in range(3)]
        BI = sb(es, [128, 2, 2, 64], F32)
        rc = sb(es, [128, 512], F32)
        rsS = sb(es, [128, 4], F32)
        rsF = sb(es, [128, 4], F32)
        t1 = [sb(es, [128, 512], F32) for _ in range(2)]
        X1 = XR
        tp = ps(es, [128, 8, 128], BF16)
        st = [ps(es, [128, 512], F32) for _ in range(4)]
        OT = [ps(es, [128, 512], F32) for _ in range(2)]
        sm = ps(es, [128, 512], F32)

        T.dma('gpsimd', 'wq_u', WQ[:, :, 0:1024], win_v[:, :, 0:1024], writes=['wq_uv'])
        T.dma('gpsimd', 'wq_q', WQ[:, :, 1024:1536], win_v[:, :, 1024:1536], writes=['wq_q'])
        T.dma('gpsimd', 'mk', MK[:], masks_d.rearrange("m p q -> p m q"), writes=['mk'])
        T.dma('sync', 'xr0', wsf, wsT_d, writes=['xr0'])
        T.dma('sync', 'gsg', gsg[:], gsgu_d[0:1, :].broadcast_to([128, 512]), writes=['gsg'])
        T.dma('sync', 'gos', gos[:], gos_d, writes=['gos'])
        T.dma('sync', 'gof', gof[:], gof_d, writes=['gof'])
        for h in range(8):
            T.dma('sync', 'Bb', Bb[(h % 2) * 64:(h % 2) * 64 + 64, h // 2, :], bsp_d[h:h + 1, :].broadcast_to([64, 128]),
                  writes=[f'Bb{h}'])
        T.op('gpsimd', ASEL(wsf, wsf, [[0, 8], [1, 128]], ALU.is_ge, 0.0, 0, -1), reads=['xr0'], writes=['xr0'])
        T.op('vector', CP(WsT[:], wsf), reads=['xr0'], writes=['wst'])
        T.op('gpsimd', MEMSET(VN[:, :, :, 1, :], 0.0), writes=['vn'])
        wout_v = wout_d.rearrange("(kt p) n -> p kt n", p=128)
        for kt in range(8):
            wsb = kt % 2
            T.dma('sync', f'xa{wsb}', wstage[wsb][:], wout_v[:, kt, :], writes=[f'xa{wsb}'])
            gg = gos[:, kt:kt + 1] if kt < 4 else gof[:, kt - 4:kt - 3]
            T.op('vector', TS(WO[:, kt, :], wstage[wsb][:], gg, None, ALU.mult), reads=[f'xa{wsb}', 'gos', 'gof'],
                 writes=['wo'])
        Bbn = [f'Bb{h}' for h in range(8)]

        sti = [0]
        pti = [0]
        kvi = [0]

        def next_st():
            k = sti[0] % 4
            sti[0] += 1
            return st[k], f'st{k}'

        bglob = 0
        for i in range(8):
            nk = NK(i)
            par = i % 2
            for bl in range(4):
                g = i * 4 + bl
                ln1_block(xown[g * 128:(g + 1) * 128, :], XA, XN, junk, ssq, rs, tp, tmpf,
                          HT[:, :, bl * 128:(bl + 1) * 128], g, 'ht')
            for p in range(4):
                m, mn = next_st()
                T.op('tensor', mmgrp([(m[:], WQ[:, kt, 1024 + p * 128:1024 + (p + 1) * 128], HT[:, kt, :], kt == 0, kt == 7)
                                      for kt in range(8)]), reads=['wq_q', 'ht'], writes=[mn])
                T.op('vector', TS(QT[:, p, :], m[:], 0.125, None, ALU.mult), reads=[mn], writes=['qt'])
            for p in range(4):
                m, mn = next_st()
                T.op('tensor', mmgrp([(m[:], WQ[:, kt, p * 128:(p + 1) * 128], HT[:, kt, :], kt == 0, kt == 7)
                                      for kt in range(8)]), reads=['wq_uv', 'ht'], writes=[mn])
                T.op('scalar', ACT(UT[:, p, :], m[:], AF.Gelu_apprx_tanh), reads=[mn], writes=['ut'])
            for bl in range(4):
                m, mn = next_st()
                T.op('tensor', mmgrp([(m[:], HT[:, kt, bl * 128:(bl + 1) * 128], WQ[:, kt, 512:1024], kt == 0, kt == 7)
                                      for kt in range(8)]), reads=['wq_uv', 'ht'], writes=[mn])
                T.op('scalar', ACT(VG[:], m[:], AF.Gelu_apprx_tanh), reads=[mn], writes=['vg'])
                T.op('vector', TT(VG2[:], VG[:], VG[:], ALU.mult), reads=['vg'], writes=['vg2'])
                T.op('vector', RED(v8[:], VG2[:].rearrange("t (h d) -> t h d", h=8), ALU.add), reads=['vg2'], writes=['v8'])
                T.op('scalar', ACT(v8[:], v8[:], AF.Ln, scale=1.0 / 64, bias=eps_t[:]), reads=['v8', 'eps'], writes=['v8'])
                T.op('scalar', ACT(v8[:], v8[:], AF.Exp, scale=-0.5), reads=['v8'], writes=['v8'])
                T.op('vector', TT(VG2[:].rearrange("t (h d) -> t h d", h=8), VG[:].rearrange("t (h d) -> t h d", h=8),
                                  v8[:].unsqueeze(2).broadcast_to([128, 8, 64]), ALU.mult), reads=['vg', 'v8'], writes=['vg2'])
                T.op('vector', TT(VN[:, bl, :, 0::2, :], VG2[:].rearrange("t (p two d) -> t p two d", p=4, two=2),
                                  gsg[:].rearrange("t (p two d) -> t p two d", p=4, two=2), ALU.mult),
                     reads=['vg2', 'gsg'], writes=['vn'])
            if i == 0:
                dump("HT", HT[:], [128, 8, 512], BF16, ['ht'])
                dump("QT", QT[:], [128, 4, 512], BF16, ['qt'])
                dump("UT", UT[:], [128, 4, 512], BF16, ['ut'])
                dump("VN", VN[:].rearrange("t b q three d -> t (b q three d)"), [128, 3072], BF16, ['vn'])
                dump("GT1", GT1[:], [128, 1024], F32, ['GT10', 'GT11'])
                dump("A1", A1[:], [128, 8], F32, ['A1'])
                dump("B1", B1[:], [128, 8], F32, ['B1'])
                dump("Call", C_all[:].rearrange("p b h -> p (b h)"), [128, 512], F32, ['Call'])
                dump("RC", RC[:].rearrange("p i h -> p (i h)"), [128, 64], F32, ['RC'])
            for bl in range(4):
                lst = []
                for p in range(4):
                    vnp = VN[:, bl, p, :, :].rearrange("t three d -> t (three d)")
                    lst.append((sm[:, p * 128:(p + 1) * 128], vnp[:, 0:128], WsT[:, 2 * p, :], True, False))
                    lst.append((sm[:, p * 128:(p + 1) * 128], vnp[:, 64:192], WsT[:, 2 * p + 1, :], False, True))
                T.op('tensor', mmgrp(lst), reads=['vn', 'wst'], writes=['sm'])
                T.op('vector', TT(zt[:], sm[:].rearrange("d (p t) -> d p t", p=4), Bb[:], ALU.add), reads=['sm'] + Bbn,
                     writes=['zt'])
                T.op('vector', TT(YS[:, :, bl * 128:(bl + 1) * 128], zt[:], UT[:, :, bl * 128:(bl + 1) * 128], ALU.mult),
                     reads=['zt', 'ut'], writes=['ys'])
            T.op('vector', TT(SQ[:], YS[:], YS[:], ALU.mult), reads=['ys'], writes=['sq'])
            T.op('tensor', mmgrp([(sm[:, bl:bl + 1], SQ[:, p, bl * 128:(bl + 1) * 128], ones_bf[:], p == 0, p == 3)
                                  for bl in range(4) for p in range(4)]), reads=['sq', 'onesbf'], writes=['sm'])
            T.op('scalar', ACT(rsS[:], sm[:, 0:4], AF.Ln, scale=1.0 / 512, bias=eps_t[:]), reads=['sm', 'eps'], writes=['rsS'])
            T.op('scalar', ACT(rsS[:], rsS[:], AF.Exp, scale=-0.5), reads=['rsS'], writes=['rsS'])
            for p in range(4):
                pq = p % 2
                for hh in range(2):
                    h = 2 * p + hh
                    T.op('vector', TS(BI[:, pq, hh, :], C_all[:, :, h], RC[:, i, h:h + 1], None, ALU.subtract),
                         reads=['Call', 'RC'], writes=[f'bi{pq}{hh}'])
                units = [(jb, hh) for jb in range(nk) for hh in range(2)]
                nchunks = (nk + 15) // 16
                chunk_buf = {}

                def load_chunk(c):
                    kb = kvi[0] % 2
                    kvi[0] += 1
                    n = min(16, nk - c * 16)
                    T.dma('sync', f'kb{kb}', KB[kb][:, 0:n * 128], KT_d[p, :, c * 2048:c * 2048 + n * 128],
                          reads=[f'KT_d{r}' for r in range(c * 4, (c * 16 + n + 3) // 4)], writes=[f'kb{kb}'])
                    T.dma('sync', f'vb{kb}', VB[kb][:, 0:n, :], V_d[p, :, c * 16:c * 16 + n, :],
                          reads=[f'V_d{r}_{p}' for r in range(c * 4, (c * 16 + n + 3) // 4)], writes=[f'vb{kb}'])
                    chunk_buf[c] = kb

                load_chunk(0)
                if nchunks > 1:
                    load_chunk(1)
                ubank = {}

                def emit_qk(n):
                    jb, hh = units[n]
                    c = jb // 16
                    kb = chunk_buf[c]
                    jl = jb - c * 16
                    m, mn = next_st()
                    ubank[n] = (m, mn)
                    r0 = hh * 64
                    lst = [(m[:], KB[kb][r0:r0 + 64, jl * 128:(jl + 1) * 128], QT[r0:r0 + 64, p, :], True, jb < nk - 8)]
                    rd = [f'kb{kb}', 'qt']
                    if jb >= nk - 8:
                        lst.append((m[:], ident_b[:], MK[:, par * 8 + (jb - (nk - 8)), :], False, True))
                        rd += ['identb', 'mk']
                    T.op('tensor', mmgrp(lst), reads=rd, writes=[mn])

                def emit_rest(n):
                    jb, hh = units[n]
                    c = jb // 16
                    kb = chunk_buf[c]
                    jl = jb - c * 16
                    m, mn = ubank.pop(n)
                    k3 = pti[0] % 3
                    pti[0] += 1
                    T.op('scalar', ACT(PT[k3][:], m[:], AF.Exp, bias=BI[:, pq, hh, jb:jb + 1]), reads=[mn, f'bi{pq}{hh}'],
                         writes=[f'pt{k3}'])
                    vb = VB[kb][:, jl, hh * 64:hh * 64 + 128]
                    T.op('tensor', mmgrp([(OT[hh][:], vb, PT[k3][:], jb == 0, jb == nk - 1)]), reads=[f'vb{kb}', f'pt{k3}'],
                         writes=[f'ot{hh}'])

                T.op('tensor', mmgrp([(sm[:], ident_b[:], MK[:, 0, :], True, True) for _ in range(WARM)]), reads=['identb', 'mk', 'sm'],
                     writes=['sm'])
                LA = 3
                for n in range(min(LA, len(units))):
                    emit_qk(n)
                for n in range(len(units)):
                    emit_rest(n)
                    jbd, hhd = units[n]
                    if hhd == 1 and jbd % 16 == 15 and jbd // 16 + 2 < nchunks:
                        load_chunk(jbd // 16 + 2)
                    nn = n + LA
                    if nn < len(units):
                        emit_qk(nn)
                T.op('vector', RECIP(rc[0:64, :], OT[0][64:128, :]), reads=['ot0'], writes=['rca'])
                T.op('vector', RECIP(rc[64:128, :], OT[1][0:64, :]), reads=['ot1'], writes=['rcb'])
                T.op('vector', TT(YF[0:64, p, :], OT[0][0:64, :], rc[0:64, :], ALU.mult), reads=['ot0', 'rca'], writes=['yfa'])
                T.op('vector', TT(YF[64:128, p, :], OT[1][64:128, :], rc[64:128, :], ALU.mult), reads=['ot1', 'rcb'],
                     writes=['yfb'])
            T.op('vector', TT(SQ[:], YF[:], YF[:], ALU.mult), reads=['yfa', 'yfb'], writes=['sq'])
            T.op('tensor', mmgrp([(sm[:, 4 + bl:5 + bl], SQ[:, p, bl * 128:(bl + 1) * 128], ones_bf[:], p == 0, p == 3)
                                  for bl in range(4) for p in range(4)]), reads=['sq', 'onesbf'], writes=['sm'])
            T.op('scalar', ACT(rsF[:], sm[:, 4:8], AF.Ln, scale=1.0 / 512, bias=eps_t[:]), reads=['sm', 'eps'], writes=['rsF'])
            T.op('scalar', ACT(rsF[:], rsF[:], AF.Exp, scale=-0.5), reads=['rsF'], writes=['rsF'])
            if i == 0:
                dump("YS", YS[:], [128, 4, 512], BF16, ['ys'])
                dump("YF", YF[:], [128, 4, 512], BF16, ['yfa', 'yfb'])
                dump("rsS", rsS[:], [128, 4], F32, ['rsS'])
                dump("rsF", rsF[:], [128, 4], F32, ['rsF'])
            for bl in range(4):
                g = i * 4 + bl
                xb = g % 2
                T.dma('sync', f'xr{xb}', XR[xb][:], xown[g * 128:(g + 1) * 128, :], writes=[f'xr{xb}'])
                for hf in range(2):
                    ms, msn = next_st()
                    T.op('tensor', mmgrp([(ms[:], YS[:, p, bl * 128:(bl + 1) * 128], WO[:, p, hf * 512:(hf + 1) * 512], p == 0, p == 3)
                                          for p in range(4)]), reads=['ys', 'wo'], writes=[msn])
                    mf, mfn = next_st()
                    T.op('tensor', mmgrp([(mf[:], YF[:, p, bl * 128:(bl + 1) * 128], WO[:, 4 + p, hf * 512:(hf + 1) * 512], p == 0, p == 3)
                                          for p in range(4)]), reads=['yfa', 'yfb', 'wo'], writes=[mfn])
                    tb = hf
                    T.op('vector', TS(t1[tb][:], ms[:], rsS[:, bl:bl + 1], None, ALU.mult), reads=[msn, 'rsS'], writes=[f't1{tb}'])
                    T.op('vector', STT(t1[tb][:], mf[:], rsF[:, bl:bl + 1], t1[tb][:], ALU.mult, ALU.add),
                         reads=[mfn, 'rsF', f't1{tb}'], writes=[f't1{tb}'])
                    T.op('vector', TT(t1[tb][:], t1[tb][:], GT1[:, hf * 512:(hf + 1) * 512], ALU.mult),
                         reads=[f't1{tb}', 'GT10', 'GT11'], writes=[f't1{tb}'])
                    T.op('vector', TT(X1[xb][:, hf * 512:(hf + 1) * 512], t1[tb][:], XR[xb][:, hf * 512:(hf + 1) * 512], ALU.add),
                         reads=[f't1{tb}', f'xr{xb}'], writes=[f'xr{xb}'])
                T.dma('gpsimd', f'x1o{xb}', X1_d[g * 128:(g + 1) * 128, :], X1[xb][:], reads=[f'xr{xb}'], writes=[f'X1_d{g}'])
        T.barrier()

    CAP = 768
    NT0 = CAP // 128
    if "C" in DEBUG["phases"]:
      csem = {}
      with ExitStack() as es0:
        IDX = sb(es0, [128, 32, 2], U32, "IDX")
        W12 = sb(es0, [128, 32, 2], F32, "W12")
        gfin = sb(es0, [128, 1024], F32, "gfin")
        T.dma('sync', 'gfin', gfin[:], gfin_d[0:1, :].broadcast_to([128, 1024]), writes=['gfin'])
        with ExitStack() as es:
            X1t = [sb(es, [128, 1024], F32) for _ in range(2)]
            xn2 = sb(es, [128, 1024], F32)
            junk = sb(es, [128, 1024], BF16)
            H2f = sb(es, [128, 8, 128], F32)
            tmpg = sb(es, [128, 8, 128], F32)
            H2row = [sb(es, [128, 1024], BF16) for _ in range(2)]
            hrt = sb(es, [128, 1024], F32)
            A2R = sb(es, [128, 1024], F32)
            B2R = sb(es, [128, 1024], F32)
            dg = sb(es, [128, 128], F32)
            onesf = sb(es, [128, 128], F32)
            WR = sb(es, [128, 8, 20], F32)
            brb = sb(es, [128, 20], F32)
            EB = sb(es, [128, 16], F32)
            Cn = sb(es, [128, 32, 16], F32)
            L = sb(es, [128, 20], F32)
            s1 = sb(es, [128, 16], F32)
            s2 = sb(es, [128, 16], F32)
            E1 = sb(es, [128, 16], F32)
            E2 = sb(es, [128, 16], F32)
            Mx = sb(es, [128, 16], F32)
            oh = sb(es, [128, 4], F32)
            mk1 = sb(es, [128, 4], F32)
            mk2 = sb(es, [128, 4], F32)
            les = sb(es, [128, 4], F32)
            le2 = sb(es, [128, 4], F32)
            sc = sb(es, [128, 12], F32)
            idf = sb(es, [128, 2], F32)
            CNTi = sb(es, [128, 16], I32)
            ssq = sb(es, [128, 32], F32)
            T.op('vector', MEMSET(ssq[:], 0.0), writes=['ssqall'])
            rs = sb(es, [128, 1], F32)
            tpc = ps(es, [128, 8, 128], F32)
            pm = ps(es, [128, 512], F32)

            T.dma('sync', 'WR', WR[:], wr_d.rearrange("(kt p) n -> p kt n", p=128), writes=['WR'])
            T.dma('sync', 'brb', brb[:], br_d[0:1, :].broadcast_to([128, 20]), writes=['brb'])
            T.dma('sync', 'EB', EB[:], eb_d[0:1, :].broadcast_to([128, 16]), writes=['EB'])
            T.op('gpsimd', MEMSET(onesf[:], 1.0), writes=['onesf'])
            for (colt, rowt, nm) in ((A2, A2R, 'A2R'), (B2, B2R, 'B2R')):
                for kt in range(8):
                    T.op('vector', TS(dg[:], ident_f[:], colt[:, kt:kt + 1], None, ALU.mult), reads=['identf', 'A2', 'B2', 'dg'],
                         writes=['dg'])
                    T.op('tensor', mmgrp([(pm[:, 0:128], onesf[:], dg[:], True, True)]), reads=['dg', 'onesf'], writes=['pm'])
                    T.op('vector', CP(rowt[:, kt * 128:(kt + 1) * 128], pm[:, 0:128]), reads=['pm'], writes=[nm])
            for g in range(32):
                xb = g % 2
                T.dma('sync', f'x1t{xb}', X1t[xb][:], X1_d[g * 128:(g + 1) * 128, :], reads=[f'X1_d{g}'], writes=[f'x1t{xb}'])
                T.op('scalar', ACT(junk[:], X1t[xb][:], AF.Square, accum_out=ssq[:, g:g + 1]), reads=[f'x1t{xb}', 'ssqall'],
                     writes=['junk', f'ssq_{g}'])
                T.op('scalar', ACT(rs[:], ssq[:, g:g + 1], AF.Ln, scale=1.0 / 1024, bias=eps_t[:]), reads=[f'ssq_{g}', 'eps'], writes=['rs'])
                T.op('scalar', ACT(rs[:], rs[:], AF.Exp, scale=-0.5), reads=['rs'], writes=['rs'])
                T.op('vector', TS(xn2[:], X1t[xb][:], rs[:, 0:1], None, ALU.mult), reads=[f'x1t{xb}', 'rs'], writes=['xn2'])
                T.op('vector', TT(hrt[:], xn2[:], A2R[:], ALU.mult), reads=['xn2', 'A2R'], writes=['hrt'])
                T.op('vector', TT(H2row[xb][:], hrt[:], B2R[:], ALU.add), reads=['hrt', 'B2R'], writes=[f'h2row{xb}'])
                T.op('tensor', tpgrp([(tpc[:, kt, :], xn2[:, kt * 128:(kt + 1) * 128], ident_f[:]) for kt in range(8)]),
                     reads=['xn2', 'identf'], writes=['tpc'])
                T.op('vector', TT(tmpg[:], tpc[:], A2[:].unsqueeze(2).broadcast_to([128, 8, 128]), ALU.mult), reads=['tpc', 'A2'],
                     writes=['tmpg'])
                T.op('vector', TT(H2f[:], tmpg[:], B2[:].unsqueeze(2).broadcast_to([128, 8, 128]), ALU.add), reads=['tmpg', 'B2'],
                     writes=['h2f'])
                T.op('tensor', mmgrp([(pm[:, 0:20], H2f[:, kt, :], WR[:, kt, :], kt == 0, kt == 7) for kt in range(8)]),
                     reads=['h2f', 'WR'], writes=['pm'])
                T.op('vector', TT(L[:], pm[:, 0:20], brb[:], ALU.add), reads=['pm', 'brb'], writes=['L'])
                rr = ['L']
                V = lambda fn: T.op('vector', fn, reads=rr, writes=rr)
                S = lambda fn: T.op('scalar', fn, reads=rr, writes=rr)
                lg = L[:, 0:4]
                le = L[:, 4:20].rearrange("t (g e) -> t g e", g=4)
                V(lambda e: e.reduce_max(out=sc[:, 0:1], in_=lg, axis=AX.X))
                V(TS(oh[:], lg, sc[:, 0:1], None, ALU.is_equal))
                V(TS(sc[:, 1:2], sc[:, 0:1], -1.0, None, ALU.mult))
                V(MEMSET(sc[:, 2:3], 0.0))
                S(ACT(s1[:, 0:4], lg, AF.Exp, bias=sc[:, 1:2], accum_out=sc[:, 2:3]))
                V(RECIP(sc[:, 3:4], sc[:, 2:3]))
                V(TT(s2[:].rearrange("t (g e) -> t g e", g=4), le, oh[:].unsqueeze(2).broadcast_to([128, 4, 4]), ALU.mult))
                V(RED(les[:], s2[:].rearrange("t (g e) -> t e g", g=4), ALU.add))
                V(lambda e: e.reduce_max(out=sc[:, 4:5], in_=les[:], axis=AX.X))
                V(TS(mk1[:], les[:], sc[:, 4:5], None, ALU.is_equal))
                V(STT(le2[:], mk1[:], NEG, les[:], ALU.mult, ALU.add))
                V(lambda e: e.reduce_max(out=sc[:, 5:6], in_=le2[:], axis=AX.X))
                V(TS(mk2[:], le2[:], sc[:, 5:6], None, ALU.is_equal))
                V(TT(sc[:, 6:7], sc[:, 5:6], sc[:, 4:5], ALU.subtract))
                S(ACT(sc[:, 7:8], sc[:, 6:7], AF.Exp))
                V(TS(sc[:, 8:9], sc[:, 7:8], 1.0, None, ALU.add))
                V(RECIP(sc[:, 8:9], sc[:, 8:9]))
                V(TT(W12[:, g, 0:1], sc[:, 8:9], sc[:, 3:4], ALU.mult))
                V(TT(W12[:, g, 1:2], sc[:, 7:8], W12[:, g, 0:1], ALU.mult))
                V(TT(E1[:].rearrange("t (g e) -> t g e", g=4), oh[:].unsqueeze(2).broadcast_to([128, 4, 4]),
                     mk1[:].unsqueeze(1).broadcast_to([128, 4, 4]), ALU.mult))
                V(TT(E2[:].rearrange("t (g e) -> t g e", g=4), oh[:].unsqueeze(2).broadcast_to([128, 4, 4]),
                     mk2[:].unsqueeze(1).broadcast_to([128, 4, 4]), ALU.mult))
                V(TT(Mx[:], E1[:], E2[:], ALU.add))
                lst = [(pm[:, 32:48], Umat[:], Mx[:], True, g == 0)]
                if g > 0:
                    lst.append((pm[:, 32:48], Sel127[:], Cn[:, g - 1, :], False, True))
                T.op('tensor', mmgrp(lst), reads=['L', 'U', 'sel127', 'Cn'], writes=['pm'])
                T.op('vector', CP(Cn[:, g, :], pm[:, 32:48]), reads=['pm'], writes=['Cn'])
                T.op('vector', TT(s1[:], Cn[:, g, :], EB[:], ALU.add), reads=['Cn', 'EB', 'L'], writes=['L'])
                V(TT(s2[:], s1[:], E1[:], ALU.mult))
                V(RED(idf[:, 0:1], s2[:], ALU.add))
                V(TT(s2[:], s1[:], E2[:], ALU.mult))
                V(RED(idf[:, 1:2], s2[:], ALU.add))
                T.op('vector', CP(IDX[:, g, :], idf[:]), reads=['L'], writes=[f'idx{g}'])
                for k in range(2):
                    T.idma(f'sc{xb}{k}', Xs_d, bass.IndirectOffsetOnAxis(ap=IDX[:, g, k:k + 1], axis=0), H2row[xb][:], None,
                           reads=[f'idx{g}', f'h2row{xb}'], writes=[f'Xs{g}_{k}'])
            T.op('tensor', mmgrp([(pm[:, 64:80], Sel127[:], Cn[:, 31, :], True, True)]), reads=['Cn', 'sel127'], writes=['pm'])
            T.op('vector', CP(CNTi[:], pm[:, 64:80]), reads=['pm'], writes=['cnti'])
            T.dma('sync', 'cntd', CNT_d, CNTi[0:1, :], reads=['cnti'], writes=['CNT_d'])
            dump("IDX", IDX[:].rearrange("p g k -> p (g k)"), [128, 64], U32, [f'idx{g}' for g in range(32)])
            dump("W12", W12[:].rearrange("p g k -> p (g k)"), [128, 64], F32, ['L'])
            dump("Cn", Cn[:].rearrange("p g e -> p (g e)"), [128, 512], F32, ['Cn'])
            T.barrier()

        with ExitStack() as es:
            WG = [sb(es, [128, 8, 512], BF16) for _ in range(2)]
            WU = [sb(es, [128, 8, 512], BF16) for _ in range(2)]
            WD = [sb(es, [128, 4, 1024], BF16) for _ in range(2)]
            XT = [sb(es, [128, 1024], BF16) for _ in range(3)]
            XsT = [sb(es, [128, 8, 512], BF16) for _ in range(2)]
            AT = [sb(es, [128, 4, 512], BF16) for _ in range(2)]
            sg = [sb(es, [128, 512], F32) for _ in range(2)]
            YT = [sb(es, [128, 1024], F32) for _ in range(2)]
            tpb = ps(es, [128, 8, 128], BF16)
            gp = [ps(es, [128, 512], F32) for _ in range(2)]
            up = [ps(es, [128, 512], F32) for _ in range(2)]
            yp = [ps(es, [128, 512], F32) for _ in range(2)]
            wg_v = wg_d.rearrange("e (kt p) n -> e p kt n", p=128)
            wu_v = wu_d.rearrange("e (kt p) n -> e p kt n", p=128)
            wd_v = wd_d.rearrange("e (kt p) n -> e p kt n", p=128)
            gi = [0]
            yi = [0]
            xi = [0]
            yti = [0]
            gri = [0]

            def load_w(ex):
                wb = ex % 2
                T.dma('gpsimd', f'wg{wb}', WG[wb][:], wg_v[ex], writes=[f'wg{wb}'])
                T.dma('gpsimd', f'wu{wb}', WU[wb][:], wu_v[ex], writes=[f'wu{wb}'])
                T.dma('gpsimd', f'wd{wb}', WD[wb][:], wd_v[ex], writes=[f'wd{wb}'])

            def emit_group(ex, grp):
                wb = ex % 2
                ab = gri[0] % 2
                gri[0] += 1
                base = ex * 4096 + grp * 512
                for tl in range(4):
                    k3 = xi[0] % 3
                    xi[0] += 1
                    r0 = base + tl * 128
                    T.dma('sync', f'xt{k3}', XT[k3][:], Xs_d[r0:r0 + 128, :], writes=[f'xt{k3}'])
                    T.op('tensor', tpgrp([(tpb[:, kt, :], XT[k3][:, kt * 128:(kt + 1) * 128], ident_b[:]) for kt in range(8)]),
                         reads=[f'xt{k3}', 'identb'], writes=['tpb'])
                    T.op('vector', CP(XsT[ab][:, :, tl * 128:(tl + 1) * 128], tpb[:]), reads=['tpb'], writes=[f'xst{ab}'])
                for ht in range(4):
                    k2 = gi[0] % 2
                    gi[0] += 1
                    T.op('tensor', mmgrp([(gp[k2][:], WG[wb][:, kt, ht * 128:(ht + 1) * 128], XsT[ab][:, kt, :], kt == 0, kt == 7)
                                          for kt in range(8)]), reads=[f'wg{wb}', f'xst{ab}'], writes=[f'gp{k2}'])
                    T.op('tensor', mmgrp([(up[k2][:], WU[wb][:, kt, ht * 128:(ht + 1) * 128], XsT[ab][:, kt, :], kt == 0, kt == 7)
                                          for kt in range(8)]), reads=[f'wu{wb}', f'xst{ab}'], writes=[f'up{k2}'])
                    T.op('scalar', ACT(sg[k2][:], gp[k2][:], AF.Silu), reads=[f'gp{k2}'], writes=[f'sg{k2}'])
                    T.op('vector', TT(AT[ab][:, ht, :], sg[k2][:], up[k2][:], ALU.mult), reads=[f'sg{k2}', f'up{k2}'],
                         writes=[f'at{ab}'])
                for tl in range(4):
                    yb = yti[0] % 2
                    yti[0] += 1
                    for nh in range(2):
                        k2 = yi[0] % 2
                        yi[0] += 1
                        T.op('tensor', mmgrp([(yp[k2][:], AT[ab][:, ht, tl * 128:(tl + 1) * 128], WD[wb][:, ht, nh * 512:(nh + 1) * 512],
                                               ht == 0, ht == 3) for ht in range(4)]), reads=[f'at{ab}', f'wd{wb}'], writes=[f'yp{k2}'])
                        T.op('scalar', ACT(YT[yb][:, nh * 512:(nh + 1) * 512], yp[k2][:], AF.Copy), reads=[f'yp{k2}'], writes=[f'yt{yb}'])
                    r0 = base + tl * 128
                    T.dma('sync', f'yt{yb}', Ys_d[r0:r0 + 128, :], YT[yb][:], reads=[f'yt{yb}'], writes=[f'Ys{ex}_{grp}_{tl}'])

            drain = ['xt0', 'xt1', 'xt2', 'yt0', 'yt1']
            load_w(0)
            for ex in range(16):
                if ex + 1 < 16:
                    load_w(ex + 1)
                emit_group(ex, 0)
                for grp in range(1, 8):
                    T.cond_begin(CNT_d[0:1, ex:ex + 1], grp * 512, drain)
                    emit_group(ex, grp)
                    T.cond_end()
            T.barrier()

        with ExitStack() as es:
            Y1 = [sb(es, [128, 1024], F32) for _ in range(2)]
            Y2 = [sb(es, [128, 1024], F32) for _ in range(2)]
            X1t = [sb(es, [128, 1024], F32) for _ in range(2)]
            junk = sb(es, [128, 1024], BF16)
            osb = [sb(es, [128, 1024], F32) for _ in range(2)]
            ssq = sb(es, [128, 32], F32)
            T.op('vector', MEMSET(ssq[:], 0.0), writes=['ssqall'])
            rs = [sb(es, [128, 1], F32) for _ in range(2)]
            def c3_loads(g):
                b = g % 2
                T.dma('sync', f'x1c{b}', X1t[b][:], X1_d[g * 128:(g + 1) * 128, :], writes=[f'x1c{b}'])
                T.idma(f'ga{b}', Y1[b][:], None, Ys_d, bass.IndirectOffsetOnAxis(ap=IDX[:, g, 0:1], axis=0), writes=[f'y1{b}'])
                T.idma(f'gb{b}', Y2[b][:], None, Ys_d, bass.IndirectOffsetOnAxis(ap=IDX[:, g, 1:2], axis=0), writes=[f'y2{b}'])

            c3_loads(0)
            for g in range(32):
                b = g % 2
                if g + 1 < 32:
                    c3_loads(g + 1)
                T.op('vector', TS(Y1[b][:], Y1[b][:], W12[:, g, 0:1], None, ALU.mult), reads=[f'y1{b}'], writes=[f'y1{b}'])
                T.op('vector', STT(Y1[b][:], Y2[b][:], W12[:, g, 1:2], Y1[b][:], ALU.mult, ALU.add), reads=[f'y1{b}', f'y2{b}'],
                     writes=[f'y1{b}'])
                T.op('vector', TT(Y1[b][:], Y1[b][:], GT2[:], ALU.mult), reads=[f'y1{b}'], writes=[f'y1{b}'])
                T.op('vector', TT(Y1[b][:], Y1[b][:], X1t[b][:], ALU.add), reads=[f'y1{b}', f'x1c{b}'], writes=[f'y1{b}'])
                T.op('scalar', ACT(junk[:], Y1[b][:], AF.Square, accum_out=ssq[:, g:g + 1]), reads=[f'y1{b}', 'ssqall'],
                     writes=['junk', f'ssq_{g}'])
                T.op('scalar', ACT(rs[b][:], ssq[:, g:g + 1], AF.Ln, scale=1.0 / 1024, bias=eps_t[:]), reads=[f'ssq_{g}', 'eps'], writes=[f'rs{b}'])
                T.op('scalar', ACT(rs[b][:], rs[b][:], AF.Exp, scale=-0.5), reads=[f'rs{b}'], writes=[f'rs{b}'])
                T.op('vector', STT(osb[b][:], Y1[b][:], rs[b][:, 0:1], gfin[:], ALU.mult, ALU.mult),
                     reads=[f'y1{b}', f'rs{b}', 'gfin'], writes=[f'osb{b}'])
                T.dma('sync', f'out{b}', out_d[g * 128:(g + 1) * 128, :], osb[b][:], reads=[f'osb{b}'], writes=[f'out{g}'])
            T.barrier()
    T.emit()
    top.close()
    return nc


_CACHE = {}


def _masks(j):
    m = np.zeros((2, 8, 128, 512), np.float32)
    ki = np.arange(128)[:, None]
    qq = np.arange(512)[None, :]
    for par in range(2):
        i = par
        run = OWN_RUNS[j][i]
        nk = NK(i)
        for r in range(8):
            jb = nk - 8 + r
            kpos = jb * 128 + ki
            qpos = run * 512 + qq
            m[par, r] = np.where(kpos <= qpos, 0.0, NEG)
    return m.reshape(16, 128, 512)


def _rsel(j):
    s = np.zeros((8, 64), np.float32)
    for i, run in enumerate(OWN_RUNS[j]):
        s[i, 4 * run + 1] = 1.0
    return s.reshape(1, 512)


def kernel(x, c, w_ada, b_ada, g_norm_mix, w_in, g_sgu, w_spatial, b_spatial, b_forget, g_out_sgu, g_out_fox, w_out,
           g_norm_ffn, w_router_group, b_router_group, w_router_expert, b_router_expert, w_gate, w_up, w_down, g_final):
    f = lambda a: np.ascontiguousarray(np.asarray(a, dtype=np.float32))
    x = f(x); c = f(c)
    if "nc" not in _CACHE:
        _CACHE["nc"] = build_program()
    nc = _CACHE["nc"]
    wr = np.concatenate([f(w_router_group)[0], f(w_router_expert)[0].transpose(1, 0, 2).reshape(1024, 16)], axis=1)
    br = np.concatenate([f(b_router_group)[0], f(b_router_expert)[0].reshape(16)])[None, :]
    shared = {
        "w_ada": f(w_ada)[0], "b_ada": f(b_ada)[0][None, :], "g1T": f(f(g_norm_mix)[0].reshape(8, 128).T),
        "g2T": f(f(g_norm_ffn)[0].reshape(8, 128).T), "w_in": f(w_in)[0], "gsgu": f(g_sgu)[0][None, :],
        "wsT": f(f(w_spatial)[0].transpose(2, 0, 1)), "bsp": f(b_spatial)[0], "bfg": f(b_forget)[0][None, :],
        "gos": f(f(g_out_sgu)[0].reshape(4, 128).T), "gof": f(f(g_out_fox)[0].reshape(4, 128).T), "w_out": f(w_out)[0],
        "wr": f(wr), "br": f(br), "w_gate": f(w_gate)[0], "w_up": f(w_up)[0], "w_down": f(w_down)[0],
        "gfin": f(g_final)[None, :],
    }
    in_maps = []
    for core in range(8):
        b, j = core // 2, core % 2
        m = dict(shared)
        m["xall"] = x[b]
        m["xown"] = f(np.concatenate([x[b, 512 * r:512 * (r + 1)] for r in OWN_RUNS[j]], axis=0))
        m["cT"] = f(c[b].reshape(8, 128).T)
        m["masks"] = _masks(j)
        m["rsel"] = _rsel(j)
        m["eb"] = (np.arange(16, dtype=np.float32) * 4096.0 - 1.0)[None, :]
        in_maps.append(m)
    res = run_bass_kernel_spmd(nc, in_maps, core_ids=list(range(8)))
    _CACHE["res"] = res
    out = np.empty((4, 8192, 1024), np.float32)
    for core in range(8):
        b, j = core // 2, core % 2
        o = res.results[core]["out"]
        for i, r in enumerate(OWN_RUNS[j]):
            out[b, 512 * r:512 * (r + 1)] = o[512 * i:512 * (i + 1)]
    return out
```

```python
import os
import numpy as np
from contextlib import ExitStack
import concourse.bass as bass
import concourse.mybir as mybir
from concourse.bass_utils import run_bass_kernel_spmd

F32 = mybir.dt.float32
BF16 = mybir.dt.bfloat16
U32 = mybir.dt.uint32
I32 = mybir.dt.int32
AF = mybir.ActivationFunctionType
ALU = mybir.AluOpType
AX = mybir.AxisListType
ENGS = ['sync', 'scalar', 'vector', 'gpsimd', 'tensor']
EPS = 1e-6
NEG = -1.0e30
OWN_RUNS = {0: [0, 3, 4, 7, 8, 11, 12, 15], 1: [1, 2, 5, 6, 9, 10, 13, 14]}
WARM = 18
PROBE = os.environ.get('MK_PROBE', '')
DEBUG = {"x1": False, "same": True, "phases": "ABC", "dump": []}


def NK(i):
    return 16 * (i // 2) + 8 + 8 * (i % 2)


class Tracker:
    def __init__(self, nc, same_engine_sync=False):
        self.nc = nc
        self.streams = {e: [] for e in ENGS}
        self.sems = {e: nc.alloc_semaphore("c_" + e) for e in ENGS}
        self.cnt = {e: 0 for e in ENGS}
        self.waited = {e: {} for e in ENGS}
        self.res = {}
        self.slots = {}
        self.same = same_engine_sync
        self.slot_q = {}

    def _deps(self, reads, writes):
        deps = []
        for r in reads:
            st = self.res.get(r)
            if st and st[0]:
                deps.append(st[0])
        for w in writes:
            st = self.res.get(w)
            if st:
                if st[0]:
                    deps.append(st[0])
                deps.extend(st[1])
        return deps

    def _update(self, reads, writes, tag):
        for r in reads:
            st = self.res.setdefault(r, [None, []])
            st[1].append(tag)
        for w in writes:
            self.res[w] = [tag, []]

    def _emit_waits(self, eng, deps, skip_same):
        mx = {}
        for key, val in deps:
            mx[key] = max(mx.get(key, 0), val)
        for key, val in mx.items():
            if key == eng and skip_same:
                continue
            if self.waited[eng].get(key, 0) >= val:
                continue
            self.waited[eng][key] = val
            sem = self.sems[key] if key in self.sems else self.slots[key][0]
            self.streams[eng].append(lambda e, sem=sem, val=val: e.wait_ge(sem, val))

    def op(self, eng, fn, reads=(), writes=()):
        deps = self._deps(reads, writes)
        self._emit_waits(eng, deps, (not self.same) or eng == 'tensor')
        self.cnt[eng] += 1
        sem = self.sems[eng]
        self.streams[eng].append(lambda e, fn=fn, sem=sem: fn(e).then_inc(sem, 1))
        self._update(reads, writes, (eng, self.cnt[eng]))

    def dma(self, q, slot, out, in_, reads=(), writes=()):
        self.slot_q[slot] = q
        if slot not in self.slots:
            self.slots[slot] = [self.nc.alloc_semaphore("d_" + slot), 0]
        deps = self._deps(reads, writes)
        self._emit_waits(q, deps, False)
        s = self.slots[slot]
        s[1] += 16
        sem = s[0]
        self.streams[q].append(lambda e, out=out, in_=in_, sem=sem: e.dma_start(out=out, in_=in_).then_inc(sem, 16))
        self._update(reads, writes, (slot, s[1]))

    def idma(self, slot, out, out_off, in_, in_off, reads=(), writes=()):
        q = 'gpsimd'
        self.slot_q[slot] = q
        if slot not in self.slots:
            self.slots[slot] = [self.nc.alloc_semaphore("d_" + slot), 0]
        deps = self._deps(reads, writes)
        self._emit_waits(q, deps, False)
        s = self.slots[slot]
        s[1] += 16
        sem = s[0]
        self.streams[q].append(lambda e, out=out, in_=in_, sem=sem, oo=out_off, io=in_off:
                               e.indirect_dma_start(out=out, out_offset=oo, in_=in_, in_offset=io).then_inc(sem, 16))
        self._update(reads, writes, (slot, s[1]))

    def cond_begin(self, cnt_ap, thr, drain_slots):
        for e in ENGS:
            deps = [(e, self.cnt[e])] if self.cnt[e] else []
            deps += [(k, self.slots[k][1]) for k in drain_slots if k in self.slots and self.slot_q.get(k) == e]
            self._emit_waits(e, deps, False)
        self.snap_cnt = dict(self.cnt)
        self.snap_slots = {k: v[1] for k, v in self.slots.items()}
        self.snap_waited = {e: dict(w) for e, w in self.waited.items()}
        for e in ENGS:
            self.streams[e].append(('regload', cnt_ap))
            self.streams[e].append(('if', thr))

    def cond_end(self):
        for e in ENGS:
            self.streams[e].append(('else',))
            n = self.cnt[e] - self.snap_cnt[e]
            if n:
                self.streams[e].append(lambda eng, sem=self.sems[e], n=n: eng.sem_inc(sem, n))
        for slot, (sem, c) in self.slots.items():
            d = c - self.snap_slots.get(slot, 0)
            if d:
                self.streams[self.slot_q[slot]].append(lambda eng, sem=sem, d=d: eng.sem_inc(sem, d))
        for e in ENGS:
            self.streams[e].append(('endif',))
        self.waited = self.snap_waited

    def barrier(self):
        deps = [(e, c) for e, c in self.cnt.items() if c > 0]
        deps += [(k, v[1]) for k, v in self.slots.items() if v[1] > 0]
        for e in ENGS:
            self._emit_waits(e, deps, True)

    def emit(self):
        self.barrier()
        streams = self.streams

        def run(e, items):
            creg = e.alloc_register("creg")
            i = 0
            n = len(items)
            while i < n:
                it = items[i]
                if not isinstance(it, tuple):
                    it(e)
                    i += 1
                    continue
                if it[0] == 'regload':
                    e.reg_load(creg, it[1])
                    i += 1
                    continue
                assert it[0] == 'if'
                thr = it[1]
                j = i + 1
                body = []
                while not (isinstance(items[j], tuple) and items[j][0] == 'else'):
                    body.append(items[j])
                    j += 1
                j += 1
                fix = []
                while not (isinstance(items[j], tuple) and items[j][0] == 'endif'):
                    fix.append(items[j])
                    j += 1
                with e.If_lt(creg, thr + 1):
                    for f in fix:
                        f(e)
                with e.Else():
                    for f in body:
                        f(e)
                i = j + 1

        with self.nc.Block() as block:
            @block.sync
            def _(e):
                run(e, streams['sync'])

            @block.scalar
            def _(e):
                run(e, streams['scalar'])

            @block.vector
            def _(e):
                run(e, streams['vector'])

            @block.gpsimd
            def _(e):
                run(e, streams['gpsimd'])

            @block.tensor
            def _(e):
                run(e, streams['tensor'])


def mmgrp(lst):
    def fn(e):
        ins = None
        for (out, lhsT, rhs, st, sp) in lst:
            ins = e.matmul(out, lhsT=lhsT, rhs=rhs, start=st, stop=sp)
        return ins
    return fn


def tpgrp(lst):
    def fn(e):
        ins = None
        for (out, in_, ident) in lst:
            ins = e.transpose(out, in_, ident)
        return ins
    return fn


def ACT(out, in_, func, **kw):
    return lambda e: e.activation(out=out, in_=in_, func=func, **kw)


def TT(out, in0, in1, op):
    return lambda e: e.tensor_tensor(out=out, in0=in0, in1=in1, op=op)


def TS(out, in0, s1, s2, op0, op1=None):
    if op1 is None:
        return lambda e: e.tensor_scalar(out=out, in0=in0, scalar1=s1, scalar2=None, op0=op0)
    return lambda e: e.tensor_scalar(out=out, in0=in0, scalar1=s1, scalar2=s2, op0=op0, op1=op1)


def STT(out, in0, scalar, in1, op0, op1):
    return lambda e: e.scalar_tensor_tensor(out=out, in0=in0, scalar=scalar, in1=in1, op0=op0, op1=op1)


def CP(out, in_):
    return lambda e: e.tensor_copy(out=out, in_=in_)


def RED(out, in_, op):
    return lambda e: e.tensor_reduce(out=out, in_=in_, axis=AX.X, op=op)


def RECIP(out, in_):
    return lambda e: e.reciprocal(out=out, in_=in_)


def MEMSET(ap, v):
    return lambda e: e.memset(ap, v)


def ASEL(out, in_, pattern, cmp, fill, base, cm):
    return lambda e: e.affine_select(out=out, in_=in_, pattern=pattern, compare_op=cmp, fill=fill, base=base,
                                     channel_multiplier=cm)


def build_program():
    nc = bass.Bass("TRN2", target_bir_lowering=False)
    T = Tracker(nc, same_engine_sync=DEBUG["same"])
    din = lambda name, shape: nc.dram_tensor(name, shape, F32, kind="ExternalInput").ap()
    xall = din("xall", [8192, 1024])
    xown = din("xown", [4096, 1024])
    cT_d = din("cT", [128, 8])
    wada_d = din("w_ada", [1024, 6144])
    bada_d = din("b_ada", [1, 6144])
    g1T_d = din("g1T", [128, 8])
    g2T_d = din("g2T", [128, 8])
    win_d = din("w_in", [1024, 2568])
    gsgu_d = din("gsgu", [1, 512])
    wsT_d = din("wsT", [128, 8, 128])
    bsp_d = din("bsp", [8, 128])
    bfg_d = din("bfg", [1, 8])
    gos_d = din("gos", [128, 4])
    gof_d = din("gof", [128, 4])
    wout_d = din("w_out", [1024, 1024])
    wr_d = din("wr", [1024, 20])
    br_d = din("br", [1, 20])
    wg_d = din("w_gate", [16, 1024, 512])
    wu_d = din("w_up", [16, 1024, 512])
    wd_d = din("w_down", [16, 512, 1024])
    gfin_d = din("gfin", [1, 1024])
    masks_d = din("masks", [16, 128, 512])
    rsel_d = din("rsel", [1, 512])
    eb_d = din("eb", [1, 16])
    out_d = nc.dram_tensor("out", [4096, 1024], F32, kind="ExternalOutput").ap()
    KT_d = nc.dram_tensor("KT_d", [4, 128, 8192], BF16).ap()
    V_d = nc.dram_tensor("V_d", [4, 128, 64, 192], BF16).ap()
    Xs_d = nc.dram_tensor("Xs_d", [65536, 1024], BF16).ap()
    Ys_d = nc.dram_tensor("Ys_d", [65536, 1024], F32).ap()
    CNT_d = nc.dram_tensor("CNT_d", [1, 16], I32).ap()
    if DEBUG["x1"]:
        X1_d = nc.dram_tensor("X1_d", [4096, 1024], F32, kind="ExternalOutput").ap()
    else:
        X1_d = nc.dram_tensor("X1_d", [4096, 1024], F32).ap()

    win_v = win_d.rearrange("(kt p) n -> p kt n", p=128)
    dumps = DEBUG.get("dump", [])

    def dump(name, ap, shape, dt, rname):
        if name not in dumps:
            return
        d = nc.dram_tensor("dbg_" + name, shape, dt, kind="ExternalOutput").ap()
        T.dma('sync', 'dbg_' + name, d, ap, reads=rname, writes=['dbg_' + name])

    top = ExitStack()
    uid = [0]

    def sb(es, shape, dt, name=None):
        uid[0] += 1
        return es.enter_context(nc.sbuf_tensor(f"{name or 't'}_{uid[0]}", shape, dt))

    def ps(es, shape, dt, name=None):
        uid[0] += 1
        return es.enter_context(nc.psum_tensor(f"{name or 'p'}_{uid[0]}", shape, dt))

    ident_f = sb(top, [128, 128], F32, "identf")
    ident_b = sb(top, [128, 128], BF16, "identb")
    Umat = sb(top, [128, 128], F32, "U")
    Sel127 = sb(top, [128, 128], F32, "sel127")
    eps_t = sb(top, [128, 1], F32, "eps")
    one_t = sb(top, [128, 1], F32, "one")
    ones_bf = sb(top, [128, 1], BF16, "onesbf")
    A1 = sb(top, [128, 8], F32, "A1")
    B1 = sb(top, [128, 8], F32, "B1")
    A2 = sb(top, [128, 8], F32, "A2")
    B2 = sb(top, [128, 8], F32, "B2")
    GT1 = sb(top, [128, 1024], F32, "GT1")
    GT2 = sb(top, [128, 1024], F32, "GT2")
    C_all = sb(top, [128, 64, 8], F32, "Call")
    RC = sb(top, [128, 8, 8], F32, "RC")

    T.op('gpsimd', MEMSET(ident_f[:], 0.0), writes=['identf'])
    T.op('gpsimd', ASEL(ident_f[:], ident_f[:], [[-1, 128]], ALU.not_equal, 1.0, 0, 1), reads=['identf'], writes=['identf'])
    T.op('gpsimd', MEMSET(Umat[:], 1.0), writes=['U'])
    T.op('gpsimd', ASEL(Umat[:], Umat[:], [[1, 128]], ALU.is_ge, 0.0, 0, -1), reads=['U'], writes=['U'])
    T.op('gpsimd', MEMSET(Sel127[:], 1.0), writes=['sel127'])
    T.op('gpsimd', ASEL(Sel127[:], Sel127[:], [[0, 128]], ALU.is_ge, 0.0, -127, 1), reads=['sel127'], writes=['sel127'])
    T.op('gpsimd', MEMSET(eps_t[:], EPS), writes=['eps'])
    T.op('gpsimd', MEMSET(one_t[:], 1.0), writes=['one'])
    T.op('gpsimd', MEMSET(ones_bf[:], 1.0), writes=['onesbf'])
    T.op('vector', CP(ident_b[:], ident_f[:]), reads=['identf'], writes=['identb'])

    with ExitStack() as es:
        cT = sb(es, [128, 8], F32)
        CB = sb(es, [128, 8, 128], F32)
        WA = [sb(es, [128, 8, 1024], F32, "wada") for _ in range(2)]
        badab = sb(es, [128, 1024], F32)
        gT1 = sb(es, [128, 8], F32)
        gT2 = sb(es, [128, 8], F32)
        rowv = sb(es, [128, 1024], F32)
        dtmp = sb(es, [128, 8, 128], F32)
        colv = sb(es, [128, 8], F32)
        pp = [ps(es, [128, 512], F32) for _ in range(2)]
        T.dma('sync', 'cT', cT[:], cT_d, writes=['cT'])
        T.dma('sync', 'gT1', gT1[:], g1T_d, writes=['gT1'])
        T.dma('sync', 'gT2', gT2[:], g2T_d, writes=['gT2'])
        T.op('scalar', ACT(cT[:], cT[:], AF.Silu), reads=['cT'], writes=['cT'])
        T.op('vector', CP(CB[:], cT[:].unsqueeze(2).broadcast_to([128, 8, 128])), reads=['cT'], writes=['CB'])
        wada_v = wada_d.rearrange("(kt p) n -> p kt n", p=128)
        order = [1, 0, 2, 4, 3, 5]
        for n, v in enumerate(order):
            wb = n % 2
            for kt in range(8):
                T.dma('sync', f'wada{wb}_{kt}', WA[wb][:, kt, :], wada_v[:, kt, v * 1024:(v + 1) * 1024],
                      writes=[f'wada{wb}_{kt}'])
            T.dma('gpsimd', 'badab', badab[:], bada_d[0:1, v * 1024:(v + 1) * 1024].broadcast_to([128, 1024]),
                  writes=['badab'])
            for hf in range(2):
                T.op('tensor', mmgrp([(pp[hf][:], CB[:, kt, :], WA[wb][:, kt, hf * 512:(hf + 1) * 512], kt == 0, kt == 7)
                                      for kt in range(8)]),
                     reads=['CB'] + [f'wada{wb}_{kt}' for kt in range(8)], writes=[f'pp{hf}'])
                dst = {2: GT1, 5: GT2}.get(v, rowv)
                dname = {2: 'GT1', 5: 'GT2'}.get(v, 'rowv') + str(hf)
                T.op('vector', TT(dst[:, hf * 512:(hf + 1) * 512], pp[hf][:], badab[:, hf * 512:(hf + 1) * 512], ALU.add),
                     reads=[f'pp{hf}', 'badab'], writes=[dname])
            if v in (2, 5):
                continue
            T.op('vector', TT(dtmp[:], rowv[:].rearrange("p (k t) -> p k t", k=8),
                              ident_f[:].unsqueeze(1).broadcast_to([128, 8, 128]), ALU.mult),
                 reads=['rowv0', 'rowv1', 'identf'], writes=['dtmp'])
            T.op('vector', RED(colv[:], dtmp[:], ALU.add), reads=['dtmp'], writes=['colv'])
            if v == 1:
                T.op('vector', STT(A1[:], colv[:], 1.0, gT1[:], ALU.add, ALU.mult), reads=['colv', 'gT1'], writes=['A1'])
            elif v == 0:
                T.op('vector', CP(B1[:], colv[:]), reads=['colv'], writes=['B1'])
            elif v == 4:
                T.op('vector', STT(A2[:], colv[:], 1.0, gT2[:], ALU.add, ALU.mult), reads=['colv', 'gT2'], writes=['A2'])
            elif v == 3:
                T.op('vector', CP(B2[:], colv[:]), reads=['colv'], writes=['B2'])
        T.barrier()

    def ln1_block(xsrc_rows, XA, XN, junk, ssq, rs, tp, tmpf, HT_dst, bi, htname):
        b = bi % 2
        T.dma('sync', f'xa{b}', XA[b][:], xsrc_rows, writes=[f'xa{b}'])
        T.op('scalar', ACT(XN[b][:], XA[b][:], AF.Square, accum_out=ssq[:, bi:bi + 1]), reads=[f'xa{b}', 'ssqall'],
             writes=[f'xn{b}', f'ssq_{bi}'])
        T.op('scalar', ACT(rs[b][:], ssq[:, bi:bi + 1], AF.Ln, scale=1.0 / 1024, bias=eps_t[:]), reads=[f'ssq_{bi}', 'eps'],
             writes=[f'rs{b}'])
        T.op('scalar', ACT(rs[b][:], rs[b][:], AF.Exp, scale=-0.5), reads=[f'rs{b}'], writes=[f'rs{b}'])
        T.op('vector', TS(XN[b][:], XA[b][:], rs[b][:, 0:1], None, ALU.mult), reads=[f'xa{b}', f'rs{b}'], writes=[f'xn{b}'])
        T.op('tensor', tpgrp([(tp[:, kt, :], XN[b][:, kt * 128:(kt + 1) * 128], ident_b[:]) for kt in range(8)]),
             reads=[f'xn{b}', 'identb'], writes=['tp'])
        T.op('vector', TT(tmpf[:], tp[:], A1[:].unsqueeze(2).broadcast_to([128, 8, 128]), ALU.mult), reads=['tp', 'A1'],
             writes=['tmpf'])
        T.op('vector', TT(HT_dst, tmpf[:], B1[:].unsqueeze(2).broadcast_to([128, 8, 128]), ALU.add), reads=['tmpf', 'B1'],
             writes=[htname])

    if "A" in DEBUG["phases"]:
      with ExitStack() as es:
        WKV = sb(es, [128, 8, 1032], BF16, "wkvf")
        XA = [sb(es, [128, 1024], F32) for _ in range(2)]
        XN = [sb(es, [128, 1024], BF16) for _ in range(2)]
        junk = None
        ssq = sb(es, [128, 64], F32)
        T.op('vector', MEMSET(ssq[:], 0.0), writes=['ssqall'])
        rs = [sb(es, [128, 1], F32) for _ in range(2)]
        tmpf = sb(es, [128, 8, 128], F32)
        HT = [sb(es, [128, 8, 512], BF16) for _ in range(2)]
        KTs = [sb(es, [128, 4, 512], BF16) for _ in range(2)]
        VBs = [sb(es, [128, 4, 4, 3, 64], BF16) for _ in range(2)]
        bfb = sb(es, [128, 8], F32)
        ft = sb(es, [128, 8], F32)
        tp = ps(es, [128, 8, 128], BF16)
        mm = [ps(es, [128, 512], F32) for _ in range(3)]
        sm = ps(es, [128, 512], F32)
        T.dma('gpsimd', 'wkvf', WKV[:], win_v[:, :, 1536:2568], writes=['wkvf'])
        T.dma('sync', 'bfb', bfb[:], bfg_d[0:1, :].broadcast_to([128, 8]), writes=['bfb'])
        for k in range(2):
            T.op('gpsimd', MEMSET(VBs[k][:, :, :, 1, :], 1.0), writes=[f'vbs{k}'])
        mi = 0
        for r in range(16):
            hb = r % 2
            for bl in range(4):
                g = r * 4 + bl
                ln1_block(xall[g * 128:(g + 1) * 128, :], XA, XN, junk, ssq, rs, tp, tmpf,
                          HT[hb][:, :, bl * 128:(bl + 1) * 128], g, f'ht{hb}')
            for p in range(4):
                m = mm[mi % 3]; mn = f'mm{mi % 3}'; mi += 1
                T.op('tensor', mmgrp([(m[:], WKV[:, kt, p * 128:(p + 1) * 128], HT[hb][:, kt, :], kt == 0, kt == 7)
                                      for kt in range(8)]), reads=['wkvf', f'ht{hb}'], writes=[mn])
                T.op('scalar', ACT(KTs[hb][:, p, :], m[:], AF.Copy), reads=[mn], writes=[f'kts{hb}'])
            T.dma('gpsimd', f'kts{hb}', KT_d[:, :, r * 512:(r + 1) * 512].rearrange("q p t -> p q t"), KTs[hb][:],
                  reads=[f'kts{hb}'], writes=[f'KT_d{r}'])
            for bl in range(4):
                g = r * 4 + bl
                m = mm[mi % 3]; mn = f'mm{mi % 3}'; mi += 1
                T.op('tensor', mmgrp([(m[:], HT[hb][:, kt, bl * 128:(bl + 1) * 128], WKV[:, kt, 512:1024], kt == 0, kt == 7)
                                      for kt in range(8)]), reads=['wkvf', f'ht{hb}'], writes=[mn])
                T.op('scalar', ACT(VBs[hb][:, bl, :, 0::2, :], m[:].rearrange("t (p two d) -> t p two d", p=4, two=2),
                                   AF.Copy), reads=[mn], writes=[f'vbs{hb}'])
                T.op('tensor', mmgrp([(sm[:, 0:8], HT[hb][:, kt, bl * 128:(bl + 1) * 128], WKV[:, kt, 1024:1032], kt == 0, kt == 7)
                                      for kt in range(8)]), reads=['wkvf', f'ht{hb}'], writes=['sm'])
                T.op('vector', TT(ft[:], sm[:, 0:8], bfb[:], ALU.add), reads=['sm', 'bfb'], writes=['ft'])
                T.op('scalar', ACT(ft[:], ft[:], AF.Exp, scale=-1.0), reads=['ft'], writes=['ft'])
                T.op('scalar', ACT(ft[:], ft[:], AF.Ln, bias=one_t[:]), reads=['ft', 'one'], writes=['ft'])
                lst = [(sm[:, 8:16], Umat[:], ft[:], True, g == 0)]
                if g > 0:
                    lst.append((sm[:, 8:16], Sel127[:], C_all[:, g - 1, :], False, True))
                T.op('tensor', mmgrp(lst), reads=['ft', 'U', 'sel127', 'Call'], writes=['sm'])
                T.op('vector', CP(C_all[:, g, :], sm[:, 8:16]), reads=['sm'], writes=['Call'])
            for q in range(4):
                T.dma('gpsimd', f'vbs{hb}_{q}', V_d[q, :, r * 4:(r + 1) * 4, :],
                      VBs[hb][:, :, q, :, :].rearrange("t b three d -> t b (three d)"), reads=[f'vbs{hb}'],
                      writes=[f'V_d{r}_{q}'])
        T.barrier()

    with ExitStack() as es:
        rselb = sb(es, [128, 8, 64], F32)
        csel = sb(es, [128, 8, 8], F32)
        tmp3 = sb(es, [128, 8, 64], F32)
        pr = ps(es, [128, 512], F32)
        T.dma('sync', 'rselb', rselb[:], rsel_d[0:1, :].broadcast_to([128, 512]).rearrange("p (i b) -> p i b", i=8),
              writes=['rselb'])
        for i in range(8):
            T.op('vector', TT(tmp3[:], C_all[:].rearrange("p b h -> p h b"),
                              rselb[:, i, :].unsqueeze(1).broadcast_to([128, 8, 64]), ALU.mult),
                 reads=['Call', 'rselb'], writes=['tmp3'])
            T.op('vector', RED(csel[:, i, :], tmp3[:], ALU.add), reads=['tmp3'], writes=['csel'])
        T.op('tensor', mmgrp([(pr[:, 0:64], Sel127[:], csel[:].rearrange("p i h -> p (i h)"), True, True)]),
             reads=['csel', 'sel127'], writes=['pr'])
        T.op('vector', CP(RC[:].rearrange("p i h -> p (i h)"), pr[:, 0:64]), reads=['pr'], writes=['RC'])
        T.barrier()

    if "B" in DEBUG["phases"]:
      with ExitStack() as es:
        WQ = sb(es, [128, 8, 1536], BF16, "wq")
        WO = sb(es, [128, 8, 1024], BF16, "wo")
        MK = sb(es, [128, 16, 512], BF16, "mk")
        WsT = sb(es, [128, 8, 128], BF16, "wst")
        Bb = sb(es, [128, 4, 128], F32)
        gsg = sb(es, [128, 512], F32)
        gos = sb(es, [128, 4], F32)
        gof = sb(es, [128, 4], F32)
        XA = [sb(es, [128, 1024], F32) for _ in range(2)]
        XR = [sb(es, [128, 1024], F32) for _ in range(2)]
        XN = [sb(es, [128, 1024], BF16) for _ in range(2)]
        wstage = XA
        wsf = XR[0][:].rearrange("p (h t) -> p h t", h=8)
        junk = None
        ssq = sb(es, [128, 64], F32)
        T.op('vector', MEMSET(ssq[:], 0.0), writes=['ssqall'])
        rs = [sb(es, [128, 1], F32) for _ in range(2)]
        tmpf = sb(es, [128, 8, 128], F32)
        HT = sb(es, [128, 8, 512], BF16)
        QT = sb(es, [128, 4, 2, 512], BF16)
        T.op('gpsimd', MEMSET(QT[:], 0.0), writes=['qt'])
        UT = sb(es, [128, 4, 512], BF16)
        VG = sb(es, [128, 512], F32)
        VG2 = sb(es, [128, 512], F32)
        v8 = sb(es, [128, 8], F32)
        VN = sb(es, [128, 4, 4, 3, 64], BF16)
        YS = sb(es, [128, 4, 512], BF16)
        YF = sb(es, [128, 4, 512], BF16)
        SQ = sb(es, [128, 4, 512], BF16)
        zt = sb(es, [128, 4, 128], F32)
        KB = [sb(es, [128, 2048], BF16) for _ in range(2)]
        VB = [sb(es, [128, 16, 192], BF16) for _ in range(2)]
        PT = [sb(es, [128, 512], BF16) for _ in range(3)]
        BI = sb(es, [128, 2, 2, 64], F32)
        rc = sb(es, [128, 512], F32)
        rsS = sb(es, [128, 4], F32)
        rsF = sb(es, [128, 4], F32)
        t1 = [sb(es, [128, 512], F32) for _ in range(2)]
        X1 = XR
        tp = ps(es, [128, 8, 128], BF16)
        st = [ps(es, [128, 512], F32) for _ in range(4)]
        OT = [ps(es, [128, 512], F32) for _ in range(2)]
        sm = ps(es, [128, 512], F32)

        T.dma('gpsimd', 'wq_u', WQ[:, :, 0:1024], win_v[:, :, 0:1024], writes=['wq_uv'])
        T.dma('gpsimd', 'wq_q', WQ[:, :, 1024:1536], win_v[:, :, 1024:1536], writes=['wq_q'])
        T.dma('gpsimd', 'mk', MK[:], masks_d.rearrange("m p q -> p m q"), writes=['mk'])
        T.dma('sync', 'xr0', wsf, wsT_d, writes=['xr0'])
        T.dma('sync', 'gsg', gsg[:], gsgu_d[0:1, :].broadcast_to([128, 512]), writes=['gsg'])
        T.dma('sync', 'gos', gos[:], gos_d, writes=['gos'])
        T.dma('sync', 'gof', gof[:], gof_d, writes=['gof'])
        for h in range(8):
            T.dma('sync', 'Bb', Bb[(h % 2) * 64:(h % 2) * 64 + 64, h // 2, :], bsp_d[h:h + 1, :].broadcast_to([64, 128]),
                  writes=[f'Bb{h}'])
        T.op('gpsimd', ASEL(wsf, wsf, [[0, 8], [1, 128]], ALU.is_ge, 0.0, 0, -1), reads=['xr0'], writes=['xr0'])
        T.op('vector', CP(WsT[:], wsf), reads=['xr0'], writes=['wst'])
        T.op('gpsimd', MEMSET(VN[:, :, :, 1, :], 0.0), writes=['vn'])
        wout_v = wout_d.rearrange("(kt p) n -> p kt n", p=128)
        for kt in range(8):
            wsb = kt % 2
            T.dma('sync', f'xa{wsb}', wstage[wsb][:], wout_v[:, kt, :], writes=[f'xa{wsb}'])
            gg = gos[:, kt:kt + 1] if kt < 4 else gof[:, kt - 4:kt - 3]
            T.op('vector', TS(WO[:, kt, :], wstage[wsb][:], gg, None, ALU.mult), reads=[f'xa{wsb}', 'gos', 'gof'],
                 writes=['wo'])
        Bbn = [f'Bb{h}' for h in range(8)]

        sti = [0]
        pti = [0]
        kvi = [0]

        def next_st():
            k = sti[0] % 4
            sti[0] += 1
            return st[k], f'st{k}'

        bglob = 0
        for i in range(8):
            nk = NK(i)
            par = i % 2
            for bl in range(4):
                g = i * 4 + bl
                ln1_block(xown[g * 128:(g + 1) * 128, :], XA, XN, junk, ssq, rs, tp, tmpf,
                          HT[:, :, bl * 128:(bl + 1) * 128], g, 'ht')
            for p in range(4):
                m, mn = next_st()
                T.op('tensor', mmgrp([(m[:], WQ[:, kt, 1024 + p * 128:1024 + (p + 1) * 128], HT[:, kt, :], kt == 0, kt == 7)
                                      for kt in range(8)]), reads=['wq_q', 'ht'], writes=[mn])
                T.op('vector', TS(QT[0:64, p, 0, :], m[0:64, :], 0.125, None, ALU.mult), reads=[mn], writes=['qt'])
                T.op('vector', TS(QT[64:128, p, 1, :], m[64:128, :], 0.125, None, ALU.mult), reads=[mn], writes=['qt'])
            for p in range(4):
                m, mn = next_st()
                T.op('tensor', mmgrp([(m[:], WQ[:, kt, p * 128:(p + 1) * 128], HT[:, kt, :], kt == 0, kt == 7)
                                      for kt in range(8)]), reads=['wq_uv', 'ht'], writes=[mn])
                T.op('scalar', ACT(UT[:, p, :], m[:], AF.Gelu_apprx_tanh), reads=[mn], writes=['ut'])
            for bl in range(4):
                m, mn = next_st()
                T.op('tensor', mmgrp([(m[:], HT[:, kt, bl * 128:(bl + 1) * 128], WQ[:, kt, 512:1024], kt == 0, kt == 7)
                                      for kt in range(8)]), reads=['wq_uv', 'ht'], writes=[mn])
                T.op('scalar', ACT(VG[:], m[:], AF.Gelu_apprx_tanh), reads=[mn], writes=['vg'])
                T.op('vector', TT(VG2[:], VG[:], VG[:], ALU.mult), reads=['vg'], writes=['vg2'])
                T.op('vector', RED(v8[:], VG2[:].rearrange("t (h d) -> t h d", h=8), ALU.add), reads=['vg2'], writes=['v8'])
                T.op('scalar', ACT(v8[:], v8[:], AF.Ln, scale=1.0 / 64, bias=eps_t[:]), reads=['v8', 'eps'], writes=['v8'])
                T.op('scalar', ACT(v8[:], v8[:], AF.Exp, scale=-0.5), reads=['v8'], writes=['v8'])
                T.op('vector', TT(VG2[:].rearrange("t (h d) -> t h d", h=8), VG[:].rearrange("t (h d) -> t h d", h=8),
                                  v8[:].unsqueeze(2).broadcast_to([128, 8, 64]), ALU.mult), reads=['vg', 'v8'], writes=['vg2'])
                T.op('vector', TT(VN[:, bl, :, 0::2, :], VG2[:].rearrange("t (p two d) -> t p two d", p=4, two=2),
                                  gsg[:].rearrange("t (p two d) -> t p two d", p=4, two=2), ALU.mult),
                     reads=['vg2', 'gsg'], writes=['vn'])
            if i == 0:
                dump("HT", HT[:], [128, 8, 512], BF16, ['ht'])
                dump("UT", UT[:], [128, 4, 512], BF16, ['ut'])
                dump("VN", VN[:].rearrange("t b q three d -> t (b q three d)"), [128, 3072], BF16, ['vn'])
                dump("GT1", GT1[:], [128, 1024], F32, ['GT10', 'GT11'])
                dump("A1", A1[:], [128, 8], F32, ['A1'])
                dump("B1", B1[:], [128, 8], F32, ['B1'])
                dump("Call", C_all[:].rearrange("p b h -> p (b h)"), [128, 512], F32, ['Call'])
                dump("RC", RC[:].rearrange("p i h -> p (i h)"), [128, 64], F32, ['RC'])
            for bl in range(4):
                lst = []
                for p in range(4):
                    vnp = VN[:, bl, p, :, :].rearrange("t three d -> t (three d)")
                    lst.append((sm[:, p * 128:(p + 1) * 128], vnp[:, 0:128], WsT[:, 2 * p, :], True, False))
                    lst.append((sm[:, p * 128:(p + 1) * 128], vnp[:, 64:192], WsT[:, 2 * p + 1, :], False, True))
                T.op('tensor', mmgrp(lst), reads=['vn', 'wst'], writes=['sm'])
                T.op('vector', TT(zt[:], sm[:].rearrange("d (p t) -> d p t", p=4), Bb[:], ALU.add), reads=['sm'] + Bbn,
                     writes=['zt'])
                T.op('vector', TT(YS[:, :, bl * 128:(bl + 1) * 128], zt[:], UT[:, :, bl * 128:(bl + 1) * 128], ALU.mult),
                     reads=['zt', 'ut'], writes=['ys'])
            T.op('vector', TT(SQ[:], YS[:], YS[:], ALU.mult), reads=['ys'], writes=['sq'])
            T.op('tensor', mmgrp([(sm[:, bl:bl + 1], SQ[:, p, bl * 128:(bl + 1) * 128], ones_bf[:], p == 0, p == 3)
                                  for bl in range(4) for p in range(4)]), reads=['sq', 'onesbf'], writes=['sm'])
            T.op('scalar', ACT(rsS[:], sm[:, 0:4], AF.Ln, scale=1.0 / 512, bias=eps_t[:]), reads=['sm', 'eps'], writes=['rsS'])
            T.op('scalar', ACT(rsS[:], rsS[:], AF.Exp, scale=-0.5), reads=['rsS'], writes=['rsS'])
            for p in range(4):
                pq = p % 2
                for hh in range(2):
                    h = 2 * p + hh
                    T.op('vector', TS(BI[:, pq, hh, :], C_all[:, :, h], RC[:, i, h:h + 1], None, ALU.subtract),
                         reads=['Call', 'RC'], writes=[f'bi{pq}{hh}'])
                units = [(jb, hh) for jb in range(nk) for hh in range(2)]
                nchunks = (nk + 15) // 16
                chunk_buf = {}

                def load_chunk(c):
                    kb = kvi[0] % 2
                    kvi[0] += 1
                    n = min(16, nk - c * 16)
                    T.dma('sync', f'kb{kb}', KB[kb][:, 0:n * 128], KT_d[p, :, c * 2048:c * 2048 + n * 128],
                          reads=[f'KT_d{r}' for r in range(c * 4, (c * 16 + n + 3) // 4)], writes=[f'kb{kb}'])
                    T.dma('sync', f'vb{kb}', VB[kb][:, 0:n, :], V_d[p, :, c * 16:c * 16 + n, :],
                          reads=[f'V_d{r}_{p}' for r in range(c * 4, (c * 16 + n + 3) // 4)], writes=[f'vb{kb}'])
                    chunk_buf[c] = kb

                load_chunk(0)
                if nchunks > 1:
                    load_chunk(1)
                ubank = {}

                def emit_qk(n):
                    jb, hh = units[n]
                    c = jb // 16
                    kb = chunk_buf[c]
                    jl = jb - c * 16
                    m, mn = next_st()
                    ubank[n] = (m, mn)
                    r0 = hh * 64
                    lst = [(m[:], KB[kb][:, jl * 128:(jl + 1) * 128], QT[:, p, hh, :], True, jb < nk - 8)]
                    rd = [f'kb{kb}', 'qt']
                    if jb >= nk - 8:
                        lst.append((m[:], ident_b[:], MK[:, par * 8 + (jb - (nk - 8)), :], False, True))
                        rd += ['identb', 'mk']
                    T.op('tensor', mmgrp(lst), reads=rd, writes=[mn])

                def emit_rest(n):
                    jb, hh = units[n]
                    c = jb // 16
                    kb = chunk_buf[c]
                    jl = jb - c * 16
                    m, mn = ubank.pop(n)
                    k3 = pti[0] % 3
                    pti[0] += 1
                    T.op('scalar', ACT(PT[k3][:], m[:], AF.Exp, bias=BI[:, pq, hh, jb:jb + 1]), reads=[mn, f'bi{pq}{hh}'],
                         writes=[f'pt{k3}'])
                    vb = VB[kb][:, jl, hh * 64:hh * 64 + 128]
                    T.op('tensor', mmgrp([(OT[hh][:], vb, PT[k3][:], jb == 0, jb == nk - 1)]),
                         reads=[f'vb{kb}'] + ([] if 'nodep' in PROBE else [f'pt{k3}']), writes=[f'ot{hh}'])

                T.op('tensor', mmgrp([(sm[:], ident_b[:], MK[:, 0, :], True, True) for _ in range(WARM)]), reads=['identb', 'mk', 'sm'],
                     writes=['sm'])
                LA = 3
                for n in range(min(LA, len(units))):
                    emit_qk(n)
                for n in range(len(units)):
                    emit_rest(n)
                    jbd, hhd = units[n]
                    if hhd == 1 and jbd % 16 == 15 and jbd // 16 + 2 < nchunks:
                        load_chunk(jbd // 16 + 2)
                    nn = n + LA
                    if nn < len(units):
                        emit_qk(nn)
                T.op('vector', RECIP(rc[0:64, :], OT[0][64:128, :]), reads=['ot0'], writes=['rca'])
                T.op('vector', RECIP(rc[64:128, :], OT[1][0:64, :]), reads=['ot1'], writes=['rcb'])
                T.op('vector', TT(YF[0:64, p, :], OT[0][0:64, :], rc[0:64, :], ALU.mult), reads=['ot0', 'rca'], writes=['yfa'])
                T.op('vector', TT(YF[64:128, p, :], OT[1][64:128, :], rc[64:128, :], ALU.mult), reads=['ot1', 'rcb'],
                     writes=['yfb'])
            T.op('vector', TT(SQ[:], YF[:], YF[:], ALU.mult), reads=['yfa', 'yfb'], writes=['sq'])
            T.op('tensor', mmgrp([(sm[:, 4 + bl:5 + bl], SQ[:, p, bl * 128:(bl + 1) * 128], ones_bf[:], p == 0, p == 3)
                                  for bl in range(4) for p in range(4)]), reads=['sq', 'onesbf'], writes=['sm'])
            T.op('scalar', ACT(rsF[:], sm[:, 4:8], AF.Ln, scale=1.0 / 512, bias=eps_t[:]), reads=['sm', 'eps'], writes=['rsF'])
            T.op('scalar', ACT(rsF[:], rsF[:], AF.Exp, scale=-0.5), reads=['rsF'], writes=['rsF'])
            if i == 0:
                dump("YS", YS[:], [128, 4, 512], BF16, ['ys'])
                dump("YF", YF[:], [128, 4, 512], BF16, ['yfa', 'yfb'])
                dump("rsS", rsS[:], [128, 4], F32, ['rsS'])
                dump("rsF", rsF[:], [128, 4], F32, ['rsF'])
            for bl in range(4):
                g = i * 4 + bl
                xb = g % 2
                T.dma('sync', f'xr{xb}', XR[xb][:], xown[g * 128:(g + 1) * 128, :], writes=[f'xr{xb}'])
                for hf in range(2):
                    ms, msn = next_st()
                    T.op('tensor', mmgrp([(ms[:], YS[:, p, bl * 128:(bl + 1) * 128], WO[:, p, hf * 512:(hf + 1) * 512], p == 0, p == 3)
                                          for p in range(4)]), reads=['ys', 'wo'], writes=[msn])
                    mf, mfn = next_st()
                    T.op('tensor', mmgrp([(mf[:], YF[:, p, bl * 128:(bl + 1) * 128], WO[:, 4 + p, hf * 512:(hf + 1) * 512], p == 0, p == 3)
                                          for p in range(4)]), reads=['yfa', 'yfb', 'wo'], writes=[mfn])
                    tb = hf
                    T.op('vector', TS(t1[tb][:], ms[:], rsS[:, bl:bl + 1], None, ALU.mult), reads=[msn, 'rsS'], writes=[f't1{tb}'])
                    T.op('vector', STT(t1[tb][:], mf[:], rsF[:, bl:bl + 1], t1[tb][:], ALU.mult, ALU.add),
                         reads=[mfn, 'rsF', f't1{tb}'], writes=[f't1{tb}'])
                    T.op('vector', TT(t1[tb][:], t1[tb][:], GT1[:, hf * 512:(hf + 1) * 512], ALU.mult),
                         reads=[f't1{tb}', 'GT10', 'GT11'], writes=[f't1{tb}'])
                    T.op('vector', TT(X1[xb][:, hf * 512:(hf + 1) * 512], t1[tb][:], XR[xb][:, hf * 512:(hf + 1) * 512], ALU.add),
                         reads=[f't1{tb}', f'xr{xb}'], writes=[f'xr{xb}'])
                T.dma('gpsimd', f'x1o{xb}', X1_d[g * 128:(g + 1) * 128, :], X1[xb][:], reads=[f'xr{xb}'], writes=[f'X1_d{g}'])
        T.barrier()

    CAP = 768
    NT0 = CAP // 128
    if "C" in DEBUG["phases"]:
      csem = {}
      with ExitStack() as es0:
        IDX = sb(es0, [128, 32, 2], U32, "IDX")
        W12 = sb(es0, [128, 32, 2], F32, "W12")
        gfin = sb(es0, [128, 1024], F32, "gfin")
        T.dma('sync', 'gfin', gfin[:], gfin_d[0:1, :].broadcast_to([128, 1024]), writes=['gfin'])
        with ExitStack() as es:
            X1t = [sb(es, [128, 1024], F32) for _ in range(2)]
            xn2 = sb(es, [128, 1024], F32)
            junk = sb(es, [128, 1024], BF16)
            H2f = sb(es, [128, 8, 128], F32)
            tmpg = sb(es, [128, 8, 128], F32)
            H2row = [sb(es, [128, 1024], BF16) for _ in range(2)]
            hrt = sb(es, [128, 1024], F32)
            A2R = sb(es, [128, 1024], F32)
            B2R = sb(es, [128, 1024], F32)
            dg = sb(es, [128, 128], F32)
            onesf = sb(es, [128, 128], F32)
            WR = sb(es, [128, 8, 20], F32)
            brb = sb(es, [128, 20], F32)
            EB = sb(es, [128, 16], F32)
            Cn = sb(es, [128, 32, 16], F32)
            L = sb(es, [128, 20], F32)
            s1 = sb(es, [128, 16], F32)
            s2 = sb(es, [128, 16], F32)
            E1 = sb(es, [128, 16], F32)
            E2 = sb(es, [128, 16], F32)
            Mx = sb(es, [128, 16], F32)
            oh = sb(es, [128, 4], F32)
            mk1 = sb(es, [128, 4], F32)
            mk2 = sb(es, [128, 4], F32)
            les = sb(es, [128, 4], F32)
            le2 = sb(es, [128, 4], F32)
            sc = sb(es, [128, 12], F32)
            idf = sb(es, [128, 2], F32)
            CNTi = sb(es, [128, 16], I32)
            ssq = sb(es, [128, 32], F32)
            T.op('vector', MEMSET(ssq[:], 0.0), writes=['ssqall'])
            rs = sb(es, [128, 1], F32)
            tpc = ps(es, [128, 8, 128], F32)
            pm = ps(es, [128, 512], F32)

            T.dma('sync', 'WR', WR[:], wr_d.rearrange("(kt p) n -> p kt n", p=128), writes=['WR'])
            T.dma('sync', 'brb', brb[:], br_d[0:1, :].broadcast_to([128, 20]), writes=['brb'])
            T.dma('sync', 'EB', EB[:], eb_d[0:1, :].broadcast_to([128, 16]), writes=['EB'])
            T.op('gpsimd', MEMSET(onesf[:], 1.0), writes=['onesf'])
            for (colt, rowt, nm) in ((A2, A2R, 'A2R'), (B2, B2R, 'B2R')):
                for kt in range(8):
                    T.op('vector', TS(dg[:], ident_f[:], colt[:, kt:kt + 1], None, ALU.mult), reads=['identf', 'A2', 'B2', 'dg'],
                         writes=['dg'])
                    T.op('tensor', mmgrp([(pm[:, 0:128], onesf[:], dg[:], True, True)]), reads=['dg', 'onesf'], writes=['pm'])
                    T.op('vector', CP(rowt[:, kt * 128:(kt + 1) * 128], pm[:, 0:128]), reads=['pm'], writes=[nm])
            for g in range(32):
                xb = g % 2
                T.dma('sync', f'x1t{xb}', X1t[xb][:], X1_d[g * 128:(g + 1) * 128, :], reads=[f'X1_d{g}'], writes=[f'x1t{xb}'])
                T.op('scalar', ACT(junk[:], X1t[xb][:], AF.Square, accum_out=ssq[:, g:g + 1]), reads=[f'x1t{xb}', 'ssqall'],
                     writes=['junk', f'ssq_{g}'])
                T.op('scalar', ACT(rs[:], ssq[:, g:g + 1], AF.Ln, scale=1.0 / 1024, bias=eps_t[:]), reads=[f'ssq_{g}', 'eps'], writes=['rs'])
                T.op('scalar', ACT(rs[:], rs[:], AF.Exp, scale=-0.5), reads=['rs'], writes=['rs'])
                T.op('vector', TS(xn2[:], X1t[xb][:], rs[:, 0:1], None, ALU.mult), reads=[f'x1t{xb}', 'rs'], writes=['xn2'])
                T.op('vector', TT(hrt[:], xn2[:], A2R[:], ALU.mult), reads=['xn2', 'A2R'], writes=['hrt'])
                T.op('vector', TT(H2row[xb][:], hrt[:], B2R[:], ALU.add), reads=['hrt', 'B2R'], writes=[f'h2row{xb}'])
                T.op('tensor', tpgrp([(tpc[:, kt, :], xn2[:, kt * 128:(kt + 1) * 128], ident_f[:]) for kt in range(8)]),
                     reads=['xn2', 'identf'], writes=['tpc'])
                T.op('vector', TT(tmpg[:], tpc[:], A2[:].unsqueeze(2).broadcast_to([128, 8, 128]), ALU.mult), reads=['tpc', 'A2'],
                     writes=['tmpg'])
                T.op('vector', TT(H2f[:], tmpg[:], B2[:].unsqueeze(2).broadcast_to([128, 8, 128]), ALU.add), reads=['tmpg', 'B2'],
                     writes=['h2f'])
                T.op('tensor', mmgrp([(pm[:, 0:20], H2f[:, kt, :], WR[:, kt, :], kt == 0, kt == 7) for kt in range(8)]),
                     reads=['h2f', 'WR'], writes=['pm'])
                T.op('vector', TT(L[:], pm[:, 0:20], brb[:], ALU.add), reads=['pm', 'brb'], writes=['L'])
                rr = ['L']
                V = lambda fn: T.op('vector', fn, reads=rr, writes=rr)
                S = lambda fn: T.op('scalar', fn, reads=rr, writes=rr)
                lg = L[:, 0:4]
                le = L[:, 4:20].rearrange("t (g e) -> t g e", g=4)
                V(lambda e: e.reduce_max(out=sc[:, 0:1], in_=lg, axis=AX.X))
                V(TS(oh[:], lg, sc[:, 0:1], None, ALU.is_equal))
                V(TS(sc[:, 1:2], sc[:, 0:1], -1.0, None, ALU.mult))
                V(MEMSET(sc[:, 2:3], 0.0))
                S(ACT(s1[:, 0:4], lg, AF.Exp, bias=sc[:, 1:2], accum_out=sc[:, 2:3]))
                V(RECIP(sc[:, 3:4], sc[:, 2:3]))
                V(TT(s2[:].rearrange("t (g e) -> t g e", g=4), le, oh[:].unsqueeze(2).broadcast_to([128, 4, 4]), ALU.mult))
                V(RED(les[:], s2[:].rearrange("t (g e) -> t e g", g=4), ALU.add))
                V(lambda e: e.reduce_max(out=sc[:, 4:5], in_=les[:], axis=AX.X))
                V(TS(mk1[:], les[:], sc[:, 4:5], None, ALU.is_equal))
                V(STT(le2[:], mk1[:], NEG, les[:], ALU.mult, ALU.add))
                V(lambda e: e.reduce_max(out=sc[:, 5:6], in_=le2[:], axis=AX.X))
                V(TS(mk2[:], le2[:], sc[:, 5:6], None, ALU.is_equal))
                V(TT(sc[:, 6:7], sc[:, 5:6], sc[:, 4:5], ALU.subtract))
                S(ACT(sc[:, 7:8], sc[:, 6:7], AF.Exp))
                V(TS(sc[:, 8:9], sc[:, 7:8], 1.0, None, ALU.add))
                V(RECIP(sc[:, 8:9], sc[:, 8:9]))
                V(TT(W12[:, g, 0:1], sc[:, 8:9], sc[:, 3:4], ALU.mult))
                V(TT(W12[:, g, 1:2], sc[:, 7:8], W12[:, g, 0:1], ALU.mult))
                V(TT(E1[:].rearrange("t (g e) -> t g e", g=4), oh[:].unsqueeze(2).broadcast_to([128, 4, 4]),
                     mk1[:].unsqueeze(1).broadcast_to([128, 4, 4]), ALU.mult))
                V(TT(E2[:].rearrange("t (g e) -> t g e", g=4), oh[:].unsqueeze(2).broadcast_to([128, 4, 4]),
                     mk2[:].unsqueeze(1).broadcast_to([128, 4, 4]), ALU.mult))
                V(TT(Mx[:], E1[:], E2[:], ALU.add))
                lst = [(pm[:, 32:48], Umat[:], Mx[:], True, g == 0)]
                if g > 0:
                    lst.append((pm[:, 32:48], Sel127[:], Cn[:, g - 1, :], False, True))
                T.op('tensor', mmgrp(lst), reads=['L', 'U', 'sel127', 'Cn'], writes=['pm'])
                T.op('vector', CP(Cn[:, g, :], pm[:, 32:48]), reads=['pm'], writes=['Cn'])
                T.op('vector', TT(s1[:], Cn[:, g, :], EB[:], ALU.add), reads=['Cn', 'EB', 'L'], writes=['L'])
                V(TT(s2[:], s1[:], E1[:], ALU.mult))
                V(RED(idf[:, 0:1], s2[:], ALU.add))
                V(TT(s2[:], s1[:], E2[:], ALU.mult))
                V(RED(idf[:, 1:2], s2[:], ALU.add))
                T.op('vector', CP(IDX[:, g, :], idf[:]), reads=['L'], writes=[f'idx{g}'])
                for k in range(2):
                    T.idma(f'sc{xb}{k}', Xs_d, bass.IndirectOffsetOnAxis(ap=IDX[:, g, k:k + 1], axis=0), H2row[xb][:], None,
                           reads=[f'idx{g}', f'h2row{xb}'], writes=[f'Xs{g}_{k}'])
            T.op('tensor', mmgrp([(pm[:, 64:80], Sel127[:], Cn[:, 31, :], True, True)]), reads=['Cn', 'sel127'], writes=['pm'])
            T.op('vector', CP(CNTi[:], pm[:, 64:80]), reads=['pm'], writes=['cnti'])
            T.dma('sync', 'cntd', CNT_d, CNTi[0:1, :], reads=['cnti'], writes=['CNT_d'])
            dump("IDX", IDX[:].rearrange("p g k -> p (g k)"), [128, 64], U32, [f'idx{g}' for g in range(32)])
            dump("W12", W12[:].rearrange("p g k -> p (g k)"), [128, 64], F32, ['L'])
            dump("Cn", Cn[:].rearrange("p g e -> p (g e)"), [128, 512], F32, ['Cn'])
            T.barrier()

        with ExitStack() as es:
            WG = [sb(es, [128, 8, 512], BF16) for _ in range(2)]
            WU = [sb(es, [128, 8, 512], BF16) for _ in range(2)]
            WD = [sb(es, [128, 4, 1024], BF16) for _ in range(2)]
            XT = [sb(es, [128, 1024], BF16) for _ in range(3)]
            XsT = [sb(es, [128, 8, 512], BF16) for _ in range(2)]
            AT = [sb(es, [128, 4, 512], BF16) for _ in range(2)]
            sg = [sb(es, [128, 512], F32) for _ in range(2)]
            YT = [sb(es, [128, 1024], F32) for _ in range(2)]
            tpb = ps(es, [128, 8, 128], BF16)
            gp = [ps(es, [128, 512], F32) for _ in range(2)]
            up = [ps(es, [128, 512], F32) for _ in range(2)]
            yp = [ps(es, [128, 512], F32) for _ in range(2)]
            wg_v = wg_d.rearrange("e (kt p) n -> e p kt n", p=128)
            wu_v = wu_d.rearrange("e (kt p) n -> e p kt n", p=128)
            wd_v = wd_d.rearrange("e (kt p) n -> e p kt n", p=128)
            gi = [0]
            yi = [0]
            xi = [0]
            yti = [0]
            gri = [0]

            def load_w(ex):
                wb = ex % 2
                T.dma('gpsimd', f'wg{wb}', WG[wb][:], wg_v[ex], writes=[f'wg{wb}'])
                T.dma('gpsimd', f'wu{wb}', WU[wb][:], wu_v[ex], writes=[f'wu{wb}'])
                T.dma('gpsimd', f'wd{wb}', WD[wb][:], wd_v[ex], writes=[f'wd{wb}'])

            def emit_group(ex, grp):
                wb = ex % 2
                ab = gri[0] % 2
                gri[0] += 1
                base = ex * 4096 + grp * 512
                for tl in range(4):
                    k3 = xi[0] % 3
                    xi[0] += 1
                    r0 = base + tl * 128
                    T.dma('sync', f'xt{k3}', XT[k3][:], Xs_d[r0:r0 + 128, :], writes=[f'xt{k3}'])
                    T.op('tensor', tpgrp([(tpb[:, kt, :], XT[k3][:, kt * 128:(kt + 1) * 128], ident_b[:]) for kt in range(8)]),
                         reads=[f'xt{k3}', 'identb'], writes=['tpb'])
                    T.op('vector', CP(XsT[ab][:, :, tl * 128:(tl + 1) * 128], tpb[:]), reads=['tpb'], writes=[f'xst{ab}'])
                for ht in range(4):
                    k2 = gi[0] % 2
                    gi[0] += 1
                    T.op('tensor', mmgrp([(gp[k2][:], WG[wb][:, kt, ht * 128:(ht + 1) * 128], XsT[ab][:, kt, :], kt == 0, kt == 7)
                                          for kt in range(8)]), reads=[f'wg{wb}', f'xst{ab}'], writes=[f'gp{k2}'])
                    T.op('tensor', mmgrp([(up[k2][:], WU[wb][:, kt, ht * 128:(ht + 1) * 128], XsT[ab][:, kt, :], kt == 0, kt == 7)
                                          for kt in range(8)]), reads=[f'wu{wb}', f'xst{ab}'], writes=[f'up{k2}'])
                    T.op('scalar', ACT(sg[k2][:], gp[k2][:], AF.Silu), reads=[f'gp{k2}'], writes=[f'sg{k2}'])
                    T.op('vector', TT(AT[ab][:, ht, :], sg[k2][:], up[k2][:], ALU.mult), reads=[f'sg{k2}', f'up{k2}'],
                         writes=[f'at{ab}'])
                for tl in range(4):
                    yb = yti[0] % 2
                    yti[0] += 1
                    for nh in range(2):
                        k2 = yi[0] % 2
                        yi[0] += 1
                        T.op('tensor', mmgrp([(yp[k2][:], AT[ab][:, ht, tl * 128:(tl + 1) * 128], WD[wb][:, ht, nh * 512:(nh + 1) * 512],
                                               ht == 0, ht == 3) for ht in range(4)]), reads=[f'at{ab}', f'wd{wb}'], writes=[f'yp{k2}'])
                        T.op('scalar', ACT(YT[yb][:, nh * 512:(nh + 1) * 512], yp[k2][:], AF.Copy), reads=[f'yp{k2}'], writes=[f'yt{yb}'])
                    r0 = base + tl * 128
                    T.dma('sync', f'yt{yb}', Ys_d[r0:r0 + 128, :], YT[yb][:], reads=[f'yt{yb}'], writes=[f'Ys{ex}_{grp}_{tl}'])

            drain = ['xt0', 'xt1', 'xt2', 'yt0', 'yt1']
            load_w(0)
            for ex in range(16):
                if ex + 1 < 16:
                    load_w(ex + 1)
                emit_group(ex, 0)
                for grp in range(1, 8):
                    T.cond_begin(CNT_d[0:1, ex:ex + 1], grp * 512, drain)
                    emit_group(ex, grp)
                    T.cond_end()
            T.barrier()

        with ExitStack() as es:
            Y1 = [sb(es, [128, 1024], F32) for _ in range(2)]
            Y2 = [sb(es, [128, 1024], F32) for _ in range(2)]
            X1t = [sb(es, [128, 1024], F32) for _ in range(2)]
            junk = sb(es, [128, 1024], BF16)
            osb = [sb(es, [128, 1024], F32) for _ in range(2)]
            ssq = sb(es, [128, 32], F32)
            T.op('vector', MEMSET(ssq[:], 0.0), writes=['ssqall'])
            rs = [sb(es, [128, 1], F32) for _ in range(2)]
            def c3_loads(g):
                b = g % 2
                T.dma('sync', f'x1c{b}', X1t[b][:], X1_d[g * 128:(g + 1) * 128, :], writes=[f'x1c{b}'])
                T.idma(f'ga{b}', Y1[b][:], None, Ys_d, bass.IndirectOffsetOnAxis(ap=IDX[:, g, 0:1], axis=0), writes=[f'y1{b}'])
                T.idma(f'gb{b}', Y2[b][:], None, Ys_d, bass.IndirectOffsetOnAxis(ap=IDX[:, g, 1:2], axis=0), writes=[f'y2{b}'])

            c3_loads(0)
            for g in range(32):
                b = g % 2
                if g + 1 < 32:
                    c3_loads(g + 1)
                T.op('vector', TS(Y1[b][:], Y1[b][:], W12[:, g, 0:1], None, ALU.mult), reads=[f'y1{b}'], writes=[f'y1{b}'])
                T.op('vector', STT(Y1[b][:], Y2[b][:], W12[:, g, 1:2], Y1[b][:], ALU.mult, ALU.add), reads=[f'y1{b}', f'y2{b}'],
                     writes=[f'y1{b}'])
                T.op('vector', TT(Y1[b][:], Y1[b][:], GT2[:], ALU.mult), reads=[f'y1{b}'], writes=[f'y1{b}'])
                T.op('vector', TT(Y1[b][:], Y1[b][:], X1t[b][:], ALU.add), reads=[f'y1{b}', f'x1c{b}'], writes=[f'y1{b}'])
                T.op('scalar', ACT(junk[:], Y1[b][:], AF.Square, accum_out=ssq[:, g:g + 1]), reads=[f'y1{b}', 'ssqall'],
                     writes=['junk', f'ssq_{g}'])
                T.op('scalar', ACT(rs[b][:], ssq[:, g:g + 1], AF.Ln, scale=1.0 / 1024, bias=eps_t[:]), reads=[f'ssq_{g}', 'eps'], writes=[f'rs{b}'])
                T.op('scalar', ACT(rs[b][:], rs[b][:], AF.Exp, scale=-0.5), reads=[f'rs{b}'], writes=[f'rs{b}'])
                T.op('vector', STT(osb[b][:], Y1[b][:], rs[b][:, 0:1], gfin[:], ALU.mult, ALU.mult),
                     reads=[f'y1{b}', f'rs{b}', 'gfin'], writes=[f'osb{b}'])
                T.dma('sync', f'out{b}', out_d[g * 128:(g + 1) * 128, :], osb[b][:], reads=[f'osb{b}'], writes=[f'out{g}'])
            T.barrier()
    T.emit()
    top.close()
    return nc


_CACHE = {}


def _masks(j):
    m = np.zeros((2, 8, 128, 512), np.float32)
    ki = np.arange(128)[:, None]
    qq = np.arange(512)[None, :]
    for par in range(2):
        i = par
        run = OWN_RUNS[j][i]
        nk = NK(i)
        for r in range(8):
            jb = nk - 8 + r
            kpos = jb * 128 + ki
            qpos = run * 512 + qq
            m[par, r] = np.where(kpos <= qpos, 0.0, NEG)
    return m.reshape(16, 128, 512)


def _rsel(j):
    s = np.zeros((8, 64), np.float32)
    for i, run in enumerate(OWN_RUNS[j]):
        s[i, 4 * run + 1] = 1.0
    return s.reshape(1, 512)


def kernel(x, c, w_ada, b_ada, g_norm_mix, w_in, g_sgu, w_spatial, b_spatial, b_forget, g_out_sgu, g_out_fox, w_out,
           g_norm_ffn, w_router_group, b_router_group, w_router_expert, b_router_expert, w_gate, w_up, w_down, g_final):
    f = lambda a: np.ascontiguousarray(np.asarray(a, dtype=np.float32))
    x = f(x); c = f(c)
    if "nc" not in _CACHE:
        _CACHE["nc"] = build_program()
    nc = _CACHE["nc"]
    wr = np.concatenate([f(w_router_group)[0], f(w_router_expert)[0].transpose(1, 0, 2).reshape(1024, 16)], axis=1)
    br = np.concatenate([f(b_router_group)[0], f(b_router_expert)[0].reshape(16)])[None, :]
    shared = {
        "w_ada": f(w_ada)[0], "b_ada": f(b_ada)[0][None, :], "g1T": f(f(g_norm_mix)[0].reshape(8, 128).T),
        "g2T": f(f(g_norm_ffn)[0].reshape(8, 128).T), "w_in": f(w_in)[0], "gsgu": f(g_sgu)[0][None, :],
        "wsT": f(f(w_spatial)[0].transpose(2, 0, 1)), "bsp": f(b_spatial)[0], "bfg": f(b_forget)[0][None, :],
        "gos": f(f(g_out_sgu)[0].reshape(4, 128).T), "gof": f(f(g_out_fox)[0].reshape(4, 128).T), "w_out": f(w_out)[0],
        "wr": f(wr), "br": f(br), "w_gate": f(w_gate)[0], "w_up": f(w_up)[0], "w_down": f(w_down)[0],
        "gfin": f(g_final)[None, :],
    }
    in_maps = []
    for core in range(8):
        b, j = core // 2, core % 2
        m = dict(shared)
        m["xall"] = x[b]
        m["xown"] = f(np.concatenate([x[b, 512 * r:512 * (r + 1)] for r in OWN_RUNS[j]], axis=0))
        m["cT"] = f(c[b].reshape(8, 128).T)
        m["masks"] = _masks(j)
        m["rsel"] = _rsel(j)
        m["eb"] = (np.arange(16, dtype=np.float32) * 4096.0 - 1.0)[None, :]
        in_maps.append(m)
    res = run_bass_kernel_spmd(nc, in_maps, core_ids=list(range(8)))
    _CACHE["res"] = res
    out = np.empty((4, 8192, 1024), np.float32)
    for core in range(8):
        b, j = core // 2, core % 2
        o = res.results[core]["out"]
        for i, r in enumerate(OWN_RUNS[j]):
            out[b, 512 * r:512 * (r + 1)] = o[512 * i:512 * (i + 1)]
    return out
```

```python
import os
import numpy as np
from contextlib import ExitStack
import concourse.bass as bass
import concourse.mybir as mybir
from concourse.bass_utils import run_bass_kernel_spmd

F32 = mybir.dt.float32
BF16 = mybir.dt.bfloat16
U32 = mybir.dt.uint32
I32 = mybir.dt.int32
AF = mybir.ActivationFunctionType
ALU = mybir.AluOpType
AX = mybir.AxisListType
ENGS = ['sync', 'scalar', 'vector', 'gpsimd', 'tensor']
EPS = 1e-6
NEG = -1.0e30
OWN_RUNS = {0: [0, 3, 4, 7, 8, 11, 12, 15], 1: [1, 2, 5, 6, 9, 10, 13, 14]}
WARM = 18
PROBE = os.environ.get('MK_PROBE', '')
DEBUG = {"x1": False, "same": True, "phases": "ABC", "dump": []}


def NK(i):
    return 16 * (i // 2) + 8 + 8 * (i % 2)


class Tracker:
    def __init__(self, nc, same_engine_sync=False):
        self.nc = nc
        self.streams = {e: [] for e in ENGS}
        self.sems = {e: nc.alloc_semaphore("c_" + e) for e in ENGS}
        self.cnt = {e: 0 for e in ENGS}
        self.waited = {e: {} for e in ENGS}
        self.res = {}
        self.slots = {}
        self.same = same_engine_sync
        self.slot_q = {}

    def _deps(self, reads, writes):
        deps = []
        for r in reads:
            st = self.res.get(r)
            if st and st[0]:
                deps.append(st[0])
        for w in writes:
            st = self.res.get(w)
            if st:
                if st[0]:
                    deps.append(st[0])
                deps.extend(st[1])
        return deps

    def _update(self, reads, writes, tag):
        for r in reads:
            st = self.res.setdefault(r, [None, []])
            st[1].append(tag)
        for w in writes:
            self.res[w] = [tag, []]

    def _emit_waits(self, eng, deps, skip_same):
        mx = {}
        for key, val in deps:
            mx[key] = max(mx.get(key, 0), val)
        for key, val in mx.items():
            if key == eng and skip_same:
                continue
            if self.waited[eng].get(key, 0) >= val:
                continue
            self.waited[eng][key] = val
            sem = self.sems[key] if key in self.sems else self.slots[key][0]
            self.streams[eng].append(lambda e, sem=sem, val=val: e.wait_ge(sem, val))

    def op(self, eng, fn, reads=(), writes=()):
        deps = self._deps(reads, writes)
        self._emit_waits(eng, deps, (not self.same) or eng == 'tensor')
        self.cnt[eng] += 1
        sem = self.sems[eng]
        self.streams[eng].append(lambda e, fn=fn, sem=sem: fn(e).then_inc(sem, 1))
        self._update(reads, writes, (eng, self.cnt[eng]))

    def dma(self, q, slot, out, in_, reads=(), writes=()):
        self.slot_q[slot] = q
        if slot not in self.slots:
            self.slots[slot] = [self.nc.alloc_semaphore("d_" + slot), 0]
        deps = self._deps(reads, writes)
        self._emit_waits(q, deps, False)
        s = self.slots[slot]
        s[1] += 16
        sem = s[0]
        self.streams[q].append(lambda e, out=out, in_=in_, sem=sem: e.dma_start(out=out, in_=in_).then_inc(sem, 16))
        self._update(reads, writes, (slot, s[1]))

    def idma(self, slot, out, out_off, in_, in_off, reads=(), writes=()):
        q = 'gpsimd'
        self.slot_q[slot] = q
        if slot not in self.slots:
            self.slots[slot] = [self.nc.alloc_semaphore("d_" + slot), 0]
        deps = self._deps(reads, writes)
        self._emit_waits(q, deps, False)
        s = self.slots[slot]
        s[1] += 16
        sem = s[0]
        self.streams[q].append(lambda e, out=out, in_=in_, sem=sem, oo=out_off, io=in_off:
                               e.indirect_dma_start(out=out, out_offset=oo, in_=in_, in_offset=io).then_inc(sem, 16))
        self._update(reads, writes, (slot, s[1]))

    def cond_begin(self, cnt_ap, thr, drain_slots):
        for e in ENGS:
            deps = [(e, self.cnt[e])] if self.cnt[e] else []
            deps += [(k, self.slots[k][1]) for k in drain_slots if k in self.slots and self.slot_q.get(k) == e]
            self._emit_waits(e, deps, False)
        self.snap_cnt = dict(self.cnt)
        self.snap_slots = {k: v[1] for k, v in self.slots.items()}
        self.snap_waited = {e: dict(w) for e, w in self.waited.items()}
        for e in ENGS:
            self.streams[e].append(('regload', cnt_ap))
            self.streams[e].append(('if', thr))

    def cond_end(self):
        for e in ENGS:
            self.streams[e].append(('else',))
            n = self.cnt[e] - self.snap_cnt[e]
            if n:
                self.streams[e].append(lambda eng, sem=self.sems[e], n=n: eng.sem_inc(sem, n))
        for slot, (sem, c) in self.slots.items():
            d = c - self.snap_slots.get(slot, 0)
            if d:
                self.streams[self.slot_q[slot]].append(lambda eng, sem=sem, d=d: eng.sem_inc(sem, d))
        for e in ENGS:
            self.streams[e].append(('endif',))
        self.waited = self.snap_waited

    def barrier(self):
        deps = [(e, c) for e, c in self.cnt.items() if c > 0]
        deps += [(k, v[1]) for k, v in self.slots.items() if v[1] > 0]
        for e in ENGS:
            self._emit_waits(e, deps, True)

    def emit(self):
        self.barrier()
        streams = self.streams

        def run(e, items):
            creg = e.alloc_register("creg")
            i = 0
            n = len(items)
            while i < n:
                it = items[i]
                if not isinstance(it, tuple):
                    it(e)
                    i += 1
                    continue
                if it[0] == 'regload':
                    e.reg_load(creg, it[1])
                    i += 1
                    continue
                assert it[0] == 'if'
                thr = it[1]
                j = i + 1
                body = []
                while not (isinstance(items[j], tuple) and items[j][0] == 'else'):
                    body.append(items[j])
                    j += 1
                j += 1
                fix = []
                while not (isinstance(items[j], tuple) and items[j][0] == 'endif'):
                    fix.append(items[j])
                    j += 1
                with e.If_lt(creg, thr + 1):
                    for f in fix:
                        f(e)
                with e.Else():
                    for f in body:
                        f(e)
                i = j + 1

        with self.nc.Block() as block:
            @block.sync
            def _(e):
                run(e, streams['sync'])

            @block.scalar
            def _(e):
                run(e, streams['scalar'])

            @block.vector
            def _(e):
                run(e, streams['vector'])

            @block.gpsimd
            def _(e):
                run(e, streams['gpsimd'])

            @block.tensor
            def _(e):
                run(e, streams['tensor'])


def mmgrp(lst):
    def fn(e):
        ins = None
        for (out, lhsT, rhs, st, sp) in lst:
            ins = e.matmul(out, lhsT=lhsT, rhs=rhs, start=st, stop=sp)
        return ins
    return fn


def tpgrp(lst):
    def fn(e):
        ins = None
        for (out, in_, ident) in lst:
            ins = e.transpose(out, in_, ident)
        return ins
    return fn


def ACT(out, in_, func, **kw):
    return lambda e: e.activation(out=out, in_=in_, func=func, **kw)


def TT(out, in0, in1, op):
    return lambda e: e.tensor_tensor(out=out, in0=in0, in1=in1, op=op)


def TS(out, in0, s1, s2, op0, op1=None):
    if op1 is None:
        return lambda e: e.tensor_scalar(out=out, in0=in0, scalar1=s1, scalar2=None, op0=op0)
    return lambda e: e.tensor_scalar(out=out, in0=in0, scalar1=s1, scalar2=s2, op0=op0, op1=op1)


def STT(out, in0, scalar, in1, op0, op1):
    return lambda e: e.scalar_tensor_tensor(out=out, in0=in0, scalar=scalar, in1=in1, op0=op0, op1=op1)


def CP(out, in_):
    return lambda e: e.tensor_copy(out=out, in_=in_)


def RED(out, in_, op):
    return lambda e: e.tensor_reduce(out=out, in_=in_, axis=AX.X, op=op)


def RMAX(out, in_):
    return lambda e: e.reduce_max(out=out, in_=in_, axis=AX.X)


def RECIP(out, in_):
    return lambda e: e.reciprocal(out=out, in_=in_)


def MEMSET(ap, v):
    return lambda e: e.memset(ap, v)


def ASEL(out, in_, pattern, cmp, fill, base, cm):
    return lambda e: e.affine_select(out=out, in_=in_, pattern=pattern, compare_op=cmp, fill=fill, base=base,
                                     channel_multiplier=cm)


def build_program():
    nc = bass.Bass("TRN2", target_bir_lowering=False)
    T = Tracker(nc, same_engine_sync=DEBUG["same"])
    din = lambda name, shape: nc.dram_tensor(name, shape, F32, kind="ExternalInput").ap()
    xall = din("xall", [8192, 1024])
    xown = din("xown", [4096, 1024])
    cT_d = din("cT", [128, 8])
    wada_d = din("w_ada", [1024, 6144])
    bada_d = din("b_ada", [1, 6144])
    g1T_d = din("g1T", [128, 8])
    g2T_d = din("g2T", [128, 8])
    win_d = din("w_in", [1024, 2568])
    gsgu_d = din("gsgu", [1, 512])
    wsT_d = din("wsT", [128, 8, 128])
    bsp_d = din("bsp", [8, 128])
    bfg_d = din("bfg", [1, 8])
    gos_d = din("gos", [128, 4])
    gof_d = din("gof", [128, 4])
    wout_d = din("w_out", [1024, 1024])
    wr_d = din("wr", [1024, 20])
    br_d = din("br", [1, 20])
    wg_d = din("w_gate", [16, 1024, 512])
    wu_d = din("w_up", [16, 1024, 512])
    wd_d = din("w_down", [16, 512, 1024])
    gfin_d = din("gfin", [1, 1024])
    masks_d = din("masks", [16, 128, 512])
    rsel_d = din("rsel", [1, 512])
    eb_d = din("eb", [1, 16])
    out_d = nc.dram_tensor("out", [4096, 1024], F32, kind="ExternalOutput").ap()
    KT_d = nc.dram_tensor("KT_d", [4, 128, 8192], BF16).ap()
    V_d = nc.dram_tensor("V_d", [4, 128, 64, 192], BF16).ap()
    Xs_d = nc.dram_tensor("Xs_d", [65536, 1024], BF16).ap()
    Ys_d = nc.dram_tensor("Ys_d", [65536, 1024], F32).ap()
    CNT_d = nc.dram_tensor("CNT_d", [1, 16], I32).ap()
    if DEBUG["x1"]:
        X1_d = nc.dram_tensor("X1_d", [4096, 1024], F32, kind="ExternalOutput").ap()
    else:
        X1_d = nc.dram_tensor("X1_d", [4096, 1024], F32).ap()

    win_v = win_d.rearrange("(kt p) n -> p kt n", p=128)
    dumps = DEBUG.get("dump", [])

    def dump(name, ap, shape, dt, rname):
        if name not in dumps:
            return
        d = nc.dram_tensor("dbg_" + name, shape, dt, kind="ExternalOutput").ap()
        T.dma('sync', 'dbg_' + name, d, ap, reads=rname, writes=['dbg_' + name])

    top = ExitStack()
    uid = [0]

    def sb(es, shape, dt, name=None):
        uid[0] += 1
        return es.enter_context(nc.sbuf_tensor(f"{name or 't'}_{uid[0]}", shape, dt))

    def ps(es, shape, dt, name=None):
        uid[0] += 1
        return es.enter_context(nc.psum_tensor(f"{name or 'p'}_{uid[0]}", shape, dt))

    ident_f = sb(top, [128, 128], F32, "identf")
    ident_b = sb(top, [128, 128], BF16, "identb")
    Umat = sb(top, [128, 128], F32, "U")
    Sel127 = sb(top, [128, 128], F32, "sel127")
    eps_t = sb(top, [128, 1], F32, "eps")
    one_t = sb(top, [128, 1], F32, "one")
    ones_bf = sb(top, [128, 1], BF16, "onesbf")
    A1 = sb(top, [128, 8], F32, "A1")
    B1 = sb(top, [128, 8], F32, "B1")
    A2 = sb(top, [128, 8], F32, "A2")
    B2 = sb(top, [128, 8], F32, "B2")
    GT1 = sb(top, [128, 1024], F32, "GT1")
    GT2 = sb(top, [128, 1024], F32, "GT2")
    C_all = sb(top, [128, 64, 8], F32, "Call")
    RC = sb(top, [128, 8, 8], F32, "RC")

    T.op('gpsimd', MEMSET(ident_f[:], 0.0), writes=['identf'])
    T.op('gpsimd', ASEL(ident_f[:], ident_f[:], [[-1, 128]], ALU.not_equal, 1.0, 0, 1), reads=['identf'], writes=['identf'])
    T.op('gpsimd', MEMSET(Umat[:], 1.0), writes=['U'])
    T.op('gpsimd', ASEL(Umat[:], Umat[:], [[1, 128]], ALU.is_ge, 0.0, 0, -1), reads=['U'], writes=['U'])
    T.op('gpsimd', MEMSET(Sel127[:], 1.0), writes=['sel127'])
    T.op('gpsimd', ASEL(Sel127[:], Sel127[:], [[0, 128]], ALU.is_ge, 0.0, -127, 1), reads=['sel127'], writes=['sel127'])
    T.op('gpsimd', MEMSET(eps_t[:], EPS), writes=['eps'])
    T.op('gpsimd', MEMSET(one_t[:], 1.0), writes=['one'])
    T.op('gpsimd', MEMSET(ones_bf[:], 1.0), writes=['onesbf'])
    T.op('vector', CP(ident_b[:], ident_f[:]), reads=['identf'], writes=['identb'])

    with ExitStack() as es:
        cT = sb(es, [128, 8], F32)
        CB = sb(es, [128, 8, 128], F32)
        WA = [sb(es, [128, 8, 1024], F32, "wada") for _ in range(2)]
        badab = sb(es, [128, 1024], F32)
        gT1 = sb(es, [128, 8], F32)
        gT2 = sb(es, [128, 8], F32)
        rowv = sb(es, [128, 1024], F32)
        dtmp = sb(es, [128, 8, 128], F32)
        colv = sb(es, [128, 8], F32)
        pp = [ps(es, [128, 512], F32) for _ in range(2)]
        T.dma('sync', 'cT', cT[:], cT_d, writes=['cT'])
        T.dma('sync', 'gT1', gT1[:], g1T_d, writes=['gT1'])
        T.dma('sync', 'gT2', gT2[:], g2T_d, writes=['gT2'])
        T.op('scalar', ACT(cT[:], cT[:], AF.Silu), reads=['cT'], writes=['cT'])
        T.op('vector', CP(CB[:], cT[:].unsqueeze(2).broadcast_to([128, 8, 128])), reads=['cT'], writes=['CB'])
        wada_v = wada_d.rearrange("(kt p) n -> p kt n", p=128)
        order = [1, 0, 2, 4, 3, 5]
        for n, v in enumerate(order):
            wb = n % 2
            for kt in range(8):
                T.dma('sync', f'wada{wb}_{kt}', WA[wb][:, kt, :], wada_v[:, kt, v * 1024:(v + 1) * 1024],
                      writes=[f'wada{wb}_{kt}'])
            T.dma('gpsimd', 'badab', badab[:], bada_d[0:1, v * 1024:(v + 1) * 1024].broadcast_to([128, 1024]),
                  writes=['badab'])
            for hf in range(2):
                T.op('tensor', mmgrp([(pp[hf][:], CB[:, kt, :], WA[wb][:, kt, hf * 512:(hf + 1) * 512], kt == 0, kt == 7)
                                      for kt in range(8)]),
                     reads=['CB'] + [f'wada{wb}_{kt}' for kt in range(8)], writes=[f'pp{hf}'])
                dst = {2: GT1, 5: GT2}.get(v, rowv)
                dname = {2: 'GT1', 5: 'GT2'}.get(v, 'rowv') + str(hf)
                T.op('vector', TT(dst[:, hf * 512:(hf + 1) * 512], pp[hf][:], badab[:, hf * 512:(hf + 1) * 512], ALU.add),
                     reads=[f'pp{hf}', 'badab'], writes=[dname])
            if v in (2, 5):
                continue
            T.op('vector', TT(dtmp[:], rowv[:].rearrange("p (k t) -> p k t", k=8),
                              ident_f[:].unsqueeze(1).broadcast_to([128, 8, 128]), ALU.mult),
                 reads=['rowv0', 'rowv1', 'identf'], writes=['dtmp'])
            T.op('vector', RED(colv[:], dtmp[:], ALU.add), reads=['dtmp'], writes=['colv'])
            if v == 1:
                T.op('vector', STT(A1[:], colv[:], 1.0, gT1[:], ALU.add, ALU.mult), reads=['colv', 'gT1'], writes=['A1'])
            elif v == 0:
                T.op('vector', CP(B1[:], colv[:]), reads=['colv'], writes=['B1'])
            elif v == 4:
                T.op('vector', STT(A2[:], colv[:], 1.0, gT2[:], ALU.add, ALU.mult), reads=['colv', 'gT2'], writes=['A2'])
            elif v == 3:
                T.op('vector', CP(B2[:], colv[:]), reads=['colv'], writes=['B2'])
        T.barrier()

    def ln1_block(xsrc_rows, XA, XN, junk, ssq, rs, tp, tmpf, HT_dst, bi, htname):
        b = bi % 2
        T.dma('sync', f'xa{b}', XA[b][:], xsrc_rows, writes=[f'xa{b}'])
        T.op('scalar', ACT(XN[b][:], XA[b][:], AF.Square, accum_out=ssq[:, bi:bi + 1]), reads=[f'xa{b}', 'ssqall'],
             writes=[f'xn{b}', f'ssq_{bi}'])
        T.op('scalar', ACT(rs[b][:], ssq[:, bi:bi + 1], AF.Ln, scale=1.0 / 1024, bias=eps_t[:]), reads=[f'ssq_{bi}', 'eps'],
             writes=[f'rs{b}'])
        T.op('scalar', ACT(rs[b][:], rs[b][:], AF.Exp, scale=-0.5), reads=[f'rs{b}'], writes=[f'rs{b}'])
        T.op('vector', TS(XN[b][:], XA[b][:], rs[b][:, 0:1], None, ALU.mult), reads=[f'xa{b}', f'rs{b}'], writes=[f'xn{b}'])
        T.op('tensor', tpgrp([(tp[:, kt, :], XN[b][:, kt * 128:(kt + 1) * 128], ident_b[:]) for kt in range(8)]),
             reads=[f'xn{b}', 'identb'], writes=['tp'])
        T.op('vector', TT(tmpf[:], tp[:], A1[:].unsqueeze(2).broadcast_to([128, 8, 128]), ALU.mult), reads=['tp', 'A1'],
             writes=['tmpf'])
        T.op('vector', TT(HT_dst, tmpf[:], B1[:].unsqueeze(2).broadcast_to([128, 8, 128]), ALU.add), reads=['tmpf', 'B1'],
             writes=[htname])

    if "A" in DEBUG["phases"]:
      with ExitStack() as es:
        WKV = sb(es, [128, 8, 1032], BF16, "wkvf")
        XA = [sb(es, [128, 1024], F32) for _ in range(2)]
        XN = [sb(es, [128, 1024], BF16) for _ in range(2)]
        junk = None
        ssq = sb(es, [128, 64], F32)
        T.op('vector', MEMSET(ssq[:], 0.0), writes=['ssqall'])
        rs = [sb(es, [128, 1], F32) for _ in range(2)]
        tmpf = sb(es, [128, 8, 128], F32)
        HT = [sb(es, [128, 8, 512], BF16) for _ in range(2)]
        KTs = [sb(es, [128, 4, 512], BF16) for _ in range(2)]
        VBs = [sb(es, [128, 4, 4, 3, 64], BF16) for _ in range(2)]
        bfb = sb(es, [128, 8], F32)
        ft = sb(es, [128, 8], F32)
        tp = ps(es, [128, 8, 128], BF16)
        mm = [ps(es, [128, 512], F32) for _ in range(3)]
        sm = ps(es, [128, 512], F32)
        T.dma('gpsimd', 'wkvf', WKV[:], win_v[:, :, 1536:2568], writes=['wkvf'])
        T.dma('sync', 'bfb', bfb[:], bfg_d[0:1, :].broadcast_to([128, 8]), writes=['bfb'])
        for k in range(2):
            T.op('gpsimd', MEMSET(VBs[k][:, :, :, 1, :], 1.0), writes=[f'vbs{k}'])
        mi = 0
        for r in range(16):
            hb = r % 2
            for bl in range(4):
                g = r * 4 + bl
                ln1_block(xall[g * 128:(g + 1) * 128, :], XA, XN, junk, ssq, rs, tp, tmpf,
                          HT[hb][:, :, bl * 128:(bl + 1) * 128], g, f'ht{hb}')
            for p in range(4):
                m = mm[mi % 3]; mn = f'mm{mi % 3}'; mi += 1
                T.op('tensor', mmgrp([(m[:], WKV[:, kt, p * 128:(p + 1) * 128], HT[hb][:, kt, :], kt == 0, kt == 7)
                                      for kt in range(8)]), reads=['wkvf', f'ht{hb}'], writes=[mn])
                T.op('scalar', ACT(KTs[hb][:, p, :], m[:], AF.Copy), reads=[mn], writes=[f'kts{hb}'])
            T.dma('gpsimd', f'kts{hb}', KT_d[:, :, r * 512:(r + 1) * 512].rearrange("q p t -> p q t"), KTs[hb][:],
                  reads=[f'kts{hb}'], writes=[f'KT_d{r}'])
            for bl in range(4):
                g = r * 4 + bl
                m = mm[mi % 3]; mn = f'mm{mi % 3}'; mi += 1
                T.op('tensor', mmgrp([(m[:], HT[hb][:, kt, bl * 128:(bl + 1) * 128], WKV[:, kt, 512:1024], kt == 0, kt == 7)
                                      for kt in range(8)]), reads=['wkvf', f'ht{hb}'], writes=[mn])
                T.op('scalar', ACT(VBs[hb][:, bl, :, 0::2, :], m[:].rearrange("t (p two d) -> t p two d", p=4, two=2),
                                   AF.Copy), reads=[mn], writes=[f'vbs{hb}'])
                T.op('tensor', mmgrp([(sm[:, 0:8], HT[hb][:, kt, bl * 128:(bl + 1) * 128], WKV[:, kt, 1024:1032], kt == 0, kt == 7)
                                      for kt in range(8)]), reads=['wkvf', f'ht{hb}'], writes=['sm'])
                T.op('vector', TT(ft[:], sm[:, 0:8], bfb[:], ALU.add), reads=['sm', 'bfb'], writes=['ft'])
                T.op('scalar', ACT(ft[:], ft[:], AF.Exp, scale=-1.0), reads=['ft'], writes=['ft'])
                T.op('scalar', ACT(ft[:], ft[:], AF.Ln, bias=one_t[:]), reads=['ft', 'one'], writes=['ft'])
                lst = [(sm[:, 8:16], Umat[:], ft[:], True, g == 0)]
                if g > 0:
                    lst.append((sm[:, 8:16], Sel127[:], C_all[:, g - 1, :], False, True))
                T.op('tensor', mmgrp(lst), reads=['ft', 'U', 'sel127', 'Call'], writes=['sm'])
                T.op('vector', CP(C_all[:, g, :], sm[:, 8:16]), reads=['sm'], writes=['Call'])
            for q in range(4):
                T.dma('gpsimd', f'vbs{hb}_{q}', V_d[q, :, r * 4:(r + 1) * 4, :],
                      VBs[hb][:, :, q, :, :].rearrange("t b three d -> t b (three d)"), reads=[f'vbs{hb}'],
                      writes=[f'V_d{r}_{q}'])
        T.barrier()

    with ExitStack() as es:
        rselb = sb(es, [128, 8, 64], F32)
        csel = sb(es, [128, 8, 8], F32)
        tmp3 = sb(es, [128, 8, 64], F32)
        pr = ps(es, [128, 512], F32)
        T.dma('sync', 'rselb', rselb[:], rsel_d[0:1, :].broadcast_to([128, 512]).rearrange("p (i b) -> p i b", i=8),
              writes=['rselb'])
        for i in range(8):
            T.op('vector', TT(tmp3[:], C_all[:].rearrange("p b h -> p h b"),
                              rselb[:, i, :].unsqueeze(1).broadcast_to([128, 8, 64]), ALU.mult),
                 reads=['Call', 'rselb'], writes=['tmp3'])
            T.op('vector', RED(csel[:, i, :], tmp3[:], ALU.add), reads=['tmp3'], writes=['csel'])
        T.op('tensor', mmgrp([(pr[:, 0:64], Sel127[:], csel[:].rearrange("p i h -> p (i h)"), True, True)]),
             reads=['csel', 'sel127'], writes=['pr'])
        T.op('vector', CP(RC[:].rearrange("p i h -> p (i h)"), pr[:, 0:64]), reads=['pr'], writes=['RC'])
        T.barrier()

    if "B" in DEBUG["phases"]:
      with ExitStack() as es:
        WQ = sb(es, [128, 8, 1536], BF16, "wq")
        WO = sb(es, [128, 8, 1024], BF16, "wo")
        MK = sb(es, [128, 16, 512], BF16, "mk")
        WsT = sb(es, [128, 8, 128], BF16, "wst")
        Bb = sb(es, [128, 4, 128], F32)
        gsg = sb(es, [128, 512], F32)
        gos = sb(es, [128, 4], F32)
        gof = sb(es, [128, 4], F32)
        XA = [sb(es, [128, 1024], F32) for _ in range(2)]
        XR = [sb(es, [128, 1024], F32) for _ in range(2)]
        XN = [sb(es, [128, 1024], BF16) for _ in range(2)]
        wstage = XA
        wsf = XR[0][:].rearrange("p (h t) -> p h t", h=8)
        junk = None
        ssq = sb(es, [128, 64], F32)
        T.op('vector', MEMSET(ssq[:], 0.0), writes=['ssqall'])
        rs = [sb(es, [128, 1], F32) for _ in range(2)]
        tmpf = sb(es, [128, 8, 128], F32)
        HT = sb(es, [128, 8, 512], BF16)
        QT = sb(es, [128, 4, 2, 512], BF16)
        T.op('gpsimd', MEMSET(QT[:], 0.0), writes=['qt'])
        UT = sb(es, [128, 4, 512], BF16)
        VG = sb(es, [128, 4, 512], F32)
        VG2 = sb(es, [128, 4, 512], F32)
        v8 = sb(es, [128, 32], F32)
        VN = sb(es, [128, 4, 4, 3, 64], BF16)
        YS = sb(es, [128, 4, 512], BF16)
        YF = sb(es, [128, 4, 512], BF16)
        SQ = sb(es, [128, 4, 512], BF16)
        zt = sb(es, [128, 4, 128], F32)
        KB = [sb(es, [128, 2048], BF16) for _ in range(3)]
        VB = [sb(es, [128, 16, 192], BF16) for _ in range(3)]
        PT = [sb(es, [128, 512], BF16) for _ in range(3)]
        BI = sb(es, [128, 2, 2, 64], F32)
        rc = sb(es, [128, 512], F32)
        rsS = sb(es, [128, 4], F32)
        rsF = sb(es, [128, 4], F32)
        t1 = [sb(es, [128, 512], F32) for _ in range(2)]
        X1 = XR
        tp = ps(es, [128, 8, 128], BF16)
        st = [ps(es, [128, 512], F32) for _ in range(4)]
        OT = [ps(es, [128, 512], F32) for _ in range(2)]
        sm = ps(es, [128, 512], F32)

        T.dma('gpsimd', 'wq_u', WQ[:, :, 0:1024], win_v[:, :, 0:1024], writes=['wq_uv'])
        T.dma('gpsimd', 'wq_q', WQ[:, :, 1024:1536], win_v[:, :, 1024:1536], writes=['wq_q'])
        T.dma('gpsimd', 'mk', MK[:], masks_d.rearrange("m p q -> p m q"), writes=['mk'])
        T.dma('sync', 'xr0', wsf, wsT_d, writes=['xr0'])
        T.dma('sync', 'gsg', gsg[:], gsgu_d[0:1, :].broadcast_to([128, 512]), writes=['gsg'])
        T.dma('sync', 'gos', gos[:], gos_d, writes=['gos'])
        T.dma('sync', 'gof', gof[:], gof_d, writes=['gof'])
        for h in range(8):
            T.dma('sync', 'Bb', Bb[(h % 2) * 64:(h % 2) * 64 + 64, h // 2, :], bsp_d[h:h + 1, :].broadcast_to([64, 128]),
                  writes=[f'Bb{h}'])
        T.op('gpsimd', ASEL(wsf, wsf, [[0, 8], [1, 128]], ALU.is_ge, 0.0, 0, -1), reads=['xr0'], writes=['xr0'])
        T.op('vector', CP(WsT[:], wsf), reads=['xr0'], writes=['wst'])
        T.op('gpsimd', MEMSET(VN[:, :, :, 1, :], 0.0), writes=['vn'])
        wout_v = wout_d.rearrange("(kt p) n -> p kt n", p=128)
        for kt in range(8):
            wsb = kt % 2
            T.dma('sync', f'xa{wsb}', wstage[wsb][:], wout_v[:, kt, :], writes=[f'xa{wsb}'])
            gg = gos[:, kt:kt + 1] if kt < 4 else gof[:, kt - 4:kt - 3]
            T.op('vector', TS(WO[:, kt, :], wstage[wsb][:], gg, None, ALU.mult), reads=[f'xa{wsb}', 'gos', 'gof'],
                 writes=['wo'])
        Bbn = [f'Bb{h}' for h in range(8)]

        sti = [0]
        pti = [0]
        kvi = [0]

        def next_st():
            k = sti[0] % 4
            sti[0] += 1
            return st[k], f'st{k}'

        bglob = 0
        for i in range(8):
            nk = NK(i)
            par = i % 2
            for bl in range(4):
                g = i * 4 + bl
                ln1_block(xown[g * 128:(g + 1) * 128, :], XA, XN, junk, ssq, rs, tp, tmpf,
                          HT[:, :, bl * 128:(bl + 1) * 128], g, 'ht')
            for p in range(4):
                m, mn = next_st()
                T.op('tensor', mmgrp([(m[:], WQ[:, kt, 1024 + p * 128:1024 + (p + 1) * 128], HT[:, kt, :], kt == 0, kt == 7)
                                      for kt in range(8)]), reads=['wq_q', 'ht'], writes=[mn])
                T.op('vector', TS(QT[0:64, p, 0, :], m[0:64, :], 0.125, None, ALU.mult), reads=[mn], writes=['qt'])
                T.op('vector', TS(QT[64:128, p, 1, :], m[64:128, :], 0.125, None, ALU.mult), reads=[mn], writes=['qt'])
            for p in range(4):
                m, mn = next_st()
                T.op('tensor', mmgrp([(m[:], WQ[:, kt, p * 128:(p + 1) * 128], HT[:, kt, :], kt == 0, kt == 7)
                                      for kt in range(8)]), reads=['wq_uv', 'ht'], writes=[mn])
                T.op('scalar', ACT(UT[:, p, :], m[:], AF.Gelu_apprx_tanh), reads=[mn], writes=['ut'])
            for bl in range(4):
                m, mn = next_st()
                T.op('tensor', mmgrp([(m[:], HT[:, kt, bl * 128:(bl + 1) * 128], WQ[:, kt, 512:1024], kt == 0, kt == 7)
                                      for kt in range(8)]), reads=['wq_uv', 'ht'], writes=[mn])
                T.op('scalar', ACT(VG[:, bl, :], m[:], AF.Gelu_apprx_tanh), reads=[mn], writes=['vg'])
            T.op('vector', TT(VG2[:], VG[:], VG[:], ALU.mult), reads=['vg'], writes=['vg2'])
            T.op('vector', RED(v8[:], VG2[:].rearrange("t b (h d) -> t (b h) d", h=8), ALU.add), reads=['vg2'], writes=['v8'])
            T.op('scalar', ACT(v8[:], v8[:], AF.Ln, scale=1.0 / 64, bias=eps_t[:]), reads=['v8', 'eps'], writes=['v8'])
            T.op('scalar', ACT(v8[:], v8[:], AF.Exp, scale=-0.5), reads=['v8'], writes=['v8'])
            T.op('vector', TT(VG2[:].rearrange("t b (h d) -> t (b h) d", h=8), VG[:].rearrange("t b (h d) -> t (b h) d", h=8),
                              v8[:].unsqueeze(2).broadcast_to([128, 32, 64]), ALU.mult), reads=['vg', 'v8'], writes=['vg2'])
            for bl in range(4):
                T.op('vector', TT(VN[:, bl, :, 0::2, :], VG2[:, bl, :].rearrange("t (p two d) -> t p two d", p=4, two=2),
                                  gsg[:].rearrange("t (p two d) -> t p two d", p=4, two=2), ALU.mult),
                     reads=['vg2', 'gsg'], writes=['vn'])
            if i == 0:
                dump("HT", HT[:], [128, 8, 512], BF16, ['ht'])
            for bl in range(4):
                lst = []
                for p in range(4):
                    vnp = VN[:, bl, p, :, :].rearrange("t three d -> t (three d)")
                    lst.append((sm[:, p * 128:(p + 1) * 128], vnp[:, 0:128], WsT[:, 2 * p, :], True, False))
                    lst.append((sm[:, p * 128:(p + 1) * 128], vnp[:, 64:192], WsT[:, 2 * p + 1, :], False, True))
                T.op('tensor', mmgrp(lst), reads=['vn', 'wst'], writes=['sm'])
                T.op('vector', TT(zt[:], sm[:].rearrange("d (p t) -> d p t", p=4), Bb[:], ALU.add), reads=['sm'] + Bbn,
                     writes=['zt'])
                T.op('vector', TT(YS[:, :, bl * 128:(bl + 1) * 128], zt[:], UT[:, :, bl * 128:(bl + 1) * 128], ALU.mult),
                     reads=['zt', 'ut'], writes=['ys'])
            T.op('vector', TT(SQ[:], YS[:], YS[:], ALU.mult), reads=['ys'], writes=['sq'])
            T.op('tensor', mmgrp([(sm[:, bl:bl + 1], SQ[:, p, bl * 128:(bl + 1) * 128], ones_bf[:], p == 0, p == 3)
                                  for bl in range(4) for p in range(4)]), reads=['sq', 'onesbf'], writes=['sm'])
            T.op('scalar', ACT(rsS[:], sm[:, 0:4], AF.Ln, scale=1.0 / 512, bias=eps_t[:]), reads=['sm', 'eps'], writes=['rsS'])
            T.op('scalar', ACT(rsS[:], rsS[:], AF.Exp, scale=-0.5), reads=['rsS'], writes=['rsS'])
            for p in range(4):
                pq = p % 2
                for hh in range(2):
                    h = 2 * p + hh
                    T.op('vector', TS(BI[:, pq, hh, :], C_all[:, :, h], RC[:, i, h:h + 1], None, ALU.subtract),
                         reads=['Call', 'RC'], writes=[f'bi{pq}{hh}'])
                units = [(jb, hh) for jb in range(nk) for hh in range(2)]
                nchunks = (nk + 15) // 16
                if p == 0:
                    seq = [(pp_, c_) for pp_ in range(4) for c_ in range(nchunks)]
                    chunk_buf = {}
                    issued = [0]

                    def ensure(upto):
                        while issued[0] <= upto and issued[0] < len(seq):
                            pp_, c = seq[issued[0]]
                            kb = kvi[0] % 3
                            kvi[0] += 1
                            n = min(16, nk - c * 16)
                            T.dma('sync', f'kb{kb}', KB[kb][:, 0:n * 128], KT_d[pp_, :, c * 2048:c * 2048 + n * 128],
                                  reads=[f'KT_d{r}' for r in range(c * 4, (c * 16 + n + 3) // 4)], writes=[f'kb{kb}'])
                            T.dma('sync', f'vb{kb}', VB[kb][:, 0:n, :], V_d[pp_, :, c * 16:c * 16 + n, :],
                                  reads=[f'V_d{r}_{pp_}' for r in range(c * 4, (c * 16 + n + 3) // 4)], writes=[f'vb{kb}'])
                            chunk_buf[(pp_, c)] = kb
                            issued[0] += 1

                    ensure(2)
                ubank = {}

                def emit_qk(n):
                    jb, hh = units[n]
                    c = jb // 16
                    kb = chunk_buf[(p, c)]
                    jl = jb - c * 16
                    m, mn = next_st()
                    ubank[n] = (m, mn)
                    r0 = hh * 64
                    lst = [(m[:], KB[kb][:, jl * 128:(jl + 1) * 128], QT[:, p, hh, :], True, jb < nk - 8)]
                    rd = [f'kb{kb}', 'qt']
                    if jb >= nk - 8:
                        lst.append((m[:], ident_b[:], MK[:, par * 8 + (jb - (nk - 8)), :], False, True))
                        rd += ['identb', 'mk']
                    T.op('tensor', mmgrp(lst), reads=rd, writes=[mn])

                def emit_rest(n):
                    jb, hh = units[n]
                    c = jb // 16
                    kb = chunk_buf[(p, c)]
                    jl = jb - c * 16
                    m, mn = ubank.pop(n)
                    k3 = pti[0] % 3
                    pti[0] += 1
                    T.op('scalar', ACT(PT[k3][:], m[:], AF.Exp, bias=BI[:, pq, hh, jb:jb + 1]), reads=[mn, f'bi{pq}{hh}'],
                         writes=[f'pt{k3}'])
                    vb = VB[kb][:, jl, hh * 64:hh * 64 + 128]
                    T.op('tensor', mmgrp([(OT[hh][:], vb, PT[k3][:], jb == 0, jb == nk - 1)]),
                         reads=[f'vb{kb}'] + ([] if 'nodep' in PROBE else [f'pt{k3}']), writes=[f'ot{hh}'])

                LA = 3
                for n in range(min(LA, len(units))):
                    emit_qk(n)
                for n in range(len(units)):
                    emit_rest(n)
                    jbd, hhd = units[n]
                    if hhd == 1 and (jbd % 16 == 15 or jbd == nk - 1):
                        ensure(p * nchunks + jbd // 16 + 3)
                    nn = n + LA
                    if nn < len(units):
                        emit_qk(nn)
                T.op('vector', RECIP(rc[0:64, :], OT[0][64:128, :]), reads=['ot0'], writes=['rca'])
                T.op('vector', RECIP(rc[64:128, :], OT[1][0:64, :]), reads=['ot1'], writes=['rcb'])
                T.op('vector', TT(YF[0:64, p, :], OT[0][0:64, :], rc[0:64, :], ALU.mult), reads=['ot0', 'rca'], writes=['yfa'])
                T.op('vector', TT(YF[64:128, p, :], OT[1][64:128, :], rc[64:128, :], ALU.mult), reads=['ot1', 'rcb'],
                     writes=['yfb'])
            T.op('vector', TT(SQ[:], YF[:], YF[:], ALU.mult), reads=['yfa', 'yfb'], writes=['sq'])
            T.op('tensor', mmgrp([(sm[:, 4 + bl:5 + bl], SQ[:, p, bl * 128:(bl + 1) * 128], ones_bf[:], p == 0, p == 3)
                                  for bl in range(4) for p in range(4)]), reads=['sq', 'onesbf'], writes=['sm'])
            T.op('scalar', ACT(rsF[:], sm[:, 4:8], AF.Ln, scale=1.0 / 512, bias=eps_t[:]), reads=['sm', 'eps'], writes=['rsF'])
            T.op('scalar', ACT(rsF[:], rsF[:], AF.Exp, scale=-0.5), reads=['rsF'], writes=['rsF'])
            if i == 0:
                dump("YS", YS[:], [128, 4, 512], BF16, ['ys'])
                dump("YF", YF[:], [128, 4, 512], BF16, ['yfa', 'yfb'])
                dump("rsS", rsS[:], [128, 4], F32, ['rsS'])
                dump("rsF", rsF[:], [128, 4], F32, ['rsF'])
            for bl in range(4):
                g = i * 4 + bl
                xb = g % 2
                T.dma('sync', f'xr{xb}', XR[xb][:], xown[g * 128:(g + 1) * 128, :], writes=[f'xr{xb}'])
                for hf in range(2):
                    ms, msn = next_st()
                    T.op('tensor', mmgrp([(ms[:], YS[:, p, bl * 128:(bl + 1) * 128], WO[:, p, hf * 512:(hf + 1) * 512], p == 0, p == 3)
                                          for p in range(4)]), reads=['ys', 'wo'], writes=[msn])
                    mf, mfn = next_st()
                    T.op('tensor', mmgrp([(mf[:], YF[:, p, bl * 128:(bl + 1) * 128], WO[:, 4 + p, hf * 512:(hf + 1) * 512], p == 0, p == 3)
                                          for p in range(4)]), reads=['yfa', 'yfb', 'wo'], writes=[mfn])
                    tb = hf
                    T.op('vector', TS(t1[tb][:], ms[:], rsS[:, bl:bl + 1], None, ALU.mult), reads=[msn, 'rsS'], writes=[f't1{tb}'])
                    T.op('vector', STT(t1[tb][:], mf[:], rsF[:, bl:bl + 1], t1[tb][:], ALU.mult, ALU.add),
                         reads=[mfn, 'rsF', f't1{tb}'], writes=[f't1{tb}'])
                    T.op('vector', TT(t1[tb][:], t1[tb][:], GT1[:, hf * 512:(hf + 1) * 512], ALU.mult),
                         reads=[f't1{tb}', 'GT10', 'GT11'], writes=[f't1{tb}'])
                    T.op('vector', TT(X1[xb][:, hf * 512:(hf + 1) * 512], t1[tb][:], XR[xb][:, hf * 512:(hf + 1) * 512], ALU.add),
                         reads=[f't1{tb}', f'xr{xb}'], writes=[f'xr{xb}'])
                T.dma('gpsimd', f'x1o{xb}', X1_d[g * 128:(g + 1) * 128, :], X1[xb][:], reads=[f'xr{xb}'], writes=[f'X1_d{g}'])
        T.barrier()

    CAP = 768
    NT0 = CAP // 128
    if "C" in DEBUG["phases"]:
      csem = {}
      with ExitStack() as es0:
        IDX = sb(es0, [128, 32, 2], U32, "IDX")
        W12 = sb(es0, [128, 32, 2], F32, "W12")
        gfin = sb(es0, [128, 1024], F32, "gfin")
        T.dma('sync', 'gfin', gfin[:], gfin_d[0:1, :].broadcast_to([128, 1024]), writes=['gfin'])
        with ExitStack() as es:
            X1t = [sb(es, [128, 1024], F32) for _ in range(2)]
            xn2_2 = [sb(es, [128, 1024], F32) for _ in range(2)]
            junk_2 = [sb(es, [128, 1024], BF16) for _ in range(2)]
            H2f_2 = [sb(es, [128, 8, 128], F32) for _ in range(2)]
            tmpg_2 = [sb(es, [128, 8, 128], F32) for _ in range(2)]
            H2row = [sb(es, [128, 1024], BF16) for _ in range(2)]
            hrt_2 = [sb(es, [128, 1024], F32) for _ in range(2)]
            A2R = sb(es, [128, 1024], F32)
            B2R = sb(es, [128, 1024], F32)
            dg = sb(es, [128, 128], F32)
            onesf = sb(es, [128, 128], F32)
            WR = sb(es, [128, 8, 20], F32)
            brb = sb(es, [128, 20], F32)
            EB = sb(es, [128, 16], F32)
            Cn = sb(es, [128, 32, 16], F32)
            L_2 = [sb(es, [128, 20], F32) for _ in range(2)]
            s1_2 = [sb(es, [128, 16], F32) for _ in range(2)]
            s2_2 = [sb(es, [128, 16], F32) for _ in range(2)]
            E1_2 = [sb(es, [128, 16], F32) for _ in range(2)]
            E2_2 = [sb(es, [128, 16], F32) for _ in range(2)]
            Mx_2 = [sb(es, [128, 16], F32) for _ in range(2)]
            oh_2 = [sb(es, [128, 4], F32) for _ in range(2)]
            mk1_2 = [sb(es, [128, 4], F32) for _ in range(2)]
            mk2_2 = [sb(es, [128, 4], F32) for _ in range(2)]
            les_2 = [sb(es, [128, 4], F32) for _ in range(2)]
            le2_2 = [sb(es, [128, 4], F32) for _ in range(2)]
            sc_2 = [sb(es, [128, 12], F32) for _ in range(2)]
            idf_2 = [sb(es, [128, 2], F32) for _ in range(2)]
            CNTi = sb(es, [128, 16], I32)
            ssq = sb(es, [128, 32], F32)
            T.op('vector', MEMSET(ssq[:], 0.0), writes=['ssqall'])
            rs_2 = [sb(es, [128, 1], F32) for _ in range(2)]
            tpc = ps(es, [128, 8, 128], F32)
            pm = ps(es, [128, 512], F32)

            T.dma('sync', 'WR', WR[:], wr_d.rearrange("(kt p) n -> p kt n", p=128), writes=['WR'])
            T.dma('sync', 'brb', brb[:], br_d[0:1, :].broadcast_to([128, 20]), writes=['brb'])
            T.dma('sync', 'EB', EB[:], eb_d[0:1, :].broadcast_to([128, 16]), writes=['EB'])
            T.op('gpsimd', MEMSET(onesf[:], 1.0), writes=['onesf'])
            for (colt, rowt, nm) in ((A2, A2R, 'A2R'), (B2, B2R, 'B2R')):
                for kt in range(8):
                    T.op('vector', TS(dg[:], ident_f[:], colt[:, kt:kt + 1], None, ALU.mult), reads=['identf', 'A2', 'B2', 'dg'],
                         writes=['dg'])
                    T.op('tensor', mmgrp([(pm[:, 0:128], onesf[:], dg[:], True, True)]), reads=['dg', 'onesf'], writes=['pm'])
                    T.op('vector', CP(rowt[:, kt * 128:(kt + 1) * 128], pm[:, 0:128]), reads=['pm'], writes=[nm])
            for g in range(32):
                xb = g % 2
                xn2 = xn2_2[xb]
                junk = junk_2[xb]
                H2f = H2f_2[xb]
                tmpg = tmpg_2[xb]
                hrt = hrt_2[xb]
                L = L_2[xb]
                s1 = s1_2[xb]
                s2 = s2_2[xb]
                E1 = E1_2[xb]
                E2 = E2_2[xb]
                Mx = Mx_2[xb]
                oh = oh_2[xb]
                mk1 = mk1_2[xb]
                mk2 = mk2_2[xb]
                les = les_2[xb]
                le2 = le2_2[xb]
                sc = sc_2[xb]
                idf = idf_2[xb]
                rs = rs_2[xb]
                T.dma('sync', f'x1t{xb}', X1t[xb][:], X1_d[g * 128:(g + 1) * 128, :], reads=[f'X1_d{g}'], writes=[f'x1t{xb}'])
                T.op('scalar', ACT(junk[:], X1t[xb][:], AF.Square, accum_out=ssq[:, g:g + 1]), reads=[f'x1t{xb}', 'ssqall'],
                     writes=[f'junk{xb}', f'ssq_{g}'])
                T.op('scalar', ACT(rs[:], ssq[:, g:g + 1], AF.Ln, scale=1.0 / 1024, bias=eps_t[:]), reads=[f'ssq_{g}', 'eps'], writes=[f'rs{xb}'])
                T.op('scalar', ACT(rs[:], rs[:], AF.Exp, scale=-0.5), reads=[f'rs{xb}'], writes=[f'rs{xb}'])
                T.op('vector', TS(xn2[:], X1t[xb][:], rs[:, 0:1], None, ALU.mult), reads=[f'x1t{xb}', f'rs{xb}'], writes=[f'xn2{xb}'])
                T.op('vector', TT(hrt[:], xn2[:], A2R[:], ALU.mult), reads=[f'xn2{xb}', 'A2R'], writes=[f'hrt{xb}'])
                T.op('vector', TT(H2row[xb][:], hrt[:], B2R[:], ALU.add), reads=[f'hrt{xb}', 'B2R'], writes=[f'h2row{xb}'])
                T.op('tensor', tpgrp([(tpc[:, kt, :], xn2[:, kt * 128:(kt + 1) * 128], ident_f[:]) for kt in range(8)]),
                     reads=[f'xn2{xb}', 'identf'], writes=['tpc'])
                T.op('vector', TT(tmpg[:], tpc[:], A2[:].unsqueeze(2).broadcast_to([128, 8, 128]), ALU.mult), reads=['tpc', 'A2'],
                     writes=[f'tmpg{xb}'])
                T.op('vector', TT(H2f[:], tmpg[:], B2[:].unsqueeze(2).broadcast_to([128, 8, 128]), ALU.add), reads=[f'tmpg{xb}', 'B2'],
                     writes=[f'h2f{xb}'])
                T.op('tensor', mmgrp([(pm[:, 0:20], H2f[:, kt, :], WR[:, kt, :], kt == 0, kt == 7) for kt in range(8)]),
                     reads=[f'h2f{xb}', 'WR'], writes=['pm'])
                T.op('vector', TT(L[:], pm[:, 0:20], brb[:], ALU.add), reads=['pm', 'brb'], writes=[f'L{xb}'])
                rr = [f'L{xb}']
                V = lambda fn: T.op('vector', fn, reads=rr, writes=rr)
                S = lambda fn: T.op('scalar', fn, reads=rr, writes=rr)
                lg = L[:, 0:4]
                le = L[:, 4:20].rearrange("t (g e) -> t g e", g=4)
                V(RMAX(sc[:, 0:1], lg))
                V(TS(oh[:], lg, sc[:, 0:1], None, ALU.is_equal))
                V(TS(sc[:, 1:2], sc[:, 0:1], -1.0, None, ALU.mult))
                V(MEMSET(sc[:, 2:3], 0.0))
                S(ACT(s1[:, 0:4], lg, AF.Exp, bias=sc[:, 1:2], accum_out=sc[:, 2:3]))
                V(RECIP(sc[:, 3:4], sc[:, 2:3]))
                V(TT(s2[:].rearrange("t (g e) -> t g e", g=4), le, oh[:].unsqueeze(2).broadcast_to([128, 4, 4]), ALU.mult))
                V(RED(les[:], s2[:].rearrange("t (g e) -> t e g", g=4), ALU.add))
                V(RMAX(sc[:, 4:5], les[:]))
                V(TS(mk1[:], les[:], sc[:, 4:5], None, ALU.is_equal))
                V(STT(le2[:], mk1[:], NEG, les[:], ALU.mult, ALU.add))
                V(RMAX(sc[:, 5:6], le2[:]))
                V(TS(mk2[:], le2[:], sc[:, 5:6], None, ALU.is_equal))
                V(TT(sc[:, 6:7], sc[:, 5:6], sc[:, 4:5], ALU.subtract))
                S(ACT(sc[:, 7:8], sc[:, 6:7], AF.Exp))
                V(TS(sc[:, 8:9], sc[:, 7:8], 1.0, None, ALU.add))
                V(RECIP(sc[:, 8:9], sc[:, 8:9]))
                V(TT(W12[:, g, 0:1], sc[:, 8:9], sc[:, 3:4], ALU.mult))
                V(TT(W12[:, g, 1:2], sc[:, 7:8], W12[:, g, 0:1], ALU.mult))
                V(TT(E1[:].rearrange("t (g e) -> t g e", g=4), oh[:].unsqueeze(2).broadcast_to([128, 4, 4]),
                     mk1[:].unsqueeze(1).broadcast_to([128, 4, 4]), ALU.mult))
                V(TT(E2[:].rearrange("t (g e) -> t g e", g=4), oh[:].unsqueeze(2).broadcast_to([128, 4, 4]),
                     mk2[:].unsqueeze(1).broadcast_to([128, 4, 4]), ALU.mult))
                V(TT(Mx[:], E1[:], E2[:], ALU.add))
                lst = [(pm[:, 32:48], Umat[:], Mx[:], True, g == 0)]
                if g > 0:
                    lst.append((pm[:, 32:48], Sel127[:], Cn[:, g - 1, :], False, True))
                T.op('tensor', mmgrp(lst), reads=[f'L{xb}', 'U', 'sel127', 'Cn'], writes=['pm'])
                T.op('vector', CP(Cn[:, g, :], pm[:, 32:48]), reads=['pm'], writes=['Cn'])
                T.op('vector', TT(s1[:], Cn[:, g, :], EB[:], ALU.add), reads=['Cn', 'EB', f'L{xb}'], writes=[f'L{xb}'])
                V(TT(s2[:], s1[:], E1[:], ALU.mult))
                V(RED(idf[:, 0:1], s2[:], ALU.add))
                V(TT(s2[:], s1[:], E2[:], ALU.mult))
                V(RED(idf[:, 1:2], s2[:], ALU.add))
                T.op('vector', CP(IDX[:, g, :], idf[:]), reads=[f'L{xb}'], writes=[f'idx{g}'])
                for k in range(2):
                    T.idma(f'sc{xb}{k}', Xs_d, bass.IndirectOffsetOnAxis(ap=IDX[:, g, k:k + 1], axis=0), H2row[xb][:], None,
                           reads=[f'idx{g}', f'h2row{xb}'], writes=[f'Xs{g}_{k}'])
            T.op('tensor', mmgrp([(pm[:, 64:80], Sel127[:], Cn[:, 31, :], True, True)]), reads=['Cn', 'sel127'], writes=['pm'])
            T.op('vector', CP(CNTi[:], pm[:, 64:80]), reads=['pm'], writes=['cnti'])
            T.dma('sync', 'cntd', CNT_d, CNTi[0:1, :], reads=['cnti'], writes=['CNT_d'])
            dump("IDX", IDX[:].rearrange("p g k -> p (g k)"), [128, 64], U32, [f'idx{g}' for g in range(32)])
            dump("W12", W12[:].rearrange("p g k -> p (g k)"), [128, 64], F32, ['L'])
            dump("Cn", Cn[:].rearrange("p g e -> p (g e)"), [128, 512], F32, ['Cn'])
            T.barrier()

        with ExitStack() as es:
            WG = [sb(es, [128, 8, 512], BF16) for _ in range(2)]
            WU = [sb(es, [128, 8, 512], BF16) for _ in range(2)]
            WD = [sb(es, [128, 4, 1024], BF16) for _ in range(2)]
            XT = [sb(es, [128, 1024], BF16) for _ in range(3)]
            XsT = [sb(es, [128, 8, 512], BF16) for _ in range(2)]
            AT = [sb(es, [128, 4, 512], BF16) for _ in range(2)]
            sg = [sb(es, [128, 512], F32) for _ in range(2)]
            YT = [sb(es, [128, 1024], F32) for _ in range(2)]
            tpb = ps(es, [128, 8, 128], BF16)
            gp = [ps(es, [128, 512], F32) for _ in range(2)]
            up = [ps(es, [128, 512], F32) for _ in range(2)]
            yp = [ps(es, [128, 512], F32) for _ in range(2)]
            wg_v = wg_d.rearrange("e (kt p) n -> e p kt n", p=128)
            wu_v = wu_d.rearrange("e (kt p) n -> e p kt n", p=128)
            wd_v = wd_d.rearrange("e (kt p) n -> e p kt n", p=128)
            gi = [0]
            yi = [0]
            xi = [0]
            yti = [0]
            gri = [0]

            def load_w(ex):
                wb = ex % 2
                T.dma('gpsimd', f'wg{wb}', WG[wb][:], wg_v[ex], writes=[f'wg{wb}'])
                T.dma('gpsimd', f'wu{wb}', WU[wb][:], wu_v[ex], writes=[f'wu{wb}'])
                T.dma('gpsimd', f'wd{wb}', WD[wb][:], wd_v[ex], writes=[f'wd{wb}'])

            def emit_group(ex, grp):
                wb = ex % 2
                ab = gri[0] % 2
                gri[0] += 1
                base = ex * 4096 + grp * 512
                for tl in range(4):
                    k3 = xi[0] % 3
                    xi[0] += 1
                    r0 = base + tl * 128
                    T.dma('sync', f'xt{k3}', XT[k3][:], Xs_d[r0:r0 + 128, :], writes=[f'xt{k3}'])
                    T.op('tensor', tpgrp([(tpb[:, kt, :], XT[k3][:, kt * 128:(kt + 1) * 128], ident_b[:]) for kt in range(8)]),
                         reads=[f'xt{k3}', 'identb'], writes=['tpb'])
                    T.op('vector', CP(XsT[ab][:, :, tl * 128:(tl + 1) * 128], tpb[:]), reads=['tpb'], writes=[f'xst{ab}'])
                for ht in range(4):
                    k2 = gi[0] % 2
                    gi[0] += 1
                    T.op('tensor', mmgrp([(gp[k2][:], WG[wb][:, kt, ht * 128:(ht + 1) * 128], XsT[ab][:, kt, :], kt == 0, kt == 7)
                                          for kt in range(8)]), reads=[f'wg{wb}', f'xst{ab}'], writes=[f'gp{k2}'])
                    T.op('tensor', mmgrp([(up[k2][:], WU[wb][:, kt, ht * 128:(ht + 1) * 128], XsT[ab][:, kt, :], kt == 0, kt == 7)
                                          for kt in range(8)]), reads=[f'wu{wb}', f'xst{ab}'], writes=[f'up{k2}'])
                    T.op('scalar', ACT(sg[k2][:], gp[k2][:], AF.Silu), reads=[f'gp{k2}'], writes=[f'sg{k2}'])
                    T.op('vector', TT(AT[ab][:, ht, :], sg[k2][:], up[k2][:], ALU.mult), reads=[f'sg{k2}', f'up{k2}'],
                         writes=[f'at{ab}'])
                for tl in range(4):
                    yb = yti[0] % 2
                    yti[0] += 1
                    for nh in range(2):
                        k2 = yi[0] % 2
                        yi[0] += 1
                        T.op('tensor', mmgrp([(yp[k2][:], AT[ab][:, ht, tl * 128:(tl + 1) * 128], WD[wb][:, ht, nh * 512:(nh + 1) * 512],
                                               ht == 0, ht == 3) for ht in range(4)]), reads=[f'at{ab}', f'wd{wb}'], writes=[f'yp{k2}'])
                        T.op('scalar', ACT(YT[yb][:, nh * 512:(nh + 1) * 512], yp[k2][:], AF.Copy), reads=[f'yp{k2}'], writes=[f'yt{yb}'])
                    r0 = base + tl * 128
                    T.dma('sync', f'yt{yb}', Ys_d[r0:r0 + 128, :], YT[yb][:], reads=[f'yt{yb}'], writes=[f'Ys{ex}_{grp}_{tl}'])

            drain = ['xt0', 'xt1', 'xt2', 'yt0', 'yt1']
            load_w(0)
            for ex in range(16):
                if ex + 1 < 16:
                    load_w(ex + 1)
                emit_group(ex, 0)
                for grp in range(1, 8):
                    T.cond_begin(CNT_d[0:1, ex:ex + 1], grp * 512, drain)
                    emit_group(ex, grp)
                    T.cond_end()
            T.barrier()

        with ExitStack() as es:
            Y1 = [sb(es, [128, 1024], F32) for _ in range(2)]
            Y2 = [sb(es, [128, 1024], F32) for _ in range(2)]
            X1t = [sb(es, [128, 1024], F32) for _ in range(2)]
            junk = sb(es, [128, 1024], BF16)
            osb = [sb(es, [128, 1024], F32) for _ in range(2)]
            ssq = sb(es, [128, 32], F32)
            T.op('vector', MEMSET(ssq[:], 0.0), writes=['ssqall'])
            rs = [sb(es, [128, 1], F32) for _ in range(2)]
            def c3_loads(g):
                b = g % 2
                T.dma('sync', f'x1c{b}', X1t[b][:], X1_d[g * 128:(g + 1) * 128, :], writes=[f'x1c{b}'])
                T.idma(f'ga{b}', Y1[b][:], None, Ys_d, bass.IndirectOffsetOnAxis(ap=IDX[:, g, 0:1], axis=0), writes=[f'y1{b}'])
                T.idma(f'gb{b}', Y2[b][:], None, Ys_d, bass.IndirectOffsetOnAxis(ap=IDX[:, g, 1:2], axis=0), writes=[f'y2{b}'])

            c3_loads(0)
            for g in range(32):
                b = g % 2
                if g + 1 < 32:
                    c3_loads(g + 1)
                T.op('vector', TS(Y1[b][:], Y1[b][:], W12[:, g, 0:1], None, ALU.mult), reads=[f'y1{b}'], writes=[f'y1{b}'])
                T.op('vector', STT(Y1[b][:], Y2[b][:], W12[:, g, 1:2], Y1[b][:], ALU.mult, ALU.add), reads=[f'y1{b}', f'y2{b}'],
                     writes=[f'y1{b}'])
                T.op('vector', TT(Y1[b][:], Y1[b][:], GT2[:], ALU.mult), reads=[f'y1{b}'], writes=[f'y1{b}'])
                T.op('vector', TT(Y1[b][:], Y1[b][:], X1t[b][:], ALU.add), reads=[f'y1{b}', f'x1c{b}'], writes=[f'y1{b}'])
                T.op('scalar', ACT(junk[:], Y1[b][:], AF.Square, accum_out=ssq[:, g:g + 1]), reads=[f'y1{b}', 'ssqall'],
                     writes=['junk', f'ssq_{g}'])
                T.op('scalar', ACT(rs[b][:], ssq[:, g:g + 1], AF.Ln, scale=1.0 / 1024, bias=eps_t[:]), reads=[f'ssq_{g}', 'eps'], writes=[f'rs{b}'])
                T.op('scalar', ACT(rs[b][:], rs[b][:], AF.Exp, scale=-0.5), reads=[f'rs{b}'], writes=[f'rs{b}'])
                T.op('vector', STT(osb[b][:], Y1[b][:], rs[b][:, 0:1], gfin[:], ALU.mult, ALU.mult),
                     reads=[f'y1{b}', f'rs{b}', 'gfin'], writes=[f'osb{b}'])
                T.dma('sync', f'out{b}', out_d[g * 128:(g + 1) * 128, :], osb[b][:], reads=[f'osb{b}'], writes=[f'out{g}'])
            T.barrier()
    T.emit()
    top.close()
    return nc


_CACHE = {}


def _masks(j):
    m = np.zeros((2, 8, 128, 512), np.float32)
    ki = np.arange(128)[:, None]
    qq = np.arange(512)[None, :]
    for par in range(2):
        i = par
        run = OWN_RUNS[j][i]
        nk = NK(i)
        for r in range(8):
            jb = nk - 8 + r
            kpos = jb * 128 + ki
            qpos = run * 512 + qq
            m[par, r] = np.where(kpos <= qpos, 0.0, NEG)
    return m.reshape(16, 128, 512)


def _rsel(j):
    s = np.zeros((8, 64), np.float32)
    for i, run in enumerate(OWN_RUNS[j]):
        s[i, 4 * run + 1] = 1.0
    return s.reshape(1, 512)


def kernel(x, c, w_ada, b_ada, g_norm_mix, w_in, g_sgu, w_spatial, b_spatial, b_forget, g_out_sgu, g_out_fox, w_out,
           g_norm_ffn, w_router_group, b_router_group, w_router_expert, b_router_expert, w_gate, w_up, w_down, g_final):
    f = lambda a: np.ascontiguousarray(np.asarray(a, dtype=np.float32))
    x = f(x); c = f(c)
    if "nc" not in _CACHE:
        _CACHE["nc"] = build_program()
    nc = _CACHE["nc"]
    wr = np.concatenate([f(w_router_group)[0], f(w_router_expert)[0].transpose(1, 0, 2).reshape(1024, 16)], axis=1)
    br = np.concatenate([f(b_router_group)[0], f(b_router_expert)[0].reshape(16)])[None, :]
    shared = {
        "w_ada": f(w_ada)[0], "b_ada": f(b_ada)[0][None, :], "g1T": f(f(g_norm_mix)[0].reshape(8, 128).T),
        "g2T": f(f(g_norm_ffn)[0].reshape(8, 128).T), "w_in": f(w_in)[0], "gsgu": f(g_sgu)[0][None, :],
        "wsT": f(f(w_spatial)[0].transpose(2, 0, 1)), "bsp": f(b_spatial)[0], "bfg": f(b_forget)[0][None, :],
        "gos": f(f(g_out_sgu)[0].reshape(4, 128).T), "gof": f(f(g_out_fox)[0].reshape(4, 128).T), "w_out": f(w_out)[0],
        "wr": f(wr), "br": f(br), "w_gate": f(w_gate)[0], "w_up": f(w_up)[0], "w_down": f(w_down)[0],
        "gfin": f(g_final)[None, :],
    }
    in_maps = []
    for core in range(8):
        b, j = core // 2, core % 2
        m = dict(shared)
        m["xall"] = x[b]
        m["xown"] = f(np.concatenate([x[b, 512 * r:512 * (r + 1)] for r in OWN_RUNS[j]], axis=0))
        m["cT"] = f(c[b].reshape(8, 128).T)
        m["masks"] = _masks(j)
        m["rsel"] = _rsel(j)
        m["eb"] = (np.arange(16, dtype=np.float32) * 4096.0 - 1.0)[None, :]
        in_maps.append(m)
    res = run_bass_kernel_spmd(nc, in_maps, core_ids=list(range(8)))
    _CACHE["res"] = res
    out = np.empty((4, 8192, 1024), np.float32)
    for core in range(8):
        b, j = core // 2, core % 2
        o = res.results[core]["out"]
        for i, r in enumerate(OWN_RUNS[j]):
            out[b, 512 * r:512 * (r + 1)] = o[512 * i:512 * (i + 1)]
    return out
```

```python
import os
import numpy as np
from contextlib import ExitStack
import concourse.bass as bass
import concourse.mybir as mybir
from concourse.bass_utils import run_bass_kernel_spmd

F32 = mybir.dt.float32
BF16 = mybir.dt.bfloat16
U32 = mybir.dt.uint32
I32 = mybir.dt.int32
AF = mybir.ActivationFunctionType
ALU = mybir.AluOpType
AX = mybir.AxisListType
ENGS = ['sync', 'scalar', 'vector', 'gpsimd', 'tensor']
EPS = 1e-6
NEG = -1.0e30
OWN_RUNS = {0: [0, 3, 4, 7, 8, 11, 12, 15], 1: [1, 2, 5, 6, 9, 10, 13, 14]}
WARM = 18
PROBE = os.environ.get('MK_PROBE', '')
DEBUG = {"x1": False, "same": True, "phases": "ABC", "dump": []}


def NK(i):
    return 16 * (i // 2) + 8 + 8 * (i % 2)


class Tracker:
    def __init__(self, nc, same_engine_sync=False):
        self.nc = nc
        self.streams = {e: [] for e in ENGS}
        self.sems = {e: nc.alloc_semaphore("c_" + e) for e in ENGS}
        self.cnt = {e: 0 for e in ENGS}
        self.waited = {e: {} for e in ENGS}
        self.res = {}
        self.slots = {}
        self.same = same_engine_sync
        self.slot_q = {}

    def _deps(self, reads, writes):
        deps = []
        for r in reads:
            st = self.res.get(r)
            if st and st[0]:
                deps.append(st[0])
        for w in writes:
            st = self.res.get(w)
            if st:
                if st[0]:
                    deps.append(st[0])
                deps.extend(st[1])
        return deps

    def _update(self, reads, writes, tag):
        for r in reads:
            st = self.res.setdefault(r, [None, []])
            st[1].append(tag)
        for w in writes:
            self.res[w] = [tag, []]

    def _emit_waits(self, eng, deps, skip_same):
        mx = {}
        for key, val in deps:
            mx[key] = max(mx.get(key, 0), val)
        for key, val in mx.items():
            if key == eng and skip_same:
                continue
            if self.waited[eng].get(key, 0) >= val:
                continue
            self.waited[eng][key] = val
            sem = self.sems[key] if key in self.sems else self.slots[key][0]
            self.streams[eng].append(lambda e, sem=sem, val=val: e.wait_ge(sem, val))

    def op(self, eng, fn, reads=(), writes=()):
        deps = self._deps(reads, writes)
        self._emit_waits(eng, deps, (not self.same) or eng == 'tensor')
        self.cnt[eng] += 1
        sem = self.sems[eng]
        self.streams[eng].append(lambda e, fn=fn, sem=sem: fn(e).then_inc(sem, 1))
        self._update(reads, writes, (eng, self.cnt[eng]))

    def dma(self, q, slot, out, in_, reads=(), writes=()):
        self.slot_q[slot] = q
        if slot not in self.slots:
            self.slots[slot] = [self.nc.alloc_semaphore("d_" + slot), 0]
        deps = self._deps(reads, writes)
        self._emit_waits(q, deps, False)
        s = self.slots[slot]
        s[1] += 16
        sem = s[0]
        self.streams[q].append(lambda e, out=out, in_=in_, sem=sem: e.dma_start(out=out, in_=in_).then_inc(sem, 16))
        self._update(reads, writes, (slot, s[1]))

    def idma(self, slot, out, out_off, in_, in_off, reads=(), writes=()):
        q = 'gpsimd'
        self.slot_q[slot] = q
        if slot not in self.slots:
            self.slots[slot] = [self.nc.alloc_semaphore("d_" + slot), 0]
        deps = self._deps(reads, writes)
        self._emit_waits(q, deps, False)
        s = self.slots[slot]
        s[1] += 16
        sem = s[0]
        self.streams[q].append(lambda e, out=out, in_=in_, sem=sem, oo=out_off, io=in_off:
                               e.indirect_dma_start(out=out, out_offset=oo, in_=in_, in_offset=io).then_inc(sem, 16))
        self._update(reads, writes, (slot, s[1]))

    def cond_begin(self, cnt_ap, thr, drain_slots):
        for e in ENGS:
            deps = [(e, self.cnt[e])] if self.cnt[e] else []
            deps += [(k, self.slots[k][1]) for k in drain_slots if k in self.slots and self.slot_q.get(k) == e]
            self._emit_waits(e, deps, False)
        self.snap_cnt = dict(self.cnt)
        self.snap_slots = {k: v[1] for k, v in self.slots.items()}
        self.snap_waited = {e: dict(w) for e, w in self.waited.items()}
        for e in ENGS:
            self.streams[e].append(('regload', cnt_ap))
            self.streams[e].append(('if', thr))

    def cond_end(self):
        for e in ENGS:
            self.streams[e].append(('else',))
            n = self.cnt[e] - self.snap_cnt[e]
            if n:
                self.streams[e].append(lambda eng, sem=self.sems[e], n=n: eng.sem_inc(sem, n))
        for slot, (sem, c) in self.slots.items():
            d = c - self.snap_slots.get(slot, 0)
            if d:
                self.streams[self.slot_q[slot]].append(lambda eng, sem=sem, d=d: eng.sem_inc(sem, d))
        for e in ENGS:
            self.streams[e].append(('endif',))
        self.waited = self.snap_waited

    def barrier(self):
        deps = [(e, c) for e, c in self.cnt.items() if c > 0]
        deps += [(k, v[1]) for k, v in self.slots.items() if v[1] > 0]
        for e in ENGS:
            self._emit_waits(e, deps, True)

    def emit(self):
        self.barrier()
        streams = self.streams

        def run(e, items):
            creg = e.alloc_register("creg")
            i = 0
            n = len(items)
            while i < n:
                it = items[i]
                if not isinstance(it, tuple):
                    it(e)
                    i += 1
                    continue
                if it[0] == 'regload':
                    e.reg_load(creg, it[1])
                    i += 1
                    continue
                assert it[0] == 'if'
                thr = it[1]
                j = i + 1
                body = []
                while not (isinstance(items[j], tuple) and items[j][0] == 'else'):
                    body.append(items[j])
                    j += 1
                j += 1
                fix = []
                while not (isinstance(items[j], tuple) and items[j][0] == 'endif'):
                    fix.append(items[j])
                    j += 1
                with e.If_lt(creg, thr + 1):
                    for f in fix:
                        f(e)
                with e.Else():
                    for f in body:
                        f(e)
                i = j + 1

        with self.nc.Block() as block:
            @block.sync
            def _(e):
                run(e, streams['sync'])

            @block.scalar
            def _(e):
                run(e, streams['scalar'])

            @block.vector
            def _(e):
                run(e, streams['vector'])

            @block.gpsimd
            def _(e):
                run(e, streams['gpsimd'])

            @block.tensor
            def _(e):
                run(e, streams['tensor'])


def mmgrp(lst):
    def fn(e):
        ins = None
        for (out, lhsT, rhs, st, sp) in lst:
            ins = e.matmul(out, lhsT=lhsT, rhs=rhs, start=st, stop=sp)
        return ins
    return fn


def tpgrp(lst):
    def fn(e):
        ins = None
        for (out, in_, ident) in lst:
            ins = e.transpose(out, in_, ident)
        return ins
    return fn


def ACT(out, in_, func, **kw):
    return lambda e: e.activation(out=out, in_=in_, func=func, **kw)


def TT(out, in0, in1, op):
    return lambda e: e.tensor_tensor(out=out, in0=in0, in1=in1, op=op)


def TS(out, in0, s1, s2, op0, op1=None):
    if op1 is None:
        return lambda e: e.tensor_scalar(out=out, in0=in0, scalar1=s1, scalar2=None, op0=op0)
    return lambda e: e.tensor_scalar(out=out, in0=in0, scalar1=s1, scalar2=s2, op0=op0, op1=op1)


def STT(out, in0, scalar, in1, op0, op1):
    return lambda e: e.scalar_tensor_tensor(out=out, in0=in0, scalar=scalar, in1=in1, op0=op0, op1=op1)


def CP(out, in_):
    return lambda e: e.tensor_copy(out=out, in_=in_)


def RED(out, in_, op):
    return lambda e: e.tensor_reduce(out=out, in_=in_, axis=AX.X, op=op)


def RMAX(out, in_):
    return lambda e: e.reduce_max(out=out, in_=in_, axis=AX.X)


def RECIP(out, in_):
    return lambda e: e.reciprocal(out=out, in_=in_)


def MEMSET(ap, v):
    return lambda e: e.memset(ap, v)


def ASEL(out, in_, pattern, cmp, fill, base, cm):
    return lambda e: e.affine_select(out=out, in_=in_, pattern=pattern, compare_op=cmp, fill=fill, base=base,
                                     channel_multiplier=cm)


def build_program():
    nc = bass.Bass("TRN2", target_bir_lowering=False)
    T = Tracker(nc, same_engine_sync=DEBUG["same"])
    din = lambda name, shape: nc.dram_tensor(name, shape, F32, kind="ExternalInput").ap()
    xall = din("xall", [8192, 1024])
    xown = din("xown", [4096, 1024])
    cT_d = din("cT", [128, 8])
    wada_d = din("w_ada", [1024, 6144])
    bada_d = din("b_ada", [1, 6144])
    g1T_d = din("g1T", [128, 8])
    g2T_d = din("g2T", [128, 8])
    win_d = din("w_in", [1024, 2568])
    gsgu_d = din("gsgu", [1, 512])
    wsT_d = din("wsT", [128, 8, 128])
    bsp_d = din("bsp", [8, 128])
    bfg_d = din("bfg", [1, 8])
    gos_d = din("gos", [128, 4])
    gof_d = din("gof", [128, 4])
    wout_d = din("w_out", [1024, 1024])
    wr_d = din("wr", [1024, 20])
    br_d = din("br", [1, 20])
    wg_d = din("w_gate", [16, 1024, 512])
    wu_d = din("w_up", [16, 1024, 512])
    wd_d = din("w_down", [16, 512, 1024])
    gfin_d = din("gfin", [1, 1024])
    masks_d = din("masks", [16, 128, 512])
    rsel_d = din("rsel", [1, 512])
    eb_d = din("eb", [1, 16])
    out_d = nc.dram_tensor("out", [4096, 1024], F32, kind="ExternalOutput").ap()
    KT_d = nc.dram_tensor("KT_d", [4, 128, 8192], BF16).ap()
    V_d = nc.dram_tensor("V_d", [4, 128, 64, 192], BF16).ap()
    Xs_d = nc.dram_tensor("Xs_d", [65536, 1024], BF16).ap()
    Ys_d = nc.dram_tensor("Ys_d", [65536, 1024], F32).ap()
    CNT_d = nc.dram_tensor("CNT_d", [1, 16], I32).ap()
    if DEBUG["x1"]:
        X1_d = nc.dram_tensor("X1_d", [4096, 1024], F32, kind="ExternalOutput").ap()
    else:
        X1_d = nc.dram_tensor("X1_d", [4096, 1024], F32).ap()

    win_v = win_d.rearrange("(kt p) n -> p kt n", p=128)
    dumps = DEBUG.get("dump", [])

    def dump(name, ap, shape, dt, rname):
        if name not in dumps:
            return
        d = nc.dram_tensor("dbg_" + name, shape, dt, kind="ExternalOutput").ap()
        T.dma('sync', 'dbg_' + name, d, ap, reads=rname, writes=['dbg_' + name])

    top = ExitStack()
    uid = [0]

    def sb(es, shape, dt, name=None):
        uid[0] += 1
        return es.enter_context(nc.sbuf_tensor(f"{name or 't'}_{uid[0]}", shape, dt))

    def ps(es, shape, dt, name=None):
        uid[0] += 1
        return es.enter_context(nc.psum_tensor(f"{name or 'p'}_{uid[0]}", shape, dt))

    ident_f = sb(top, [128, 128], F32, "identf")
    ident_b = sb(top, [128, 128], BF16, "identb")
    Umat = sb(top, [128, 128], F32, "U")
    Sel127 = sb(top, [128, 128], F32, "sel127")
    eps_t = sb(top, [128, 1], F32, "eps")
    one_t = sb(top, [128, 1], F32, "one")
    ones_bf = sb(top, [128, 1], BF16, "onesbf")
    A1 = sb(top, [128, 8], F32, "A1")
    B1 = sb(top, [128, 8], F32, "B1")
    A2 = sb(top, [128, 8], F32, "A2")
    B2 = sb(top, [128, 8], F32, "B2")
    GT1 = sb(top, [128, 1024], F32, "GT1")
    GT2 = sb(top, [128, 1024], F32, "GT2")
    C_all = sb(top, [128, 64, 8], F32, "Call")
    RC = sb(top, [128, 8, 8], F32, "RC")

    T.op('gpsimd', MEMSET(ident_f[:], 0.0), writes=['identf'])
    T.op('gpsimd', ASEL(ident_f[:], ident_f[:], [[-1, 128]], ALU.not_equal, 1.0, 0, 1), reads=['identf'], writes=['identf'])
    T.op('gpsimd', MEMSET(Umat[:], 1.0), writes=['U'])
    T.op('gpsimd', ASEL(Umat[:], Umat[:], [[1, 128]], ALU.is_ge, 0.0, 0, -1), reads=['U'], writes=['U'])
    T.op('gpsimd', MEMSET(Sel127[:], 1.0), writes=['sel127'])
    T.op('gpsimd', ASEL(Sel127[:], Sel127[:], [[0, 128]], ALU.is_ge, 0.0, -127, 1), reads=['sel127'], writes=['sel127'])
    T.op('gpsimd', MEMSET(eps_t[:], EPS), writes=['eps'])
    T.op('gpsimd', MEMSET(one_t[:], 1.0), writes=['one'])
    T.op('gpsimd', MEMSET(ones_bf[:], 1.0), writes=['onesbf'])
    T.op('vector', CP(ident_b[:], ident_f[:]), reads=['identf'], writes=['identb'])

    with ExitStack() as es:
        cT = sb(es, [128, 8], F32)
        CB = sb(es, [128, 8, 128], F32)
        WA = [sb(es, [128, 8, 1024], F32, "wada") for _ in range(2)]
        badab = sb(es, [128, 1024], F32)
        gT1 = sb(es, [128, 8], F32)
        gT2 = sb(es, [128, 8], F32)
        rowv = sb(es, [128, 1024], F32)
        dtmp = sb(es, [128, 8, 128], F32)
        colv = sb(es, [128, 8], F32)
        pp = [ps(es, [128, 512], F32) for _ in range(2)]
        T.dma('sync', 'cT', cT[:], cT_d, writes=['cT'])
        T.dma('sync', 'gT1', gT1[:], g1T_d, writes=['gT1'])
        T.dma('sync', 'gT2', gT2[:], g2T_d, writes=['gT2'])
        T.op('scalar', ACT(cT[:], cT[:], AF.Silu), reads=['cT'], writes=['cT'])
        T.op('vector', CP(CB[:], cT[:].unsqueeze(2).broadcast_to([128, 8, 128])), reads=['cT'], writes=['CB'])
        wada_v = wada_d.rearrange("(kt p) n -> p kt n", p=128)
        order = [1, 0, 2, 4, 3, 5]
        for n, v in enumerate(order):
            wb = n % 2
            for kt in range(8):
                T.dma('sync', f'wada{wb}_{kt}', WA[wb][:, kt, :], wada_v[:, kt, v * 1024:(v + 1) * 1024],
                      writes=[f'wada{wb}_{kt}'])
            T.dma('gpsimd', 'badab', badab[:], bada_d[0:1, v * 1024:(v + 1) * 1024].broadcast_to([128, 1024]),
                  writes=['badab'])
            for hf in range(2):
                T.op('tensor', mmgrp([(pp[hf][:], CB[:, kt, :], WA[wb][:, kt, hf * 512:(hf + 1) * 512], kt == 0, kt == 7)
                                      for kt in range(8)]),
                     reads=['CB'] + [f'wada{wb}_{kt}' for kt in range(8)], writes=[f'pp{hf}'])
                dst = {2: GT1, 5: GT2}.get(v, rowv)
                dname = {2: 'GT1', 5: 'GT2'}.get(v, 'rowv') + str(hf)
                T.op('vector', TT(dst[:, hf * 512:(hf + 1) * 512], pp[hf][:], badab[:, hf * 512:(hf + 1) * 512], ALU.add),
                     reads=[f'pp{hf}', 'badab'], writes=[dname])
            if v in (2, 5):
                continue
            T.op('vector', TT(dtmp[:], rowv[:].rearrange("p (k t) -> p k t", k=8),
                              ident_f[:].unsqueeze(1).broadcast_to([128, 8, 128]), ALU.mult),
                 reads=['rowv0', 'rowv1', 'identf'], writes=['dtmp'])
            T.op('vector', RED(colv[:], dtmp[:], ALU.add), reads=['dtmp'], writes=['colv'])
            if v == 1:
                T.op('vector', STT(A1[:], colv[:], 1.0, gT1[:], ALU.add, ALU.mult), reads=['colv', 'gT1'], writes=['A1'])
            elif v == 0:
                T.op('vector', CP(B1[:], colv[:]), reads=['colv'], writes=['B1'])
            elif v == 4:
                T.op('vector', STT(A2[:], colv[:], 1.0, gT2[:], ALU.add, ALU.mult), reads=['colv', 'gT2'], writes=['A2'])
            elif v == 3:
                T.op('vector', CP(B2[:], colv[:]), reads=['colv'], writes=['B2'])
        T.barrier()

    def ln1_block(xsrc_rows, XA, XN, junk, ssq, rs, tp, tmpf, HT_dst, bi, htname):
        b = bi % 2
        T.dma('sync', f'xa{b}', XA[b][:], xsrc_rows, writes=[f'xa{b}'])
        T.op('scalar', ACT(XN[b][:], XA[b][:], AF.Square, accum_out=ssq[:, bi:bi + 1]), reads=[f'xa{b}', 'ssqall'],
             writes=[f'xn{b}', f'ssq_{bi}'])
        T.op('scalar', ACT(rs[b][:], ssq[:, bi:bi + 1], AF.Ln, scale=1.0 / 1024, bias=eps_t[:]), reads=[f'ssq_{bi}', 'eps'],
             writes=[f'rs{b}'])
        T.op('scalar', ACT(rs[b][:], rs[b][:], AF.Exp, scale=-0.5), reads=[f'rs{b}'], writes=[f'rs{b}'])
        T.op('vector', TS(XN[b][:], XA[b][:], rs[b][:, 0:1], None, ALU.mult), reads=[f'xa{b}', f'rs{b}'], writes=[f'xn{b}'])
        T.op('tensor', tpgrp([(tp[:, kt, :], XN[b][:, kt * 128:(kt + 1) * 128], ident_b[:]) for kt in range(8)]),
             reads=[f'xn{b}', 'identb'], writes=['tp'])
        T.op('vector', TT(tmpf[:], tp[:], A1[:].unsqueeze(2).broadcast_to([128, 8, 128]), ALU.mult), reads=['tp', 'A1'],
             writes=['tmpf'])
        T.op('vector', TT(HT_dst, tmpf[:], B1[:].unsqueeze(2).broadcast_to([128, 8, 128]), ALU.add), reads=['tmpf', 'B1'],
             writes=[htname])

    if "A" in DEBUG["phases"]:
      with ExitStack() as es:
        WKV = sb(es, [128, 8, 1032], BF16, "wkvf")
        XA = [sb(es, [128, 1024], F32) for _ in range(2)]
        XN = [sb(es, [128, 1024], BF16) for _ in range(2)]
        junk = None
        ssq = sb(es, [128, 64], F32)
        T.op('vector', MEMSET(ssq[:], 0.0), writes=['ssqall'])
        rs = [sb(es, [128, 1], F32) for _ in range(2)]
        tmpf = sb(es, [128, 8, 128], F32)
        HT = [sb(es, [128, 8, 512], BF16) for _ in range(2)]
        KTs = [sb(es, [128, 4, 512], BF16) for _ in range(2)]
        VBs = [sb(es, [128, 4, 4, 3, 64], BF16) for _ in range(2)]
        bfb = sb(es, [128, 8], F32)
        tp = ps(es, [128, 8, 128], BF16)
        mm = [ps(es, [128, 512], F32) for _ in range(3)]
        sm = ps(es, [128, 512], F32)
        T.dma('gpsimd', 'wkvf', WKV[:], win_v[:, :, 1536:2568], writes=['wkvf'])
        T.dma('sync', 'bfb', bfb[:], bfg_d[0:1, :].broadcast_to([128, 8]), writes=['bfb'])
        for k in range(2):
            T.op('gpsimd', MEMSET(VBs[k][:, :, :, 1, :], 1.0), writes=[f'vbs{k}'])
        mi = [0]
        ftt = [sb(es, [128, 8], F32) for _ in range(4)]
        smb = [sm] + [ps(es, [128, 512], F32) for _ in range(3)]

        def lnA(r):
            hb = r % 2
            for bl in range(4):
                g = r * 4 + bl
                ln1_block(xall[g * 128:(g + 1) * 128, :], XA, XN, junk, ssq, rs, tp, tmpf,
                          HT[hb][:, :, bl * 128:(bl + 1) * 128], g, f'ht{hb}')

        def kvfA(r):
            hb = r % 2
            for p in range(4):
                m = mm[mi[0] % 3]; mn = f'mm{mi[0] % 3}'; mi[0] += 1
                T.op('tensor', mmgrp([(m[:], WKV[:, kt, p * 128:(p + 1) * 128], HT[hb][:, kt, :], kt == 0, kt == 7)
                                      for kt in range(8)]), reads=['wkvf', f'ht{hb}'], writes=[mn])
                T.op('scalar', ACT(KTs[hb][:, p, :], m[:], AF.Copy), reads=[mn], writes=[f'kts{hb}'])
            T.dma('gpsimd', f'kts{hb}', KT_d[:, :, r * 512:(r + 1) * 512].rearrange("q p t -> p q t"), KTs[hb][:],
                  reads=[f'kts{hb}'], writes=[f'KT_d{r}'])
            for bl in range(4):
                m = mm[mi[0] % 3]; mn = f'mm{mi[0] % 3}'; mi[0] += 1
                T.op('tensor', mmgrp([(m[:], HT[hb][:, kt, bl * 128:(bl + 1) * 128], WKV[:, kt, 512:1024], kt == 0, kt == 7)
                                      for kt in range(8)]), reads=['wkvf', f'ht{hb}'], writes=[mn])
                T.op('tensor', mmgrp([(smb[bl][:, 0:8], HT[hb][:, kt, bl * 128:(bl + 1) * 128], WKV[:, kt, 1024:1032], kt == 0, kt == 7)
                                      for kt in range(8)]), reads=['wkvf', f'ht{hb}'], writes=[f'smb{bl}'])
                T.op('scalar', ACT(VBs[hb][:, bl, :, 0::2, :], m[:].rearrange("t (p two d) -> t p two d", p=4, two=2),
                                   AF.Copy), reads=[mn], writes=[f'vbs{hb}'])
                ft = ftt[bl]
                T.op('vector', TT(ft[:], smb[bl][:, 0:8], bfb[:], ALU.add), reads=[f'smb{bl}', 'bfb'], writes=[f'ft{bl}'])
                T.op('scalar', ACT(ft[:], ft[:], AF.Exp, scale=-1.0), reads=[f'ft{bl}'], writes=[f'ft{bl}'])
                T.op('scalar', ACT(ft[:], ft[:], AF.Ln, bias=one_t[:]), reads=[f'ft{bl}', 'one'], writes=[f'ft{bl}'])
            for q in range(4):
                T.dma('gpsimd', f'vbs{hb}_{q}', V_d[q, :, r * 4:(r + 1) * 4, :],
                      VBs[hb][:, :, q, :, :].rearrange("t b three d -> t b (three d)"), reads=[f'vbs{hb}'],
                      writes=[f'V_d{r}_{q}'])
            for bl in range(4):
                g = r * 4 + bl
                cs = smb[bl][:, 8:16]
                lst = [(cs, Umat[:], ftt[bl][:], True, g == 0)]
                if g > 0:
                    lst.append((cs, Sel127[:], C_all[:, g - 1, :], False, True))
                T.op('tensor', mmgrp(lst), reads=[f'ft{bl}', 'U', 'sel127', 'Call'], writes=[f'smb{bl}'])
                T.op('vector', CP(C_all[:, g, :], cs), reads=[f'smb{bl}'], writes=['Call'])

        lnA(0)
        for r in range(16):
            if r + 1 < 16:
                lnA(r + 1)
            kvfA(r)
        T.barrier()

    with ExitStack() as es:
        rselb = sb(es, [128, 8, 64], F32)
        csel = sb(es, [128, 8, 8], F32)
        tmp3 = sb(es, [128, 8, 64], F32)
        pr = ps(es, [128, 512], F32)
        T.dma('sync', 'rselb', rselb[:], rsel_d[0:1, :].broadcast_to([128, 512]).rearrange("p (i b) -> p i b", i=8),
              writes=['rselb'])
        for i in range(8):
            T.op('vector', TT(tmp3[:], C_all[:].rearrange("p b h -> p h b"),
                              rselb[:, i, :].unsqueeze(1).broadcast_to([128, 8, 64]), ALU.mult),
                 reads=['Call', 'rselb'], writes=['tmp3'])
            T.op('vector', RED(csel[:, i, :], tmp3[:], ALU.add), reads=['tmp3'], writes=['csel'])
        T.op('tensor', mmgrp([(pr[:, 0:64], Sel127[:], csel[:].rearrange("p i h -> p (i h)"), True, True)]),
             reads=['csel', 'sel127'], writes=['pr'])
        T.op('vector', CP(RC[:].rearrange("p i h -> p (i h)"), pr[:, 0:64]), reads=['pr'], writes=['RC'])
        T.barrier()

    if "B" in DEBUG["phases"]:
      with ExitStack() as es:
        WQ = sb(es, [128, 8, 1536], BF16, "wq")
        WO = sb(es, [128, 8, 1024], BF16, "wo")
        MK = sb(es, [128, 16, 512], BF16, "mk")
        WsT = sb(es, [128, 8, 128], BF16, "wst")
        Bb = sb(es, [128, 4, 128], F32)
        gsg = sb(es, [128, 512], F32)
        gos = sb(es, [128, 4], F32)
        gof = sb(es, [128, 4], F32)
        XA = [sb(es, [128, 1024], F32) for _ in range(2)]
        XR = [sb(es, [128, 1024], F32) for _ in range(2)]
        XN = [sb(es, [128, 1024], BF16) for _ in range(2)]
        wstage = XA
        wsf = XR[0][:].rearrange("p (h t) -> p h t", h=8)
        junk = None
        ssq = sb(es, [128, 64], F32)
        T.op('vector', MEMSET(ssq[:], 0.0), writes=['ssqall'])
        rs = [sb(es, [128, 1], F32) for _ in range(2)]
        tmpf = sb(es, [128, 8, 128], F32)
        HT = sb(es, [128, 8, 512], BF16)
        QT = sb(es, [128, 4, 2, 512], BF16)
        T.op('gpsimd', MEMSET(QT[:], 0.0), writes=['qt'])
        UT = sb(es, [128, 4, 512], BF16)
        VG = sb(es, [128, 4, 512], F32)
        VG2 = sb(es, [128, 4, 512], F32)
        v8 = sb(es, [128, 32], F32)
        VN = sb(es, [128, 4, 4, 3, 64], BF16)
        YS = sb(es, [128, 4, 512], BF16)
        YF = sb(es, [128, 4, 512], BF16)
        SQ = sb(es, [128, 4, 512], BF16)
        zt = sb(es, [128, 4, 128], F32)
        KB = [sb(es, [128, 2048], BF16) for _ in range(3)]
        VB = [sb(es, [128, 16, 192], BF16) for _ in range(3)]
        PT = [sb(es, [128, 512], BF16) for _ in range(3)]
        BI = sb(es, [128, 2, 2, 64], F32)
        rc = sb(es, [128, 512], F32)
        rsS = sb(es, [128, 4], F32)
        rsF = sb(es, [128, 4], F32)
        t1 = [sb(es, [128, 512], F32) for _ in range(2)]
        X1 = XR
        tp = ps(es, [128, 8, 128], BF16)
        st = [ps(es, [128, 512], F32) for _ in range(4)]
        OT = [ps(es, [128, 512], F32) for _ in range(2)]
        sm = ps(es, [128, 512], F32)

        T.dma('gpsimd', 'wq_u', WQ[:, :, 0:1024], win_v[:, :, 0:1024], writes=['wq_uv'])
        T.dma('gpsimd', 'wq_q', WQ[:, :, 1024:1536], win_v[:, :, 1024:1536], writes=['wq_q'])
        T.dma('gpsimd', 'mk', MK[:], masks_d.rearrange("m p q -> p m q"), writes=['mk'])
        T.dma('sync', 'xr0', wsf, wsT_d, writes=['xr0'])
        T.dma('sync', 'gsg', gsg[:], gsgu_d[0:1, :].broadcast_to([128, 512]), writes=['gsg'])
        T.dma('sync', 'gos', gos[:], gos_d, writes=['gos'])
        T.dma('sync', 'gof', gof[:], gof_d, writes=['gof'])
        for h in range(8):
            T.dma('sync', 'Bb', Bb[(h % 2) * 64:(h % 2) * 64 + 64, h // 2, :], bsp_d[h:h + 1, :].broadcast_to([64, 128]),
                  writes=[f'Bb{h}'])
        T.op('gpsimd', ASEL(wsf, wsf, [[0, 8], [1, 128]], ALU.is_ge, 0.0, 0, -1), reads=['xr0'], writes=['xr0'])
        T.op('vector', CP(WsT[:], wsf), reads=['xr0'], writes=['wst'])
        T.op('gpsimd', MEMSET(VN[:, :, :, 1, :], 0.0), writes=['vn'])
        wout_v = wout_d.rearrange("(kt p) n -> p kt n", p=128)
        for kt in range(8):
            wsb = kt % 2
            T.dma('sync', f'xa{wsb}', wstage[wsb][:], wout_v[:, kt, :], writes=[f'xa{wsb}'])
            gg = gos[:, kt:kt + 1] if kt < 4 else gof[:, kt - 4:kt - 3]
            T.op('vector', TS(WO[:, kt, :], wstage[wsb][:], gg, None, ALU.mult), reads=[f'xa{wsb}', 'gos', 'gof'],
                 writes=['wo'])
        Bbn = [f'Bb{h}' for h in range(8)]

        sti = [0]
        pti = [0]
        kvi = [0]

        def next_st():
            k = sti[0] % 4
            sti[0] += 1
            return st[k], f'st{k}'

        bglob = 0
        for i in range(8):
            nk = NK(i)
            par = i % 2
            for bl in range(4):
                g = i * 4 + bl
                ln1_block(xown[g * 128:(g + 1) * 128, :], XA, XN, junk, ssq, rs, tp, tmpf,
                          HT[:, :, bl * 128:(bl + 1) * 128], g, 'ht')
            for p in range(4):
                m, mn = next_st()
                T.op('tensor', mmgrp([(m[:], WQ[:, kt, 1024 + p * 128:1024 + (p + 1) * 128], HT[:, kt, :], kt == 0, kt == 7)
                                      for kt in range(8)]), reads=['wq_q', 'ht'], writes=[mn])
                T.op('vector', TS(QT[0:64, p, 0, :], m[0:64, :], 0.125, None, ALU.mult), reads=[mn], writes=['qt'])
                T.op('vector', TS(QT[64:128, p, 1, :], m[64:128, :], 0.125, None, ALU.mult), reads=[mn], writes=['qt'])
            for p in range(4):
                m, mn = next_st()
                T.op('tensor', mmgrp([(m[:], WQ[:, kt, p * 128:(p + 1) * 128], HT[:, kt, :], kt == 0, kt == 7)
                                      for kt in range(8)]), reads=['wq_uv', 'ht'], writes=[mn])
                T.op('scalar', ACT(UT[:, p, :], m[:], AF.Gelu_apprx_tanh), reads=[mn], writes=['ut'])
            for bl in range(4):
                m, mn = next_st()
                T.op('tensor', mmgrp([(m[:], HT[:, kt, bl * 128:(bl + 1) * 128], WQ[:, kt, 512:1024], kt == 0, kt == 7)
                                      for kt in range(8)]), reads=['wq_uv', 'ht'], writes=[mn])
                T.op('scalar', ACT(VG[:, bl, :], m[:], AF.Gelu_apprx_tanh), reads=[mn], writes=['vg'])
            T.op('vector', TT(VG2[:], VG[:], VG[:], ALU.mult), reads=['vg'], writes=['vg2'])
            T.op('vector', RED(v8[:], VG2[:].rearrange("t b (h d) -> t (b h) d", h=8), ALU.add), reads=['vg2'], writes=['v8'])
            T.op('scalar', ACT(v8[:], v8[:], AF.Ln, scale=1.0 / 64, bias=eps_t[:]), reads=['v8', 'eps'], writes=['v8'])
            T.op('scalar', ACT(v8[:], v8[:], AF.Exp, scale=-0.5), reads=['v8'], writes=['v8'])
            T.op('vector', TT(VG2[:].rearrange("t b (h d) -> t (b h) d", h=8), VG[:].rearrange("t b (h d) -> t (b h) d", h=8),
                              v8[:].unsqueeze(2).broadcast_to([128, 32, 64]), ALU.mult), reads=['vg', 'v8'], writes=['vg2'])
            for bl in range(4):
                T.op('vector', TT(VN[:, bl, :, 0::2, :], VG2[:, bl, :].rearrange("t (p two d) -> t p two d", p=4, two=2),
                                  gsg[:].rearrange("t (p two d) -> t p two d", p=4, two=2), ALU.mult),
                     reads=['vg2', 'gsg'], writes=['vn'])
            if i == 0:
                dump("HT", HT[:], [128, 8, 512], BF16, ['ht'])
            for bl in range(4):
                lst = []
                for p in range(4):
                    vnp = VN[:, bl, p, :, :].rearrange("t three d -> t (three d)")
                    lst.append((sm[:, p * 128:(p + 1) * 128], vnp[:, 0:128], WsT[:, 2 * p, :], True, False))
                    lst.append((sm[:, p * 128:(p + 1) * 128], vnp[:, 64:192], WsT[:, 2 * p + 1, :], False, True))
                T.op('tensor', mmgrp(lst), reads=['vn', 'wst'], writes=['sm'])
                T.op('vector', TT(zt[:], sm[:].rearrange("d (p t) -> d p t", p=4), Bb[:], ALU.add), reads=['sm'] + Bbn,
                     writes=['zt'])
                T.op('vector', TT(YS[:, :, bl * 128:(bl + 1) * 128], zt[:], UT[:, :, bl * 128:(bl + 1) * 128], ALU.mult),
                     reads=['zt', 'ut'], writes=['ys'])
            T.op('vector', TT(SQ[:], YS[:], YS[:], ALU.mult), reads=['ys'], writes=['sq'])
            T.op('tensor', mmgrp([(sm[:, bl:bl + 1], SQ[:, p, bl * 128:(bl + 1) * 128], ones_bf[:], p == 0, p == 3)
                                  for bl in range(4) for p in range(4)]), reads=['sq', 'onesbf'], writes=['sm'])
            T.op('scalar', ACT(rsS[:], sm[:, 0:4], AF.Ln, scale=1.0 / 512, bias=eps_t[:]), reads=['sm', 'eps'], writes=['rsS'])
            T.op('scalar', ACT(rsS[:], rsS[:], AF.Exp, scale=-0.5), reads=['rsS'], writes=['rsS'])
            for p in range(4):
                pq = p % 2
                for hh in range(2):
                    h = 2 * p + hh
                    T.op('vector', TS(BI[:, pq, hh, :], C_all[:, :, h], RC[:, i, h:h + 1], None, ALU.subtract),
                         reads=['Call', 'RC'], writes=[f'bi{pq}{hh}'])
                units = [(jb, hh) for jb in range(nk) for hh in range(2)]
                nchunks = (nk + 15) // 16
                if p == 0:
                    seq = [(pp_, c_) for pp_ in range(4) for c_ in range(nchunks)]
                    chunk_buf = {}
                    issued = [0]

                    def ensure(upto):
                        while issued[0] <= upto and issued[0] < len(seq):
                            pp_, c = seq[issued[0]]
                            kb = kvi[0] % 3
                            kvi[0] += 1
                            n = min(16, nk - c * 16)
                            T.dma('sync', f'kb{kb}', KB[kb][:, 0:n * 128], KT_d[pp_, :, c * 2048:c * 2048 + n * 128],
                                  reads=[f'KT_d{r}' for r in range(c * 4, (c * 16 + n + 3) // 4)], writes=[f'kb{kb}'])
                            T.dma('sync', f'vb{kb}', VB[kb][:, 0:n, :], V_d[pp_, :, c * 16:c * 16 + n, :],
                                  reads=[f'V_d{r}_{pp_}' for r in range(c * 4, (c * 16 + n + 3) // 4)], writes=[f'vb{kb}'])
                            chunk_buf[(pp_, c)] = kb
                            issued[0] += 1

                    ensure(2)
                ubank = {}

                def emit_qk(n):
                    jb, hh = units[n]
                    c = jb // 16
                    kb = chunk_buf[(p, c)]
                    jl = jb - c * 16
                    m, mn = next_st()
                    ubank[n] = (m, mn)
                    r0 = hh * 64
                    lst = [(m[:], KB[kb][:, jl * 128:(jl + 1) * 128], QT[:, p, hh, :], True, jb < nk - 8)]
                    rd = [f'kb{kb}', 'qt']
                    if jb >= nk - 8:
                        lst.append((m[:], ident_b[:], MK[:, par * 8 + (jb - (nk - 8)), :], False, True))
                        rd += ['identb', 'mk']
                    T.op('tensor', mmgrp(lst), reads=rd, writes=[mn])

                def emit_rest(n):
                    jb, hh = units[n]
                    c = jb // 16
                    kb = chunk_buf[(p, c)]
                    jl = jb - c * 16
                    m, mn = ubank.pop(n)
                    k3 = pti[0] % 3
                    pti[0] += 1
                    T.op('scalar', ACT(PT[k3][:], m[:], AF.Exp, bias=BI[:, pq, hh, jb:jb + 1]), reads=[mn, f'bi{pq}{hh}'],
                         writes=[f'pt{k3}'])
                    vb = VB[kb][:, jl, hh * 64:hh * 64 + 128]
                    T.op('tensor', mmgrp([(OT[hh][:], vb, PT[k3][:], jb == 0, jb == nk - 1)]),
                         reads=[f'vb{kb}'] + ([] if 'nodep' in PROBE else [f'pt{k3}']), writes=[f'ot{hh}'])

                LA = 3
                for n in range(min(LA, len(units))):
                    emit_qk(n)
                for n in range(len(units)):
                    emit_rest(n)
                    jbd, hhd = units[n]
                    if hhd == 1 and (jbd % 16 == 15 or jbd == nk - 1):
                        ensure(p * nchunks + jbd // 16 + 3)
                    nn = n + LA
                    if nn < len(units):
                        emit_qk(nn)
                T.op('vector', RECIP(rc[0:64, :], OT[0][64:128, :]), reads=['ot0'], writes=['rca'])
                T.op('vector', RECIP(rc[64:128, :], OT[1][0:64, :]), reads=['ot1'], writes=['rcb'])
                T.op('vector', TT(YF[0:64, p, :], OT[0][0:64, :], rc[0:64, :], ALU.mult), reads=['ot0', 'rca'], writes=['yfa'])
                T.op('vector', TT(YF[64:128, p, :], OT[1][64:128, :], rc[64:128, :], ALU.mult), reads=['ot1', 'rcb'],
                     writes=['yfb'])
            T.op('vector', TT(SQ[:], YF[:], YF[:], ALU.mult), reads=['yfa', 'yfb'], writes=['sq'])
            T.op('tensor', mmgrp([(sm[:, 4 + bl:5 + bl], SQ[:, p, bl * 128:(bl + 1) * 128], ones_bf[:], p == 0, p == 3)
                                  for bl in range(4) for p in range(4)]), reads=['sq', 'onesbf'], writes=['sm'])
            T.op('scalar', ACT(rsF[:], sm[:, 4:8], AF.Ln, scale=1.0 / 512, bias=eps_t[:]), reads=['sm', 'eps'], writes=['rsF'])
            T.op('scalar', ACT(rsF[:], rsF[:], AF.Exp, scale=-0.5), reads=['rsF'], writes=['rsF'])
            if i == 0:
                dump("YS", YS[:], [128, 4, 512], BF16, ['ys'])
                dump("YF", YF[:], [128, 4, 512], BF16, ['yfa', 'yfb'])
                dump("rsS", rsS[:], [128, 4], F32, ['rsS'])
                dump("rsF", rsF[:], [128, 4], F32, ['rsF'])
            for bl in range(4):
                g = i * 4 + bl
                xb = g % 2
                T.dma('sync', f'xr{xb}', XR[xb][:], xown[g * 128:(g + 1) * 128, :], writes=[f'xr{xb}'])
                for hf in range(2):
                    ms, msn = next_st()
                    T.op('tensor', mmgrp([(ms[:], YS[:, p, bl * 128:(bl + 1) * 128], WO[:, p, hf * 512:(hf + 1) * 512], p == 0, p == 3)
                                          for p in range(4)]), reads=['ys', 'wo'], writes=[msn])
                    mf, mfn = next_st()
                    T.op('tensor', mmgrp([(mf[:], YF[:, p, bl * 128:(bl + 1) * 128], WO[:, 4 + p, hf * 512:(hf + 1) * 512], p == 0, p == 3)
                                          for p in range(4)]), reads=['yfa', 'yfb', 'wo'], writes=[mfn])
                    tb = hf
                    T.op('vector', TS(t1[tb][:], ms[:], rsS[:, bl:bl + 1], None, ALU.mult), reads=[msn, 'rsS'], writes=[f't1{tb}'])
                    T.op('vector', STT(t1[tb][:], mf[:], rsF[:, bl:bl + 1], t1[tb][:], ALU.mult, ALU.add),
                         reads=[mfn, 'rsF', f't1{tb}'], writes=[f't1{tb}'])
                    T.op('vector', TT(t1[tb][:], t1[tb][:], GT1[:, hf * 512:(hf + 1) * 512], ALU.mult),
                         reads=[f't1{tb}', 'GT10', 'GT11'], writes=[f't1{tb}'])
                    T.op('vector', TT(X1[xb][:, hf * 512:(hf + 1) * 512], t1[tb][:], XR[xb][:, hf * 512:(hf + 1) * 512], ALU.add),
                         reads=[f't1{tb}', f'xr{xb}'], writes=[f'xr{xb}'])
                T.dma('gpsimd', f'x1o{xb}', X1_d[g * 128:(g + 1) * 128, :], X1[xb][:], reads=[f'xr{xb}'], writes=[f'X1_d{g}'])
        T.barrier()

    CAP = 768
    NT0 = CAP // 128
    if "C" in DEBUG["phases"]:
      csem = {}
      with ExitStack() as es0:
        IDX = sb(es0, [128, 32, 2], U32, "IDX")
        W12 = sb(es0, [128, 32, 2], F32, "W12")
        gfin = sb(es0, [128, 1024], F32, "gfin")
        T.dma('sync', 'gfin', gfin[:], gfin_d[0:1, :].broadcast_to([128, 1024]), writes=['gfin'])
        with ExitStack() as es:
            X1t = [sb(es, [128, 1024], F32) for _ in range(2)]
            xn2_2 = [sb(es, [128, 1024], F32) for _ in range(2)]
            junk_2 = [sb(es, [128, 1024], BF16) for _ in range(2)]
            H2f_2 = [sb(es, [128, 8, 128], F32) for _ in range(2)]
            tmpg_2 = [sb(es, [128, 8, 128], F32) for _ in range(2)]
            H2row = [sb(es, [128, 1024], BF16) for _ in range(2)]
            hrt_2 = [sb(es, [128, 1024], F32) for _ in range(2)]
            A2R = sb(es, [128, 1024], F32)
            B2R = sb(es, [128, 1024], F32)
            dg = sb(es, [128, 128], F32)
            onesf = sb(es, [128, 128], F32)
            WR = sb(es, [128, 8, 20], F32)
            brb = sb(es, [128, 20], F32)
            EB = sb(es, [128, 16], F32)
            Cn = sb(es, [128, 32, 16], F32)
            L_2 = [sb(es, [128, 20], F32) for _ in range(2)]
            s1_2 = [sb(es, [128, 16], F32) for _ in range(2)]
            s2_2 = [sb(es, [128, 16], F32) for _ in range(2)]
            E1_2 = [sb(es, [128, 16], F32) for _ in range(2)]
            E2_2 = [sb(es, [128, 16], F32) for _ in range(2)]
            Mx_2 = [sb(es, [128, 16], F32) for _ in range(2)]
            oh_2 = [sb(es, [128, 4], F32) for _ in range(2)]
            mk1_2 = [sb(es, [128, 4], F32) for _ in range(2)]
            mk2_2 = [sb(es, [128, 4], F32) for _ in range(2)]
            les_2 = [sb(es, [128, 4], F32) for _ in range(2)]
            le2_2 = [sb(es, [128, 4], F32) for _ in range(2)]
            sc_2 = [sb(es, [128, 12], F32) for _ in range(2)]
            idf_2 = [sb(es, [128, 2], F32) for _ in range(2)]
            CNTi = sb(es, [128, 16], I32)
            ssq = sb(es, [128, 32], F32)
            T.op('vector', MEMSET(ssq[:], 0.0), writes=['ssqall'])
            rs_2 = [sb(es, [128, 1], F32) for _ in range(2)]
            tpc = ps(es, [128, 8, 128], F32)
            pm = ps(es, [128, 512], F32)
            pmr = [ps(es, [128, 512], F32) for _ in range(2)]

            T.dma('sync', 'WR', WR[:], wr_d.rearrange("(kt p) n -> p kt n", p=128), writes=['WR'])
            T.dma('sync', 'brb', brb[:], br_d[0:1, :].broadcast_to([128, 20]), writes=['brb'])
            T.dma('sync', 'EB', EB[:], eb_d[0:1, :].broadcast_to([128, 16]), writes=['EB'])
            T.op('gpsimd', MEMSET(onesf[:], 1.0), writes=['onesf'])
            for (colt, rowt, nm) in ((A2, A2R, 'A2R'), (B2, B2R, 'B2R')):
                for kt in range(8):
                    T.op('vector', TS(dg[:], ident_f[:], colt[:, kt:kt + 1], None, ALU.mult), reads=['identf', 'A2', 'B2', 'dg'],
                         writes=['dg'])
                    T.op('tensor', mmgrp([(pm[:, 0:128], onesf[:], dg[:], True, True)]), reads=['dg', 'onesf'], writes=['pm'])
                    T.op('vector', CP(rowt[:, kt * 128:(kt + 1) * 128], pm[:, 0:128]), reads=['pm'], writes=[nm])
            def c1_stage1(g):
                    xb = g % 2
                    xn2 = xn2_2[xb]
                    junk = junk_2[xb]
                    H2f = H2f_2[xb]
                    tmpg = tmpg_2[xb]
                    hrt = hrt_2[xb]
                    L = L_2[xb]
                    s1 = s1_2[xb]
                    s2 = s2_2[xb]
                    E1 = E1_2[xb]
                    E2 = E2_2[xb]
                    Mx = Mx_2[xb]
                    oh = oh_2[xb]
                    mk1 = mk1_2[xb]
                    mk2 = mk2_2[xb]
                    les = les_2[xb]
                    le2 = le2_2[xb]
                    sc = sc_2[xb]
                    idf = idf_2[xb]
                    rs = rs_2[xb]
                    T.dma('sync', f'x1t{xb}', X1t[xb][:], X1_d[g * 128:(g + 1) * 128, :], reads=[f'X1_d{g}'], writes=[f'x1t{xb}'])
                    T.op('scalar', ACT(junk[:], X1t[xb][:], AF.Square, accum_out=ssq[:, g:g + 1]), reads=[f'x1t{xb}', 'ssqall'],
                         writes=[f'junk{xb}', f'ssq_{g}'])
                    T.op('scalar', ACT(rs[:], ssq[:, g:g + 1], AF.Ln, scale=1.0 / 1024, bias=eps_t[:]), reads=[f'ssq_{g}', 'eps'], writes=[f'rs{xb}'])
                    T.op('scalar', ACT(rs[:], rs[:], AF.Exp, scale=-0.5), reads=[f'rs{xb}'], writes=[f'rs{xb}'])
                    T.op('vector', TS(xn2[:], X1t[xb][:], rs[:, 0:1], None, ALU.mult), reads=[f'x1t{xb}', f'rs{xb}'], writes=[f'xn2{xb}'])
                    T.op('gpsimd', TT(hrt[:], xn2[:], A2R[:], ALU.mult), reads=[f'xn2{xb}', 'A2R'], writes=[f'hrt{xb}'])
                    T.op('gpsimd', TT(H2row[xb][:], hrt[:], B2R[:], ALU.add), reads=[f'hrt{xb}', 'B2R'], writes=[f'h2row{xb}'])
                    T.op('tensor', tpgrp([(tpc[:, kt, :], xn2[:, kt * 128:(kt + 1) * 128], ident_f[:]) for kt in range(8)]),
                         reads=[f'xn2{xb}', 'identf'], writes=['tpc'])
                    for kt in range(8):
                        T.op('scalar', ACT(H2f[:, kt, :], tpc[:, kt, :], AF.Identity, scale=A2[:, kt:kt + 1], bias=B2[:, kt:kt + 1]),
                             reads=['tpc', 'A2', 'B2'], writes=[f'h2f{xb}'])
                    T.op('tensor', mmgrp([(pmr[xb][:, 0:20], H2f[:, kt, :], WR[:, kt, :], kt == 0, kt == 7) for kt in range(8)]),
                         reads=[f'h2f{xb}', 'WR'], writes=[f'pmr{xb}'])

            def c1_stage2(g):
                    xb = g % 2
                    xn2 = xn2_2[xb]
                    junk = junk_2[xb]
                    H2f = H2f_2[xb]
                    tmpg = tmpg_2[xb]
                    hrt = hrt_2[xb]
                    L = L_2[xb]
                    s1 = s1_2[xb]
                    s2 = s2_2[xb]
                    E1 = E1_2[xb]
                    E2 = E2_2[xb]
                    Mx = Mx_2[xb]
                    oh = oh_2[xb]
                    mk1 = mk1_2[xb]
                    mk2 = mk2_2[xb]
                    les = les_2[xb]
                    le2 = le2_2[xb]
                    sc = sc_2[xb]
                    idf = idf_2[xb]
                    rs = rs_2[xb]
                    T.op('vector', TT(L[:], pmr[xb][:, 0:20], brb[:], ALU.add), reads=[f'pmr{xb}', 'brb'], writes=[f'L{xb}'])
                    rr = [f'L{xb}']
                    V = lambda fn: T.op('vector', fn, reads=rr, writes=rr)
                    S = lambda fn: T.op('scalar', fn, reads=rr, writes=rr)
                    lg = L[:, 0:4]
                    le = L[:, 4:20].rearrange("t (g e) -> t g e", g=4)
                    V(RMAX(sc[:, 0:1], lg))
                    V(TS(oh[:], lg, sc[:, 0:1], None, ALU.is_equal))
                    V(TS(sc[:, 1:2], sc[:, 0:1], -1.0, None, ALU.mult))
                    V(MEMSET(sc[:, 2:3], 0.0))
                    S(ACT(s1[:, 0:4], lg, AF.Exp, bias=sc[:, 1:2], accum_out=sc[:, 2:3]))
                    V(RECIP(sc[:, 3:4], sc[:, 2:3]))
                    V(TT(s2[:].rearrange("t (g e) -> t g e", g=4), le, oh[:].unsqueeze(2).broadcast_to([128, 4, 4]), ALU.mult))
                    V(RED(les[:], s2[:].rearrange("t (g e) -> t e g", g=4), ALU.add))
                    V(RMAX(sc[:, 4:5], les[:]))
                    V(TS(mk1[:], les[:], sc[:, 4:5], None, ALU.is_equal))
                    V(STT(le2[:], mk1[:], NEG, les[:], ALU.mult, ALU.add))
                    V(RMAX(sc[:, 5:6], le2[:]))
                    V(TS(mk2[:], le2[:], sc[:, 5:6], None, ALU.is_equal))
                    V(TT(sc[:, 6:7], sc[:, 5:6], sc[:, 4:5], ALU.subtract))
                    S(ACT(sc[:, 7:8], sc[:, 6:7], AF.Exp))
                    V(TS(sc[:, 8:9], sc[:, 7:8], 1.0, None, ALU.add))
                    V(RECIP(sc[:, 8:9], sc[:, 8:9]))
                    V(TT(W12[:, g, 0:1], sc[:, 8:9], sc[:, 3:4], ALU.mult))
                    V(TT(W12[:, g, 1:2], sc[:, 7:8], W12[:, g, 0:1], ALU.mult))
                    V(TT(E1[:].rearrange("t (g e) -> t g e", g=4), oh[:].unsqueeze(2).broadcast_to([128, 4, 4]),
                         mk1[:].unsqueeze(1).broadcast_to([128, 4, 4]), ALU.mult))
                    V(TT(E2[:].rearrange("t (g e) -> t g e", g=4), oh[:].unsqueeze(2).broadcast_to([128, 4, 4]),
                         mk2[:].unsqueeze(1).broadcast_to([128, 4, 4]), ALU.mult))
                    V(TT(Mx[:], E1[:], E2[:], ALU.add))
                    lst = [(pm[:, 32:48], Umat[:], Mx[:], True, g == 0)]
                    if g > 0:
                        lst.append((pm[:, 32:48], Sel127[:], Cn[:, g - 1, :], False, True))
                    T.op('tensor', mmgrp(lst), reads=[f'L{xb}', 'U', 'sel127', 'Cn'], writes=['pm'])
                    T.op('vector', CP(Cn[:, g, :], pm[:, 32:48]), reads=['pm'], writes=['Cn'])
                    T.op('vector', TT(s1[:], Cn[:, g, :], EB[:], ALU.add), reads=['Cn', 'EB', f'L{xb}'], writes=[f'L{xb}'])
                    V(TT(s2[:], s1[:], E1[:], ALU.mult))
                    V(RED(idf[:, 0:1], s2[:], ALU.add))
                    V(TT(s2[:], s1[:], E2[:], ALU.mult))
                    V(RED(idf[:, 1:2], s2[:], ALU.add))
                    T.op('vector', CP(IDX[:, g, :], idf[:]), reads=[f'L{xb}'], writes=[f'idx{g}'])
                    for k in range(2):
                        T.idma(f'sc{xb}{k}', Xs_d, bass.IndirectOffsetOnAxis(ap=IDX[:, g, k:k + 1], axis=0), H2row[xb][:], None,
                               reads=[f'idx{g}', f'h2row{xb}'], writes=[f'Xs{g}_{k}'])

            c1_stage1(0)
            for g in range(32):
                if g + 1 < 32:
                    c1_stage1(g + 1)
                c1_stage2(g)
            T.op('tensor', mmgrp([(pm[:, 64:80], Sel127[:], Cn[:, 31, :], True, True)]), reads=['Cn', 'sel127'], writes=['pm'])
            T.op('vector', CP(CNTi[:], pm[:, 64:80]), reads=['pm'], writes=['cnti'])
            T.dma('sync', 'cntd', CNT_d, CNTi[0:1, :], reads=['cnti'], writes=['CNT_d'])
            dump("IDX", IDX[:].rearrange("p g k -> p (g k)"), [128, 64], U32, [f'idx{g}' for g in range(32)])
            dump("W12", W12[:].rearrange("p g k -> p (g k)"), [128, 64], F32, ['L'])
            dump("Cn", Cn[:].rearrange("p g e -> p (g e)"), [128, 512], F32, ['Cn'])
            T.barrier()

        with ExitStack() as es:
            WG = [sb(es, [128, 8, 512], BF16) for _ in range(2)]
            WU = [sb(es, [128, 8, 512], BF16) for _ in range(2)]
            WD = [sb(es, [128, 4, 1024], BF16) for _ in range(2)]
            XT = [sb(es, [128, 1024], BF16) for _ in range(3)]
            XsT = [sb(es, [128, 8, 512], BF16) for _ in range(2)]
            AT = [sb(es, [128, 4, 512], BF16) for _ in range(2)]
            sg = [sb(es, [128, 512], F32) for _ in range(2)]
            YT = [sb(es, [128, 1024], F32) for _ in range(2)]
            tpb = ps(es, [128, 8, 128], BF16)
            gp = [ps(es, [128, 512], F32) for _ in range(2)]
            up = [ps(es, [128, 512], F32) for _ in range(2)]
            yp = [ps(es, [128, 512], F32) for _ in range(2)]
            wg_v = wg_d.rearrange("e (kt p) n -> e p kt n", p=128)
            wu_v = wu_d.rearrange("e (kt p) n -> e p kt n", p=128)
            wd_v = wd_d.rearrange("e (kt p) n -> e p kt n", p=128)
            gi = [0]
            yi = [0]
            xi = [0]
            yti = [0]
            gri = [0]

            SG = [sb(es, [128, 4096], F32) for _ in range(3)]

            def load_w(ex):
                wb = ex % 2
                T.dma('sync', 'sg0', SG[0][:].rearrange("p (k n) -> p k n", k=8), wg_v[ex], writes=['sg0'])
                T.dma('sync', 'sg1', SG[1][:].rearrange("p (k n) -> p k n", k=8), wu_v[ex], writes=['sg1'])
                T.dma('sync', 'sg2', SG[2][:].rearrange("p (k n) -> p k n", k=4), wd_v[ex], writes=['sg2'])
                T.op('vector', CP(WG[wb][:].rearrange("p k n -> p (k n)"), SG[0][:]), reads=['sg0'], writes=[f'wg{wb}'])
                T.op('vector', CP(WU[wb][:].rearrange("p k n -> p (k n)"), SG[1][:]), reads=['sg1'], writes=[f'wu{wb}'])
                T.op('vector', CP(WD[wb][:].rearrange("p k n -> p (k n)"), SG[2][:]), reads=['sg2'], writes=[f'wd{wb}'])

            def emit_group(ex, grp):
                wb = ex % 2
                ab = gri[0] % 2
                gri[0] += 1
                base = ex * 4096 + grp * 512
                for tl in range(4):
                    k3 = xi[0] % 3
                    xi[0] += 1
                    r0 = base + tl * 128
                    T.dma('sync', f'xt{k3}', XT[k3][:], Xs_d[r0:r0 + 128, :], writes=[f'xt{k3}'])
                    T.op('tensor', tpgrp([(tpb[:, kt, :], XT[k3][:, kt * 128:(kt + 1) * 128], ident_b[:]) for kt in range(8)]),
                         reads=[f'xt{k3}', 'identb'], writes=['tpb'])
                    T.op('vector', CP(XsT[ab][:, :, tl * 128:(tl + 1) * 128], tpb[:]), reads=['tpb'], writes=[f'xst{ab}'])
                for ht in range(4):
                    k2 = gi[0] % 2
                    gi[0] += 1
                    T.op('tensor', mmgrp([(gp[k2][:], WG[wb][:, kt, ht * 128:(ht + 1) * 128], XsT[ab][:, kt, :], kt == 0, kt == 7)
                                          for kt in range(8)]), reads=[f'wg{wb}', f'xst{ab}'], writes=[f'gp{k2}'])
                    T.op('tensor', mmgrp([(up[k2][:], WU[wb][:, kt, ht * 128:(ht + 1) * 128], XsT[ab][:, kt, :], kt == 0, kt == 7)
                                          for kt in range(8)]), reads=[f'wu{wb}', f'xst{ab}'], writes=[f'up{k2}'])
                    T.op('scalar', ACT(sg[k2][:], gp[k2][:], AF.Silu), reads=[f'gp{k2}'], writes=[f'sg{k2}'])
                    T.op('vector', TT(AT[ab][:, ht, :], sg[k2][:], up[k2][:], ALU.mult), reads=[f'sg{k2}', f'up{k2}'],
                         writes=[f'at{ab}'])
                for tl in range(4):
                    yb = yti[0] % 2
                    yti[0] += 1
                    for nh in range(2):
                        k2 = yi[0] % 2
                        yi[0] += 1
                        T.op('tensor', mmgrp([(yp[k2][:], AT[ab][:, ht, tl * 128:(tl + 1) * 128], WD[wb][:, ht, nh * 512:(nh + 1) * 512],
                                               ht == 0, ht == 3) for ht in range(4)]), reads=[f'at{ab}', f'wd{wb}'], writes=[f'yp{k2}'])
                        T.op('scalar', ACT(YT[yb][:, nh * 512:(nh + 1) * 512], yp[k2][:], AF.Copy), reads=[f'yp{k2}'], writes=[f'yt{yb}'])
                    r0 = base + tl * 128
                    T.dma('sync', f'yt{yb}', Ys_d[r0:r0 + 128, :], YT[yb][:], reads=[f'yt{yb}'], writes=[f'Ys{ex}_{grp}_{tl}'])

            drain = ['xt0', 'xt1', 'xt2', 'yt0', 'yt1']
            load_w(0)
            for ex in range(16):
                if ex + 1 < 16:
                    load_w(ex + 1)
                emit_group(ex, 0)
                for grp in range(1, 8):
                    T.cond_begin(CNT_d[0:1, ex:ex + 1], grp * 512, drain)
                    emit_group(ex, grp)
                    T.cond_end()
            T.barrier()

        with ExitStack() as es:
            Y1 = [sb(es, [128, 1024], F32) for _ in range(2)]
            Y2 = [sb(es, [128, 1024], F32) for _ in range(2)]
            X1t = [sb(es, [128, 1024], F32) for _ in range(2)]
            junk = sb(es, [128, 1024], BF16)
            osb = [sb(es, [128, 1024], F32) for _ in range(2)]
            ssq = sb(es, [128, 32], F32)
            T.op('vector', MEMSET(ssq[:], 0.0), writes=['ssqall'])
            rs = [sb(es, [128, 1], F32) for _ in range(2)]
            def c3_loads(g):
                b = g % 2
                T.dma('sync', f'x1c{b}', X1t[b][:], X1_d[g * 128:(g + 1) * 128, :], writes=[f'x1c{b}'])
                T.idma(f'ga{b}', Y1[b][:], None, Ys_d, bass.IndirectOffsetOnAxis(ap=IDX[:, g, 0:1], axis=0), writes=[f'y1{b}'])
                T.idma(f'gb{b}', Y2[b][:], None, Ys_d, bass.IndirectOffsetOnAxis(ap=IDX[:, g, 1:2], axis=0), writes=[f'y2{b}'])

            c3_loads(0)
            for g in range(32):
                b = g % 2
                if g + 1 < 32:
                    c3_loads(g + 1)
                T.op('vector', TS(Y1[b][:], Y1[b][:], W12[:, g, 0:1], None, ALU.mult), reads=[f'y1{b}'], writes=[f'y1{b}'])
                T.op('vector', STT(Y1[b][:], Y2[b][:], W12[:, g, 1:2], Y1[b][:], ALU.mult, ALU.add), reads=[f'y1{b}', f'y2{b}'],
                     writes=[f'y1{b}'])
                T.op('vector', TT(Y1[b][:], Y1[b][:], GT2[:], ALU.mult), reads=[f'y1{b}'], writes=[f'y1{b}'])
                T.op('vector', TT(Y1[b][:], Y1[b][:], X1t[b][:], ALU.add), reads=[f'y1{b}', f'x1c{b}'], writes=[f'y1{b}'])
                T.op('scalar', ACT(junk[:], Y1[b][:], AF.Square, accum_out=ssq[:, g:g + 1]), reads=[f'y1{b}', 'ssqall'],
                     writes=['junk', f'ssq_{g}'])
                T.op('scalar', ACT(rs[b][:], ssq[:, g:g + 1], AF.Ln, scale=1.0 / 1024, bias=eps_t[:]), reads=[f'ssq_{g}', 'eps'], writes=[f'rs{b}'])
                T.op('scalar', ACT(rs[b][:], rs[b][:], AF.Exp, scale=-0.5), reads=[f'rs{b}'], writes=[f'rs{b}'])
                T.op('vector', STT(osb[b][:], Y1[b][:], rs[b][:, 0:1], gfin[:], ALU.mult, ALU.mult),
                     reads=[f'y1{b}', f'rs{b}', 'gfin'], writes=[f'osb{b}'])
                T.dma('sync', f'out{b}', out_d[g * 128:(g + 1) * 128, :], osb[b][:], reads=[f'osb{b}'], writes=[f'out{g}'])
            T.barrier()
    T.emit()
    top.close()
    return nc


_CACHE = {}


def _masks(j):
    m = np.zeros((2, 8, 128, 512), np.float32)
    ki = np.arange(128)[:, None]
    qq = np.arange(512)[None, :]
    for par in range(2):
        i = par
        run = OWN_RUNS[j][i]
        nk = NK(i)
        for r in range(8):
            jb = nk - 8 + r
            kpos = jb * 128 + ki
            qpos = run * 512 + qq
            m[par, r] = np.where(kpos <= qpos, 0.0, NEG)
    return m.reshape(16, 128, 512)


def _rsel(j):
    s = np.zeros((8, 64), np.float32)
    for i, run in enumerate(OWN_RUNS[j]):
        s[i, 4 * run + 1] = 1.0
    return s.reshape(1, 512)


def kernel(x, c, w_ada, b_ada, g_norm_mix, w_in, g_sgu, w_spatial, b_spatial, b_forget, g_out_sgu, g_out_fox, w_out,
           g_norm_ffn, w_router_group, b_router_group, w_router_expert, b_router_expert, w_gate, w_up, w_down, g_final):
    f = lambda a: np.ascontiguousarray(np.asarray(a, dtype=np.float32))
    x = f(x); c = f(c)
    if "nc" not in _CACHE:
        _CACHE["nc"] = build_program()
    nc = _CACHE["nc"]
    wr = np.concatenate([f(w_router_group)[0], f(w_router_expert)[0].transpose(1, 0, 2).reshape(1024, 16)], axis=1)
    br = np.concatenate([f(b_router_group)[0], f(b_router_expert)[0].reshape(16)])[None, :]
    shared = {
        "w_ada": f(w_ada)[0], "b_ada": f(b_ada)[0][None, :], "g1T": f(f(g_norm_mix)[0].reshape(8, 128).T),
        "g2T": f(f(g_norm_ffn)[0].reshape(8, 128).T), "w_in": f(w_in)[0], "gsgu": f(g_sgu)[0][None, :],
        "wsT": f(f(w_spatial)[0].transpose(2, 0, 1)), "bsp": f(b_spatial)[0], "bfg": f(b_forget)[0][None, :],
        "gos": f(f(g_out_sgu)[0].reshape(4, 128).T), "gof": f(f(g_out_fox)[0].reshape(4, 128).T), "w_out": f(w_out)[0],
        "wr": f(wr), "br": f(br), "w_gate": f(w_gate)[0], "w_up": f(w_up)[0], "w_down": f(w_down)[0],
        "gfin": f(g_final)[None, :],
    }
    in_maps = []
    for core in range(8):
        b, j = core // 2, core % 2
        m = dict(shared)
        m["xall"] = x[b]
        m["xown"] = f(np.concatenate([x[b, 512 * r:512 * (r + 1)] for r in OWN_RUNS[j]], axis=0))
        m["cT"] = f(c[b].reshape(8, 128).T)
        m["masks"] = _masks(j)
        m["rsel"] = _rsel(j)
        m["eb"] = (np.arange(16, dtype=np.float32) * 4096.0 - 1.0)[None, :]
        in_maps.append(m)
    res = run_bass_kernel_spmd(nc, in_maps, core_ids=list(range(8)))
    _CACHE["res"] = res
    out = np.empty((4, 8192, 1024), np.float32)
    for core in range(8):
        b, j = core // 2, core % 2
        o = res.results[core]["out"]
        for i, r in enumerate(OWN_RUNS[j]):
            out[b, 512 * r:512 * (r + 1)] = o[512 * i:512 * (i + 1)]
    return out
```

```python
import os
import numpy as np
from contextlib import ExitStack
import concourse.bass as bass
import concourse.mybir as mybir
from concourse.bass_utils import run_bass_kernel_spmd

F32 = mybir.dt.float32
BF16 = mybir.dt.bfloat16
U32 = mybir.dt.uint32
I32 = mybir.dt.int32
AF = mybir.ActivationFunctionType
ALU = mybir.AluOpType
AX = mybir.AxisListType
ENGS = ['sync', 'scalar', 'vector', 'gpsimd', 'tensor']
EPS = 1e-6
NEG = -1.0e30
OWN_RUNS = {0: [0, 3, 4, 7, 8, 11, 12, 15], 1: [1, 2, 5, 6, 9, 10, 13, 14]}
WARM = 18
PROBE = os.environ.get('MK_PROBE', '')
DEBUG = {"x1": False, "same": True, "phases": "ABC", "dump": []}


def NK(i):
    return 16 * (i // 2) + 8 + 8 * (i % 2)


class Tracker:
    def __init__(self, nc, same_engine_sync=False):
        self.nc = nc
        self.streams = {e: [] for e in ENGS}
        self.sems = {e: nc.alloc_semaphore("c_" + e) for e in ENGS}
        self.cnt = {e: 0 for e in ENGS}
        self.waited = {e: {} for e in ENGS}
        self.res = {}
        self.slots = {}
        self.same = same_engine_sync
        self.slot_q = {}

    def _deps(self, reads, writes):
        deps = []
        for r in reads:
            st = self.res.get(r)
            if st and st[0]:
                deps.append(st[0])
        for w in writes:
            st = self.res.get(w)
            if st:
                if st[0]:
                    deps.append(st[0])
                deps.extend(st[1])
        return deps

    def _update(self, reads, writes, tag):
        for r in reads:
            st = self.res.setdefault(r, [None, []])
            st[1].append(tag)
        for w in writes:
            self.res[w] = [tag, []]

    def _emit_waits(self, eng, deps, skip_same):
        mx = {}
        for key, val in deps:
            mx[key] = max(mx.get(key, 0), val)
        for key, val in mx.items():
            if key == eng and skip_same:
                continue
            if self.waited[eng].get(key, 0) >= val:
                continue
            self.waited[eng][key] = val
            sem = self.sems[key] if key in self.sems else self.slots[key][0]
            self.streams[eng].append(lambda e, sem=sem, val=val: e.wait_ge(sem, val))

    def op(self, eng, fn, reads=(), writes=()):
        deps = self._deps(reads, writes)
        self._emit_waits(eng, deps, (not self.same) or eng == 'tensor')
        self.cnt[eng] += 1
        sem = self.sems[eng]
        self.streams[eng].append(lambda e, fn=fn, sem=sem: fn(e).then_inc(sem, 1))
        self._update(reads, writes, (eng, self.cnt[eng]))

    def dma(self, q, slot, out, in_, reads=(), writes=()):
        self.slot_q[slot] = q
        if slot not in self.slots:
            self.slots[slot] = [self.nc.alloc_semaphore("d_" + slot), 0]
        deps = self._deps(reads, writes)
        self._emit_waits(q, deps, False)
        s = self.slots[slot]
        s[1] += 16
        sem = s[0]
        self.streams[q].append(lambda e, out=out, in_=in_, sem=sem: e.dma_start(out=out, in_=in_).then_inc(sem, 16))
        self._update(reads, writes, (slot, s[1]))

    def idma(self, slot, out, out_off, in_, in_off, reads=(), writes=()):
        q = 'gpsimd'
        self.slot_q[slot] = q
        if slot not in self.slots:
            self.slots[slot] = [self.nc.alloc_semaphore("d_" + slot), 0]
        deps = self._deps(reads, writes)
        self._emit_waits(q, deps, False)
        s = self.slots[slot]
        s[1] += 16
        sem = s[0]
        self.streams[q].append(lambda e, out=out, in_=in_, sem=sem, oo=out_off, io=in_off:
                               e.indirect_dma_start(out=out, out_offset=oo, in_=in_, in_offset=io).then_inc(sem, 16))
        self._update(reads, writes, (slot, s[1]))

    def cond_begin(self, cnt_ap, thr, drain_slots):
        for e in ENGS:
            deps = [(e, self.cnt[e])] if self.cnt[e] else []
            deps += [(k, self.slots[k][1]) for k in drain_slots if k in self.slots and self.slot_q.get(k) == e]
            self._emit_waits(e, deps, False)
        self.snap_cnt = dict(self.cnt)
        self.snap_slots = {k: v[1] for k, v in self.slots.items()}
        self.snap_waited = {e: dict(w) for e, w in self.waited.items()}
        for e in ENGS:
            self.streams[e].append(('regload', cnt_ap))
            self.streams[e].append(('if', thr))

    def cond_end(self):
        for e in ENGS:
            self.streams[e].append(('else',))
            n = self.cnt[e] - self.snap_cnt[e]
            if n:
                self.streams[e].append(lambda eng, sem=self.sems[e], n=n: eng.sem_inc(sem, n))
        for slot, (sem, c) in self.slots.items():
            d = c - self.snap_slots.get(slot, 0)
            if d:
                self.streams[self.slot_q[slot]].append(lambda eng, sem=sem, d=d: eng.sem_inc(sem, d))
        for e in ENGS:
            self.streams[e].append(('endif',))
        self.waited = self.snap_waited

    def barrier(self):
        deps = [(e, c) for e, c in self.cnt.items() if c > 0]
        deps += [(k, v[1]) for k, v in self.slots.items() if v[1] > 0]
        for e in ENGS:
            self._emit_waits(e, deps, True)

    def emit(self):
        self.barrier()
        streams = self.streams

        def run(e, items):
            creg = e.alloc_register("creg")
            i = 0
            n = len(items)
            while i < n:
                it = items[i]
                if not isinstance(it, tuple):
                    it(e)
                    i += 1
                    continue
                if it[0] == 'regload':
                    e.reg_load(creg, it[1])
                    i += 1
                    continue
                assert it[0] == 'if'
                thr = it[1]
                j = i + 1
                body = []
                while not (isinstance(items[j], tuple) and items[j][0] == 'else'):
                    body.append(items[j])
                    j += 1
                j += 1
                fix = []
                while not (isinstance(items[j], tuple) and items[j][0] == 'endif'):
                    fix.append(items[j])
                    j += 1
                with e.If_lt(creg, thr + 1):
                    for f in fix:
                        f(e)
                with e.Else():
                    for f in body:
                        f(e)
                i = j + 1

        with self.nc.Block() as block:
            @block.sync
            def _(e):
                run(e, streams['sync'])

            @block.scalar
            def _(e):
                run(e, streams['scalar'])

            @block.vector
            def _(e):
                run(e, streams['vector'])

            @block.gpsimd
            def _(e):
                run(e, streams['gpsimd'])

            @block.tensor
            def _(e):
                run(e, streams['tensor'])


def mmgrp(lst):
    def fn(e):
        ins = None
        for (out, lhsT, rhs, st, sp) in lst:
            ins = e.matmul(out, lhsT=lhsT, rhs=rhs, start=st, stop=sp)
        return ins
    return fn


def tpgrp(lst):
    def fn(e):
        ins = None
        for (out, in_, ident) in lst:
            ins = e.transpose(out, in_, ident)
        return ins
    return fn


def ACT(out, in_, func, **kw):
    return lambda e: e.activation(out=out, in_=in_, func=func, **kw)


def TT(out, in0, in1, op):
    return lambda e: e.tensor_tensor(out=out, in0=in0, in1=in1, op=op)


def TS(out, in0, s1, s2, op0, op1=None):
    if op1 is None:
        return lambda e: e.tensor_scalar(out=out, in0=in0, scalar1=s1, scalar2=None, op0=op0)
    return lambda e: e.tensor_scalar(out=out, in0=in0, scalar1=s1, scalar2=s2, op0=op0, op1=op1)


def STT(out, in0, scalar, in1, op0, op1):
    return lambda e: e.scalar_tensor_tensor(out=out, in0=in0, scalar=scalar, in1=in1, op0=op0, op1=op1)


def CP(out, in_):
    return lambda e: e.tensor_copy(out=out, in_=in_)


def RED(out, in_, op):
    return lambda e: e.tensor_reduce(out=out, in_=in_, axis=AX.X, op=op)


def RMAX(out, in_):
    return lambda e: e.reduce_max(out=out, in_=in_, axis=AX.X)


def RECIP(out, in_):
    return lambda e: e.reciprocal(out=out, in_=in_)


def MEMSET(ap, v):
    return lambda e: e.memset(ap, v)


def ASEL(out, in_, pattern, cmp, fill, base, cm):
    return lambda e: e.affine_select(out=out, in_=in_, pattern=pattern, compare_op=cmp, fill=fill, base=base,
                                     channel_multiplier=cm)


def build_program():
    nc = bass.Bass("TRN2", target_bir_lowering=False)
    T = Tracker(nc, same_engine_sync=DEBUG["same"])
    din = lambda name, shape: nc.dram_tensor(name, shape, F32, kind="ExternalInput").ap()
    xall = din("xall", [8192, 1024])
    xown = din("xown", [4096, 1024])
    cT_d = din("cT", [128, 8])
    wada_d = din("w_ada", [1024, 6144])
    bada_d = din("b_ada", [1, 6144])
    g1T_d = din("g1T", [128, 8])
    g2T_d = din("g2T", [128, 8])
    win_d = din("w_in", [1024, 2568])
    gsgu_d = din("gsgu", [1, 512])
    wsT_d = din("wsT", [128, 8, 128])
    bsp_d = din("bsp", [8, 128])
    bfg_d = din("bfg", [1, 8])
    gos_d = din("gos", [128, 4])
    gof_d = din("gof", [128, 4])
    wout_d = din("w_out", [1024, 1024])
    wr_d = din("wr", [1024, 20])
    br_d = din("br", [1, 20])
    wg_d = din("w_gate", [16, 1024, 512])
    wu_d = din("w_up", [16, 1024, 512])
    wd_d = din("w_down", [16, 512, 1024])
    gfin_d = din("gfin", [1, 1024])
    masks_d = din("masks", [16, 128, 512])
    rsel_d = din("rsel", [1, 512])
    eb_d = din("eb", [1, 16])
    out_d = nc.dram_tensor("out", [4096, 1024], F32, kind="ExternalOutput").ap()
    KT_d = nc.dram_tensor("KT_d", [4, 128, 8192], BF16).ap()
    V_d = nc.dram_tensor("V_d", [4, 128, 64, 192], BF16).ap()
    Xs_d = nc.dram_tensor("Xs_d", [65536, 1024], BF16).ap()
    Ys_d = nc.dram_tensor("Ys_d", [65536, 1024], F32).ap()
    CNT_d = nc.dram_tensor("CNT_d", [1, 16], I32).ap()
    if DEBUG["x1"]:
        X1_d = nc.dram_tensor("X1_d", [4096, 1024], F32, kind="ExternalOutput").ap()
    else:
        X1_d = nc.dram_tensor("X1_d", [4096, 1024], F32).ap()

    win_v = win_d.rearrange("(kt p) n -> p kt n", p=128)
    dumps = DEBUG.get("dump", [])

    def dump(name, ap, shape, dt, rname):
        if name not in dumps:
            return
        d = nc.dram_tensor("dbg_" + name, shape, dt, kind="ExternalOutput").ap()
        T.dma('sync', 'dbg_' + name, d, ap, reads=rname, writes=['dbg_' + name])

    top = ExitStack()
    uid = [0]

    def sb(es, shape, dt, name=None):
        uid[0] += 1
        return es.enter_context(nc.sbuf_tensor(f"{name or 't'}_{uid[0]}", shape, dt))

    def ps(es, shape, dt, name=None):
        uid[0] += 1
        return es.enter_context(nc.psum_tensor(f"{name or 'p'}_{uid[0]}", shape, dt))

    ident_f = sb(top, [128, 128], F32, "identf")
    ident_b = sb(top, [128, 128], BF16, "identb")
    Umat = sb(top, [128, 128], F32, "U")
    Sel127 = sb(top, [128, 128], F32, "sel127")
    eps_t = sb(top, [128, 1], F32, "eps")
    one_t = sb(top, [128, 1], F32, "one")
    ones_bf = sb(top, [128, 1], BF16, "onesbf")
    A1 = sb(top, [128, 8], F32, "A1")
    B1 = sb(top, [128, 8], F32, "B1")
    A2 = sb(top, [128, 8], F32, "A2")
    B2 = sb(top, [128, 8], F32, "B2")
    GT1 = sb(top, [128, 1024], F32, "GT1")
    GT2 = sb(top, [128, 1024], F32, "GT2")
    C_all = sb(top, [128, 64, 8], F32, "Call")
    RC = sb(top, [128, 8, 8], F32, "RC")

    T.op('gpsimd', MEMSET(ident_f[:], 0.0), writes=['identf'])
    T.op('gpsimd', ASEL(ident_f[:], ident_f[:], [[-1, 128]], ALU.not_equal, 1.0, 0, 1), reads=['identf'], writes=['identf'])
    T.op('gpsimd', MEMSET(Umat[:], 1.0), writes=['U'])
    T.op('gpsimd', ASEL(Umat[:], Umat[:], [[1, 128]], ALU.is_ge, 0.0, 0, -1), reads=['U'], writes=['U'])
    T.op('gpsimd', MEMSET(Sel127[:], 1.0), writes=['sel127'])
    T.op('gpsimd', ASEL(Sel127[:], Sel127[:], [[0, 128]], ALU.is_ge, 0.0, -127, 1), reads=['sel127'], writes=['sel127'])
    T.op('gpsimd', MEMSET(eps_t[:], EPS), writes=['eps'])
    T.op('gpsimd', MEMSET(one_t[:], 1.0), writes=['one'])
    T.op('gpsimd', MEMSET(ones_bf[:], 1.0), writes=['onesbf'])
    T.op('vector', CP(ident_b[:], ident_f[:]), reads=['identf'], writes=['identb'])

    with ExitStack() as es:
        cT = sb(es, [128, 8], F32)
        CB = sb(es, [128, 8, 128], F32)
        WA = [sb(es, [128, 8, 1024], F32, "wada") for _ in range(2)]
        badab = sb(es, [128, 1024], F32)
        gT1 = sb(es, [128, 8], F32)
        gT2 = sb(es, [128, 8], F32)
        rowv = sb(es, [128, 1024], F32)
        dtmp = sb(es, [128, 8, 128], F32)
        colv = sb(es, [128, 8], F32)
        pp = [ps(es, [128, 512], F32) for _ in range(2)]
        T.dma('sync', 'cT', cT[:], cT_d, writes=['cT'])
        T.dma('sync', 'gT1', gT1[:], g1T_d, writes=['gT1'])
        T.dma('sync', 'gT2', gT2[:], g2T_d, writes=['gT2'])
        T.op('scalar', ACT(cT[:], cT[:], AF.Silu), reads=['cT'], writes=['cT'])
        T.op('vector', CP(CB[:], cT[:].unsqueeze(2).broadcast_to([128, 8, 128])), reads=['cT'], writes=['CB'])
        wada_v = wada_d.rearrange("(kt p) n -> p kt n", p=128)
        order = [1, 0, 2, 4, 3, 5]
        for n, v in enumerate(order):
            wb = n % 2
            for kt in range(8):
                T.dma('sync', f'wada{wb}_{kt}', WA[wb][:, kt, :], wada_v[:, kt, v * 1024:(v + 1) * 1024],
                      writes=[f'wada{wb}_{kt}'])
            T.dma('gpsimd', 'badab', badab[:], bada_d[0:1, v * 1024:(v + 1) * 1024].broadcast_to([128, 1024]),
                  writes=['badab'])
            for hf in range(2):
                T.op('tensor', mmgrp([(pp[hf][:], CB[:, kt, :], WA[wb][:, kt, hf * 512:(hf + 1) * 512], kt == 0, kt == 7)
                                      for kt in range(8)]),
                     reads=['CB'] + [f'wada{wb}_{kt}' for kt in range(8)], writes=[f'pp{hf}'])
                dst = {2: GT1, 5: GT2}.get(v, rowv)
                dname = {2: 'GT1', 5: 'GT2'}.get(v, 'rowv') + str(hf)
                T.op('vector', TT(dst[:, hf * 512:(hf + 1) * 512], pp[hf][:], badab[:, hf * 512:(hf + 1) * 512], ALU.add),
                     reads=[f'pp{hf}', 'badab'], writes=[dname])
            if v in (2, 5):
                continue
            T.op('vector', TT(dtmp[:], rowv[:].rearrange("p (k t) -> p k t", k=8),
                              ident_f[:].unsqueeze(1).broadcast_to([128, 8, 128]), ALU.mult),
                 reads=['rowv0', 'rowv1', 'identf'], writes=['dtmp'])
            T.op('vector', RED(colv[:], dtmp[:], ALU.add), reads=['dtmp'], writes=['colv'])
            if v == 1:
                T.op('vector', STT(A1[:], colv[:], 1.0, gT1[:], ALU.add, ALU.mult), reads=['colv', 'gT1'], writes=['A1'])
            elif v == 0:
                T.op('vector', CP(B1[:], colv[:]), reads=['colv'], writes=['B1'])
            elif v == 4:
                T.op('vector', STT(A2[:], colv[:], 1.0, gT2[:], ALU.add, ALU.mult), reads=['colv', 'gT2'], writes=['A2'])
            elif v == 3:
                T.op('vector', CP(B2[:], colv[:]), reads=['colv'], writes=['B2'])
        T.barrier()

    def ln1_block(xsrc_rows, XA, XN, junk, ssq, rs, tp, tmpf, HT_dst, bi, htname):
        b = bi % 2
        T.dma('sync', f'xa{b}', XA[b][:], xsrc_rows, writes=[f'xa{b}'])
        T.op('scalar', ACT(XN[b][:], XA[b][:], AF.Square, accum_out=ssq[:, bi:bi + 1]), reads=[f'xa{b}', 'ssqall'],
             writes=[f'xn{b}', f'ssq_{bi}'])
        T.op('scalar', ACT(rs[b][:], ssq[:, bi:bi + 1], AF.Ln, scale=1.0 / 1024, bias=eps_t[:]), reads=[f'ssq_{bi}', 'eps'],
             writes=[f'rs{b}'])
        T.op('scalar', ACT(rs[b][:], rs[b][:], AF.Exp, scale=-0.5), reads=[f'rs{b}'], writes=[f'rs{b}'])
        T.op('vector', TS(XN[b][:], XA[b][:], rs[b][:, 0:1], None, ALU.mult), reads=[f'xa{b}', f'rs{b}'], writes=[f'xn{b}'])
        T.op('tensor', tpgrp([(tp[:, kt, :], XN[b][:, kt * 128:(kt + 1) * 128], ident_b[:]) for kt in range(8)]),
             reads=[f'xn{b}', 'identb'], writes=['tp'])
        T.op('vector', TT(tmpf[:], tp[:], A1[:].unsqueeze(2).broadcast_to([128, 8, 128]), ALU.mult), reads=['tp', 'A1'],
             writes=['tmpf'])
        T.op('vector', TT(HT_dst, tmpf[:], B1[:].unsqueeze(2).broadcast_to([128, 8, 128]), ALU.add), reads=['tmpf', 'B1'],
             writes=[htname])

    if "A" in DEBUG["phases"]:
      with ExitStack() as es:
        WKV = sb(es, [128, 8, 1032], BF16, "wkvf")
        XA = [sb(es, [128, 1024], F32) for _ in range(2)]
        XN = [sb(es, [128, 1024], BF16) for _ in range(2)]
        junk = None
        ssq = sb(es, [128, 64], F32)
        T.op('vector', MEMSET(ssq[:], 0.0), writes=['ssqall'])
        rs = [sb(es, [128, 1], F32) for _ in range(2)]
        tmpf = sb(es, [128, 8, 128], F32)
        HT = [sb(es, [128, 8, 512], BF16) for _ in range(2)]
        KTs = [sb(es, [128, 4, 512], BF16) for _ in range(2)]
        VBs = [sb(es, [128, 4, 4, 3, 64], BF16) for _ in range(2)]
        bfb = sb(es, [128, 8], F32)
        tp = ps(es, [128, 8, 128], BF16)
        mm = [ps(es, [128, 512], F32) for _ in range(3)]
        sm = ps(es, [128, 512], F32)
        T.dma('gpsimd', 'wkvf', WKV[:], win_v[:, :, 1536:2568], writes=['wkvf'])
        T.dma('sync', 'bfb', bfb[:], bfg_d[0:1, :].broadcast_to([128, 8]), writes=['bfb'])
        for k in range(2):
            T.op('gpsimd', MEMSET(VBs[k][:, :, :, 1, :], 1.0), writes=[f'vbs{k}'])
        mi = [0]
        ftt = [sb(es, [128, 8], F32) for _ in range(4)]
        smb = [sm] + [ps(es, [128, 512], F32) for _ in range(3)]

        def lnA(r):
            hb = r % 2
            for bl in range(4):
                g = r * 4 + bl
                ln1_block(xall[g * 128:(g + 1) * 128, :], XA, XN, junk, ssq, rs, tp, tmpf,
                          HT[hb][:, :, bl * 128:(bl + 1) * 128], g, f'ht{hb}')

        def kvfA(r):
            hb = r % 2
            for p in range(4):
                m = mm[mi[0] % 3]; mn = f'mm{mi[0] % 3}'; mi[0] += 1
                T.op('tensor', mmgrp([(m[:], WKV[:, kt, p * 128:(p + 1) * 128], HT[hb][:, kt, :], kt == 0, kt == 7)
                                      for kt in range(8)]), reads=['wkvf', f'ht{hb}'], writes=[mn])
                T.op('scalar', ACT(KTs[hb][:, p, :], m[:], AF.Copy), reads=[mn], writes=[f'kts{hb}'])
            T.dma('gpsimd', f'kts{hb}', KT_d[:, :, r * 512:(r + 1) * 512].rearrange("q p t -> p q t"), KTs[hb][:],
                  reads=[f'kts{hb}'], writes=[f'KT_d{r}'])
            for bl in range(4):
                m = mm[mi[0] % 3]; mn = f'mm{mi[0] % 3}'; mi[0] += 1
                T.op('tensor', mmgrp([(m[:], HT[hb][:, kt, bl * 128:(bl + 1) * 128], WKV[:, kt, 512:1024], kt == 0, kt == 7)
                                      for kt in range(8)]), reads=['wkvf', f'ht{hb}'], writes=[mn])
                T.op('tensor', mmgrp([(smb[bl][:, 0:8], HT[hb][:, kt, bl * 128:(bl + 1) * 128], WKV[:, kt, 1024:1032], kt == 0, kt == 7)
                                      for kt in range(8)]), reads=['wkvf', f'ht{hb}'], writes=[f'smb{bl}'])
                T.op('scalar', ACT(VBs[hb][:, bl, :, 0::2, :], m[:].rearrange("t (p two d) -> t p two d", p=4, two=2),
                                   AF.Copy), reads=[mn], writes=[f'vbs{hb}'])
                ft = ftt[bl]
                T.op('vector', TT(ft[:], smb[bl][:, 0:8], bfb[:], ALU.add), reads=[f'smb{bl}', 'bfb'], writes=[f'ft{bl}'])
                T.op('scalar', ACT(ft[:], ft[:], AF.Exp, scale=-1.0), reads=[f'ft{bl}'], writes=[f'ft{bl}'])
                T.op('scalar', ACT(ft[:], ft[:], AF.Ln, bias=one_t[:]), reads=[f'ft{bl}', 'one'], writes=[f'ft{bl}'])
            for q in range(4):
                T.dma('gpsimd', f'vbs{hb}_{q}', V_d[q, :, r * 4:(r + 1) * 4, :],
                      VBs[hb][:, :, q, :, :].rearrange("t b three d -> t b (three d)"), reads=[f'vbs{hb}'],
                      writes=[f'V_d{r}_{q}'])
            for bl in range(4):
                g = r * 4 + bl
                cs = smb[bl][:, 8:16]
                lst = [(cs, Umat[:], ftt[bl][:], True, g == 0)]
                if g > 0:
                    lst.append((cs, Sel127[:], C_all[:, g - 1, :], False, True))
                T.op('tensor', mmgrp(lst), reads=[f'ft{bl}', 'U', 'sel127', 'Call'], writes=[f'smb{bl}'])
                T.op('vector', CP(C_all[:, g, :], cs), reads=[f'smb{bl}'], writes=['Call'])

        lnA(0)
        for r in range(16):
            if r + 1 < 16:
                lnA(r + 1)
            kvfA(r)
        T.barrier()

    with ExitStack() as es:
        rselb = sb(es, [128, 8, 64], F32)
        csel = sb(es, [128, 8, 8], F32)
        tmp3 = sb(es, [128, 8, 64], F32)
        pr = ps(es, [128, 512], F32)
        T.dma('sync', 'rselb', rselb[:], rsel_d[0:1, :].broadcast_to([128, 512]).rearrange("p (i b) -> p i b", i=8),
              writes=['rselb'])
        for i in range(8):
            T.op('vector', TT(tmp3[:], C_all[:].rearrange("p b h -> p h b"),
                              rselb[:, i, :].unsqueeze(1).broadcast_to([128, 8, 64]), ALU.mult),
                 reads=['Call', 'rselb'], writes=['tmp3'])
            T.op('vector', RED(csel[:, i, :], tmp3[:], ALU.add), reads=['tmp3'], writes=['csel'])
        T.op('tensor', mmgrp([(pr[:, 0:64], Sel127[:], csel[:].rearrange("p i h -> p (i h)"), True, True)]),
             reads=['csel', 'sel127'], writes=['pr'])
        T.op('vector', CP(RC[:].rearrange("p i h -> p (i h)"), pr[:, 0:64]), reads=['pr'], writes=['RC'])
        T.barrier()

    if "B" in DEBUG["phases"]:
      with ExitStack() as es:
        WQ = sb(es, [128, 8, 1536], BF16, "wq")
        WO = sb(es, [128, 8, 1024], BF16, "wo")
        MK = sb(es, [128, 16, 512], BF16, "mk")
        WsT = sb(es, [128, 8, 128], BF16, "wst")
        Bb = sb(es, [128, 4, 128], F32)
        gsg = sb(es, [128, 512], F32)
        gos = sb(es, [128, 4], F32)
        gof = sb(es, [128, 4], F32)
        XA = [sb(es, [128, 1024], F32) for _ in range(2)]
        XR = [sb(es, [128, 1024], F32) for _ in range(2)]
        XN = [sb(es, [128, 1024], BF16) for _ in range(2)]
        wstage = XA
        wsf = XR[0][:].rearrange("p (h t) -> p h t", h=8)
        junk = None
        ssq = sb(es, [128, 64], F32)
        T.op('vector', MEMSET(ssq[:], 0.0), writes=['ssqall'])
        rs = [sb(es, [128, 1], F32) for _ in range(2)]
        tmpf = sb(es, [128, 8, 128], F32)
        HT = sb(es, [128, 8, 512], BF16)
        QT = sb(es, [128, 4, 2, 512], BF16)
        T.op('gpsimd', MEMSET(QT[:], 0.0), writes=['qt'])
        UT = sb(es, [128, 4, 512], BF16)
        VG = sb(es, [128, 4, 512], F32)
        VG2 = sb(es, [128, 4, 512], F32)
        v8 = sb(es, [128, 32], F32)
        VN = sb(es, [128, 4, 4, 3, 64], BF16)
        YS = sb(es, [128, 4, 512], BF16)
        YF = sb(es, [128, 4, 512], BF16)
        SQ = sb(es, [128, 4, 512], BF16)
        zt = sb(es, [128, 4, 128], F32)
        KB = [sb(es, [128, 2048], BF16) for _ in range(3)]
        VB = [sb(es, [128, 16, 192], BF16) for _ in range(3)]
        PT = [sb(es, [128, 512], BF16) for _ in range(3)]
        BI = sb(es, [128, 2, 2, 64], F32)
        rc = sb(es, [128, 512], F32)
        rsS = sb(es, [128, 4], F32)
        rsF = sb(es, [128, 4], F32)
        t1 = [sb(es, [128, 512], F32) for _ in range(2)]
        X1 = XR
        tp = ps(es, [128, 8, 128], BF16)
        st = [ps(es, [128, 512], F32) for _ in range(4)]
        OT = [ps(es, [128, 512], F32) for _ in range(2)]
        sm = ps(es, [128, 512], F32)

        T.dma('gpsimd', 'wq_u', WQ[:, :, 0:1024], win_v[:, :, 0:1024], writes=['wq_uv'])
        T.dma('gpsimd', 'wq_q', WQ[:, :, 1024:1536], win_v[:, :, 1024:1536], writes=['wq_q'])
        T.dma('gpsimd', 'mk', MK[:], masks_d.rearrange("m p q -> p m q"), writes=['mk'])
        T.dma('sync', 'xr0', wsf, wsT_d, writes=['xr0'])
        T.dma('sync', 'gsg', gsg[:], gsgu_d[0:1, :].broadcast_to([128, 512]), writes=['gsg'])
        T.dma('sync', 'gos', gos[:], gos_d, writes=['gos'])
        T.dma('sync', 'gof', gof[:], gof_d, writes=['gof'])
        for h in range(8):
            T.dma('sync', 'Bb', Bb[(h % 2) * 64:(h % 2) * 64 + 64, h // 2, :], bsp_d[h:h + 1, :].broadcast_to([64, 128]),
                  writes=[f'Bb{h}'])
        T.op('gpsimd', ASEL(wsf, wsf, [[0, 8], [1, 128]], ALU.is_ge, 0.0, 0, -1), reads=['xr0'], writes=['xr0'])
        T.op('vector', CP(WsT[:], wsf), reads=['xr0'], writes=['wst'])
        T.op('gpsimd', MEMSET(VN[:, :, :, 1, :], 0.0), writes=['vn'])
        wout_v = wout_d.rearrange("(kt p) n -> p kt n", p=128)
        for kt in range(8):
            wsb = kt % 2
            T.dma('sync', f'xa{wsb}', wstage[wsb][:], wout_v[:, kt, :], writes=[f'xa{wsb}'])
            gg = gos[:, kt:kt + 1] if kt < 4 else gof[:, kt - 4:kt - 3]
            T.op('vector', TS(WO[:, kt, :], wstage[wsb][:], gg, None, ALU.mult), reads=[f'xa{wsb}', 'gos', 'gof'],
                 writes=['wo'])
        Bbn = [f'Bb{h}' for h in range(8)]

        sti = [0]
        pti = [0]
        kvi = [0]

        def next_st():
            k = sti[0] % 4
            sti[0] += 1
            return st[k], f'st{k}'

        bglob = 0
        for i in range(8):
            nk = NK(i)
            par = i % 2
            for bl in range(4):
                g = i * 4 + bl
                ln1_block(xown[g * 128:(g + 1) * 128, :], XA, XN, junk, ssq, rs, tp, tmpf,
                          HT[:, :, bl * 128:(bl + 1) * 128], g, 'ht')
            for p in range(4):
                m, mn = next_st()
                T.op('tensor', mmgrp([(m[:], WQ[:, kt, 1024 + p * 128:1024 + (p + 1) * 128], HT[:, kt, :], kt == 0, kt == 7)
                                      for kt in range(8)]), reads=['wq_q', 'ht'], writes=[mn])
                T.op('vector', TS(QT[0:64, p, 0, :], m[0:64, :], 0.125, None, ALU.mult), reads=[mn], writes=['qt'])
                T.op('vector', TS(QT[64:128, p, 1, :], m[64:128, :], 0.125, None, ALU.mult), reads=[mn], writes=['qt'])
            for p in range(4):
                m, mn = next_st()
                T.op('tensor', mmgrp([(m[:], WQ[:, kt, p * 128:(p + 1) * 128], HT[:, kt, :], kt == 0, kt == 7)
                                      for kt in range(8)]), reads=['wq_uv', 'ht'], writes=[mn])
                T.op('scalar', ACT(UT[:, p, :], m[:], AF.Gelu_apprx_tanh), reads=[mn], writes=['ut'])
            for bl in range(4):
                m, mn = next_st()
                T.op('tensor', mmgrp([(m[:], HT[:, kt, bl * 128:(bl + 1) * 128], WQ[:, kt, 512:1024], kt == 0, kt == 7)
                                      for kt in range(8)]), reads=['wq_uv', 'ht'], writes=[mn])
                T.op('scalar', ACT(VG[:, bl, :], m[:], AF.Gelu_apprx_tanh), reads=[mn], writes=['vg'])
            T.op('vector', TT(VG2[:], VG[:], VG[:], ALU.mult), reads=['vg'], writes=['vg2'])
            T.op('vector', RED(v8[:], VG2[:].rearrange("t b (h d) -> t (b h) d", h=8), ALU.add), reads=['vg2'], writes=['v8'])
            T.op('scalar', ACT(v8[:], v8[:], AF.Ln, scale=1.0 / 64, bias=eps_t[:]), reads=['v8', 'eps'], writes=['v8'])
            T.op('scalar', ACT(v8[:], v8[:], AF.Exp, scale=-0.5), reads=['v8'], writes=['v8'])
            T.op('vector', TT(VG2[:].rearrange("t b (h d) -> t (b h) d", h=8), VG[:].rearrange("t b (h d) -> t (b h) d", h=8),
                              v8[:].unsqueeze(2).broadcast_to([128, 32, 64]), ALU.mult), reads=['vg', 'v8'], writes=['vg2'])
            for bl in range(4):
                T.op('vector', TT(VN[:, bl, :, 0::2, :], VG2[:, bl, :].rearrange("t (p two d) -> t p two d", p=4, two=2),
                                  gsg[:].rearrange("t (p two d) -> t p two d", p=4, two=2), ALU.mult),
                     reads=['vg2', 'gsg'], writes=['vn'])
            if i == 0:
                dump("HT", HT[:], [128, 8, 512], BF16, ['ht'])
            for bl in range(4):
                lst = []
                for p in range(4):
                    vnp = VN[:, bl, p, :, :].rearrange("t three d -> t (three d)")
                    lst.append((sm[:, p * 128:(p + 1) * 128], vnp[:, 0:128], WsT[:, 2 * p, :], True, False))
                    lst.append((sm[:, p * 128:(p + 1) * 128], vnp[:, 64:192], WsT[:, 2 * p + 1, :], False, True))
                T.op('tensor', mmgrp(lst), reads=['vn', 'wst'], writes=['sm'])
                T.op('vector', TT(zt[:], sm[:].rearrange("d (p t) -> d p t", p=4), Bb[:], ALU.add), reads=['sm'] + Bbn,
                     writes=['zt'])
                T.op('vector', TT(YS[:, :, bl * 128:(bl + 1) * 128], zt[:], UT[:, :, bl * 128:(bl + 1) * 128], ALU.mult),
                     reads=['zt', 'ut'], writes=['ys'])
            T.op('vector', TT(SQ[:], YS[:], YS[:], ALU.mult), reads=['ys'], writes=['sq'])
            T.op('tensor', mmgrp([(sm[:, bl:bl + 1], SQ[:, p, bl * 128:(bl + 1) * 128], ones_bf[:], p == 0, p == 3)
                                  for bl in range(4) for p in range(4)]), reads=['sq', 'onesbf'], writes=['sm'])
            T.op('scalar', ACT(rsS[:], sm[:, 0:4], AF.Ln, scale=1.0 / 512, bias=eps_t[:]), reads=['sm', 'eps'], writes=['rsS'])
            T.op('scalar', ACT(rsS[:], rsS[:], AF.Exp, scale=-0.5), reads=['rsS'], writes=['rsS'])
            for p in range(4):
                pq = p % 2
                for hh in range(2):
                    h = 2 * p + hh
                    T.op('vector', TS(BI[:, pq, hh, :], C_all[:, :, h], RC[:, i, h:h + 1], None, ALU.subtract),
                         reads=['Call', 'RC'], writes=[f'bi{pq}{hh}'])
                units = [(jb, hh) for jb in range(nk) for hh in range(2)]
                nchunks = (nk + 15) // 16
                if p == 0:
                    seq = [(pp_, c_) for pp_ in range(4) for c_ in range(nchunks)]
                    chunk_buf = {}
                    issued = [0]

                    def ensure(upto):
                        while issued[0] <= upto and issued[0] < len(seq):
                            pp_, c = seq[issued[0]]
                            kb = kvi[0] % 3
                            kvi[0] += 1
                            n = min(16, nk - c * 16)
                            T.dma('sync', f'kb{kb}', KB[kb][:, 0:n * 128], KT_d[pp_, :, c * 2048:c * 2048 + n * 128],
                                  reads=[f'KT_d{r}' for r in range(c * 4, (c * 16 + n + 3) // 4)], writes=[f'kb{kb}'])
                            T.dma('sync', f'vb{kb}', VB[kb][:, 0:n, :], V_d[pp_, :, c * 16:c * 16 + n, :],
                                  reads=[f'V_d{r}_{pp_}' for r in range(c * 4, (c * 16 + n + 3) // 4)], writes=[f'vb{kb}'])
                            chunk_buf[(pp_, c)] = kb
                            issued[0] += 1

                    ensure(2)
                ubank = {}

                def emit_qk(n):
                    jb, hh = units[n]
                    c = jb // 16
                    kb = chunk_buf[(p, c)]
                    jl = jb - c * 16
                    m, mn = next_st()
                    ubank[n] = (m, mn)
                    r0 = hh * 64
                    lst = [(m[:], KB[kb][:, jl * 128:(jl + 1) * 128], QT[:, p, hh, :], True, jb < nk - 8)]
                    rd = [f'kb{kb}', 'qt']
                    if jb >= nk - 8:
                        lst.append((m[:], ident_b[:], MK[:, par * 8 + (jb - (nk - 8)), :], False, True))
                        rd += ['identb', 'mk']
                    T.op('tensor', mmgrp(lst), reads=rd, writes=[mn])

                def emit_rest(n):
                    jb, hh = units[n]
                    c = jb // 16
                    kb = chunk_buf[(p, c)]
                    jl = jb - c * 16
                    m, mn = ubank.pop(n)
                    k3 = pti[0] % 3
                    pti[0] += 1
                    T.op('scalar', ACT(PT[k3][:], m[:], AF.Exp, bias=BI[:, pq, hh, jb:jb + 1]), reads=[mn, f'bi{pq}{hh}'],
                         writes=[f'pt{k3}'])
                    vb = VB[kb][:, jl, hh * 64:hh * 64 + 128]
                    T.op('tensor', mmgrp([(OT[hh][:], vb, PT[k3][:], jb == 0, jb == nk - 1)]),
                         reads=[f'vb{kb}'] + ([] if 'nodep' in PROBE else [f'pt{k3}']), writes=[f'ot{hh}'])

                LA = 3
                for n in range(min(LA, len(units))):
                    emit_qk(n)
                for n in range(len(units)):
                    emit_rest(n)
                    jbd, hhd = units[n]
                    if hhd == 1 and (jbd % 16 == 15 or jbd == nk - 1):
                        ensure(p * nchunks + jbd // 16 + 3)
                    nn = n + LA
                    if nn < len(units):
                        emit_qk(nn)
                T.op('vector', RECIP(rc[0:64, :], OT[0][64:128, :]), reads=['ot0'], writes=['rca'])
                T.op('vector', RECIP(rc[64:128, :], OT[1][0:64, :]), reads=['ot1'], writes=['rcb'])
                T.op('vector', TT(YF[0:64, p, :], OT[0][0:64, :], rc[0:64, :], ALU.mult), reads=['ot0', 'rca'], writes=['yfa'])
                T.op('vector', TT(YF[64:128, p, :], OT[1][64:128, :], rc[64:128, :], ALU.mult), reads=['ot1', 'rcb'],
                     writes=['yfb'])
            T.op('vector', TT(SQ[:], YF[:], YF[:], ALU.mult), reads=['yfa', 'yfb'], writes=['sq'])
            T.op('tensor', mmgrp([(sm[:, 4 + bl:5 + bl], SQ[:, p, bl * 128:(bl + 1) * 128], ones_bf[:], p == 0, p == 3)
                                  for bl in range(4) for p in range(4)]), reads=['sq', 'onesbf'], writes=['sm'])
            T.op('scalar', ACT(rsF[:], sm[:, 4:8], AF.Ln, scale=1.0 / 512, bias=eps_t[:]), reads=['sm', 'eps'], writes=['rsF'])
            T.op('scalar', ACT(rsF[:], rsF[:], AF.Exp, scale=-0.5), reads=['rsF'], writes=['rsF'])
            if i == 0:
                dump("YS", YS[:], [128, 4, 512], BF16, ['ys'])
                dump("YF", YF[:], [128, 4, 512], BF16, ['yfa', 'yfb'])
                dump("rsS", rsS[:], [128, 4], F32, ['rsS'])
                dump("rsF", rsF[:], [128, 4], F32, ['rsF'])
            for bl in range(4):
                g = i * 4 + bl
                xb = g % 2
                T.dma('sync', f'xr{xb}', XR[xb][:], xown[g * 128:(g + 1) * 128, :], writes=[f'xr{xb}'])
                for hf in range(2):
                    ms, msn = next_st()
                    T.op('tensor', mmgrp([(ms[:], YS[:, p, bl * 128:(bl + 1) * 128], WO[:, p, hf * 512:(hf + 1) * 512], p == 0, p == 3)
                                          for p in range(4)]), reads=['ys', 'wo'], writes=[msn])
                    mf, mfn = next_st()
                    T.op('tensor', mmgrp([(mf[:], YF[:, p, bl * 128:(bl + 1) * 128], WO[:, 4 + p, hf * 512:(hf + 1) * 512], p == 0, p == 3)
                                          for p in range(4)]), reads=['yfa', 'yfb', 'wo'], writes=[mfn])
                    tb = hf
                    T.op('vector', TS(t1[tb][:], ms[:], rsS[:, bl:bl + 1], None, ALU.mult), reads=[msn, 'rsS'], writes=[f't1{tb}'])
                    T.op('vector', STT(t1[tb][:], mf[:], rsF[:, bl:bl + 1], t1[tb][:], ALU.mult, ALU.add),
                         reads=[mfn, 'rsF', f't1{tb}'], writes=[f't1{tb}'])
                    T.op('vector', TT(t1[tb][:], t1[tb][:], GT1[:, hf * 512:(hf + 1) * 512], ALU.mult),
                         reads=[f't1{tb}', 'GT10', 'GT11'], writes=[f't1{tb}'])
                    T.op('vector', TT(X1[xb][:, hf * 512:(hf + 1) * 512], t1[tb][:], XR[xb][:, hf * 512:(hf + 1) * 512], ALU.add),
                         reads=[f't1{tb}', f'xr{xb}'], writes=[f'xr{xb}'])
                T.dma('gpsimd', f'x1o{xb}', X1_d[g * 128:(g + 1) * 128, :], X1[xb][:], reads=[f'xr{xb}'], writes=[f'X1_d{g}'])
        T.barrier()

    CAP = 768
    NT0 = CAP // 128
    if "C" in DEBUG["phases"]:
      csem = {}
      with ExitStack() as es0:
        IDX = sb(es0, [128, 32, 2], U32, "IDX")
        W12 = sb(es0, [128, 32, 2], F32, "W12")
        gfin = sb(es0, [128, 1024], F32, "gfin")
        T.dma('sync', 'gfin', gfin[:], gfin_d[0:1, :].broadcast_to([128, 1024]), writes=['gfin'])
        with ExitStack() as es:
            X1t = [sb(es, [128, 1024], F32) for _ in range(2)]
            xn2_2 = [sb(es, [128, 1024], F32) for _ in range(2)]
            junk_2 = [sb(es, [128, 1024], BF16) for _ in range(2)]
            H2f_2 = [sb(es, [128, 8, 128], F32) for _ in range(2)]
            tmpg_2 = [sb(es, [128, 8, 128], F32) for _ in range(2)]
            H2row = [sb(es, [128, 1024], BF16) for _ in range(2)]
            hrt_2 = [sb(es, [128, 1024], F32) for _ in range(2)]
            A2R = sb(es, [128, 1024], F32)
            B2R = sb(es, [128, 1024], F32)
            dg = sb(es, [128, 128], F32)
            onesf = sb(es, [128, 128], F32)
            WR = sb(es, [128, 8, 20], F32)
            brb = sb(es, [128, 20], F32)
            EB = sb(es, [128, 16], F32)
            Cn = sb(es, [128, 32, 16], F32)
            L_2 = [sb(es, [128, 20], F32) for _ in range(2)]
            s1_2 = [sb(es, [128, 16], F32) for _ in range(2)]
            s2_2 = [sb(es, [128, 16], F32) for _ in range(2)]
            E1_2 = [sb(es, [128, 16], F32) for _ in range(2)]
            E2_2 = [sb(es, [128, 16], F32) for _ in range(2)]
            Mx_2 = [sb(es, [128, 16], F32) for _ in range(2)]
            oh_2 = [sb(es, [128, 4], F32) for _ in range(2)]
            mk1_2 = [sb(es, [128, 4], F32) for _ in range(2)]
            mk2_2 = [sb(es, [128, 4], F32) for _ in range(2)]
            les_2 = [sb(es, [128, 4], F32) for _ in range(2)]
            le2_2 = [sb(es, [128, 4], F32) for _ in range(2)]
            sc_2 = [sb(es, [128, 12], F32) for _ in range(2)]
            idf_2 = [sb(es, [128, 2], F32) for _ in range(2)]
            CNTi = sb(es, [128, 16], I32)
            ssq = sb(es, [128, 32], F32)
            T.op('vector', MEMSET(ssq[:], 0.0), writes=['ssqall'])
            rs_2 = [sb(es, [128, 1], F32) for _ in range(2)]
            tpc = ps(es, [128, 8, 128], F32)
            pm = ps(es, [128, 512], F32)
            pmr = [ps(es, [128, 512], F32) for _ in range(2)]

            T.dma('sync', 'WR', WR[:], wr_d.rearrange("(kt p) n -> p kt n", p=128), writes=['WR'])
            T.dma('sync', 'brb', brb[:], br_d[0:1, :].broadcast_to([128, 20]), writes=['brb'])
            T.dma('sync', 'EB', EB[:], eb_d[0:1, :].broadcast_to([128, 16]), writes=['EB'])
            T.op('gpsimd', MEMSET(onesf[:], 1.0), writes=['onesf'])
            for (colt, rowt, nm) in ((A2, A2R, 'A2R'), (B2, B2R, 'B2R')):
                for kt in range(8):
                    T.op('vector', TS(dg[:], ident_f[:], colt[:, kt:kt + 1], None, ALU.mult), reads=['identf', 'A2', 'B2', 'dg'],
                         writes=['dg'])
                    T.op('tensor', mmgrp([(pm[:, 0:128], onesf[:], dg[:], True, True)]), reads=['dg', 'onesf'], writes=['pm'])
                    T.op('vector', CP(rowt[:, kt * 128:(kt + 1) * 128], pm[:, 0:128]), reads=['pm'], writes=[nm])
            def c1_stage1(g):
                    xb = g % 2
                    xn2 = xn2_2[xb]
                    junk = junk_2[xb]
                    H2f = H2f_2[xb]
                    tmpg = tmpg_2[xb]
                    hrt = hrt_2[xb]
                    L = L_2[xb]
                    s1 = s1_2[xb]
                    s2 = s2_2[xb]
                    E1 = E1_2[xb]
                    E2 = E2_2[xb]
                    Mx = Mx_2[xb]
                    oh = oh_2[xb]
                    mk1 = mk1_2[xb]
                    mk2 = mk2_2[xb]
                    les = les_2[xb]
                    le2 = le2_2[xb]
                    sc = sc_2[xb]
                    idf = idf_2[xb]
                    rs = rs_2[xb]
                    T.dma('sync', f'x1t{xb}', X1t[xb][:], X1_d[g * 128:(g + 1) * 128, :], reads=[f'X1_d{g}'], writes=[f'x1t{xb}'])
                    T.op('scalar', ACT(junk[:], X1t[xb][:], AF.Square, accum_out=ssq[:, g:g + 1]), reads=[f'x1t{xb}', 'ssqall'],
                         writes=[f'junk{xb}', f'ssq_{g}'])
                    T.op('scalar', ACT(rs[:], ssq[:, g:g + 1], AF.Ln, scale=1.0 / 1024, bias=eps_t[:]), reads=[f'ssq_{g}', 'eps'], writes=[f'rs{xb}'])
                    T.op('scalar', ACT(rs[:], rs[:], AF.Exp, scale=-0.5), reads=[f'rs{xb}'], writes=[f'rs{xb}'])
                    T.op('vector', TS(xn2[:], X1t[xb][:], rs[:, 0:1], None, ALU.mult), reads=[f'x1t{xb}', f'rs{xb}'], writes=[f'xn2{xb}'])
                    T.op('gpsimd', TT(hrt[:], xn2[:], A2R[:], ALU.mult), reads=[f'xn2{xb}', 'A2R'], writes=[f'hrt{xb}'])
                    T.op('gpsimd', TT(H2row[xb][:], hrt[:], B2R[:], ALU.add), reads=[f'hrt{xb}', 'B2R'], writes=[f'h2row{xb}'])
                    T.op('tensor', tpgrp([(tpc[:, kt, :], xn2[:, kt * 128:(kt + 1) * 128], ident_f[:]) for kt in range(8)]),
                         reads=[f'xn2{xb}', 'identf'], writes=['tpc'])
                    for kt in range(8):
                        T.op('scalar', ACT(H2f[:, kt, :], tpc[:, kt, :], AF.Identity, scale=A2[:, kt:kt + 1], bias=B2[:, kt:kt + 1]),
                             reads=['tpc', 'A2', 'B2'], writes=[f'h2f{xb}'])
                    T.op('tensor', mmgrp([(pmr[xb][:, 0:20], H2f[:, kt, :], WR[:, kt, :], kt == 0, kt == 7) for kt in range(8)]),
                         reads=[f'h2f{xb}', 'WR'], writes=[f'pmr{xb}'])

            def c1_stage2(g):
                    xb = g % 2
                    xn2 = xn2_2[xb]
                    junk = junk_2[xb]
                    H2f = H2f_2[xb]
                    tmpg = tmpg_2[xb]
                    hrt = hrt_2[xb]
                    L = L_2[xb]
                    s1 = s1_2[xb]
                    s2 = s2_2[xb]
                    E1 = E1_2[xb]
                    E2 = E2_2[xb]
                    Mx = Mx_2[xb]
                    oh = oh_2[xb]
                    mk1 = mk1_2[xb]
                    mk2 = mk2_2[xb]
                    les = les_2[xb]
                    le2 = le2_2[xb]
                    sc = sc_2[xb]
                    idf = idf_2[xb]
                    rs = rs_2[xb]
                    T.op('vector', TT(L[:], pmr[xb][:, 0:20], brb[:], ALU.add), reads=[f'pmr{xb}', 'brb'], writes=[f'L{xb}'])
                    rr = [f'L{xb}']
                    V = lambda fn: T.op('vector', fn, reads=rr, writes=rr)
                    S = lambda fn: T.op('scalar', fn, reads=rr, writes=rr)
                    lg = L[:, 0:4]
                    le = L[:, 4:20].rearrange("t (g e) -> t g e", g=4)
                    V(RMAX(sc[:, 0:1], lg))
                    V(TS(oh[:], lg, sc[:, 0:1], None, ALU.is_equal))
                    V(TS(sc[:, 1:2], sc[:, 0:1], -1.0, None, ALU.mult))
                    V(MEMSET(sc[:, 2:3], 0.0))
                    S(ACT(s1[:, 0:4], lg, AF.Exp, bias=sc[:, 1:2], accum_out=sc[:, 2:3]))
                    V(RECIP(sc[:, 3:4], sc[:, 2:3]))
                    V(TT(s2[:].rearrange("t (g e) -> t g e", g=4), le, oh[:].unsqueeze(2).broadcast_to([128, 4, 4]), ALU.mult))
                    V(RED(les[:], s2[:].rearrange("t (g e) -> t e g", g=4), ALU.add))
                    V(RMAX(sc[:, 4:5], les[:]))
                    V(TS(mk1[:], les[:], sc[:, 4:5], None, ALU.is_equal))
                    V(STT(le2[:], mk1[:], NEG, les[:], ALU.mult, ALU.add))
                    V(RMAX(sc[:, 5:6], le2[:]))
                    V(TS(mk2[:], le2[:], sc[:, 5:6], None, ALU.is_equal))
                    V(TT(sc[:, 6:7], sc[:, 5:6], sc[:, 4:5], ALU.subtract))
                    S(ACT(sc[:, 7:8], sc[:, 6:7], AF.Exp))
                    V(TS(sc[:, 8:9], sc[:, 7:8], 1.0, None, ALU.add))
                    V(RECIP(sc[:, 8:9], sc[:, 8:9]))
                    V(TT(W12[:, g, 0:1], sc[:, 8:9], sc[:, 3:4], ALU.mult))
                    V(TT(W12[:, g, 1:2], sc[:, 7:8], W12[:, g, 0:1], ALU.mult))
                    V(TT(E1[:].rearrange("t (g e) -> t g e", g=4), oh[:].unsqueeze(2).broadcast_to([128, 4, 4]),
                         mk1[:].unsqueeze(1).broadcast_to([128, 4, 4]), ALU.mult))
                    V(TT(E2[:].rearrange("t (g e) -> t g e", g=4), oh[:].unsqueeze(2).broadcast_to([128, 4, 4]),
                         mk2[:].unsqueeze(1).broadcast_to([128, 4, 4]), ALU.mult))
                    V(TT(Mx[:], E1[:], E2[:], ALU.add))
                    lst = [(pm[:, 32:48], Umat[:], Mx[:], True, g == 0)]
                    if g > 0:
                        lst.append((pm[:, 32:48], Sel127[:], Cn[:, g - 1, :], False, True))
                    T.op('tensor', mmgrp(lst), reads=[f'L{xb}', 'U', 'sel127', 'Cn'], writes=['pm'])
                    T.op('vector', CP(Cn[:, g, :], pm[:, 32:48]), reads=['pm'], writes=['Cn'])
                    T.op('vector', TT(s1[:], Cn[:, g, :], EB[:], ALU.add), reads=['Cn', 'EB', f'L{xb}'], writes=[f'L{xb}'])
                    V(TT(s2[:], s1[:], E1[:], ALU.mult))
                    V(RED(idf[:, 0:1], s2[:], ALU.add))
                    V(TT(s2[:], s1[:], E2[:], ALU.mult))
                    V(RED(idf[:, 1:2], s2[:], ALU.add))
                    T.op('vector', CP(IDX[:, g, :], idf[:]), reads=[f'L{xb}'], writes=[f'idx{g}'])
                    for k in range(2):
                        T.idma(f'sc{xb}{k}', Xs_d, bass.IndirectOffsetOnAxis(ap=IDX[:, g, k:k + 1], axis=0), H2row[xb][:], None,
                               reads=[f'idx{g}', f'h2row{xb}'], writes=[f'Xs{g}_{k}'])

            c1_stage1(0)
            for g in range(32):
                if g + 1 < 32:
                    c1_stage1(g + 1)
                c1_stage2(g)
            T.op('tensor', mmgrp([(pm[:, 64:80], Sel127[:], Cn[:, 31, :], True, True)]), reads=['Cn', 'sel127'], writes=['pm'])
            T.op('vector', CP(CNTi[:], pm[:, 64:80]), reads=['pm'], writes=['cnti'])
            T.dma('sync', 'cntd', CNT_d, CNTi[0:1, :], reads=['cnti'], writes=['CNT_d'])
            dump("IDX", IDX[:].rearrange("p g k -> p (g k)"), [128, 64], U32, [f'idx{g}' for g in range(32)])
            dump("W12", W12[:].rearrange("p g k -> p (g k)"), [128, 64], F32, ['L'])
            dump("Cn", Cn[:].rearrange("p g e -> p (g e)"), [128, 512], F32, ['Cn'])
            T.barrier()

        with ExitStack() as es:
            WG = [sb(es, [128, 8, 512], BF16) for _ in range(2)]
            WU = [sb(es, [128, 8, 512], BF16) for _ in range(2)]
            WD = [sb(es, [128, 4, 1024], BF16) for _ in range(2)]
            XT = [sb(es, [128, 1024], BF16) for _ in range(3)]
            XsT = [sb(es, [128, 8, 512], BF16) for _ in range(2)]
            AT = [sb(es, [128, 4, 512], BF16) for _ in range(2)]
            sg = [sb(es, [128, 512], F32) for _ in range(2)]
            YT = [sb(es, [128, 1024], F32) for _ in range(2)]
            tpb = ps(es, [128, 8, 128], BF16)
            gp = [ps(es, [128, 512], F32) for _ in range(2)]
            up = [ps(es, [128, 512], F32) for _ in range(2)]
            yp = [ps(es, [128, 512], F32) for _ in range(2)]
            wg_v = wg_d.rearrange("e (kt p) n -> e p kt n", p=128)
            wu_v = wu_d.rearrange("e (kt p) n -> e p kt n", p=128)
            wd_v = wd_d.rearrange("e (kt p) n -> e p kt n", p=128)
            gi = [0]
            yi = [0]
            xi = [0]
            yti = [0]
            gri = [0]

            def load_w(ex):
                wb = ex % 2
                T.dma('gpsimd', f'wg{wb}', WG[wb][:], wg_v[ex], writes=[f'wg{wb}'])
                T.dma('gpsimd', f'wu{wb}', WU[wb][:], wu_v[ex], writes=[f'wu{wb}'])
                T.dma('gpsimd', f'wd{wb}', WD[wb][:], wd_v[ex], writes=[f'wd{wb}'])

            def emit_group(ex, grp):
                wb = ex % 2
                ab = gri[0] % 2
                gri[0] += 1
                base = ex * 4096 + grp * 512
                for tl in range(4):
                    k3 = xi[0] % 3
                    xi[0] += 1
                    r0 = base + tl * 128
                    T.dma('sync', f'xt{k3}', XT[k3][:], Xs_d[r0:r0 + 128, :], writes=[f'xt{k3}'])
                    T.op('tensor', tpgrp([(tpb[:, kt, :], XT[k3][:, kt * 128:(kt + 1) * 128], ident_b[:]) for kt in range(8)]),
                         reads=[f'xt{k3}', 'identb'], writes=['tpb'])
                    T.op('vector', CP(XsT[ab][:, :, tl * 128:(tl + 1) * 128], tpb[:]), reads=['tpb'], writes=[f'xst{ab}'])
                for ht in range(4):
                    k2 = gi[0] % 2
                    gi[0] += 1
                    T.op('tensor', mmgrp([(gp[k2][:], WG[wb][:, kt, ht * 128:(ht + 1) * 128], XsT[ab][:, kt, :], kt == 0, kt == 7)
                                          for kt in range(8)]), reads=[f'wg{wb}', f'xst{ab}'], writes=[f'gp{k2}'])
                    T.op('tensor', mmgrp([(up[k2][:], WU[wb][:, kt, ht * 128:(ht + 1) * 128], XsT[ab][:, kt, :], kt == 0, kt == 7)
                                          for kt in range(8)]), reads=[f'wu{wb}', f'xst{ab}'], writes=[f'up{k2}'])
                    T.op('scalar', ACT(sg[k2][:], gp[k2][:], AF.Silu), reads=[f'gp{k2}'], writes=[f'sg{k2}'])
                    T.op('vector', TT(AT[ab][:, ht, :], sg[k2][:], up[k2][:], ALU.mult), reads=[f'sg{k2}', f'up{k2}'],
                         writes=[f'at{ab}'])
                for tl in range(4):
                    yb = yti[0] % 2
                    yti[0] += 1
                    for nh in range(2):
                        k2 = yi[0] % 2
                        yi[0] += 1
                        T.op('tensor', mmgrp([(yp[k2][:], AT[ab][:, ht, tl * 128:(tl + 1) * 128], WD[wb][:, ht, nh * 512:(nh + 1) * 512],
                                               ht == 0, ht == 3) for ht in range(4)]), reads=[f'at{ab}', f'wd{wb}'], writes=[f'yp{k2}'])
                        T.op('scalar', ACT(YT[yb][:, nh * 512:(nh + 1) * 512], yp[k2][:], AF.Copy), reads=[f'yp{k2}'], writes=[f'yt{yb}'])
                    r0 = base + tl * 128
                    T.dma('sync', f'yt{yb}', Ys_d[r0:r0 + 128, :], YT[yb][:], reads=[f'yt{yb}'], writes=[f'Ys{ex}_{grp}_{tl}'])

            drain = ['xt0', 'xt1', 'xt2', 'yt0', 'yt1']
            load_w(0)
            for ex in range(16):
                if ex + 1 < 16:
                    load_w(ex + 1)
                emit_group(ex, 0)
                for grp in range(1, 8):
                    T.cond_begin(CNT_d[0:1, ex:ex + 1], grp * 512, drain)
                    emit_group(ex, grp)
                    T.cond_end()
            T.barrier()

        with ExitStack() as es:
            Y1 = [sb(es, [128, 1024], F32) for _ in range(2)]
            Y2 = [sb(es, [128, 1024], F32) for _ in range(2)]
            X1t = [sb(es, [128, 1024], F32) for _ in range(2)]
            junk = sb(es, [128, 1024], BF16)
            osb = [sb(es, [128, 1024], F32) for _ in range(2)]
            ssq = sb(es, [128, 32], F32)
            T.op('vector', MEMSET(ssq[:], 0.0), writes=['ssqall'])
            rs = [sb(es, [128, 1], F32) for _ in range(2)]
            def c3_loads(g):
                b = g % 2
                T.dma('sync', f'x1c{b}', X1t[b][:], X1_d[g * 128:(g + 1) * 128, :], writes=[f'x1c{b}'])
                T.idma(f'ga{b}', Y1[b][:], None, Ys_d, bass.IndirectOffsetOnAxis(ap=IDX[:, g, 0:1], axis=0), writes=[f'y1{b}'])
                T.idma(f'gb{b}', Y2[b][:], None, Ys_d, bass.IndirectOffsetOnAxis(ap=IDX[:, g, 1:2], axis=0), writes=[f'y2{b}'])

            c3_loads(0)
            for g in range(32):
                b = g % 2
                if g + 1 < 32:
                    c3_loads(g + 1)
                T.op('vector', TS(Y1[b][:], Y1[b][:], W12[:, g, 0:1], None, ALU.mult), reads=[f'y1{b}'], writes=[f'y1{b}'])
                T.op('vector', STT(Y1[b][:], Y2[b][:], W12[:, g, 1:2], Y1[b][:], ALU.mult, ALU.add), reads=[f'y1{b}', f'y2{b}'],
                     writes=[f'y1{b}'])
                T.op('vector', TT(Y1[b][:], Y1[b][:], GT2[:], ALU.mult), reads=[f'y1{b}'], writes=[f'y1{b}'])
                T.op('vector', TT(Y1[b][:], Y1[b][:], X1t[b][:], ALU.add), reads=[f'y1{b}', f'x1c{b}'], writes=[f'y1{b}'])
                T.op('scalar', ACT(junk[:], Y1[b][:], AF.Square, accum_out=ssq[:, g:g + 1]), reads=[f'y1{b}', 'ssqall'],
                     writes=['junk', f'ssq_{g}'])
                T.op('scalar', ACT(rs[b][:], ssq[:, g:g + 1], AF.Ln, scale=1.0 / 1024, bias=eps_t[:]), reads=[f'ssq_{g}', 'eps'], writes=[f'rs{b}'])
                T.op('scalar', ACT(rs[b][:], rs[b][:], AF.Exp, scale=-0.5), reads=[f'rs{b}'], writes=[f'rs{b}'])
                T.op('vector', STT(osb[b][:], Y1[b][:], rs[b][:, 0:1], gfin[:], ALU.mult, ALU.mult),
                     reads=[f'y1{b}', f'rs{b}', 'gfin'], writes=[f'osb{b}'])
                T.dma('sync', f'out{b}', out_d[g * 128:(g + 1) * 128, :], osb[b][:], reads=[f'osb{b}'], writes=[f'out{g}'])
            T.barrier()
    T.emit()
    top.close()
    return nc


_CACHE = {}


def _masks(j):
    m = np.zeros((2, 8, 128, 512), np.float32)
    ki = np.arange(128)[:, None]
    qq = np.arange(512)[None, :]
    for par in range(2):
        i = par
        run = OWN_RUNS[j][i]
        nk = NK(i)
        for r in range(8):
            jb = nk - 8 + r
            kpos = jb * 128 + ki
            qpos = run * 512 + qq
            m[par, r] = np.where(kpos <= qpos, 0.0, NEG)
    return m.reshape(16, 128, 512)


def _rsel(j):
    s = np.zeros((8, 64), np.float32)
    for i, run in enumerate(OWN_RUNS[j]):
        s[i, 4 * run + 1] = 1.0
    return s.reshape(1, 512)


def kernel(x, c, w_ada, b_ada, g_norm_mix, w_in, g_sgu, w_spatial, b_spatial, b_forget, g_out_sgu, g_out_fox, w_out,
           g_norm_ffn, w_router_group, b_router_group, w_router_expert, b_router_expert, w_gate, w_up, w_down, g_final):
    f = lambda a: np.ascontiguousarray(np.asarray(a, dtype=np.float32))
    x = f(x); c = f(c)
    if "nc" not in _CACHE:
        _CACHE["nc"] = build_program()
    nc = _CACHE["nc"]
    wrg = f(w_router_group)[0]; wre = f(w_router_expert)[0]
    brg = f(b_router_group)[0]; bre = f(b_router_expert)[0]
    wg_all = f(w_gate)[0]; wu_all = f(w_up)[0]; wd_all = f(w_down)[0]
    shared = {
        "w_ada": f(w_ada)[0], "b_ada": f(b_ada)[0][None, :], "g1T": f(f(g_norm_mix)[0].reshape(8, 128).T),
        "g2T": f(f(g_norm_ffn)[0].reshape(8, 128).T), "w_in": f(w_in)[0], "gsgu": f(g_sgu)[0][None, :],
        "wsT": f(f(w_spatial)[0].transpose(2, 0, 1)), "bsp": f(b_spatial)[0], "bfg": f(b_forget)[0][None, :],
        "gos": f(f(g_out_sgu)[0].reshape(4, 128).T), "gof": f(f(g_out_fox)[0].reshape(4, 128).T), "w_out": f(w_out)[0],
        "gfin": f(g_final)[None, :],
    }
    percore = {}
    for core in range(8):
        gmap = [(G + core % 4) % 4 for G in range(4)]
        emap = [(j + 2 * (core // 4)) % 4 for j in range(4)]
        eorder = [4 * gmap[G] + emap[j] for G in range(4) for j in range(4)]
        wr = np.concatenate([wrg[:, gmap]] + [wre[gmap[G]][:, emap] for G in range(4)], axis=1)
        br = np.concatenate([brg[gmap]] + [bre[gmap[G]][emap] for G in range(4)])[None, :]
        percore[core] = {"wr": f(wr), "br": f(br), "w_gate": f(wg_all[eorder]), "w_up": f(wu_all[eorder]),
                         "w_down": f(wd_all[eorder])}
    in_maps = []
    for core in range(8):
        b, j = core // 2, core % 2
        m = dict(shared)
        m.update(percore[core])
        m["xall"] = x[b]
        m["xown"] = f(np.concatenate([x[b, 512 * r:512 * (r + 1)] for r in OWN_RUNS[j]], axis=0))
        m["cT"] = f(c[b].reshape(8, 128).T)
        m["masks"] = _masks(j)
        m["rsel"] = _rsel(j)
        m["eb"] = (np.arange(16, dtype=np.float32) * 4096.0 - 1.0)[None, :]
        in_maps.append(m)
    res = run_bass_kernel_spmd(nc, in_maps, core_ids=list(range(8)))
    _CACHE["res"] = res
    out = np.empty((4, 8192, 1024), np.float32)
    for core in range(8):
        b, j = core // 2, core % 2
        o = res.results[core]["out"]
        for i, r in enumerate(OWN_RUNS[j]):
            out[b, 512 * r:512 * (r + 1)] = o[512 * i:512 * (i + 1)]
    return out
```

```python
import os
import numpy as np
from contextlib import ExitStack
import concourse.bass as bass
import concourse.mybir as mybir
from concourse.bass_utils import run_bass_kernel_spmd

F32 = mybir.dt.float32
BF16 = mybir.dt.bfloat16
U32 = mybir.dt.uint32
I32 = mybir.dt.int32
AF = mybir.ActivationFunctionType
ALU = mybir.AluOpType
AX = mybir.AxisListType
ENGS = ['sync', 'scalar', 'vector', 'gpsimd', 'tensor']
EPS = 1e-6
NEG = -1.0e30
OWN_RUNS = {0: [0, 3, 4, 7, 8, 11, 12, 15], 1: [1, 2, 5, 6, 9, 10, 13, 14]}
WARM = 18
PROBE = os.environ.get('MK_PROBE', '')
DEBUG = {"x1": False, "same": True, "phases": "ABC", "dump": []}


def NK(i):
    return 16 * (i // 2) + 8 + 8 * (i % 2)


class Tracker:
    def __init__(self, nc, same_engine_sync=False):
        self.nc = nc
        self.streams = {e: [] for e in ENGS}
        self.sems = {e: nc.alloc_semaphore("c_" + e) for e in ENGS}
        self.cnt = {e: 0 for e in ENGS}
        self.waited = {e: {} for e in ENGS}
        self.res = {}
        self.slots = {}
        self.same = same_engine_sync
        self.slot_q = {}

    def _deps(self, reads, writes):
        deps = []
        for r in reads:
            st = self.res.get(r)
            if st and st[0]:
                deps.append(st[0])
        for w in writes:
            st = self.res.get(w)
            if st:
                if st[0]:
                    deps.append(st[0])
                deps.extend(st[1])
        return deps

    def _update(self, reads, writes, tag):
        for r in reads:
            st = self.res.setdefault(r, [None, []])
            st[1].append(tag)
        for w in writes:
            self.res[w] = [tag, []]

    def _emit_waits(self, eng, deps, skip_same):
        mx = {}
        for key, val in deps:
            mx[key] = max(mx.get(key, 0), val)
        for key, val in mx.items():
            if key == eng and skip_same:
                continue
            if self.waited[eng].get(key, 0) >= val:
                continue
            self.waited[eng][key] = val
            sem = self.sems[key] if key in self.sems else self.slots[key][0]
            self.streams[eng].append(lambda e, sem=sem, val=val: e.wait_ge(sem, val))

    def op(self, eng, fn, reads=(), writes=()):
        deps = self._deps(reads, writes)
        self._emit_waits(eng, deps, (not self.same) or eng == 'tensor')
        self.cnt[eng] += 1
        sem = self.sems[eng]
        self.streams[eng].append(lambda e, fn=fn, sem=sem: fn(e).then_inc(sem, 1))
        self._update(reads, writes, (eng, self.cnt[eng]))

    def dma(self, q, slot, out, in_, reads=(), writes=()):
        self.slot_q[slot] = q
        if slot not in self.slots:
            self.slots[slot] = [self.nc.alloc_semaphore("d_" + slot), 0]
        deps = self._deps(reads, writes)
        self._emit_waits(q, deps, False)
        s = self.slots[slot]
        s[1] += 16
        sem = s[0]
        self.streams[q].append(lambda e, out=out, in_=in_, sem=sem: e.dma_start(out=out, in_=in_).then_inc(sem, 16))
        self._update(reads, writes, (slot, s[1]))

    def idma(self, slot, out, out_off, in_, in_off, reads=(), writes=()):
        q = 'gpsimd'
        self.slot_q[slot] = q
        if slot not in self.slots:
            self.slots[slot] = [self.nc.alloc_semaphore("d_" + slot), 0]
        deps = self._deps(reads, writes)
        self._emit_waits(q, deps, False)
        s = self.slots[slot]
        s[1] += 16
        sem = s[0]
        self.streams[q].append(lambda e, out=out, in_=in_, sem=sem, oo=out_off, io=in_off:
                               e.indirect_dma_start(out=out, out_offset=oo, in_=in_, in_offset=io).then_inc(sem, 16))
        self._update(reads, writes, (slot, s[1]))

    def cond_begin(self, cnt_ap, thr, drain_slots):
        for e in ENGS:
            deps = [(e, self.cnt[e])] if self.cnt[e] else []
            deps += [(k, self.slots[k][1]) for k in drain_slots if k in self.slots and self.slot_q.get(k) == e]
            self._emit_waits(e, deps, False)
        self.snap_cnt = dict(self.cnt)
        self.snap_slots = {k: v[1] for k, v in self.slots.items()}
        self.snap_waited = {e: dict(w) for e, w in self.waited.items()}
        for e in ENGS:
            if cnt_ap is not None:
                self.streams[e].append(('regload', cnt_ap))
            self.streams[e].append(('if', thr))

    def cond_end(self):
        for e in ENGS:
            self.streams[e].append(('else',))
            n = self.cnt[e] - self.snap_cnt[e]
            if n:
                self.streams[e].append(lambda eng, sem=self.sems[e], n=n: eng.sem_inc(sem, n))
        for slot, (sem, c) in self.slots.items():
            d = c - self.snap_slots.get(slot, 0)
            if d:
                self.streams[self.slot_q[slot]].append(lambda eng, sem=sem, d=d: eng.sem_inc(sem, d))
        for e in ENGS:
            self.streams[e].append(('endif',))
        self.waited = self.snap_waited

    def barrier(self):
        deps = [(e, c) for e, c in self.cnt.items() if c > 0]
        deps += [(k, v[1]) for k, v in self.slots.items() if v[1] > 0]
        for e in ENGS:
            self._emit_waits(e, deps, True)

    def emit(self):
        self.barrier()
        streams = self.streams

        def run(e, items):
            creg = e.alloc_register("creg")
            i = 0
            n = len(items)
            while i < n:
                it = items[i]
                if not isinstance(it, tuple):
                    it(e)
                    i += 1
                    continue
                if it[0] == 'regload':
                    e.reg_load(creg, it[1])
                    i += 1
                    continue
                assert it[0] == 'if'
                thr = it[1]
                j = i + 1
                body = []
                while not (isinstance(items[j], tuple) and items[j][0] == 'else'):
                    body.append(items[j])
                    j += 1
                j += 1
                fix = []
                while not (isinstance(items[j], tuple) and items[j][0] == 'endif'):
                    fix.append(items[j])
                    j += 1
                with e.If_lt(creg, thr + 1):
                    for f in fix:
                        f(e)
                with e.Else():
                    for f in body:
                        f(e)
                i = j + 1

        with self.nc.Block() as block:
            @block.sync
            def _(e):
                run(e, streams['sync'])

            @block.scalar
            def _(e):
                run(e, streams['scalar'])

            @block.vector
            def _(e):
                run(e, streams['vector'])

            @block.gpsimd
            def _(e):
                run(e, streams['gpsimd'])

            @block.tensor
            def _(e):
                run(e, streams['tensor'])


def mmgrp(lst):
    def fn(e):
        ins = None
        for (out, lhsT, rhs, st, sp) in lst:
            ins = e.matmul(out, lhsT=lhsT, rhs=rhs, start=st, stop=sp)
        return ins
    return fn


def tpgrp(lst):
    def fn(e):
        ins = None
        for (out, in_, ident) in lst:
            ins = e.transpose(out, in_, ident)
        return ins
    return fn


def ACT(out, in_, func, **kw):
    return lambda e: e.activation(out=out, in_=in_, func=func, **kw)


def TT(out, in0, in1, op):
    return lambda e: e.tensor_tensor(out=out, in0=in0, in1=in1, op=op)


def TS(out, in0, s1, s2, op0, op1=None):
    if op1 is None:
        return lambda e: e.tensor_scalar(out=out, in0=in0, scalar1=s1, scalar2=None, op0=op0)
    return lambda e: e.tensor_scalar(out=out, in0=in0, scalar1=s1, scalar2=s2, op0=op0, op1=op1)


def STT(out, in0, scalar, in1, op0, op1):
    return lambda e: e.scalar_tensor_tensor(out=out, in0=in0, scalar=scalar, in1=in1, op0=op0, op1=op1)


def CP(out, in_):
    return lambda e: e.tensor_copy(out=out, in_=in_)


def RED(out, in_, op):
    return lambda e: e.tensor_reduce(out=out, in_=in_, axis=AX.X, op=op)


def RMAX(out, in_):
    return lambda e: e.reduce_max(out=out, in_=in_, axis=AX.X)


def RECIP(out, in_):
    return lambda e: e.reciprocal(out=out, in_=in_)


def MEMSET(ap, v):
    return lambda e: e.memset(ap, v)


def ASEL(out, in_, pattern, cmp, fill, base, cm):
    return lambda e: e.affine_select(out=out, in_=in_, pattern=pattern, compare_op=cmp, fill=fill, base=base,
                                     channel_multiplier=cm)


def build_program():
    nc = bass.Bass("TRN2", target_bir_lowering=False)
    T = Tracker(nc, same_engine_sync=DEBUG["same"])
    din = lambda name, shape: nc.dram_tensor(name, shape, F32, kind="ExternalInput").ap()
    xall = din("xall", [8192, 1024])
    xown = din("xown", [4096, 1024])
    cT_d = din("cT", [128, 8])
    wada_d = din("w_ada", [1024, 6144])
    bada_d = din("b_ada", [1, 6144])
    g1T_d = din("g1T", [128, 8])
    g2T_d = din("g2T", [128, 8])
    win_d = din("w_in", [1024, 2568])
    gsgu_d = din("gsgu", [1, 512])
    wsT_d = din("wsT", [128, 8, 128])
    bsp_d = din("bsp", [8, 128])
    bfg_d = din("bfg", [1, 8])
    gos_d = din("gos", [128, 4])
    gof_d = din("gof", [128, 4])
    wout_d = din("w_out", [1024, 1024])
    wr_d = din("wr", [1024, 20])
    br_d = din("br", [1, 20])
    wg_d = din("w_gate", [16, 1024, 512])
    wu_d = din("w_up", [16, 1024, 512])
    wd_d = din("w_down", [16, 512, 1024])
    gfin_d = din("gfin", [1, 1024])
    masks_d = din("masks", [16, 128, 512])
    rsel_d = din("rsel", [1, 512])
    eb_d = din("eb", [1, 16])
    out_d = nc.dram_tensor("out", [4096, 1024], F32, kind="ExternalOutput").ap()
    KT_d = nc.dram_tensor("KT_d", [4, 128, 8192], BF16).ap()
    V_d = nc.dram_tensor("V_d", [4, 128, 64, 192], BF16).ap()
    Xs_d = nc.dram_tensor("Xs_d", [65536, 1024], BF16).ap()
    Ys_d = nc.dram_tensor("Ys_d", [65536, 1024], F32).ap()
    CNT_d = nc.dram_tensor("CNT_d", [1, 16], I32).ap()
    if DEBUG["x1"]:
        X1_d = nc.dram_tensor("X1_d", [4096, 1024], F32, kind="ExternalOutput").ap()
    else:
        X1_d = nc.dram_tensor("X1_d", [4096, 1024], F32).ap()

    win_v = win_d.rearrange("(kt p) n -> p kt n", p=128)
    dumps = DEBUG.get("dump", [])

    def dump(name, ap, shape, dt, rname):
        if name not in dumps:
            return
        d = nc.dram_tensor("dbg_" + name, shape, dt, kind="ExternalOutput").ap()
        T.dma('sync', 'dbg_' + name, d, ap, reads=rname, writes=['dbg_' + name])

    top = ExitStack()
    uid = [0]

    def sb(es, shape, dt, name=None):
        uid[0] += 1
        return es.enter_context(nc.sbuf_tensor(f"{name or 't'}_{uid[0]}", shape, dt))

    def ps(es, shape, dt, name=None):
        uid[0] += 1
        return es.enter_context(nc.psum_tensor(f"{name or 'p'}_{uid[0]}", shape, dt))

    ident_f = sb(top, [128, 128], F32, "identf")
    ident_b = sb(top, [128, 128], BF16, "identb")
    Umat = sb(top, [128, 128], F32, "U")
    Sel127 = sb(top, [128, 128], F32, "sel127")
    eps_t = sb(top, [128, 1], F32, "eps")
    one_t = sb(top, [128, 1], F32, "one")
    ones_bf = sb(top, [128, 1], BF16, "onesbf")
    A1 = sb(top, [128, 8], F32, "A1")
    B1 = sb(top, [128, 8], F32, "B1")
    A2 = sb(top, [128, 8], F32, "A2")
    B2 = sb(top, [128, 8], F32, "B2")
    GT1 = sb(top, [128, 1024], F32, "GT1")
    GT2 = sb(top, [128, 1024], F32, "GT2")
    C_all = sb(top, [128, 64, 8], F32, "Call")
    RC = sb(top, [128, 8, 8], F32, "RC")

    T.op('gpsimd', MEMSET(ident_f[:], 0.0), writes=['identf'])
    T.op('gpsimd', ASEL(ident_f[:], ident_f[:], [[-1, 128]], ALU.not_equal, 1.0, 0, 1), reads=['identf'], writes=['identf'])
    T.op('gpsimd', MEMSET(Umat[:], 1.0), writes=['U'])
    T.op('gpsimd', ASEL(Umat[:], Umat[:], [[1, 128]], ALU.is_ge, 0.0, 0, -1), reads=['U'], writes=['U'])
    T.op('gpsimd', MEMSET(Sel127[:], 1.0), writes=['sel127'])
    T.op('gpsimd', ASEL(Sel127[:], Sel127[:], [[0, 128]], ALU.is_ge, 0.0, -127, 1), reads=['sel127'], writes=['sel127'])
    T.op('gpsimd', MEMSET(eps_t[:], EPS), writes=['eps'])
    T.op('gpsimd', MEMSET(one_t[:], 1.0), writes=['one'])
    T.op('gpsimd', MEMSET(ones_bf[:], 1.0), writes=['onesbf'])
    T.op('vector', CP(ident_b[:], ident_f[:]), reads=['identf'], writes=['identb'])

    with ExitStack() as es:
        cT = sb(es, [128, 8], F32)
        CB = sb(es, [128, 8, 128], F32)
        WA = [sb(es, [128, 8, 1024], F32, "wada") for _ in range(2)]
        badab = sb(es, [128, 1024], F32)
        gT1 = sb(es, [128, 8], F32)
        gT2 = sb(es, [128, 8], F32)
        rowv = sb(es, [128, 1024], F32)
        dtmp = sb(es, [128, 8, 128], F32)
        colv = sb(es, [128, 8], F32)
        pp = [ps(es, [128, 512], F32) for _ in range(2)]
        T.dma('sync', 'cT', cT[:], cT_d, writes=['cT'])
        T.dma('sync', 'gT1', gT1[:], g1T_d, writes=['gT1'])
        T.dma('sync', 'gT2', gT2[:], g2T_d, writes=['gT2'])
        T.op('scalar', ACT(cT[:], cT[:], AF.Silu), reads=['cT'], writes=['cT'])
        T.op('vector', CP(CB[:], cT[:].unsqueeze(2).broadcast_to([128, 8, 128])), reads=['cT'], writes=['CB'])
        wada_v = wada_d.rearrange("(kt p) n -> p kt n", p=128)
        order = [1, 0, 2, 4, 3, 5]
        for n, v in enumerate(order):
            wb = n % 2
            for kt in range(8):
                T.dma('sync', f'wada{wb}_{kt}', WA[wb][:, kt, :], wada_v[:, kt, v * 1024:(v + 1) * 1024],
                      writes=[f'wada{wb}_{kt}'])
            T.dma('gpsimd', 'badab', badab[:], bada_d[0:1, v * 1024:(v + 1) * 1024].broadcast_to([128, 1024]),
                  writes=['badab'])
            for hf in range(2):
                T.op('tensor', mmgrp([(pp[hf][:], CB[:, kt, :], WA[wb][:, kt, hf * 512:(hf + 1) * 512], kt == 0, kt == 7)
                                      for kt in range(8)]),
                     reads=['CB'] + [f'wada{wb}_{kt}' for kt in range(8)], writes=[f'pp{hf}'])
                dst = {2: GT1, 5: GT2}.get(v, rowv)
                dname = {2: 'GT1', 5: 'GT2'}.get(v, 'rowv') + str(hf)
                T.op('vector', TT(dst[:, hf * 512:(hf + 1) * 512], pp[hf][:], badab[:, hf * 512:(hf + 1) * 512], ALU.add),
                     reads=[f'pp{hf}', 'badab'], writes=[dname])
            if v in (2, 5):
                continue
            T.op('vector', TT(dtmp[:], rowv[:].rearrange("p (k t) -> p k t", k=8),
                              ident_f[:].unsqueeze(1).broadcast_to([128, 8, 128]), ALU.mult),
                 reads=['rowv0', 'rowv1', 'identf'], writes=['dtmp'])
            T.op('vector', RED(colv[:], dtmp[:], ALU.add), reads=['dtmp'], writes=['colv'])
            if v == 1:
                T.op('vector', STT(A1[:], colv[:], 1.0, gT1[:], ALU.add, ALU.mult), reads=['colv', 'gT1'], writes=['A1'])
            elif v == 0:
                T.op('vector', CP(B1[:], colv[:]), reads=['colv'], writes=['B1'])
            elif v == 4:
                T.op('vector', STT(A2[:], colv[:], 1.0, gT2[:], ALU.add, ALU.mult), reads=['colv', 'gT2'], writes=['A2'])
            elif v == 3:
                T.op('vector', CP(B2[:], colv[:]), reads=['colv'], writes=['B2'])
        T.barrier()

    def ln1_block(xsrc_rows, XA, XN, junk, ssq, rs, tp, tmpf, HT_dst, bi, htname):
        b = bi % 2
        T.dma('sync', f'xa{b}', XA[b][:], xsrc_rows, writes=[f'xa{b}'])
        T.op('scalar', ACT(XN[b][:], XA[b][:], AF.Square, accum_out=ssq[:, bi:bi + 1]), reads=[f'xa{b}', 'ssqall'],
             writes=[f'xn{b}', f'ssq_{bi}'])
        T.op('scalar', ACT(rs[b][:], ssq[:, bi:bi + 1], AF.Ln, scale=1.0 / 1024, bias=eps_t[:]), reads=[f'ssq_{bi}', 'eps'],
             writes=[f'rs{b}'])
        T.op('scalar', ACT(rs[b][:], rs[b][:], AF.Exp, scale=-0.5), reads=[f'rs{b}'], writes=[f'rs{b}'])
        T.op('vector', TS(XN[b][:], XA[b][:], rs[b][:, 0:1], None, ALU.mult), reads=[f'xa{b}', f'rs{b}'], writes=[f'xn{b}'])
        T.op('tensor', tpgrp([(tp[:, kt, :], XN[b][:, kt * 128:(kt + 1) * 128], ident_b[:]) for kt in range(8)]),
             reads=[f'xn{b}', 'identb'], writes=['tp'])
        T.op('vector', TT(tmpf[:], tp[:], A1[:].unsqueeze(2).broadcast_to([128, 8, 128]), ALU.mult), reads=['tp', 'A1'],
             writes=['tmpf'])
        T.op('vector', TT(HT_dst, tmpf[:], B1[:].unsqueeze(2).broadcast_to([128, 8, 128]), ALU.add), reads=['tmpf', 'B1'],
             writes=[htname])

    if "A" in DEBUG["phases"]:
      with ExitStack() as es:
        WKV = sb(es, [128, 8, 1032], BF16, "wkvf")
        XA = [sb(es, [128, 1024], F32) for _ in range(2)]
        XN = [sb(es, [128, 1024], BF16) for _ in range(2)]
        junk = None
        ssq = sb(es, [128, 64], F32)
        T.op('vector', MEMSET(ssq[:], 0.0), writes=['ssqall'])
        rs = [sb(es, [128, 1], F32) for _ in range(2)]
        tmpf = sb(es, [128, 8, 128], F32)
        HT = [sb(es, [128, 8, 512], BF16) for _ in range(2)]
        KTs = [sb(es, [128, 4, 512], BF16) for _ in range(2)]
        VBs = [sb(es, [128, 4, 4, 3, 64], BF16) for _ in range(2)]
        bfb = sb(es, [128, 8], F32)
        tp = ps(es, [128, 8, 128], BF16)
        mm = [ps(es, [128, 512], F32) for _ in range(3)]
        sm = ps(es, [128, 512], F32)
        T.dma('gpsimd', 'wkvf', WKV[:], win_v[:, :, 1536:2568], writes=['wkvf'])
        T.dma('sync', 'bfb', bfb[:], bfg_d[0:1, :].broadcast_to([128, 8]), writes=['bfb'])
        for k in range(2):
            T.op('gpsimd', MEMSET(VBs[k][:, :, :, 1, :], 1.0), writes=[f'vbs{k}'])
        mi = [0]
        ftt = [sb(es, [128, 8], F32) for _ in range(4)]
        smb = [sm] + [ps(es, [128, 512], F32) for _ in range(3)]

        def lnA(r):
            hb = r % 2
            for bl in range(4):
                g = r * 4 + bl
                ln1_block(xall[g * 128:(g + 1) * 128, :], XA, XN, junk, ssq, rs, tp, tmpf,
                          HT[hb][:, :, bl * 128:(bl + 1) * 128], g, f'ht{hb}')

        def kvfA(r):
            hb = r % 2
            for p in range(4):
                m = mm[mi[0] % 3]; mn = f'mm{mi[0] % 3}'; mi[0] += 1
                T.op('tensor', mmgrp([(m[:], WKV[:, kt, p * 128:(p + 1) * 128], HT[hb][:, kt, :], kt == 0, kt == 7)
                                      for kt in range(8)]), reads=['wkvf', f'ht{hb}'], writes=[mn])
                T.op('scalar', ACT(KTs[hb][:, p, :], m[:], AF.Copy), reads=[mn], writes=[f'kts{hb}'])
            T.dma('gpsimd', f'kts{hb}', KT_d[:, :, r * 512:(r + 1) * 512].rearrange("q p t -> p q t"), KTs[hb][:],
                  reads=[f'kts{hb}'], writes=[f'KT_d{r}'])
            for bl in range(4):
                m = mm[mi[0] % 3]; mn = f'mm{mi[0] % 3}'; mi[0] += 1
                T.op('tensor', mmgrp([(m[:], HT[hb][:, kt, bl * 128:(bl + 1) * 128], WKV[:, kt, 512:1024], kt == 0, kt == 7)
                                      for kt in range(8)]), reads=['wkvf', f'ht{hb}'], writes=[mn])
                T.op('tensor', mmgrp([(smb[bl][:, 0:8], HT[hb][:, kt, bl * 128:(bl + 1) * 128], WKV[:, kt, 1024:1032], kt == 0, kt == 7)
                                      for kt in range(8)]), reads=['wkvf', f'ht{hb}'], writes=[f'smb{bl}'])
                T.op('scalar', ACT(VBs[hb][:, bl, :, 0::2, :], m[:].rearrange("t (p two d) -> t p two d", p=4, two=2),
                                   AF.Copy), reads=[mn], writes=[f'vbs{hb}'])
                ft = ftt[bl]
                T.op('vector', TT(ft[:], smb[bl][:, 0:8], bfb[:], ALU.add), reads=[f'smb{bl}', 'bfb'], writes=[f'ft{bl}'])
                T.op('scalar', ACT(ft[:], ft[:], AF.Exp, scale=-1.0), reads=[f'ft{bl}'], writes=[f'ft{bl}'])
                T.op('scalar', ACT(ft[:], ft[:], AF.Ln, bias=one_t[:]), reads=[f'ft{bl}', 'one'], writes=[f'ft{bl}'])
            for q in range(4):
                T.dma('gpsimd', f'vbs{hb}_{q}', V_d[q, :, r * 4:(r + 1) * 4, :],
                      VBs[hb][:, :, q, :, :].rearrange("t b three d -> t b (three d)"), reads=[f'vbs{hb}'],
                      writes=[f'V_d{r}_{q}'])
            for bl in range(4):
                g = r * 4 + bl
                cs = smb[bl][:, 8:16]
                lst = [(cs, Umat[:], ftt[bl][:], True, g == 0)]
                if g > 0:
                    lst.append((cs, Sel127[:], C_all[:, g - 1, :], False, True))
                T.op('tensor', mmgrp(lst), reads=[f'ft{bl}', 'U', 'sel127', 'Call'], writes=[f'smb{bl}'])
                T.op('vector', CP(C_all[:, g, :], cs), reads=[f'smb{bl}'], writes=['Call'])

        lnA(0)
        for r in range(16):
            if r + 1 < 16:
                lnA(r + 1)
            kvfA(r)
        T.barrier()

    with ExitStack() as es:
        rselb = sb(es, [128, 8, 64], F32)
        csel = sb(es, [128, 8, 8], F32)
        tmp3 = sb(es, [128, 8, 64], F32)
        pr = ps(es, [128, 512], F32)
        T.dma('sync', 'rselb', rselb[:], rsel_d[0:1, :].broadcast_to([128, 512]).rearrange("p (i b) -> p i b", i=8),
              writes=['rselb'])
        for i in range(8):
            T.op('vector', TT(tmp3[:], C_all[:].rearrange("p b h -> p h b"),
                              rselb[:, i, :].unsqueeze(1).broadcast_to([128, 8, 64]), ALU.mult),
                 reads=['Call', 'rselb'], writes=['tmp3'])
            T.op('vector', RED(csel[:, i, :], tmp3[:], ALU.add), reads=['tmp3'], writes=['csel'])
        T.op('tensor', mmgrp([(pr[:, 0:64], Sel127[:], csel[:].rearrange("p i h -> p (i h)"), True, True)]),
             reads=['csel', 'sel127'], writes=['pr'])
        T.op('vector', CP(RC[:].rearrange("p i h -> p (i h)"), pr[:, 0:64]), reads=['pr'], writes=['RC'])
        T.barrier()

    if "B" in DEBUG["phases"]:
      with ExitStack() as es:
        WQ = sb(es, [128, 8, 1536], BF16, "wq")
        WO = sb(es, [128, 8, 1024], BF16, "wo")
        MK = sb(es, [128, 16, 512], BF16, "mk")
        WsT = sb(es, [128, 8, 128], BF16, "wst")
        Bb = sb(es, [128, 4, 128], F32)
        gsg = sb(es, [128, 512], F32)
        gos = sb(es, [128, 4], F32)
        gof = sb(es, [128, 4], F32)
        XA = [sb(es, [128, 1024], F32) for _ in range(2)]
        XR = [sb(es, [128, 1024], F32) for _ in range(2)]
        XN = [sb(es, [128, 1024], BF16) for _ in range(2)]
        wstage = XA
        wsf = XR[0][:].rearrange("p (h t) -> p h t", h=8)
        junk = None
        ssq = sb(es, [128, 64], F32)
        T.op('vector', MEMSET(ssq[:], 0.0), writes=['ssqall'])
        rs = [sb(es, [128, 1], F32) for _ in range(2)]
        tmpf = sb(es, [128, 8, 128], F32)
        HT = sb(es, [128, 8, 512], BF16)
        QT = sb(es, [128, 4, 2, 512], BF16)
        T.op('gpsimd', MEMSET(QT[:], 0.0), writes=['qt'])
        UT = sb(es, [128, 4, 512], BF16)
        VG = sb(es, [128, 4, 512], F32)
        VG2 = sb(es, [128, 4, 512], F32)
        v8 = sb(es, [128, 32], F32)
        VN = sb(es, [128, 4, 4, 3, 64], BF16)
        YS = sb(es, [128, 4, 512], BF16)
        YF = sb(es, [128, 4, 512], BF16)
        SQ = sb(es, [128, 4, 512], BF16)
        zt = sb(es, [128, 4, 128], F32)
        KB = [sb(es, [128, 2048], BF16) for _ in range(3)]
        VB = [sb(es, [128, 16, 192], BF16) for _ in range(3)]
        PT = [sb(es, [128, 512], BF16) for _ in range(3)]
        BI = sb(es, [128, 2, 2, 64], F32)
        rc = sb(es, [128, 512], F32)
        rsS = sb(es, [128, 4], F32)
        rsF = sb(es, [128, 4], F32)
        t1 = [sb(es, [128, 512], F32) for _ in range(2)]
        X1 = XR
        tp = ps(es, [128, 8, 128], BF16)
        st = [ps(es, [128, 512], F32) for _ in range(4)]
        OT = [ps(es, [128, 512], F32) for _ in range(2)]
        sm = ps(es, [128, 512], F32)

        T.dma('gpsimd', 'wq_u', WQ[:, :, 0:1024], win_v[:, :, 0:1024], writes=['wq_uv'])
        T.dma('gpsimd', 'wq_q', WQ[:, :, 1024:1536], win_v[:, :, 1024:1536], writes=['wq_q'])
        T.dma('gpsimd', 'mk', MK[:], masks_d.rearrange("m p q -> p m q"), writes=['mk'])
        T.dma('sync', 'xr0', wsf, wsT_d, writes=['xr0'])
        T.dma('sync', 'gsg', gsg[:], gsgu_d[0:1, :].broadcast_to([128, 512]), writes=['gsg'])
        T.dma('sync', 'gos', gos[:], gos_d, writes=['gos'])
        T.dma('sync', 'gof', gof[:], gof_d, writes=['gof'])
        for h in range(8):
            T.dma('sync', 'Bb', Bb[(h % 2) * 64:(h % 2) * 64 + 64, h // 2, :], bsp_d[h:h + 1, :].broadcast_to([64, 128]),
                  writes=[f'Bb{h}'])
        T.op('gpsimd', ASEL(wsf, wsf, [[0, 8], [1, 128]], ALU.is_ge, 0.0, 0, -1), reads=['xr0'], writes=['xr0'])
        T.op('vector', CP(WsT[:], wsf), reads=['xr0'], writes=['wst'])
        T.op('gpsimd', MEMSET(VN[:, :, :, 1, :], 0.0), writes=['vn'])
        wout_v = wout_d.rearrange("(kt p) n -> p kt n", p=128)
        for kt in range(8):
            wsb = kt % 2
            T.dma('sync', f'xa{wsb}', wstage[wsb][:], wout_v[:, kt, :], writes=[f'xa{wsb}'])
            gg = gos[:, kt:kt + 1] if kt < 4 else gof[:, kt - 4:kt - 3]
            T.op('vector', TS(WO[:, kt, :], wstage[wsb][:], gg, None, ALU.mult), reads=[f'xa{wsb}', 'gos', 'gof'],
                 writes=['wo'])
        Bbn = [f'Bb{h}' for h in range(8)]

        sti = [0]
        pti = [0]
        kvi = [0]

        def next_st():
            k = sti[0] % 4
            sti[0] += 1
            return st[k], f'st{k}'

        bglob = 0
        for i in range(8):
            nk = NK(i)
            par = i % 2
            for bl in range(4):
                g = i * 4 + bl
                ln1_block(xown[g * 128:(g + 1) * 128, :], XA, XN, junk, ssq, rs, tp, tmpf,
                          HT[:, :, bl * 128:(bl + 1) * 128], g, 'ht')
            for p in range(4):
                m, mn = next_st()
                T.op('tensor', mmgrp([(m[:], WQ[:, kt, 1024 + p * 128:1024 + (p + 1) * 128], HT[:, kt, :], kt == 0, kt == 7)
                                      for kt in range(8)]), reads=['wq_q', 'ht'], writes=[mn])
                T.op('vector', TS(QT[0:64, p, 0, :], m[0:64, :], 0.125, None, ALU.mult), reads=[mn], writes=['qt'])
                T.op('vector', TS(QT[64:128, p, 1, :], m[64:128, :], 0.125, None, ALU.mult), reads=[mn], writes=['qt'])
            for p in range(4):
                m, mn = next_st()
                T.op('tensor', mmgrp([(m[:], WQ[:, kt, p * 128:(p + 1) * 128], HT[:, kt, :], kt == 0, kt == 7)
                                      for kt in range(8)]), reads=['wq_uv', 'ht'], writes=[mn])
                T.op('scalar', ACT(UT[:, p, :], m[:], AF.Gelu_apprx_tanh), reads=[mn], writes=['ut'])
            for bl in range(4):
                m, mn = next_st()
                T.op('tensor', mmgrp([(m[:], HT[:, kt, bl * 128:(bl + 1) * 128], WQ[:, kt, 512:1024], kt == 0, kt == 7)
                                      for kt in range(8)]), reads=['wq_uv', 'ht'], writes=[mn])
                T.op('scalar', ACT(VG[:, bl, :], m[:], AF.Gelu_apprx_tanh), reads=[mn], writes=['vg'])
            T.op('vector', TT(VG2[:], VG[:], VG[:], ALU.mult), reads=['vg'], writes=['vg2'])
            T.op('vector', RED(v8[:], VG2[:].rearrange("t b (h d) -> t (b h) d", h=8), ALU.add), reads=['vg2'], writes=['v8'])
            T.op('scalar', ACT(v8[:], v8[:], AF.Ln, scale=1.0 / 64, bias=eps_t[:]), reads=['v8', 'eps'], writes=['v8'])
            T.op('scalar', ACT(v8[:], v8[:], AF.Exp, scale=-0.5), reads=['v8'], writes=['v8'])
            T.op('vector', TT(VG2[:].rearrange("t b (h d) -> t (b h) d", h=8), VG[:].rearrange("t b (h d) -> t (b h) d", h=8),
                              v8[:].unsqueeze(2).broadcast_to([128, 32, 64]), ALU.mult), reads=['vg', 'v8'], writes=['vg2'])
            for bl in range(4):
                T.op('vector', TT(VN[:, bl, :, 0::2, :], VG2[:, bl, :].rearrange("t (p two d) -> t p two d", p=4, two=2),
                                  gsg[:].rearrange("t (p two d) -> t p two d", p=4, two=2), ALU.mult),
                     reads=['vg2', 'gsg'], writes=['vn'])
            if i == 0:
                dump("HT", HT[:], [128, 8, 512], BF16, ['ht'])
            for bl in range(4):
                lst = []
                for p in range(4):
                    vnp = VN[:, bl, p, :, :].rearrange("t three d -> t (three d)")
                    lst.append((sm[:, p * 128:(p + 1) * 128], vnp[:, 0:128], WsT[:, 2 * p, :], True, False))
                    lst.append((sm[:, p * 128:(p + 1) * 128], vnp[:, 64:192], WsT[:, 2 * p + 1, :], False, True))
                T.op('tensor', mmgrp(lst), reads=['vn', 'wst'], writes=['sm'])
                T.op('vector', TT(zt[:], sm[:].rearrange("d (p t) -> d p t", p=4), Bb[:], ALU.add), reads=['sm'] + Bbn,
                     writes=['zt'])
                T.op('vector', TT(YS[:, :, bl * 128:(bl + 1) * 128], zt[:], UT[:, :, bl * 128:(bl + 1) * 128], ALU.mult),
                     reads=['zt', 'ut'], writes=['ys'])
            T.op('vector', TT(SQ[:], YS[:], YS[:], ALU.mult), reads=['ys'], writes=['sq'])
            T.op('tensor', mmgrp([(sm[:, bl:bl + 1], SQ[:, p, bl * 128:(bl + 1) * 128], ones_bf[:], p == 0, p == 3)
                                  for bl in range(4) for p in range(4)]), reads=['sq', 'onesbf'], writes=['sm'])
            T.op('scalar', ACT(rsS[:], sm[:, 0:4], AF.Ln, scale=1.0 / 512, bias=eps_t[:]), reads=['sm', 'eps'], writes=['rsS'])
            T.op('scalar', ACT(rsS[:], rsS[:], AF.Exp, scale=-0.5), reads=['rsS'], writes=['rsS'])
            for p in range(4):
                pq = p % 2
                for hh in range(2):
                    h = 2 * p + hh
                    T.op('vector', TS(BI[:, pq, hh, :], C_all[:, :, h], RC[:, i, h:h + 1], None, ALU.subtract),
                         reads=['Call', 'RC'], writes=[f'bi{pq}{hh}'])
                units = [(jb, hh) for jb in range(nk) for hh in range(2)]
                nchunks = (nk + 15) // 16
                if p == 0:
                    seq = [(pp_, c_) for pp_ in range(4) for c_ in range(nchunks)]
                    chunk_buf = {}
                    issued = [0]

                    def ensure(upto):
                        while issued[0] <= upto and issued[0] < len(seq):
                            pp_, c = seq[issued[0]]
                            kb = kvi[0] % 3
                            kvi[0] += 1
                            n = min(16, nk - c * 16)
                            T.dma('sync', f'kb{kb}', KB[kb][:, 0:n * 128], KT_d[pp_, :, c * 2048:c * 2048 + n * 128],
                                  reads=[f'KT_d{r}' for r in range(c * 4, (c * 16 + n + 3) // 4)], writes=[f'kb{kb}'])
                            T.dma('sync', f'vb{kb}', VB[kb][:, 0:n, :], V_d[pp_, :, c * 16:c * 16 + n, :],
                                  reads=[f'V_d{r}_{pp_}' for r in range(c * 4, (c * 16 + n + 3) // 4)], writes=[f'vb{kb}'])
                            chunk_buf[(pp_, c)] = kb
                            issued[0] += 1

                    ensure(2)
                ubank = {}

                def emit_qk(n):
                    jb, hh = units[n]
                    c = jb // 16
                    kb = chunk_buf[(p, c)]
                    jl = jb - c * 16
                    m, mn = next_st()
                    ubank[n] = (m, mn)
                    r0 = hh * 64
                    lst = [(m[:], KB[kb][:, jl * 128:(jl + 1) * 128], QT[:, p, hh, :], True, jb < nk - 8)]
                    rd = [f'kb{kb}', 'qt']
                    if jb >= nk - 8:
                        lst.append((m[:], ident_b[:], MK[:, par * 8 + (jb - (nk - 8)), :], False, True))
                        rd += ['identb', 'mk']
                    T.op('tensor', mmgrp(lst), reads=rd, writes=[mn])

                def emit_rest(n):
                    jb, hh = units[n]
                    c = jb // 16
                    kb = chunk_buf[(p, c)]
                    jl = jb - c * 16
                    m, mn = ubank.pop(n)
                    k3 = pti[0] % 3
                    pti[0] += 1
                    T.op('scalar', ACT(PT[k3][:], m[:], AF.Exp, bias=BI[:, pq, hh, jb:jb + 1]), reads=[mn, f'bi{pq}{hh}'],
                         writes=[f'pt{k3}'])
                    vb = VB[kb][:, jl, hh * 64:hh * 64 + 128]
                    T.op('tensor', mmgrp([(OT[hh][:], vb, PT[k3][:], jb == 0, jb == nk - 1)]),
                         reads=[f'vb{kb}'] + ([] if 'nodep' in PROBE else [f'pt{k3}']), writes=[f'ot{hh}'])

                LA = 3
                for n in range(min(LA, len(units))):
                    emit_qk(n)
                for n in range(len(units)):
                    emit_rest(n)
                    jbd, hhd = units[n]
                    if hhd == 1 and (jbd % 16 == 15 or jbd == nk - 1):
                        ensure(p * nchunks + jbd // 16 + 3)
                    nn = n + LA
                    if nn < len(units):
                        emit_qk(nn)
                T.op('vector', RECIP(rc[0:64, :], OT[0][64:128, :]), reads=['ot0'], writes=['rca'])
                T.op('vector', RECIP(rc[64:128, :], OT[1][0:64, :]), reads=['ot1'], writes=['rcb'])
                T.op('vector', TT(YF[0:64, p, :], OT[0][0:64, :], rc[0:64, :], ALU.mult), reads=['ot0', 'rca'], writes=['yfa'])
                T.op('vector', TT(YF[64:128, p, :], OT[1][64:128, :], rc[64:128, :], ALU.mult), reads=['ot1', 'rcb'],
                     writes=['yfb'])
            T.op('vector', TT(SQ[:], YF[:], YF[:], ALU.mult), reads=['yfa', 'yfb'], writes=['sq'])
            T.op('tensor', mmgrp([(sm[:, 4 + bl:5 + bl], SQ[:, p, bl * 128:(bl + 1) * 128], ones_bf[:], p == 0, p == 3)
                                  for bl in range(4) for p in range(4)]), reads=['sq', 'onesbf'], writes=['sm'])
            T.op('scalar', ACT(rsF[:], sm[:, 4:8], AF.Ln, scale=1.0 / 512, bias=eps_t[:]), reads=['sm', 'eps'], writes=['rsF'])
            T.op('scalar', ACT(rsF[:], rsF[:], AF.Exp, scale=-0.5), reads=['rsF'], writes=['rsF'])
            if i == 0:
                dump("YS", YS[:], [128, 4, 512], BF16, ['ys'])
                dump("YF", YF[:], [128, 4, 512], BF16, ['yfa', 'yfb'])
                dump("rsS", rsS[:], [128, 4], F32, ['rsS'])
                dump("rsF", rsF[:], [128, 4], F32, ['rsF'])
            for bl in range(4):
                g = i * 4 + bl
                xb = g % 2
                T.dma('sync', f'xr{xb}', XR[xb][:], xown[g * 128:(g + 1) * 128, :], writes=[f'xr{xb}'])
                for hf in range(2):
                    ms, msn = next_st()
                    T.op('tensor', mmgrp([(ms[:], YS[:, p, bl * 128:(bl + 1) * 128], WO[:, p, hf * 512:(hf + 1) * 512], p == 0, p == 3)
                                          for p in range(4)]), reads=['ys', 'wo'], writes=[msn])
                    mf, mfn = next_st()
                    T.op('tensor', mmgrp([(mf[:], YF[:, p, bl * 128:(bl + 1) * 128], WO[:, 4 + p, hf * 512:(hf + 1) * 512], p == 0, p == 3)
                                          for p in range(4)]), reads=['yfa', 'yfb', 'wo'], writes=[mfn])
                    tb = hf
                    T.op('vector', TS(t1[tb][:], ms[:], rsS[:, bl:bl + 1], None, ALU.mult), reads=[msn, 'rsS'], writes=[f't1{tb}'])
                    T.op('vector', STT(t1[tb][:], mf[:], rsF[:, bl:bl + 1], t1[tb][:], ALU.mult, ALU.add),
                         reads=[mfn, 'rsF', f't1{tb}'], writes=[f't1{tb}'])
                    T.op('vector', TT(t1[tb][:], t1[tb][:], GT1[:, hf * 512:(hf + 1) * 512], ALU.mult),
                         reads=[f't1{tb}', 'GT10', 'GT11'], writes=[f't1{tb}'])
                    T.op('vector', TT(X1[xb][:, hf * 512:(hf + 1) * 512], t1[tb][:], XR[xb][:, hf * 512:(hf + 1) * 512], ALU.add),
                         reads=[f't1{tb}', f'xr{xb}'], writes=[f'xr{xb}'])
                T.dma('gpsimd', f'x1o{xb}', X1_d[g * 128:(g + 1) * 128, :], X1[xb][:], reads=[f'xr{xb}'], writes=[f'X1_d{g}'])
        T.barrier()

    CAP = 768
    NT0 = CAP // 128
    if "C" in DEBUG["phases"]:
      csem = {}
      with ExitStack() as es0:
        IDX = sb(es0, [128, 32, 2], U32, "IDX")
        W12 = sb(es0, [128, 32, 2], F32, "W12")
        gfin = sb(es0, [128, 1024], F32, "gfin")
        T.dma('sync', 'gfin', gfin[:], gfin_d[0:1, :].broadcast_to([128, 1024]), writes=['gfin'])
        with ExitStack() as es:
            X1t = [sb(es, [128, 1024], F32) for _ in range(2)]
            xn2_2 = [sb(es, [128, 1024], F32) for _ in range(2)]
            junk_2 = [sb(es, [128, 1024], BF16) for _ in range(2)]
            H2f_2 = [sb(es, [128, 8, 128], F32) for _ in range(2)]
            tmpg_2 = [sb(es, [128, 8, 128], F32) for _ in range(2)]
            H2row = [sb(es, [128, 1024], BF16) for _ in range(2)]
            hrt_2 = [sb(es, [128, 1024], F32) for _ in range(2)]
            A2R = sb(es, [128, 1024], F32)
            B2R = sb(es, [128, 1024], F32)
            dg = sb(es, [128, 128], F32)
            onesf = sb(es, [128, 128], F32)
            WR = sb(es, [128, 8, 20], F32)
            brb = sb(es, [128, 20], F32)
            EB = sb(es, [128, 16], F32)
            Cn = sb(es, [128, 32, 16], F32)
            L_2 = [sb(es, [128, 20], F32) for _ in range(2)]
            s1_2 = [sb(es, [128, 16], F32) for _ in range(2)]
            s2_2 = [sb(es, [128, 16], F32) for _ in range(2)]
            E1_2 = [sb(es, [128, 16], F32) for _ in range(2)]
            E2_2 = [sb(es, [128, 16], F32) for _ in range(2)]
            Mx_2 = [sb(es, [128, 16], F32) for _ in range(2)]
            oh_2 = [sb(es, [128, 4], F32) for _ in range(2)]
            mk1_2 = [sb(es, [128, 4], F32) for _ in range(2)]
            mk2_2 = [sb(es, [128, 4], F32) for _ in range(2)]
            les_2 = [sb(es, [128, 4], F32) for _ in range(2)]
            le2_2 = [sb(es, [128, 4], F32) for _ in range(2)]
            sc_2 = [sb(es, [128, 12], F32) for _ in range(2)]
            idf_2 = [sb(es, [128, 2], F32) for _ in range(2)]
            CNTi = sb(es, [128, 16], I32)
            ssq = sb(es, [128, 32], F32)
            T.op('vector', MEMSET(ssq[:], 0.0), writes=['ssqall'])
            rs_2 = [sb(es, [128, 1], F32) for _ in range(2)]
            tpc = ps(es, [128, 8, 128], F32)
            pm = ps(es, [128, 512], F32)
            pmr = [ps(es, [128, 512], F32) for _ in range(2)]

            T.dma('sync', 'WR', WR[:], wr_d.rearrange("(kt p) n -> p kt n", p=128), writes=['WR'])
            T.dma('sync', 'brb', brb[:], br_d[0:1, :].broadcast_to([128, 20]), writes=['brb'])
            T.dma('sync', 'EB', EB[:], eb_d[0:1, :].broadcast_to([128, 16]), writes=['EB'])
            T.op('gpsimd', MEMSET(onesf[:], 1.0), writes=['onesf'])
            for (colt, rowt, nm) in ((A2, A2R, 'A2R'), (B2, B2R, 'B2R')):
                for kt in range(8):
                    T.op('vector', TS(dg[:], ident_f[:], colt[:, kt:kt + 1], None, ALU.mult), reads=['identf', 'A2', 'B2', 'dg'],
                         writes=['dg'])
                    T.op('tensor', mmgrp([(pm[:, 0:128], onesf[:], dg[:], True, True)]), reads=['dg', 'onesf'], writes=['pm'])
                    T.op('vector', CP(rowt[:, kt * 128:(kt + 1) * 128], pm[:, 0:128]), reads=['pm'], writes=[nm])
            def c1_stage1(g):
                    xb = g % 2
                    xn2 = xn2_2[xb]
                    junk = junk_2[xb]
                    H2f = H2f_2[xb]
                    tmpg = tmpg_2[xb]
                    hrt = hrt_2[xb]
                    L = L_2[xb]
                    s1 = s1_2[xb]
                    s2 = s2_2[xb]
                    E1 = E1_2[xb]
                    E2 = E2_2[xb]
                    Mx = Mx_2[xb]
                    oh = oh_2[xb]
                    mk1 = mk1_2[xb]
                    mk2 = mk2_2[xb]
                    les = les_2[xb]
                    le2 = le2_2[xb]
                    sc = sc_2[xb]
                    idf = idf_2[xb]
                    rs = rs_2[xb]
                    T.dma('sync', f'x1t{xb}', X1t[xb][:], X1_d[g * 128:(g + 1) * 128, :], reads=[f'X1_d{g}'], writes=[f'x1t{xb}'])
                    T.op('scalar', ACT(junk[:], X1t[xb][:], AF.Square, accum_out=ssq[:, g:g + 1]), reads=[f'x1t{xb}', 'ssqall'],
                         writes=[f'junk{xb}', f'ssq_{g}'])
                    T.op('scalar', ACT(rs[:], ssq[:, g:g + 1], AF.Ln, scale=1.0 / 1024, bias=eps_t[:]), reads=[f'ssq_{g}', 'eps'], writes=[f'rs{xb}'])
                    T.op('scalar', ACT(rs[:], rs[:], AF.Exp, scale=-0.5), reads=[f'rs{xb}'], writes=[f'rs{xb}'])
                    T.op('vector', TS(xn2[:], X1t[xb][:], rs[:, 0:1], None, ALU.mult), reads=[f'x1t{xb}', f'rs{xb}'], writes=[f'xn2{xb}'])
                    T.op('gpsimd', TT(hrt[:], xn2[:], A2R[:], ALU.mult), reads=[f'xn2{xb}', 'A2R'], writes=[f'hrt{xb}'])
                    T.op('gpsimd', TT(H2row[xb][:], hrt[:], B2R[:], ALU.add), reads=[f'hrt{xb}', 'B2R'], writes=[f'h2row{xb}'])
                    T.op('tensor', tpgrp([(tpc[:, kt, :], xn2[:, kt * 128:(kt + 1) * 128], ident_f[:]) for kt in range(8)]),
                         reads=[f'xn2{xb}', 'identf'], writes=['tpc'])
                    for kt in range(8):
                        T.op('scalar', ACT(H2f[:, kt, :], tpc[:, kt, :], AF.Identity, scale=A2[:, kt:kt + 1], bias=B2[:, kt:kt + 1]),
                             reads=['tpc', 'A2', 'B2'], writes=[f'h2f{xb}'])
                    T.op('tensor', mmgrp([(pmr[xb][:, 0:20], H2f[:, kt, :], WR[:, kt, :], kt == 0, kt == 7) for kt in range(8)]),
                         reads=[f'h2f{xb}', 'WR'], writes=[f'pmr{xb}'])

            def c1_stage2(g):
                    xb = g % 2
                    xn2 = xn2_2[xb]
                    junk = junk_2[xb]
                    H2f = H2f_2[xb]
                    tmpg = tmpg_2[xb]
                    hrt = hrt_2[xb]
                    L = L_2[xb]
                    s1 = s1_2[xb]
                    s2 = s2_2[xb]
                    E1 = E1_2[xb]
                    E2 = E2_2[xb]
                    Mx = Mx_2[xb]
                    oh = oh_2[xb]
                    mk1 = mk1_2[xb]
                    mk2 = mk2_2[xb]
                    les = les_2[xb]
                    le2 = le2_2[xb]
                    sc = sc_2[xb]
                    idf = idf_2[xb]
                    rs = rs_2[xb]
                    T.op('vector', TT(L[:], pmr[xb][:, 0:20], brb[:], ALU.add), reads=[f'pmr{xb}', 'brb'], writes=[f'L{xb}'])
                    rr = [f'L{xb}']
                    V = lambda fn: T.op('vector', fn, reads=rr, writes=rr)
                    S = lambda fn: T.op('scalar', fn, reads=rr, writes=rr)
                    lg = L[:, 0:4]
                    le = L[:, 4:20].rearrange("t (g e) -> t g e", g=4)
                    V(RMAX(sc[:, 0:1], lg))
                    V(TS(oh[:], lg, sc[:, 0:1], None, ALU.is_equal))
                    V(TS(sc[:, 1:2], sc[:, 0:1], -1.0, None, ALU.mult))
                    V(MEMSET(sc[:, 2:3], 0.0))
                    S(ACT(s1[:, 0:4], lg, AF.Exp, bias=sc[:, 1:2], accum_out=sc[:, 2:3]))
                    V(RECIP(sc[:, 3:4], sc[:, 2:3]))
                    V(TT(s2[:].rearrange("t (g e) -> t g e", g=4), le, oh[:].unsqueeze(2).broadcast_to([128, 4, 4]), ALU.mult))
                    V(RED(les[:], s2[:].rearrange("t (g e) -> t e g", g=4), ALU.add))
                    V(RMAX(sc[:, 4:5], les[:]))
                    V(TS(mk1[:], les[:], sc[:, 4:5], None, ALU.is_equal))
                    V(STT(le2[:], mk1[:], NEG, les[:], ALU.mult, ALU.add))
                    V(RMAX(sc[:, 5:6], le2[:]))
                    V(TS(mk2[:], le2[:], sc[:, 5:6], None, ALU.is_equal))
                    V(TT(sc[:, 6:7], sc[:, 5:6], sc[:, 4:5], ALU.subtract))
                    S(ACT(sc[:, 7:8], sc[:, 6:7], AF.Exp))
                    V(TS(sc[:, 8:9], sc[:, 7:8], 1.0, None, ALU.add))
                    V(RECIP(sc[:, 8:9], sc[:, 8:9]))
                    V(TT(W12[:, g, 0:1], sc[:, 8:9], sc[:, 3:4], ALU.mult))
                    V(TT(W12[:, g, 1:2], sc[:, 7:8], W12[:, g, 0:1], ALU.mult))
                    V(TT(E1[:].rearrange("t (g e) -> t g e", g=4), oh[:].unsqueeze(2).broadcast_to([128, 4, 4]),
                         mk1[:].unsqueeze(1).broadcast_to([128, 4, 4]), ALU.mult))
                    V(TT(E2[:].rearrange("t (g e) -> t g e", g=4), oh[:].unsqueeze(2).broadcast_to([128, 4, 4]),
                         mk2[:].unsqueeze(1).broadcast_to([128, 4, 4]), ALU.mult))
                    V(TT(Mx[:], E1[:], E2[:], ALU.add))
                    lst = [(pm[:, 32:48], Umat[:], Mx[:], True, g == 0)]
                    if g > 0:
                        lst.append((pm[:, 32:48], Sel127[:], Cn[:, g - 1, :], False, True))
                    T.op('tensor', mmgrp(lst), reads=[f'L{xb}', 'U', 'sel127', 'Cn'], writes=['pm'])
                    T.op('vector', CP(Cn[:, g, :], pm[:, 32:48]), reads=['pm'], writes=['Cn'])
                    T.op('vector', TT(s1[:], Cn[:, g, :], EB[:], ALU.add), reads=['Cn', 'EB', f'L{xb}'], writes=[f'L{xb}'])
                    V(TT(s2[:], s1[:], E1[:], ALU.mult))
                    V(RED(idf[:, 0:1], s2[:], ALU.add))
                    V(TT(s2[:], s1[:], E2[:], ALU.mult))
                    V(RED(idf[:, 1:2], s2[:], ALU.add))
                    T.op('vector', CP(IDX[:, g, :], idf[:]), reads=[f'L{xb}'], writes=[f'idx{g}'])
                    for k in range(2):
                        T.idma(f'sc{xb}{k}', Xs_d, bass.IndirectOffsetOnAxis(ap=IDX[:, g, k:k + 1], axis=0), H2row[xb][:], None,
                               reads=[f'idx{g}', f'h2row{xb}'], writes=[f'Xs{g}_{k}'])

            c1_stage1(0)
            for g in range(32):
                if g + 1 < 32:
                    c1_stage1(g + 1)
                c1_stage2(g)
            T.op('tensor', mmgrp([(pm[:, 64:80], Sel127[:], Cn[:, 31, :], True, True)]), reads=['Cn', 'sel127'], writes=['pm'])
            T.op('vector', CP(CNTi[:], pm[:, 64:80]), reads=['pm'], writes=['cnti'])
            T.dma('sync', 'cntd', CNT_d, CNTi[0:1, :], reads=['cnti'], writes=['CNT_d'])
            dump("IDX", IDX[:].rearrange("p g k -> p (g k)"), [128, 64], U32, [f'idx{g}' for g in range(32)])
            dump("W12", W12[:].rearrange("p g k -> p (g k)"), [128, 64], F32, ['L'])
            dump("Cn", Cn[:].rearrange("p g e -> p (g e)"), [128, 512], F32, ['Cn'])
            T.barrier()

        with ExitStack() as es:
            WG = [sb(es, [128, 8, 512], BF16) for _ in range(2)]
            WU = [sb(es, [128, 8, 512], BF16) for _ in range(2)]
            WD = [sb(es, [128, 4, 1024], BF16) for _ in range(2)]
            XT = [sb(es, [128, 1024], BF16) for _ in range(3)]
            XsT = [sb(es, [128, 8, 512], BF16) for _ in range(2)]
            AT = [sb(es, [128, 4, 512], BF16) for _ in range(2)]
            sg = [sb(es, [128, 512], F32) for _ in range(2)]
            YT = [sb(es, [128, 1024], F32) for _ in range(2)]
            tpb = ps(es, [128, 8, 128], BF16)
            gp = [ps(es, [128, 512], F32) for _ in range(2)]
            up = [ps(es, [128, 512], F32) for _ in range(2)]
            yp = [ps(es, [128, 512], F32) for _ in range(2)]
            wg_v = wg_d.rearrange("e (kt p) n -> e p kt n", p=128)
            wu_v = wu_d.rearrange("e (kt p) n -> e p kt n", p=128)
            wd_v = wd_d.rearrange("e (kt p) n -> e p kt n", p=128)
            gi = [0]
            yi = [0]
            xi = [0]
            yti = [0]
            gri = [0]

            def load_w(ex):
                wb = ex % 2
                T.dma('gpsimd', f'wg{wb}', WG[wb][:], wg_v[ex], writes=[f'wg{wb}'])
                T.dma('gpsimd', f'wu{wb}', WU[wb][:], wu_v[ex], writes=[f'wu{wb}'])
                T.dma('gpsimd', f'wd{wb}', WD[wb][:], wd_v[ex], writes=[f'wd{wb}'])

            def emit_group(ex, grp):
                wb = ex % 2
                ab = gri[0] % 2
                gri[0] += 1
                base = ex * 4096 + grp * 512
                for tl in range(4):
                    k3 = xi[0] % 3
                    xi[0] += 1
                    r0 = base + tl * 128
                    T.dma('sync', f'xt{k3}p{ab}', XT[k3][:], Xs_d[r0:r0 + 128, :], writes=[f'xt{k3}'])
                    T.op('tensor', tpgrp([(tpb[:, kt, :], XT[k3][:, kt * 128:(kt + 1) * 128], ident_b[:]) for kt in range(8)]),
                         reads=[f'xt{k3}', 'identb'], writes=['tpb'])
                    T.op('vector', CP(XsT[ab][:, :, tl * 128:(tl + 1) * 128], tpb[:]), reads=['tpb'], writes=[f'xst{ab}'])
                for ht in range(4):
                    k2 = gi[0] % 2
                    gi[0] += 1
                    T.op('tensor', mmgrp([(gp[k2][:], WG[wb][:, kt, ht * 128:(ht + 1) * 128], XsT[ab][:, kt, :], kt == 0, kt == 7)
                                          for kt in range(8)]), reads=[f'wg{wb}', f'xst{ab}'], writes=[f'gp{k2}'])
                    T.op('tensor', mmgrp([(up[k2][:], WU[wb][:, kt, ht * 128:(ht + 1) * 128], XsT[ab][:, kt, :], kt == 0, kt == 7)
                                          for kt in range(8)]), reads=[f'wu{wb}', f'xst{ab}'], writes=[f'up{k2}'])
                    T.op('scalar', ACT(sg[k2][:], gp[k2][:], AF.Silu), reads=[f'gp{k2}'], writes=[f'sg{k2}'])
                    T.op('vector', TT(AT[ab][:, ht, :], sg[k2][:], up[k2][:], ALU.mult), reads=[f'sg{k2}', f'up{k2}'],
                         writes=[f'at{ab}'])
                for tl in range(4):
                    yb = yti[0] % 2
                    yti[0] += 1
                    for nh in range(2):
                        k2 = yi[0] % 2
                        yi[0] += 1
                        T.op('tensor', mmgrp([(yp[k2][:], AT[ab][:, ht, tl * 128:(tl + 1) * 128], WD[wb][:, ht, nh * 512:(nh + 1) * 512],
                                               ht == 0, ht == 3) for ht in range(4)]), reads=[f'at{ab}', f'wd{wb}'], writes=[f'yp{k2}'])
                        T.op('scalar', ACT(YT[yb][:, nh * 512:(nh + 1) * 512], yp[k2][:], AF.Copy), reads=[f'yp{k2}'], writes=[f'yt{yb}'])
                    r0 = base + tl * 128
                    T.dma('sync', f'yt{yb}p{ab}', Ys_d[r0:r0 + 128, :], YT[yb][:], reads=[f'yt{yb}'], writes=[f'Ys{ex}_{grp}_{tl}'])

            load_w(0)
            for ex in range(16):
                if ex + 1 < 16:
                    load_w(ex + 1)
                emit_group(ex, 0)
                for grp in range(1, 8):
                    par = gri[0] % 2
                    drain = [f'xt{k}p{par}' for k in range(3)] + [f'yt{k}p{par}' for k in range(2)]
                    T.cond_begin(CNT_d[0:1, ex:ex + 1] if grp == 1 else None, grp * 512, drain)
                    emit_group(ex, grp)
                    T.cond_end()
            T.barrier()

        with ExitStack() as es:
            Y1 = [sb(es, [128, 1024], F32) for _ in range(2)]
            Y2 = [sb(es, [128, 1024], F32) for _ in range(2)]
            X1t = [sb(es, [128, 1024], F32) for _ in range(2)]
            junk = sb(es, [128, 1024], BF16)
            osb = [sb(es, [128, 1024], F32) for _ in range(2)]
            ssq = sb(es, [128, 32], F32)
            T.op('vector', MEMSET(ssq[:], 0.0), writes=['ssqall'])
            rs = [sb(es, [128, 1], F32) for _ in range(2)]
            def c3_loads(g):
                b = g % 2
                T.dma('sync', f'x1c{b}', X1t[b][:], X1_d[g * 128:(g + 1) * 128, :], writes=[f'x1c{b}'])
                T.idma(f'ga{b}', Y1[b][:], None, Ys_d, bass.IndirectOffsetOnAxis(ap=IDX[:, g, 0:1], axis=0), writes=[f'y1{b}'])
                T.idma(f'gb{b}', Y2[b][:], None, Ys_d, bass.IndirectOffsetOnAxis(ap=IDX[:, g, 1:2], axis=0), writes=[f'y2{b}'])

            c3_loads(0)
            for g in range(32):
                b = g % 2
                if g + 1 < 32:
                    c3_loads(g + 1)
                T.op('vector', TS(Y1[b][:], Y1[b][:], W12[:, g, 0:1], None, ALU.mult), reads=[f'y1{b}'], writes=[f'y1{b}'])
                T.op('vector', STT(Y1[b][:], Y2[b][:], W12[:, g, 1:2], Y1[b][:], ALU.mult, ALU.add), reads=[f'y1{b}', f'y2{b}'],
                     writes=[f'y1{b}'])
                T.op('vector', TT(Y1[b][:], Y1[b][:], GT2[:], ALU.mult), reads=[f'y1{b}'], writes=[f'y1{b}'])
                T.op('vector', TT(Y1[b][:], Y1[b][:], X1t[b][:], ALU.add), reads=[f'y1{b}', f'x1c{b}'], writes=[f'y1{b}'])
                T.op('scalar', ACT(junk[:], Y1[b][:], AF.Square, accum_out=ssq[:, g:g + 1]), reads=[f'y1{b}', 'ssqall'],
                     writes=['junk', f'ssq_{g}'])
                T.op('scalar', ACT(rs[b][:], ssq[:, g:g + 1], AF.Ln, scale=1.0 / 1024, bias=eps_t[:]), reads=[f'ssq_{g}', 'eps'], writes=[f'rs{b}'])
                T.op('scalar', ACT(rs[b][:], rs[b][:], AF.Exp, scale=-0.5), reads=[f'rs{b}'], writes=[f'rs{b}'])
                T.op('vector', STT(osb[b][:], Y1[b][:], rs[b][:, 0:1], gfin[:], ALU.mult, ALU.mult),
                     reads=[f'y1{b}', f'rs{b}', 'gfin'], writes=[f'osb{b}'])
                T.dma('sync', f'out{b}', out_d[g * 128:(g + 1) * 128, :], osb[b][:], reads=[f'osb{b}'], writes=[f'out{g}'])
            T.barrier()
    T.emit()
    top.close()
    return nc


_CACHE = {}


def _masks(j):
    m = np.zeros((2, 8, 128, 512), np.float32)
    ki = np.arange(128)[:, None]
    qq = np.arange(512)[None, :]
    for par in range(2):
        i = par
        run = OWN_RUNS[j][i]
        nk = NK(i)
        for r in range(8):
            jb = nk - 8 + r
            kpos = jb * 128 + ki
            qpos = run * 512 + qq
            m[par, r] = np.where(kpos <= qpos, 0.0, NEG)
    return m.reshape(16, 128, 512)


def _rsel(j):
    s = np.zeros((8, 64), np.float32)
    for i, run in enumerate(OWN_RUNS[j]):
        s[i, 4 * run + 1] = 1.0
    return s.reshape(1, 512)


def kernel(x, c, w_ada, b_ada, g_norm_mix, w_in, g_sgu, w_spatial, b_spatial, b_forget, g_out_sgu, g_out_fox, w_out,
           g_norm_ffn, w_router_group, b_router_group, w_router_expert, b_router_expert, w_gate, w_up, w_down, g_final):
    f = lambda a: np.ascontiguousarray(np.asarray(a, dtype=np.float32))
    x = f(x); c = f(c)
    if "nc" not in _CACHE:
        _CACHE["nc"] = build_program()
    nc = _CACHE["nc"]
    wrg = f(w_router_group)[0]; wre = f(w_router_expert)[0]
    brg = f(b_router_group)[0]; bre = f(b_router_expert)[0]
    wg_all = f(w_gate)[0]; wu_all = f(w_up)[0]; wd_all = f(w_down)[0]
    shared = {
        "w_ada": f(w_ada)[0], "b_ada": f(b_ada)[0][None, :], "g1T": f(f(g_norm_mix)[0].reshape(8, 128).T),
        "g2T": f(f(g_norm_ffn)[0].reshape(8, 128).T), "w_in": f(w_in)[0], "gsgu": f(g_sgu)[0][None, :],
        "wsT": f(f(w_spatial)[0].transpose(2, 0, 1)), "bsp": f(b_spatial)[0], "bfg": f(b_forget)[0][None, :],
        "gos": f(f(g_out_sgu)[0].reshape(4, 128).T), "gof": f(f(g_out_fox)[0].reshape(4, 128).T), "w_out": f(w_out)[0],
        "gfin": f(g_final)[None, :],
    }
    percore = {}
    for core in range(8):
        gmap = [(G + core % 4) % 4 for G in range(4)]
        emap = [(j + 2 * (core // 4)) % 4 for j in range(4)]
        eorder = [4 * gmap[G] + emap[j] for G in range(4) for j in range(4)]
        wr = np.concatenate([wrg[:, gmap]] + [wre[gmap[G]][:, emap] for G in range(4)], axis=1)
        br = np.concatenate([brg[gmap]] + [bre[gmap[G]][emap] for G in range(4)])[None, :]
        percore[core] = {"wr": f(wr), "br": f(br), "w_gate": f(wg_all[eorder]), "w_up": f(wu_all[eorder]),
                         "w_down": f(wd_all[eorder])}
    in_maps = []
    for core in range(8):
        b, j = core // 2, core % 2
        m = dict(shared)
        m.update(percore[core])
        m["xall"] = x[b]
        m["xown"] = f(np.concatenate([x[b, 512 * r:512 * (r + 1)] for r in OWN_RUNS[j]], axis=0))
        m["cT"] = f(c[b].reshape(8, 128).T)
        m["masks"] = _masks(j)
        m["rsel"] = _rsel(j)
        m["eb"] = (np.arange(16, dtype=np.float32) * 4096.0 - 1.0)[None, :]
        in_maps.append(m)
    res = run_bass_kernel_spmd(nc, in_maps, core_ids=list(range(8)))
    _CACHE["res"] = res
    out = np.empty((4, 8192, 1024), np.float32)
    for core in range(8):
        b, j = core // 2, core % 2
        o = res.results[core]["out"]
        for i, r in enumerate(OWN_RUNS[j]):
            out[b, 512 * r:512 * (r + 1)] = o[512 * i:512 * (i + 1)]
    return out
```

```python
import os
import numpy as np
from contextlib import ExitStack
import concourse.bass as bass
import concourse.mybir as mybir
from concourse.bass_utils import run_bass_kernel_spmd

F32 = mybir.dt.float32
BF16 = mybir.dt.bfloat16
U32 = mybir.dt.uint32
I32 = mybir.dt.int32
AF = mybir.ActivationFunctionType
ALU = mybir.AluOpType
AX = mybir.AxisListType
ENGS = ['sync', 'scalar', 'vector', 'gpsimd', 'tensor']
EPS = 1e-6
NEG = -1.0e30
OWN_RUNS = {0: [0, 3, 4, 7, 8, 11, 12, 15], 1: [1, 2, 5, 6, 9, 10, 13, 14]}
WARM = 18
PROBE = os.environ.get('MK_PROBE', '')
DEBUG = {"x1": False, "same": True, "phases": "ABC", "dump": []}


def NK(i):
    return 16 * (i // 2) + 8 + 8 * (i % 2)


class Tracker:
    def __init__(self, nc, same_engine_sync=False):
        self.nc = nc
        self.streams = {e: [] for e in ENGS}
        self.sems = {e: nc.alloc_semaphore("c_" + e) for e in ENGS}
        self.cnt = {e: 0 for e in ENGS}
        self.waited = {e: {} for e in ENGS}
        self.res = {}
        self.slots = {}
        self.same = same_engine_sync
        self.slot_q = {}

    def _deps(self, reads, writes):
        deps = []
        for r in reads:
            st = self.res.get(r)
            if st and st[0]:
                deps.append(st[0])
        for w in writes:
            st = self.res.get(w)
            if st:
                if st[0]:
                    deps.append(st[0])
                deps.extend(st[1])
        return deps

    def _update(self, reads, writes, tag):
        for r in reads:
            st = self.res.setdefault(r, [None, []])
            st[1].append(tag)
        for w in writes:
            self.res[w] = [tag, []]

    def _emit_waits(self, eng, deps, skip_same):
        mx = {}
        for key, val in deps:
            mx[key] = max(mx.get(key, 0), val)
        for key, val in mx.items():
            if key == eng and skip_same:
                continue
            if self.waited[eng].get(key, 0) >= val:
                continue
            self.waited[eng][key] = val
            sem = self.sems[key] if key in self.sems else self.slots[key][0]
            self.streams[eng].append(lambda e, sem=sem, val=val: e.wait_ge(sem, val))

    def op(self, eng, fn, reads=(), writes=()):
        deps = self._deps(reads, writes)
        self._emit_waits(eng, deps, (not self.same) or eng == 'tensor')
        self.cnt[eng] += 1
        sem = self.sems[eng]
        self.streams[eng].append(lambda e, fn=fn, sem=sem: fn(e).then_inc(sem, 1))
        self._update(reads, writes, (eng, self.cnt[eng]))

    def dma(self, q, slot, out, in_, reads=(), writes=()):
        self.slot_q[slot] = q
        if slot not in self.slots:
            self.slots[slot] = [self.nc.alloc_semaphore("d_" + slot), 0]
        deps = self._deps(reads, writes)
        self._emit_waits(q, deps, False)
        s = self.slots[slot]
        s[1] += 16
        sem = s[0]
        self.streams[q].append(lambda e, out=out, in_=in_, sem=sem: e.dma_start(out=out, in_=in_).then_inc(sem, 16))
        self._update(reads, writes, (slot, s[1]))

    def idma(self, slot, out, out_off, in_, in_off, reads=(), writes=()):
        q = 'gpsimd'
        self.slot_q[slot] = q
        if slot not in self.slots:
            self.slots[slot] = [self.nc.alloc_semaphore("d_" + slot), 0]
        deps = self._deps(reads, writes)
        self._emit_waits(q, deps, False)
        s = self.slots[slot]
        s[1] += 16
        sem = s[0]
        self.streams[q].append(lambda e, out=out, in_=in_, sem=sem, oo=out_off, io=in_off:
                               e.indirect_dma_start(out=out, out_offset=oo, in_=in_, in_offset=io).then_inc(sem, 16))
        self._update(reads, writes, (slot, s[1]))

    def cond_begin(self, cnt_ap, thr, drain_slots):
        for e in ENGS:
            deps = [(e, self.cnt[e])] if self.cnt[e] else []
            deps += [(k, self.slots[k][1]) for k in drain_slots if k in self.slots and self.slot_q.get(k) == e]
            self._emit_waits(e, deps, False)
        if not hasattr(self, 'cstack'):
            self.cstack = []
        self.cstack.append((dict(self.cnt), {k: v[1] for k, v in self.slots.items()},
                            {e: dict(w) for e, w in self.waited.items()}))
        for e in ENGS:
            if cnt_ap is not None:
                self.streams[e].append(('regload', cnt_ap))
            self.streams[e].append(('if', thr))

    def cond_end(self):
        snap_cnt, snap_slots, snap_waited = self.cstack.pop()
        for e in ENGS:
            self.streams[e].append(('else',))
            n = self.cnt[e] - snap_cnt[e]
            if n:
                self.streams[e].append(lambda eng, sem=self.sems[e], n=n: eng.sem_inc(sem, n))
        for slot, (sem, c) in self.slots.items():
            d = c - snap_slots.get(slot, 0)
            if d:
                self.streams[self.slot_q[slot]].append(lambda eng, sem=sem, d=d: eng.sem_inc(sem, d))
        for e in ENGS:
            self.streams[e].append(('endif',))
        self.waited = snap_waited

    def barrier(self):
        deps = [(e, c) for e, c in self.cnt.items() if c > 0]
        deps += [(k, v[1]) for k, v in self.slots.items() if v[1] > 0]
        for e in ENGS:
            self._emit_waits(e, deps, True)

    def emit(self):
        self.barrier()
        streams = self.streams

        def run(e, items):
            creg = e.alloc_register("creg")

            def emit_seq(i, stop):
                while i < len(items):
                    it = items[i]
                    if not isinstance(it, tuple):
                        it(e)
                        i += 1
                    elif it[0] == 'regload':
                        e.reg_load(creg, it[1])
                        i += 1
                    elif it[0] == 'if':
                        i = emit_if(i)
                    else:
                        assert it[0] in stop, it
                        return i
                return i

            def skip_seq(i, stop):
                depth = 0
                while True:
                    it = items[i]
                    if isinstance(it, tuple):
                        if it[0] == 'if':
                            depth += 1
                        elif it[0] == 'endif' and depth > 0:
                            depth -= 1
                        elif depth == 0 and it[0] in stop:
                            return i
                    i += 1

            def emit_if(i):
                thr = items[i][1]
                j_else = skip_seq(i + 1, ('else',))
                j_end = skip_seq(j_else + 1, ('endif',))
                with e.If_lt(creg, thr + 1):
                    for f in items[j_else + 1:j_end]:
                        f(e)
                with e.Else():
                    k = emit_seq(i + 1, ('else',))
                    assert k == j_else
                return j_end + 1

            k = emit_seq(0, ())
            assert k == len(items)

        with self.nc.Block() as block:
            @block.sync
            def _(e):
                run(e, streams['sync'])

            @block.scalar
            def _(e):
                run(e, streams['scalar'])

            @block.vector
            def _(e):
                run(e, streams['vector'])

            @block.gpsimd
            def _(e):
                run(e, streams['gpsimd'])

            @block.tensor
            def _(e):
                run(e, streams['tensor'])


def mmgrp(lst):
    def fn(e):
        ins = None
        for (out, lhsT, rhs, st, sp) in lst:
            ins = e.matmul(out, lhsT=lhsT, rhs=rhs, start=st, stop=sp)
        return ins
    return fn


def tpgrp(lst):
    def fn(e):
        ins = None
        for (out, in_, ident) in lst:
            ins = e.transpose(out, in_, ident)
        return ins
    return fn


def ACT(out, in_, func, **kw):
    return lambda e: e.activation(out=out, in_=in_, func=func, **kw)


def TT(out, in0, in1, op):
    return lambda e: e.tensor_tensor(out=out, in0=in0, in1=in1, op=op)


def TS(out, in0, s1, s2, op0, op1=None):
    if op1 is None:
        return lambda e: e.tensor_scalar(out=out, in0=in0, scalar1=s1, scalar2=None, op0=op0)
    return lambda e: e.tensor_scalar(out=out, in0=in0, scalar1=s1, scalar2=s2, op0=op0, op1=op1)


def STT(out, in0, scalar, in1, op0, op1):
    return lambda e: e.scalar_tensor_tensor(out=out, in0=in0, scalar=scalar, in1=in1, op0=op0, op1=op1)


def CP(out, in_):
    return lambda e: e.tensor_copy(out=out, in_=in_)


def RED(out, in_, op):
    return lambda e: e.tensor_reduce(out=out, in_=in_, axis=AX.X, op=op)


def RMAX(out, in_):
    return lambda e: e.reduce_max(out=out, in_=in_, axis=AX.X)


def RECIP(out, in_):
    return lambda e: e.reciprocal(out=out, in_=in_)


def MEMSET(ap, v):
    return lambda e: e.memset(ap, v)


def ASEL(out, in_, pattern, cmp, fill, base, cm):
    return lambda e: e.affine_select(out=out, in_=in_, pattern=pattern, compare_op=cmp, fill=fill, base=base,
                                     channel_multiplier=cm)


def build_program():
    nc = bass.Bass("TRN2", target_bir_lowering=False)
    T = Tracker(nc, same_engine_sync=DEBUG["same"])
    din = lambda name, shape: nc.dram_tensor(name, shape, F32, kind="ExternalInput").ap()
    xall = din("xall", [8192, 1024])
    xown = din("xown", [4096, 1024])
    cT_d = din("cT", [128, 8])
    wada_d = din("w_ada", [1024, 6144])
    bada_d = din("b_ada", [1, 6144])
    g1T_d = din("g1T", [128, 8])
    g2T_d = din("g2T", [128, 8])
    win_d = din("w_in", [1024, 2568])
    gsgu_d = din("gsgu", [1, 512])
    wsT_d = din("wsT", [128, 8, 128])
    bsp_d = din("bsp", [8, 128])
    bfg_d = din("bfg", [1, 8])
    gos_d = din("gos", [128, 4])
    gof_d = din("gof", [128, 4])
    wout_d = din("w_out", [1024, 1024])
    wr_d = din("wr", [1024, 20])
    br_d = din("br", [1, 20])
    wg_d = din("w_gate", [16, 1024, 512])
    wu_d = din("w_up", [16, 1024, 512])
    wd_d = din("w_down", [16, 512, 1024])
    gfin_d = din("gfin", [1, 1024])
    masks_d = din("masks", [16, 128, 512])
    rsel_d = din("rsel", [1, 512])
    eb_d = din("eb", [1, 16])
    out_d = nc.dram_tensor("out", [4096, 1024], F32, kind="ExternalOutput").ap()
    KT_d = nc.dram_tensor("KT_d", [4, 128, 8192], BF16).ap()
    V_d = nc.dram_tensor("V_d", [4, 128, 64, 192], BF16).ap()
    Xs_d = nc.dram_tensor("Xs_d", [65536, 1024], BF16).ap()
    Ys_d = nc.dram_tensor("Ys_d", [65536, 1024], F32).ap()
    CNT_d = nc.dram_tensor("CNT_d", [1, 16], I32).ap()
    if DEBUG["x1"]:
        X1_d = nc.dram_tensor("X1_d", [4096, 1024], F32, kind="ExternalOutput").ap()
    else:
        X1_d = nc.dram_tensor("X1_d", [4096, 1024], F32).ap()

    win_v = win_d.rearrange("(kt p) n -> p kt n", p=128)
    dumps = DEBUG.get("dump", [])

    def dump(name, ap, shape, dt, rname):
        if name not in dumps:
            return
        d = nc.dram_tensor("dbg_" + name, shape, dt, kind="ExternalOutput").ap()
        T.dma('sync', 'dbg_' + name, d, ap, reads=rname, writes=['dbg_' + name])

    top = ExitStack()
    uid = [0]

    def sb(es, shape, dt, name=None):
        uid[0] += 1
        return es.enter_context(nc.sbuf_tensor(f"{name or 't'}_{uid[0]}", shape, dt))

    def ps(es, shape, dt, name=None):
        uid[0] += 1
        return es.enter_context(nc.psum_tensor(f"{name or 'p'}_{uid[0]}", shape, dt))

    ident_f = sb(top, [128, 128], F32, "identf")
    ident_b = sb(top, [128, 128], BF16, "identb")
    Umat = sb(top, [128, 128], F32, "U")
    Sel127 = sb(top, [128, 128], F32, "sel127")
    eps_t = sb(top, [128, 1], F32, "eps")
    one_t = sb(top, [128, 1], F32, "one")
    ones_bf = sb(top, [128, 1], BF16, "onesbf")
    A1 = sb(top, [128, 8], F32, "A1")
    B1 = sb(top, [128, 8], F32, "B1")
    A2 = sb(top, [128, 8], F32, "A2")
    B2 = sb(top, [128, 8], F32, "B2")
    GT1 = sb(top, [128, 1024], F32, "GT1")
    GT2 = sb(top, [128, 1024], F32, "GT2")
    C_all = sb(top, [128, 64, 8], F32, "Call")
    RC = sb(top, [128, 8, 8], F32, "RC")

    T.op('gpsimd', MEMSET(ident_f[:], 0.0), writes=['identf'])
    T.op('gpsimd', ASEL(ident_f[:], ident_f[:], [[-1, 128]], ALU.not_equal, 1.0, 0, 1), reads=['identf'], writes=['identf'])
    T.op('gpsimd', MEMSET(Umat[:], 1.0), writes=['U'])
    T.op('gpsimd', ASEL(Umat[:], Umat[:], [[1, 128]], ALU.is_ge, 0.0, 0, -1), reads=['U'], writes=['U'])
    T.op('gpsimd', MEMSET(Sel127[:], 1.0), writes=['sel127'])
    T.op('gpsimd', ASEL(Sel127[:], Sel127[:], [[0, 128]], ALU.is_ge, 0.0, -127, 1), reads=['sel127'], writes=['sel127'])
    T.op('gpsimd', MEMSET(eps_t[:], EPS), writes=['eps'])
    T.op('gpsimd', MEMSET(one_t[:], 1.0), writes=['one'])
    T.op('gpsimd', MEMSET(ones_bf[:], 1.0), writes=['onesbf'])
    T.op('vector', CP(ident_b[:], ident_f[:]), reads=['identf'], writes=['identb'])

    with ExitStack() as es:
        cT = sb(es, [128, 8], F32)
        CB = sb(es, [128, 8, 128], F32)
        WA = [sb(es, [128, 8, 1024], F32, "wada") for _ in range(2)]
        badab = sb(es, [128, 1024], F32)
        gT1 = sb(es, [128, 8], F32)
        gT2 = sb(es, [128, 8], F32)
        rowv = sb(es, [128, 1024], F32)
        dtmp = sb(es, [128, 8, 128], F32)
        colv = sb(es, [128, 8], F32)
        pp = [ps(es, [128, 512], F32) for _ in range(2)]
        T.dma('sync', 'cT', cT[:], cT_d, writes=['cT'])
        T.dma('sync', 'gT1', gT1[:], g1T_d, writes=['gT1'])
        T.dma('sync', 'gT2', gT2[:], g2T_d, writes=['gT2'])
        T.op('scalar', ACT(cT[:], cT[:], AF.Silu), reads=['cT'], writes=['cT'])
        T.op('vector', CP(CB[:], cT[:].unsqueeze(2).broadcast_to([128, 8, 128])), reads=['cT'], writes=['CB'])
        wada_v = wada_d.rearrange("(kt p) n -> p kt n", p=128)
        order = [1, 0, 2, 4, 3, 5]
        for n, v in enumerate(order):
            wb = n % 2
            for kt in range(8):
                T.dma('sync', f'wada{wb}_{kt}', WA[wb][:, kt, :], wada_v[:, kt, v * 1024:(v + 1) * 1024],
                      writes=[f'wada{wb}_{kt}'])
            T.dma('gpsimd', 'badab', badab[:], bada_d[0:1, v * 1024:(v + 1) * 1024].broadcast_to([128, 1024]),
                  writes=['badab'])
            for hf in range(2):
                T.op('tensor', mmgrp([(pp[hf][:], CB[:, kt, :], WA[wb][:, kt, hf * 512:(hf + 1) * 512], kt == 0, kt == 7)
                                      for kt in range(8)]),
                     reads=['CB'] + [f'wada{wb}_{kt}' for kt in range(8)], writes=[f'pp{hf}'])
                dst = {2: GT1, 5: GT2}.get(v, rowv)
                dname = {2: 'GT1', 5: 'GT2'}.get(v, 'rowv') + str(hf)
                T.op('vector', TT(dst[:, hf * 512:(hf + 1) * 512], pp[hf][:], badab[:, hf * 512:(hf + 1) * 512], ALU.add),
                     reads=[f'pp{hf}', 'badab'], writes=[dname])
            if v in (2, 5):
                continue
            T.op('vector', TT(dtmp[:], rowv[:].rearrange("p (k t) -> p k t", k=8),
                              ident_f[:].unsqueeze(1).broadcast_to([128, 8, 128]), ALU.mult),
                 reads=['rowv0', 'rowv1', 'identf'], writes=['dtmp'])
            T.op('vector', RED(colv[:], dtmp[:], ALU.add), reads=['dtmp'], writes=['colv'])
            if v == 1:
                T.op('vector', STT(A1[:], colv[:], 1.0, gT1[:], ALU.add, ALU.mult), reads=['colv', 'gT1'], writes=['A1'])
            elif v == 0:
                T.op('vector', CP(B1[:], colv[:]), reads=['colv'], writes=['B1'])
            elif v == 4:
                T.op('vector', STT(A2[:], colv[:], 1.0, gT2[:], ALU.add, ALU.mult), reads=['colv', 'gT2'], writes=['A2'])
            elif v == 3:
                T.op('vector', CP(B2[:], colv[:]), reads=['colv'], writes=['B2'])
        T.barrier()

    def ln1_block(xsrc_rows, XA, XN, junk, ssq, rs, tp, tmpf, HT_dst, bi, htname):
        b = bi % 2
        T.dma('sync', f'xa{b}', XA[b][:], xsrc_rows, writes=[f'xa{b}'])
        T.op('scalar', ACT(XN[b][:], XA[b][:], AF.Square, accum_out=ssq[:, bi:bi + 1]), reads=[f'xa{b}', 'ssqall'],
             writes=[f'xn{b}', f'ssq_{bi}'])
        T.op('scalar', ACT(rs[b][:], ssq[:, bi:bi + 1], AF.Ln, scale=1.0 / 1024, bias=eps_t[:]), reads=[f'ssq_{bi}', 'eps'],
             writes=[f'rs{b}'])
        T.op('scalar', ACT(rs[b][:], rs[b][:], AF.Exp, scale=-0.5), reads=[f'rs{b}'], writes=[f'rs{b}'])
        T.op('vector', TS(XN[b][:], XA[b][:], rs[b][:, 0:1], None, ALU.mult), reads=[f'xa{b}', f'rs{b}'], writes=[f'xn{b}'])
        T.op('tensor', tpgrp([(tp[:, kt, :], XN[b][:, kt * 128:(kt + 1) * 128], ident_b[:]) for kt in range(8)]),
             reads=[f'xn{b}', 'identb'], writes=['tp'])
        T.op('vector', TT(tmpf[:], tp[:], A1[:].unsqueeze(2).broadcast_to([128, 8, 128]), ALU.mult), reads=['tp', 'A1'],
             writes=['tmpf'])
        T.op('vector', TT(HT_dst, tmpf[:], B1[:].unsqueeze(2).broadcast_to([128, 8, 128]), ALU.add), reads=['tmpf', 'B1'],
             writes=[htname])

    if "A" in DEBUG["phases"]:
      with ExitStack() as es:
        WKV = sb(es, [128, 8, 1032], BF16, "wkvf")
        XA = [sb(es, [128, 1024], F32) for _ in range(2)]
        XN = [sb(es, [128, 1024], BF16) for _ in range(2)]
        junk = None
        ssq = sb(es, [128, 64], F32)
        T.op('vector', MEMSET(ssq[:], 0.0), writes=['ssqall'])
        rs = [sb(es, [128, 1], F32) for _ in range(2)]
        tmpf = sb(es, [128, 8, 128], F32)
        HT = [sb(es, [128, 8, 512], BF16) for _ in range(2)]
        KTs = [sb(es, [128, 4, 512], BF16) for _ in range(2)]
        VBs = [sb(es, [128, 4, 4, 3, 64], BF16) for _ in range(2)]
        bfb = sb(es, [128, 8], F32)
        tp = ps(es, [128, 8, 128], BF16)
        mm = [ps(es, [128, 512], F32) for _ in range(3)]
        sm = ps(es, [128, 512], F32)
        T.dma('gpsimd', 'wkvf', WKV[:], win_v[:, :, 1536:2568], writes=['wkvf'])
        T.dma('sync', 'bfb', bfb[:], bfg_d[0:1, :].broadcast_to([128, 8]), writes=['bfb'])
        for k in range(2):
            T.op('gpsimd', MEMSET(VBs[k][:, :, :, 1, :], 1.0), writes=[f'vbs{k}'])
        mi = [0]
        ftt = [sb(es, [128, 8], F32) for _ in range(4)]
        smb = [sm] + [ps(es, [128, 512], F32) for _ in range(3)]

        def lnA(r):
            hb = r % 2
            for bl in range(4):
                g = r * 4 + bl
                ln1_block(xall[g * 128:(g + 1) * 128, :], XA, XN, junk, ssq, rs, tp, tmpf,
                          HT[hb][:, :, bl * 128:(bl + 1) * 128], g, f'ht{hb}')

        def kvfA(r):
            hb = r % 2
            for p in range(4):
                m = mm[mi[0] % 3]; mn = f'mm{mi[0] % 3}'; mi[0] += 1
                T.op('tensor', mmgrp([(m[:], WKV[:, kt, p * 128:(p + 1) * 128], HT[hb][:, kt, :], kt == 0, kt == 7)
                                      for kt in range(8)]), reads=['wkvf', f'ht{hb}'], writes=[mn])
                T.op('scalar', ACT(KTs[hb][:, p, :], m[:], AF.Copy), reads=[mn], writes=[f'kts{hb}'])
            T.dma('gpsimd', f'kts{hb}', KT_d[:, :, r * 512:(r + 1) * 512].rearrange("q p t -> p q t"), KTs[hb][:],
                  reads=[f'kts{hb}'], writes=[f'KT_d{r}'])
            for bl in range(4):
                m = mm[mi[0] % 3]; mn = f'mm{mi[0] % 3}'; mi[0] += 1
                T.op('tensor', mmgrp([(m[:], HT[hb][:, kt, bl * 128:(bl + 1) * 128], WKV[:, kt, 512:1024], kt == 0, kt == 7)
                                      for kt in range(8)]), reads=['wkvf', f'ht{hb}'], writes=[mn])
                T.op('tensor', mmgrp([(smb[bl][:, 0:8], HT[hb][:, kt, bl * 128:(bl + 1) * 128], WKV[:, kt, 1024:1032], kt == 0, kt == 7)
                                      for kt in range(8)]), reads=['wkvf', f'ht{hb}'], writes=[f'smb{bl}'])
                T.op('scalar', ACT(VBs[hb][:, bl, :, 0::2, :], m[:].rearrange("t (p two d) -> t p two d", p=4, two=2),
                                   AF.Copy), reads=[mn], writes=[f'vbs{hb}'])
                ft = ftt[bl]
                T.op('vector', TT(ft[:], smb[bl][:, 0:8], bfb[:], ALU.add), reads=[f'smb{bl}', 'bfb'], writes=[f'ft{bl}'])
                T.op('scalar', ACT(ft[:], ft[:], AF.Exp, scale=-1.0), reads=[f'ft{bl}'], writes=[f'ft{bl}'])
                T.op('scalar', ACT(ft[:], ft[:], AF.Ln, bias=one_t[:]), reads=[f'ft{bl}', 'one'], writes=[f'ft{bl}'])
            for q in range(4):
                T.dma('gpsimd', f'vbs{hb}_{q}', V_d[q, :, r * 4:(r + 1) * 4, :],
                      VBs[hb][:, :, q, :, :].rearrange("t b three d -> t b (three d)"), reads=[f'vbs{hb}'],
                      writes=[f'V_d{r}_{q}'])
            for bl in range(4):
                g = r * 4 + bl
                cs = smb[bl][:, 8:16]
                lst = [(cs, Umat[:], ftt[bl][:], True, g == 0)]
                if g > 0:
                    lst.append((cs, Sel127[:], C_all[:, g - 1, :], False, True))
                T.op('tensor', mmgrp(lst), reads=[f'ft{bl}', 'U', 'sel127', 'Call'], writes=[f'smb{bl}'])
                T.op('vector', CP(C_all[:, g, :], cs), reads=[f'smb{bl}'], writes=['Call'])

        lnA(0)
        for r in range(16):
            if r + 1 < 16:
                lnA(r + 1)
            kvfA(r)
        T.barrier()

    with ExitStack() as es:
        rselb = sb(es, [128, 8, 64], F32)
        csel = sb(es, [128, 8, 8], F32)
        tmp3 = sb(es, [128, 8, 64], F32)
        pr = ps(es, [128, 512], F32)
        T.dma('sync', 'rselb', rselb[:], rsel_d[0:1, :].broadcast_to([128, 512]).rearrange("p (i b) -> p i b", i=8),
              writes=['rselb'])
        for i in range(8):
            T.op('vector', TT(tmp3[:], C_all[:].rearrange("p b h -> p h b"),
                              rselb[:, i, :].unsqueeze(1).broadcast_to([128, 8, 64]), ALU.mult),
                 reads=['Call', 'rselb'], writes=['tmp3'])
            T.op('vector', RED(csel[:, i, :], tmp3[:], ALU.add), reads=['tmp3'], writes=['csel'])
        T.op('tensor', mmgrp([(pr[:, 0:64], Sel127[:], csel[:].rearrange("p i h -> p (i h)"), True, True)]),
             reads=['csel', 'sel127'], writes=['pr'])
        T.op('vector', CP(RC[:].rearrange("p i h -> p (i h)"), pr[:, 0:64]), reads=['pr'], writes=['RC'])
        T.barrier()

    if "B" in DEBUG["phases"]:
      with ExitStack() as es:
        WQ = sb(es, [128, 8, 1536], BF16, "wq")
        WO = sb(es, [128, 8, 1024], BF16, "wo")
        MK = sb(es, [128, 16, 512], BF16, "mk")
        WsT = sb(es, [128, 8, 128], BF16, "wst")
        Bb = sb(es, [128, 4, 128], F32)
        gsg = sb(es, [128, 512], F32)
        gos = sb(es, [128, 4], F32)
        gof = sb(es, [128, 4], F32)
        XA = [sb(es, [128, 1024], F32) for _ in range(2)]
        XR = [sb(es, [128, 1024], F32) for _ in range(2)]
        XN = [sb(es, [128, 1024], BF16) for _ in range(2)]
        wstage = XA
        wsf = XR[0][:].rearrange("p (h t) -> p h t", h=8)
        junk = None
        ssq = sb(es, [128, 64], F32)
        T.op('vector', MEMSET(ssq[:], 0.0), writes=['ssqall'])
        rs = [sb(es, [128, 1], F32) for _ in range(2)]
        tmpf = sb(es, [128, 8, 128], F32)
        HT = sb(es, [128, 8, 512], BF16)
        QT = sb(es, [128, 4, 2, 512], BF16)
        T.op('gpsimd', MEMSET(QT[:], 0.0), writes=['qt'])
        UT = sb(es, [128, 4, 512], BF16)
        VG = sb(es, [128, 4, 512], F32)
        VG2 = sb(es, [128, 4, 512], F32)
        v8 = sb(es, [128, 32], F32)
        VN = sb(es, [128, 4, 4, 3, 64], BF16)
        YS = sb(es, [128, 4, 512], BF16)
        YF = sb(es, [128, 4, 512], BF16)
        SQ = sb(es, [128, 4, 512], BF16)
        zt = sb(es, [128, 4, 128], F32)
        KB = [sb(es, [128, 2048], BF16) for _ in range(3)]
        VB = [sb(es, [128, 16, 192], BF16) for _ in range(3)]
        PT = [sb(es, [128, 512], BF16) for _ in range(3)]
        BI = sb(es, [128, 2, 2, 64], F32)
        rc = sb(es, [128, 512], F32)
        rsS = sb(es, [128, 4], F32)
        rsF = sb(es, [128, 4], F32)
        t1 = [sb(es, [128, 512], F32) for _ in range(2)]
        X1 = XR
        tp = ps(es, [128, 8, 128], BF16)
        st = [ps(es, [128, 512], F32) for _ in range(4)]
        OT = [ps(es, [128, 512], F32) for _ in range(2)]
        sm = ps(es, [128, 512], F32)

        T.dma('gpsimd', 'wq_u', WQ[:, :, 0:1024], win_v[:, :, 0:1024], writes=['wq_uv'])
        T.dma('gpsimd', 'wq_q', WQ[:, :, 1024:1536], win_v[:, :, 1024:1536], writes=['wq_q'])
        T.dma('gpsimd', 'mk', MK[:], masks_d.rearrange("m p q -> p m q"), writes=['mk'])
        T.dma('sync', 'xr0', wsf, wsT_d, writes=['xr0'])
        T.dma('sync', 'gsg', gsg[:], gsgu_d[0:1, :].broadcast_to([128, 512]), writes=['gsg'])
        T.dma('sync', 'gos', gos[:], gos_d, writes=['gos'])
        T.dma('sync', 'gof', gof[:], gof_d, writes=['gof'])
        for h in range(8):
            T.dma('sync', 'Bb', Bb[(h % 2) * 64:(h % 2) * 64 + 64, h // 2, :], bsp_d[h:h + 1, :].broadcast_to([64, 128]),
                  writes=[f'Bb{h}'])
        T.op('gpsimd', ASEL(wsf, wsf, [[0, 8], [1, 128]], ALU.is_ge, 0.0, 0, -1), reads=['xr0'], writes=['xr0'])
        T.op('vector', CP(WsT[:], wsf), reads=['xr0'], writes=['wst'])
        T.op('gpsimd', MEMSET(VN[:, :, :, 1, :], 0.0), writes=['vn'])
        wout_v = wout_d.rearrange("(kt p) n -> p kt n", p=128)
        for kt in range(8):
            wsb = kt % 2
            T.dma('sync', f'xa{wsb}', wstage[wsb][:], wout_v[:, kt, :], writes=[f'xa{wsb}'])
            gg = gos[:, kt:kt + 1] if kt < 4 else gof[:, kt - 4:kt - 3]
            T.op('vector', TS(WO[:, kt, :], wstage[wsb][:], gg, None, ALU.mult), reads=[f'xa{wsb}', 'gos', 'gof'],
                 writes=['wo'])
        Bbn = [f'Bb{h}' for h in range(8)]

        sti = [0]
        pti = [0]
        kvi = [0]

        def next_st():
            k = sti[0] % 4
            sti[0] += 1
            return st[k], f'st{k}'

        bglob = 0
        for i in range(8):
            nk = NK(i)
            par = i % 2
            for bl in range(4):
                g = i * 4 + bl
                ln1_block(xown[g * 128:(g + 1) * 128, :], XA, XN, junk, ssq, rs, tp, tmpf,
                          HT[:, :, bl * 128:(bl + 1) * 128], g, 'ht')
            for p in range(4):
                m, mn = next_st()
                T.op('tensor', mmgrp([(m[:], WQ[:, kt, 1024 + p * 128:1024 + (p + 1) * 128], HT[:, kt, :], kt == 0, kt == 7)
                                      for kt in range(8)]), reads=['wq_q', 'ht'], writes=[mn])
                T.op('vector', TS(QT[0:64, p, 0, :], m[0:64, :], 0.125, None, ALU.mult), reads=[mn], writes=['qt'])
                T.op('vector', TS(QT[64:128, p, 1, :], m[64:128, :], 0.125, None, ALU.mult), reads=[mn], writes=['qt'])
            for p in range(4):
                m, mn = next_st()
                T.op('tensor', mmgrp([(m[:], WQ[:, kt, p * 128:(p + 1) * 128], HT[:, kt, :], kt == 0, kt == 7)
                                      for kt in range(8)]), reads=['wq_uv', 'ht'], writes=[mn])
                T.op('scalar', ACT(UT[:, p, :], m[:], AF.Gelu_apprx_tanh), reads=[mn], writes=['ut'])
            for bl in range(4):
                m, mn = next_st()
                T.op('tensor', mmgrp([(m[:], HT[:, kt, bl * 128:(bl + 1) * 128], WQ[:, kt, 512:1024], kt == 0, kt == 7)
                                      for kt in range(8)]), reads=['wq_uv', 'ht'], writes=[mn])
                T.op('scalar', ACT(VG[:, bl, :], m[:], AF.Gelu_apprx_tanh), reads=[mn], writes=['vg'])
            T.op('vector', TT(VG2[:], VG[:], VG[:], ALU.mult), reads=['vg'], writes=['vg2'])
            T.op('vector', RED(v8[:], VG2[:].rearrange("t b (h d) -> t (b h) d", h=8), ALU.add), reads=['vg2'], writes=['v8'])
            T.op('scalar', ACT(v8[:], v8[:], AF.Ln, scale=1.0 / 64, bias=eps_t[:]), reads=['v8', 'eps'], writes=['v8'])
            T.op('scalar', ACT(v8[:], v8[:], AF.Exp, scale=-0.5), reads=['v8'], writes=['v8'])
            T.op('vector', TT(VG2[:].rearrange("t b (h d) -> t (b h) d", h=8), VG[:].rearrange("t b (h d) -> t (b h) d", h=8),
                              v8[:].unsqueeze(2).broadcast_to([128, 32, 64]), ALU.mult), reads=['vg', 'v8'], writes=['vg2'])
            for bl in range(4):
                T.op('vector', TT(VN[:, bl, :, 0::2, :], VG2[:, bl, :].rearrange("t (p two d) -> t p two d", p=4, two=2),
                                  gsg[:].rearrange("t (p two d) -> t p two d", p=4, two=2), ALU.mult),
                     reads=['vg2', 'gsg'], writes=['vn'])
            if i == 0:
                dump("HT", HT[:], [128, 8, 512], BF16, ['ht'])
            for bl in range(4):
                lst = []
                for p in range(4):
                    vnp = VN[:, bl, p, :, :].rearrange("t three d -> t (three d)")
                    lst.append((sm[:, p * 128:(p + 1) * 128], vnp[:, 0:128], WsT[:, 2 * p, :], True, False))
                    lst.append((sm[:, p * 128:(p + 1) * 128], vnp[:, 64:192], WsT[:, 2 * p + 1, :], False, True))
                T.op('tensor', mmgrp(lst), reads=['vn', 'wst'], writes=['sm'])
                T.op('vector', TT(zt[:], sm[:].rearrange("d (p t) -> d p t", p=4), Bb[:], ALU.add), reads=['sm'] + Bbn,
                     writes=['zt'])
                T.op('vector', TT(YS[:, :, bl * 128:(bl + 1) * 128], zt[:], UT[:, :, bl * 128:(bl + 1) * 128], ALU.mult),
                     reads=['zt', 'ut'], writes=['ys'])
            T.op('vector', TT(SQ[:], YS[:], YS[:], ALU.mult), reads=['ys'], writes=['sq'])
            T.op('tensor', mmgrp([(sm[:, bl:bl + 1], SQ[:, p, bl * 128:(bl + 1) * 128], ones_bf[:], p == 0, p == 3)
                                  for bl in range(4) for p in range(4)]), reads=['sq', 'onesbf'], writes=['sm'])
            T.op('scalar', ACT(rsS[:], sm[:, 0:4], AF.Ln, scale=1.0 / 512, bias=eps_t[:]), reads=['sm', 'eps'], writes=['rsS'])
            T.op('scalar', ACT(rsS[:], rsS[:], AF.Exp, scale=-0.5), reads=['rsS'], writes=['rsS'])
            for p in range(4):
                pq = p % 2
                for hh in range(2):
                    h = 2 * p + hh
                    T.op('vector', TS(BI[:, pq, hh, :], C_all[:, :, h], RC[:, i, h:h + 1], None, ALU.subtract),
                         reads=['Call', 'RC'], writes=[f'bi{pq}{hh}'])
                units = [(jb, hh) for jb in range(nk) for hh in range(2)]
                nchunks = (nk + 15) // 16
                if p == 0:
                    seq = [(pp_, c_) for pp_ in range(4) for c_ in range(nchunks)]
                    chunk_buf = {}
                    issued = [0]

                    def ensure(upto):
                        while issued[0] <= upto and issued[0] < len(seq):
                            pp_, c = seq[issued[0]]
                            kb = kvi[0] % 3
                            kvi[0] += 1
                            n = min(16, nk - c * 16)
                            T.dma('sync', f'kb{kb}', KB[kb][:, 0:n * 128], KT_d[pp_, :, c * 2048:c * 2048 + n * 128],
                                  reads=[f'KT_d{r}' for r in range(c * 4, (c * 16 + n + 3) // 4)], writes=[f'kb{kb}'])
                            T.dma('sync', f'vb{kb}', VB[kb][:, 0:n, :], V_d[pp_, :, c * 16:c * 16 + n, :],
                                  reads=[f'V_d{r}_{pp_}' for r in range(c * 4, (c * 16 + n + 3) // 4)], writes=[f'vb{kb}'])
                            chunk_buf[(pp_, c)] = kb
                            issued[0] += 1

                    ensure(2)
                ubank = {}

                def emit_qk(n):
                    jb, hh = units[n]
                    c = jb // 16
                    kb = chunk_buf[(p, c)]
                    jl = jb - c * 16
                    m, mn = next_st()
                    ubank[n] = (m, mn)
                    r0 = hh * 64
                    lst = [(m[:], KB[kb][:, jl * 128:(jl + 1) * 128], QT[:, p, hh, :], True, jb < nk - 8)]
                    rd = [f'kb{kb}', 'qt']
                    if jb >= nk - 8:
                        lst.append((m[:], ident_b[:], MK[:, par * 8 + (jb - (nk - 8)), :], False, True))
                        rd += ['identb', 'mk']
                    T.op('tensor', mmgrp(lst), reads=rd, writes=[mn])

                def emit_rest(n):
                    jb, hh = units[n]
                    c = jb // 16
                    kb = chunk_buf[(p, c)]
                    jl = jb - c * 16
                    m, mn = ubank.pop(n)
                    k3 = pti[0] % 3
                    pti[0] += 1
                    T.op('scalar', ACT(PT[k3][:], m[:], AF.Exp, bias=BI[:, pq, hh, jb:jb + 1]), reads=[mn, f'bi{pq}{hh}'],
                         writes=[f'pt{k3}'])
                    vb = VB[kb][:, jl, hh * 64:hh * 64 + 128]
                    T.op('tensor', mmgrp([(OT[hh][:], vb, PT[k3][:], jb == 0, jb == nk - 1)]),
                         reads=[f'vb{kb}'] + ([] if 'nodep' in PROBE else [f'pt{k3}']), writes=[f'ot{hh}'])

                LA = 3
                for n in range(min(LA, len(units))):
                    emit_qk(n)
                for n in range(len(units)):
                    emit_rest(n)
                    jbd, hhd = units[n]
                    if hhd == 1 and (jbd % 16 == 15 or jbd == nk - 1):
                        ensure(p * nchunks + jbd // 16 + 3)
                    nn = n + LA
                    if nn < len(units):
                        emit_qk(nn)
                T.op('vector', RECIP(rc[0:64, :], OT[0][64:128, :]), reads=['ot0'], writes=['rca'])
                T.op('vector', RECIP(rc[64:128, :], OT[1][0:64, :]), reads=['ot1'], writes=['rcb'])
                T.op('vector', TT(YF[0:64, p, :], OT[0][0:64, :], rc[0:64, :], ALU.mult), reads=['ot0', 'rca'], writes=['yfa'])
                T.op('vector', TT(YF[64:128, p, :], OT[1][64:128, :], rc[64:128, :], ALU.mult), reads=['ot1', 'rcb'],
                     writes=['yfb'])
            T.op('vector', TT(SQ[:], YF[:], YF[:], ALU.mult), reads=['yfa', 'yfb'], writes=['sq'])
            T.op('tensor', mmgrp([(sm[:, 4 + bl:5 + bl], SQ[:, p, bl * 128:(bl + 1) * 128], ones_bf[:], p == 0, p == 3)
                                  for bl in range(4) for p in range(4)]), reads=['sq', 'onesbf'], writes=['sm'])
            T.op('scalar', ACT(rsF[:], sm[:, 4:8], AF.Ln, scale=1.0 / 512, bias=eps_t[:]), reads=['sm', 'eps'], writes=['rsF'])
            T.op('scalar', ACT(rsF[:], rsF[:], AF.Exp, scale=-0.5), reads=['rsF'], writes=['rsF'])
            if i == 0:
                dump("YS", YS[:], [128, 4, 512], BF16, ['ys'])
                dump("YF", YF[:], [128, 4, 512], BF16, ['yfa', 'yfb'])
                dump("rsS", rsS[:], [128, 4], F32, ['rsS'])
                dump("rsF", rsF[:], [128, 4], F32, ['rsF'])
            for bl in range(4):
                g = i * 4 + bl
                xb = g % 2
                T.dma('sync', f'xr{xb}', XR[xb][:], xown[g * 128:(g + 1) * 128, :], writes=[f'xr{xb}'])
                for hf in range(2):
                    ms, msn = next_st()
                    T.op('tensor', mmgrp([(ms[:], YS[:, p, bl * 128:(bl + 1) * 128], WO[:, p, hf * 512:(hf + 1) * 512], p == 0, p == 3)
                                          for p in range(4)]), reads=['ys', 'wo'], writes=[msn])
                    mf, mfn = next_st()
                    T.op('tensor', mmgrp([(mf[:], YF[:, p, bl * 128:(bl + 1) * 128], WO[:, 4 + p, hf * 512:(hf + 1) * 512], p == 0, p == 3)
                                          for p in range(4)]), reads=['yfa', 'yfb', 'wo'], writes=[mfn])
                    tb = hf
                    T.op('vector', TS(t1[tb][:], ms[:], rsS[:, bl:bl + 1], None, ALU.mult), reads=[msn, 'rsS'], writes=[f't1{tb}'])
                    T.op('vector', STT(t1[tb][:], mf[:], rsF[:, bl:bl + 1], t1[tb][:], ALU.mult, ALU.add),
                         reads=[mfn, 'rsF', f't1{tb}'], writes=[f't1{tb}'])
                    T.op('vector', TT(t1[tb][:], t1[tb][:], GT1[:, hf * 512:(hf + 1) * 512], ALU.mult),
                         reads=[f't1{tb}', 'GT10', 'GT11'], writes=[f't1{tb}'])
                    T.op('vector', TT(X1[xb][:, hf * 512:(hf + 1) * 512], t1[tb][:], XR[xb][:, hf * 512:(hf + 1) * 512], ALU.add),
                         reads=[f't1{tb}', f'xr{xb}'], writes=[f'xr{xb}'])
                T.dma('gpsimd', f'x1o{xb}', X1_d[g * 128:(g + 1) * 128, :], X1[xb][:], reads=[f'xr{xb}'], writes=[f'X1_d{g}'])
        T.barrier()

    CAP = 768
    NT0 = CAP // 128
    if "C" in DEBUG["phases"]:
      csem = {}
      with ExitStack() as es0:
        IDX = sb(es0, [128, 32, 2], U32, "IDX")
        W12 = sb(es0, [128, 32, 2], F32, "W12")
        gfin = sb(es0, [128, 1024], F32, "gfin")
        T.dma('sync', 'gfin', gfin[:], gfin_d[0:1, :].broadcast_to([128, 1024]), writes=['gfin'])
        with ExitStack() as es:
            X1t = [sb(es, [128, 1024], F32) for _ in range(2)]
            xn2_2 = [sb(es, [128, 1024], F32) for _ in range(2)]
            junk_2 = [sb(es, [128, 1024], BF16) for _ in range(2)]
            H2f_2 = [sb(es, [128, 8, 128], F32) for _ in range(2)]
            tmpg_2 = [sb(es, [128, 8, 128], F32) for _ in range(2)]
            H2row = [sb(es, [128, 1024], BF16) for _ in range(2)]
            hrt_2 = [sb(es, [128, 1024], F32) for _ in range(2)]
            A2R = sb(es, [128, 1024], F32)
            B2R = sb(es, [128, 1024], F32)
            dg = sb(es, [128, 128], F32)
            onesf = sb(es, [128, 128], F32)
            WR = sb(es, [128, 8, 20], F32)
            brb = sb(es, [128, 20], F32)
            EB = sb(es, [128, 16], F32)
            Cn = sb(es, [128, 32, 16], F32)
            L_2 = [sb(es, [128, 20], F32) for _ in range(2)]
            s1_2 = [sb(es, [128, 16], F32) for _ in range(2)]
            s2_2 = [sb(es, [128, 16], F32) for _ in range(2)]
            E1_2 = [sb(es, [128, 16], F32) for _ in range(2)]
            E2_2 = [sb(es, [128, 16], F32) for _ in range(2)]
            Mx_2 = [sb(es, [128, 16], F32) for _ in range(2)]
            oh_2 = [sb(es, [128, 4], F32) for _ in range(2)]
            mk1_2 = [sb(es, [128, 4], F32) for _ in range(2)]
            mk2_2 = [sb(es, [128, 4], F32) for _ in range(2)]
            les_2 = [sb(es, [128, 4], F32) for _ in range(2)]
            le2_2 = [sb(es, [128, 4], F32) for _ in range(2)]
            sc_2 = [sb(es, [128, 12], F32) for _ in range(2)]
            idf_2 = [sb(es, [128, 2], F32) for _ in range(2)]
            CNTi = sb(es, [128, 16], I32)
            ssq = sb(es, [128, 32], F32)
            T.op('vector', MEMSET(ssq[:], 0.0), writes=['ssqall'])
            rs_2 = [sb(es, [128, 1], F32) for _ in range(2)]
            tpc = ps(es, [128, 8, 128], F32)
            pm = ps(es, [128, 512], F32)
            pmr = [ps(es, [128, 512], F32) for _ in range(2)]

            T.dma('sync', 'WR', WR[:], wr_d.rearrange("(kt p) n -> p kt n", p=128), writes=['WR'])
            T.dma('sync', 'brb', brb[:], br_d[0:1, :].broadcast_to([128, 20]), writes=['brb'])
            T.dma('sync', 'EB', EB[:], eb_d[0:1, :].broadcast_to([128, 16]), writes=['EB'])
            T.op('gpsimd', MEMSET(onesf[:], 1.0), writes=['onesf'])
            for (colt, rowt, nm) in ((A2, A2R, 'A2R'), (B2, B2R, 'B2R')):
                for kt in range(8):
                    T.op('vector', TS(dg[:], ident_f[:], colt[:, kt:kt + 1], None, ALU.mult), reads=['identf', 'A2', 'B2', 'dg'],
                         writes=['dg'])
                    T.op('tensor', mmgrp([(pm[:, 0:128], onesf[:], dg[:], True, True)]), reads=['dg', 'onesf'], writes=['pm'])
                    T.op('vector', CP(rowt[:, kt * 128:(kt + 1) * 128], pm[:, 0:128]), reads=['pm'], writes=[nm])
            def c1_stage1(g):
                    xb = g % 2
                    xn2 = xn2_2[xb]
                    junk = junk_2[xb]
                    H2f = H2f_2[xb]
                    tmpg = tmpg_2[xb]
                    hrt = hrt_2[xb]
                    L = L_2[xb]
                    s1 = s1_2[xb]
                    s2 = s2_2[xb]
                    E1 = E1_2[xb]
                    E2 = E2_2[xb]
                    Mx = Mx_2[xb]
                    oh = oh_2[xb]
                    mk1 = mk1_2[xb]
                    mk2 = mk2_2[xb]
                    les = les_2[xb]
                    le2 = le2_2[xb]
                    sc = sc_2[xb]
                    idf = idf_2[xb]
                    rs = rs_2[xb]
                    T.dma('sync', f'x1t{xb}', X1t[xb][:], X1_d[g * 128:(g + 1) * 128, :], reads=[f'X1_d{g}'], writes=[f'x1t{xb}'])
                    T.op('scalar', ACT(junk[:], X1t[xb][:], AF.Square, accum_out=ssq[:, g:g + 1]), reads=[f'x1t{xb}', 'ssqall'],
                         writes=[f'junk{xb}', f'ssq_{g}'])
                    T.op('scalar', ACT(rs[:], ssq[:, g:g + 1], AF.Ln, scale=1.0 / 1024, bias=eps_t[:]), reads=[f'ssq_{g}', 'eps'], writes=[f'rs{xb}'])
                    T.op('scalar', ACT(rs[:], rs[:], AF.Exp, scale=-0.5), reads=[f'rs{xb}'], writes=[f'rs{xb}'])
                    T.op('vector', TS(xn2[:], X1t[xb][:], rs[:, 0:1], None, ALU.mult), reads=[f'x1t{xb}', f'rs{xb}'], writes=[f'xn2{xb}'])
                    T.op('gpsimd', TT(hrt[:], xn2[:], A2R[:], ALU.mult), reads=[f'xn2{xb}', 'A2R'], writes=[f'hrt{xb}'])
                    T.op('gpsimd', TT(H2row[xb][:], hrt[:], B2R[:], ALU.add), reads=[f'hrt{xb}', 'B2R'], writes=[f'h2row{xb}'])
                    T.op('tensor', tpgrp([(tpc[:, kt, :], xn2[:, kt * 128:(kt + 1) * 128], ident_f[:]) for kt in range(8)]),
                         reads=[f'xn2{xb}', 'identf'], writes=['tpc'])
                    for kt in range(8):
                        T.op('scalar', ACT(H2f[:, kt, :], tpc[:, kt, :], AF.Identity, scale=A2[:, kt:kt + 1], bias=B2[:, kt:kt + 1]),
                             reads=['tpc', 'A2', 'B2'], writes=[f'h2f{xb}'])
                    T.op('tensor', mmgrp([(pmr[xb][:, 0:20], H2f[:, kt, :], WR[:, kt, :], kt == 0, kt == 7) for kt in range(8)]),
                         reads=[f'h2f{xb}', 'WR'], writes=[f'pmr{xb}'])

            def c1_stage2(g):
                    xb = g % 2
                    xn2 = xn2_2[xb]
                    junk = junk_2[xb]
                    H2f = H2f_2[xb]
                    tmpg = tmpg_2[xb]
                    hrt = hrt_2[xb]
                    L = L_2[xb]
                    s1 = s1_2[xb]
                    s2 = s2_2[xb]
                    E1 = E1_2[xb]
                    E2 = E2_2[xb]
                    Mx = Mx_2[xb]
                    oh = oh_2[xb]
                    mk1 = mk1_2[xb]
                    mk2 = mk2_2[xb]
                    les = les_2[xb]
                    le2 = le2_2[xb]
                    sc = sc_2[xb]
                    idf = idf_2[xb]
                    rs = rs_2[xb]
                    T.op('vector', TT(L[:], pmr[xb][:, 0:20], brb[:], ALU.add), reads=[f'pmr{xb}', 'brb'], writes=[f'L{xb}'])
                    rr = [f'L{xb}']
                    V = lambda fn: T.op('vector', fn, reads=rr, writes=rr)
                    S = lambda fn: T.op('scalar', fn, reads=rr, writes=rr)
                    lg = L[:, 0:4]
                    le = L[:, 4:20].rearrange("t (g e) -> t g e", g=4)
                    V(RMAX(sc[:, 0:1], lg))
                    V(TS(oh[:], lg, sc[:, 0:1], None, ALU.is_equal))
                    V(TS(sc[:, 1:2], sc[:, 0:1], -1.0, None, ALU.mult))
                    V(MEMSET(sc[:, 2:3], 0.0))
                    S(ACT(s1[:, 0:4], lg, AF.Exp, bias=sc[:, 1:2], accum_out=sc[:, 2:3]))
                    V(RECIP(sc[:, 3:4], sc[:, 2:3]))
                    V(TT(s2[:].rearrange("t (g e) -> t g e", g=4), le, oh[:].unsqueeze(2).broadcast_to([128, 4, 4]), ALU.mult))
                    V(RED(les[:], s2[:].rearrange("t (g e) -> t e g", g=4), ALU.add))
                    V(RMAX(sc[:, 4:5], les[:]))
                    V(TS(mk1[:], les[:], sc[:, 4:5], None, ALU.is_equal))
                    V(STT(le2[:], mk1[:], NEG, les[:], ALU.mult, ALU.add))
                    V(RMAX(sc[:, 5:6], le2[:]))
                    V(TS(mk2[:], le2[:], sc[:, 5:6], None, ALU.is_equal))
                    V(TT(sc[:, 6:7], sc[:, 5:6], sc[:, 4:5], ALU.subtract))
                    S(ACT(sc[:, 7:8], sc[:, 6:7], AF.Exp))
                    V(TS(sc[:, 8:9], sc[:, 7:8], 1.0, None, ALU.add))
                    V(RECIP(sc[:, 8:9], sc[:, 8:9]))
                    V(TT(W12[:, g, 0:1], sc[:, 8:9], sc[:, 3:4], ALU.mult))
                    V(TT(W12[:, g, 1:2], sc[:, 7:8], W12[:, g, 0:1], ALU.mult))
                    V(TT(E1[:].rearrange("t (g e) -> t g e", g=4), oh[:].unsqueeze(2).broadcast_to([128, 4, 4]),
                         mk1[:].unsqueeze(1).broadcast_to([128, 4, 4]), ALU.mult))
                    V(TT(E2[:].rearrange("t (g e) -> t g e", g=4), oh[:].unsqueeze(2).broadcast_to([128, 4, 4]),
                         mk2[:].unsqueeze(1).broadcast_to([128, 4, 4]), ALU.mult))
                    V(TT(Mx[:], E1[:], E2[:], ALU.add))
                    lst = [(pm[:, 32:48], Umat[:], Mx[:], True, g == 0)]
                    if g > 0:
                        lst.append((pm[:, 32:48], Sel127[:], Cn[:, g - 1, :], False, True))
                    T.op('tensor', mmgrp(lst), reads=[f'L{xb}', 'U', 'sel127', 'Cn'], writes=['pm'])
                    T.op('vector', CP(Cn[:, g, :], pm[:, 32:48]), reads=['pm'], writes=['Cn'])
                    T.op('vector', TT(s1[:], Cn[:, g, :], EB[:], ALU.add), reads=['Cn', 'EB', f'L{xb}'], writes=[f'L{xb}'])
                    V(TT(s2[:], s1[:], E1[:], ALU.mult))
                    V(RED(idf[:, 0:1], s2[:], ALU.add))
                    V(TT(s2[:], s1[:], E2[:], ALU.mult))
                    V(RED(idf[:, 1:2], s2[:], ALU.add))
                    T.op('vector', CP(IDX[:, g, :], idf[:]), reads=[f'L{xb}'], writes=[f'idx{g}'])
                    for k in range(2):
                        T.idma(f'sc{xb}{k}', Xs_d, bass.IndirectOffsetOnAxis(ap=IDX[:, g, k:k + 1], axis=0), H2row[xb][:], None,
                               reads=[f'idx{g}', f'h2row{xb}'], writes=[f'Xs{g}_{k}'])

            c1_stage1(0)
            for g in range(32):
                if g + 1 < 32:
                    c1_stage1(g + 1)
                c1_stage2(g)
            T.op('tensor', mmgrp([(pm[:, 64:80], Sel127[:], Cn[:, 31, :], True, True)]), reads=['Cn', 'sel127'], writes=['pm'])
            T.op('vector', CP(CNTi[:], pm[:, 64:80]), reads=['pm'], writes=['cnti'])
            T.dma('sync', 'cntd', CNT_d, CNTi[0:1, :], reads=['cnti'], writes=['CNT_d'])
            dump("IDX", IDX[:].rearrange("p g k -> p (g k)"), [128, 64], U32, [f'idx{g}' for g in range(32)])
            dump("W12", W12[:].rearrange("p g k -> p (g k)"), [128, 64], F32, ['L'])
            dump("Cn", Cn[:].rearrange("p g e -> p (g e)"), [128, 512], F32, ['Cn'])
            T.barrier()

        with ExitStack() as es:
            WG = [sb(es, [128, 8, 512], BF16) for _ in range(2)]
            WU = [sb(es, [128, 8, 512], BF16) for _ in range(2)]
            WD = [sb(es, [128, 4, 1024], BF16) for _ in range(2)]
            XT = [sb(es, [128, 1024], BF16) for _ in range(3)]
            XsT = [sb(es, [128, 8, 512], BF16) for _ in range(2)]
            AT = [sb(es, [128, 4, 512], BF16) for _ in range(2)]
            sg = [sb(es, [128, 512], F32) for _ in range(2)]
            YT = [sb(es, [128, 1024], F32) for _ in range(2)]
            tpb = ps(es, [128, 8, 128], BF16)
            gp = [ps(es, [128, 512], F32) for _ in range(2)]
            up = [ps(es, [128, 512], F32) for _ in range(2)]
            yp = [ps(es, [128, 512], F32) for _ in range(2)]
            wg_v = wg_d.rearrange("e (kt p) n -> e p kt n", p=128)
            wu_v = wu_d.rearrange("e (kt p) n -> e p kt n", p=128)
            wd_v = wd_d.rearrange("e (kt p) n -> e p kt n", p=128)
            gi = [0]
            yi = [0]
            xi = [0]
            yti = [0]
            gri = [0]

            def load_w(ex):
                wb = ex % 2
                T.dma('gpsimd', f'wg{wb}', WG[wb][:], wg_v[ex], writes=[f'wg{wb}'])
                T.dma('gpsimd', f'wu{wb}', WU[wb][:], wu_v[ex], writes=[f'wu{wb}'])
                T.dma('gpsimd', f'wd{wb}', WD[wb][:], wd_v[ex], writes=[f'wd{wb}'])

            def emit_group(ex, grp):
                wb = ex % 2
                ab = gri[0] % 2
                gri[0] += 1
                base = ex * 4096 + grp * 512
                for tl in range(4):
                    k3 = xi[0] % 3
                    xi[0] += 1
                    r0 = base + tl * 128
                    T.dma('sync', f'xt{k3}p{ab}', XT[k3][:], Xs_d[r0:r0 + 128, :], writes=[f'xt{k3}'])
                    T.op('tensor', tpgrp([(tpb[:, kt, :], XT[k3][:, kt * 128:(kt + 1) * 128], ident_b[:]) for kt in range(8)]),
                         reads=[f'xt{k3}', 'identb'], writes=['tpb'])
                    T.op('vector', CP(XsT[ab][:, :, tl * 128:(tl + 1) * 128], tpb[:]), reads=['tpb'], writes=[f'xst{ab}'])
                for ht in range(4):
                    k2 = gi[0] % 2
                    gi[0] += 1
                    T.op('tensor', mmgrp([(gp[k2][:], WG[wb][:, kt, ht * 128:(ht + 1) * 128], XsT[ab][:, kt, :], kt == 0, kt == 7)
                                          for kt in range(8)]), reads=[f'wg{wb}', f'xst{ab}'], writes=[f'gp{k2}'])
                    T.op('tensor', mmgrp([(up[k2][:], WU[wb][:, kt, ht * 128:(ht + 1) * 128], XsT[ab][:, kt, :], kt == 0, kt == 7)
                                          for kt in range(8)]), reads=[f'wu{wb}', f'xst{ab}'], writes=[f'up{k2}'])
                    T.op('scalar', ACT(sg[k2][:], gp[k2][:], AF.Silu), reads=[f'gp{k2}'], writes=[f'sg{k2}'])
                    T.op('vector', TT(AT[ab][:, ht, :], sg[k2][:], up[k2][:], ALU.mult), reads=[f'sg{k2}', f'up{k2}'],
                         writes=[f'at{ab}'])
                for tl in range(4):
                    yb = yti[0] % 2
                    yti[0] += 1
                    for nh in range(2):
                        k2 = yi[0] % 2
                        yi[0] += 1
                        T.op('tensor', mmgrp([(yp[k2][:], AT[ab][:, ht, tl * 128:(tl + 1) * 128], WD[wb][:, ht, nh * 512:(nh + 1) * 512],
                                               ht == 0, ht == 3) for ht in range(4)]), reads=[f'at{ab}', f'wd{wb}'], writes=[f'yp{k2}'])
                        T.op('scalar', ACT(YT[yb][:, nh * 512:(nh + 1) * 512], yp[k2][:], AF.Copy), reads=[f'yp{k2}'], writes=[f'yt{yb}'])
                    r0 = base + tl * 128
                    T.dma('sync', f'yt{yb}p{ab}', Ys_d[r0:r0 + 128, :], YT[yb][:], reads=[f'yt{yb}'], writes=[f'Ys{ex}_{grp}_{tl}'])

            load_w(0)
            for ex in range(16):
                if ex + 1 < 16:
                    load_w(ex + 1)
                emit_group(ex, 0)
                for grp in range(1, 8):
                    par = gri[0] % 2
                    pars = (0, 1) if grp == 1 else (par,)
                    drain = [f'xt{k}p{q}' for k in range(3) for q in pars] + [f'yt{k}p{q}' for k in range(2) for q in pars]
                    T.cond_begin(CNT_d[0:1, ex:ex + 1] if grp == 1 else None, grp * 512, drain)
                    emit_group(ex, grp)
                for grp in range(1, 8):
                    T.cond_end()
            T.barrier()

        with ExitStack() as es:
            Y1 = [sb(es, [128, 1024], F32) for _ in range(2)]
            Y2 = [sb(es, [128, 1024], F32) for _ in range(2)]
            X1t = [sb(es, [128, 1024], F32) for _ in range(2)]
            junk = sb(es, [128, 1024], BF16)
            osb = [sb(es, [128, 1024], F32) for _ in range(2)]
            ssq = sb(es, [128, 32], F32)
            T.op('vector', MEMSET(ssq[:], 0.0), writes=['ssqall'])
            rs = [sb(es, [128, 1], F32) for _ in range(2)]
            def c3_loads(g):
                b = g % 2
                T.dma('sync', f'x1c{b}', X1t[b][:], X1_d[g * 128:(g + 1) * 128, :], writes=[f'x1c{b}'])
                T.idma(f'ga{b}', Y1[b][:], None, Ys_d, bass.IndirectOffsetOnAxis(ap=IDX[:, g, 0:1], axis=0), writes=[f'y1{b}'])
                T.idma(f'gb{b}', Y2[b][:], None, Ys_d, bass.IndirectOffsetOnAxis(ap=IDX[:, g, 1:2], axis=0), writes=[f'y2{b}'])

            c3_loads(0)
            for g in range(32):
                b = g % 2
                if g + 1 < 32:
                    c3_loads(g + 1)
                T.op('vector', TS(Y1[b][:], Y1[b][:], W12[:, g, 0:1], None, ALU.mult), reads=[f'y1{b}'], writes=[f'y1{b}'])
                T.op('vector', STT(Y1[b][:], Y2[b][:], W12[:, g, 1:2], Y1[b][:], ALU.mult, ALU.add), reads=[f'y1{b}', f'y2{b}'],
                     writes=[f'y1{b}'])
                T.op('vector', TT(Y1[b][:], Y1[b][:], GT2[:], ALU.mult), reads=[f'y1{b}'], writes=[f'y1{b}'])
                T.op('vector', TT(Y1[b][:], Y1[b][:], X1t[b][:], ALU.add), reads=[f'y1{b}', f'x1c{b}'], writes=[f'y1{b}'])
                T.op('scalar', ACT(junk[:], Y1[b][:], AF.Square, accum_out=ssq[:, g:g + 1]), reads=[f'y1{b}', 'ssqall'],
                     writes=['junk', f'ssq_{g}'])
                T.op('scalar', ACT(rs[b][:], ssq[:, g:g + 1], AF.Ln, scale=1.0 / 1024, bias=eps_t[:]), reads=[f'ssq_{g}', 'eps'], writes=[f'rs{b}'])
                T.op('scalar', ACT(rs[b][:], rs[b][:], AF.Exp, scale=-0.5), reads=[f'rs{b}'], writes=[f'rs{b}'])
                T.op('vector', STT(osb[b][:], Y1[b][:], rs[b][:, 0:1], gfin[:], ALU.mult, ALU.mult),
                     reads=[f'y1{b}', f'rs{b}', 'gfin'], writes=[f'osb{b}'])
                T.dma('sync', f'out{b}', out_d[g * 128:(g + 1) * 128, :], osb[b][:], reads=[f'osb{b}'], writes=[f'out{g}'])
            T.barrier()
    T.emit()
    top.close()
    return nc


_CACHE = {}


def _masks(j):
    m = np.zeros((2, 8, 128, 512), np.float32)
    ki = np.arange(128)[:, None]
    qq = np.arange(512)[None, :]
    for par in range(2):
        i = par
        run = OWN_RUNS[j][i]
        nk = NK(i)
        for r in range(8):
            jb = nk - 8 + r
            kpos = jb * 128 + ki
            qpos = run * 512 + qq
            m[par, r] = np.where(kpos <= qpos, 0.0, NEG)
    return m.reshape(16, 128, 512)


def _rsel(j):
    s = np.zeros((8, 64), np.float32)
    for i, run in enumerate(OWN_RUNS[j]):
        s[i, 4 * run + 1] = 1.0
    return s.reshape(1, 512)


def kernel(x, c, w_ada, b_ada, g_norm_mix, w_in, g_sgu, w_spatial, b_spatial, b_forget, g_out_sgu, g_out_fox, w_out,
           g_norm_ffn, w_router_group, b_router_group, w_router_expert, b_router_expert, w_gate, w_up, w_down, g_final):
    f = lambda a: np.ascontiguousarray(np.asarray(a, dtype=np.float32))
    x = f(x); c = f(c)
    if "nc" not in _CACHE:
        _CACHE["nc"] = build_program()
    nc = _CACHE["nc"]
    wrg = f(w_router_group)[0]; wre = f(w_router_expert)[0]
    brg = f(b_router_group)[0]; bre = f(b_router_expert)[0]
    wg_all = f(w_gate)[0]; wu_all = f(w_up)[0]; wd_all = f(w_down)[0]
    shared = {
        "w_ada": f(w_ada)[0], "b_ada": f(b_ada)[0][None, :], "g1T": f(f(g_norm_mix)[0].reshape(8, 128).T),
        "g2T": f(f(g_norm_ffn)[0].reshape(8, 128).T), "w_in": f(w_in)[0], "gsgu": f(g_sgu)[0][None, :],
        "wsT": f(f(w_spatial)[0].transpose(2, 0, 1)), "bsp": f(b_spatial)[0], "bfg": f(b_forget)[0][None, :],
        "gos": f(f(g_out_sgu)[0].reshape(4, 128).T), "gof": f(f(g_out_fox)[0].reshape(4, 128).T), "w_out": f(w_out)[0],
        "gfin": f(g_final)[None, :],
    }
    percore = {}
    for core in range(8):
        gmap = [(G + core % 4) % 4 for G in range(4)]
        emap = [(j + 2 * (core // 4)) % 4 for j in range(4)]
        eorder = [4 * gmap[G] + emap[j] for G in range(4) for j in range(4)]
        wr = np.concatenate([wrg[:, gmap]] + [wre[gmap[G]][:, emap] for G in range(4)], axis=1)
        br = np.concatenate([brg[gmap]] + [bre[gmap[G]][emap] for G in range(4)])[None, :]
        percore[core] = {"wr": f(wr), "br": f(br), "w_gate": f(wg_all[eorder]), "w_up": f(wu_all[eorder]),
                         "w_down": f(wd_all[eorder])}
    in_maps = []
    for core in range(8):
        b, j = core // 2, core % 2
        m = dict(shared)
        m.update(percore[core])
        m["xall"] = x[b]
        m["xown"] = f(np.concatenate([x[b, 512 * r:512 * (r + 1)] for r in OWN_RUNS[j]], axis=0))
        m["cT"] = f(c[b].reshape(8, 128).T)
        m["masks"] = _masks(j)
        m["rsel"] = _rsel(j)
        m["eb"] = (np.arange(16, dtype=np.float32) * 4096.0 - 1.0)[None, :]
        in_maps.append(m)
    res = run_bass_kernel_spmd(nc, in_maps, core_ids=list(range(8)))
    _CACHE["res"] = res
    out = np.empty((4, 8192, 1024), np.float32)
    for core in range(8):
        b, j = core // 2, core % 2
        o = res.results[core]["out"]
        for i, r in enumerate(OWN_RUNS[j]):
            out[b, 512 * r:512 * (r + 1)] = o[512 * i:512 * (i + 1)]
    return out
```
